# Optimizing a Trainium2 kernel written in Bass

```python
import math
import jax, jax.numpy as jnp
from jax import lax
import numpy as np

D_MODEL = 1024
BATCH = 4
SEQ = 4096
DEPTH = 1

MEM_LEN = 256
SSD_HEADS = 16
SSD_HEAD_DIM = 64
SSD_INNER = SSD_HEADS * SSD_HEAD_DIM
SSD_GROUPS = 4
SSD_STATE = 128
SSD_CONV = 4
SSD_CHUNK = 128
SSD_XBC = SSD_INNER + 2 * SSD_GROUPS * SSD_STATE
ATT_HEADS = 16
ATT_HEAD_DIM = 64
ATT_WIDTH = ATT_HEADS * ATT_HEAD_DIM
KV_RANK = 256
IDX_HEADS = 16
IDX_DIM = 64
TOPK_MAX = 256
Q_BLOCK = 128
MEM_HEADS = 4
MEM_HEAD_DIM = D_MODEL // MEM_HEADS
D_FF = 2816
ALPHA = (2.0 * DEPTH) ** 0.25
BETA = (8.0 * DEPTH) ** -0.25
LN_EPS = 1e-5
RMS_EPS = 1e-6
IN_SPLITS = (D_MODEL, D_MODEL, SSD_INNER, SSD_XBC, SSD_HEADS,
             ATT_WIDTH, KV_RANK, IDX_HEADS * IDX_DIM, IDX_DIM, IDX_HEADS)
D_IN = 2 * D_MODEL + SSD_INNER + SSD_XBC + SSD_HEADS + ATT_WIDTH + KV_RANK + IDX_HEADS * IDX_DIM + IDX_DIM + IDX_HEADS

kernel_name = "hybrid_ssd_dsa_gated_macaron_deepnorm"


def layer_norm(x, g, b):
    xf = x.astype(jnp.float32)
    mu = jnp.mean(xf, -1, keepdims=True)
    var = jnp.mean(jnp.square(xf - mu), -1, keepdims=True)
    return ((xf - mu) * lax.rsqrt(var + LN_EPS) * g + b).astype(x.dtype)


def rms_norm(x, g):
    xf = x.astype(jnp.float32)
    y = xf * lax.rsqrt(jnp.mean(jnp.square(xf), -1, keepdims=True) + RMS_EPS)
    return (y * g).astype(x.dtype)


def swiglu_ffn(x, w_in, w_out):
    gate, up = jnp.split(x @ w_in, 2, axis=-1)
    return (jax.nn.silu(gate) * up) @ w_out


def split_cols(h, sizes):
    out, off = [], 0
    for s in sizes:
        out.append(h[..., off:off + s])
        off += s
    return out


def causal_dwconv(u, w, b):
    width, ch = w.shape
    out = lax.conv_general_dilated(u, w[:, None, :], window_strides=(1,),
                                   padding=[(width - 1, 0)],
                                   dimension_numbers=('NWC', 'WIO', 'NWC'),
                                   feature_group_count=ch)
    return out + b


def segsum(a):
    l = a.shape[-1]
    cs = jnp.cumsum(a, axis=-1)
    diff = cs[..., :, None] - cs[..., None, :]
    mask = jnp.tril(jnp.ones((l, l), dtype=bool))
    return jnp.where(mask, diff, -jnp.inf)


def ssd_chunked_scan(x, dt, a_head, bmat, cmat):
    bsz, seqlen, n_heads, p = x.shape
    g, n = bmat.shape[2], bmat.shape[3]
    r = n_heads // g
    l = SSD_CHUNK
    c = seqlen // l
    x = x.astype(jnp.float32).reshape(bsz, c, l, g, r, p)
    dt = dt.reshape(bsz, c, l, g, r)
    bmat = bmat.astype(jnp.float32).reshape(bsz, c, l, g, n)
    cmat = cmat.astype(jnp.float32).reshape(bsz, c, l, g, n)
    a = (dt * a_head.reshape(g, r)).transpose(0, 3, 4, 1, 2)
    a_cs = jnp.cumsum(a, axis=-1)
    xdt = x * dt[..., None]
    decay_in = jnp.exp(segsum(a))
    cb = jnp.einsum('bclgn,bcsgn->bcgls', cmat, bmat)
    y_diag = jnp.einsum('bcgls,bgrcls,bcsgrp->bclgrp', cb, decay_in, xdt)
    decay_to_end = jnp.exp(a_cs[..., -1:] - a_cs)
    chunk_states = jnp.einsum('bclgn,bgrcl,bclgrp->bcgrpn', bmat, decay_to_end, xdt)
    chunk_decay = jnp.exp(a_cs[..., -1])

    def step(h, inp):
        s_c, d_c = inp
        return d_c[..., None, None] * h + s_c, h

    h0 = jnp.zeros((bsz, g, r, p, n), jnp.float32)
    _, prev = lax.scan(step, h0, (jnp.moveaxis(chunk_states, 1, 0),
                                  jnp.moveaxis(chunk_decay, -1, 0)))
    prev = jnp.moveaxis(prev, 0, 1)
    y_off = jnp.einsum('bclgn,bcgrpn,bgrcl->bclgrp', cmat, prev, jnp.exp(a_cs))
    return (y_diag + y_off).reshape(bsz, seqlen, n_heads, p)


def ssd_branch(z, xbc, dt_raw, conv_w, conv_b, dt_bias, a_log, d_skip, norm_w):
    bsz, seqlen, _ = z.shape
    xbc = jax.nn.silu(causal_dwconv(xbc, conv_w, conv_b))
    gn = SSD_GROUPS * SSD_STATE
    xs, bm, cm = split_cols(xbc, (SSD_INNER, gn, gn))
    xh = xs.reshape(bsz, seqlen, SSD_HEADS, SSD_HEAD_DIM)
    dt = jax.nn.softplus(dt_raw.astype(jnp.float32) + dt_bias)
    a_head = -jnp.exp(a_log.astype(jnp.float32))
    y = ssd_chunked_scan(xh, dt, a_head,
                         bm.reshape(bsz, seqlen, SSD_GROUPS, SSD_STATE),
                         cm.reshape(bsz, seqlen, SSD_GROUPS, SSD_STATE))
    y = y + d_skip[None, None, :, None] * xh.astype(jnp.float32)
    y = y.reshape(bsz, seqlen, SSD_INNER) * jax.nn.silu(z.astype(jnp.float32))
    yg = y.reshape(bsz, seqlen, SSD_GROUPS, SSD_INNER // SSD_GROUPS)
    yg = yg * lax.rsqrt(jnp.mean(jnp.square(yg), -1, keepdims=True) + RMS_EPS)
    return (yg.reshape(bsz, seqlen, SSD_INNER) * norm_w).astype(z.dtype)


def dsa_branch(q, c_kv, q_idx, k_idx, w_idx, kv_norm_w, w_uk, w_uv):
    bsz, seqlen, _ = q.shape
    topk = min(TOPK_MAX, seqlen // 4)
    n_blocks = seqlen // Q_BLOCK
    q = q.reshape(bsz, seqlen, ATT_HEADS, ATT_HEAD_DIM)
    q_lat = jnp.einsum('bthd,hdr->bthr', q, w_uk) * (ATT_HEAD_DIM ** -0.5)
    c_kv = rms_norm(c_kv, kv_norm_w)
    q_idx = q_idx.reshape(bsz, seqlen, IDX_HEADS, IDX_DIM)
    w_idx = w_idx * (IDX_HEADS ** -0.5)
    slopes = 2.0 ** (-8.0 * jnp.arange(1, ATT_HEADS + 1, dtype=jnp.float32) / ATT_HEADS)
    key_pos = jnp.arange(seqlen, dtype=jnp.int32)

    def to_blocks(a):
        return a.reshape(bsz, n_blocks, Q_BLOCK, *a.shape[2:]).swapaxes(0, 1)

    t_blocks = key_pos.reshape(n_blocks, Q_BLOCK)

    def block(args):
        ql, qi, wi, t = args
        logits = jax.nn.relu(jnp.einsum('bqhd,bsd->bqhs', qi, k_idx))
        score = jnp.einsum('bqhs,bqh->bqs', logits, wi).astype(jnp.float32)
        causal = key_pos[None, :] <= t[:, None]
        score = jnp.where(causal[None], score, -jnp.inf)
        _, idx = lax.top_k(score, topk)
        sel = jax.vmap(lambda c, i: c[i])(c_kv, idx)
        s = jnp.einsum('bqhr,bqkr->bqhk', ql, sel).astype(jnp.float32)
        dist = (t[None, :, None] - idx).astype(jnp.float32)
        s = s - slopes[None, None, :, None] * dist[:, :, None, :]
        valid = idx <= t[None, :, None]
        s = jnp.where(valid[:, :, None, :], s, -jnp.inf)
        p = jax.nn.softmax(s, axis=-1).astype(sel.dtype)
        return jnp.einsum('bqhk,bqkr->bqhr', p, sel)

    o = lax.map(block, (to_blocks(q_lat), to_blocks(q_idx), to_blocks(w_idx), t_blocks))
    o = o.swapaxes(0, 1).reshape(bsz, seqlen, ATT_HEADS, KV_RANK)
    return jnp.einsum('bthr,hrd->bthd', o, w_uv).reshape(bsz, seqlen, ATT_WIDTH)


def memory_xattn(x, mem, w_mq, w_mkv, w_mo):
    bsz, seqlen, _ = x.shape
    q = (x @ w_mq).reshape(bsz, seqlen, MEM_HEADS, MEM_HEAD_DIM)
    k, v = jnp.split(mem @ w_mkv, 2, axis=-1)
    k = k.reshape(bsz, -1, MEM_HEADS, MEM_HEAD_DIM)
    v = v.reshape(bsz, -1, MEM_HEADS, MEM_HEAD_DIM)
    s = jnp.einsum('bthd,bmhd->bhtm', q, k).astype(jnp.float32) * (MEM_HEAD_DIM ** -0.5)
    p = jax.nn.softmax(s, axis=-1).astype(v.dtype)
    o = jnp.einsum('bhtm,bmhd->bthd', p, v).reshape(bsz, seqlen, D_MODEL)
    return o @ w_mo


def hybrid_layer(x, mem, ffn1_w_in, ffn1_w_out, ln1_g, ln1_b,
                 w_in, conv_w, conv_b, dt_bias, a_log, d_skip, ssd_norm_w,
                 kv_norm_w, w_uk, w_uv, w_proj_ssd, w_proj_att, w_out, ln2_g, ln2_b,
                 w_mq, w_mkv, w_mo, ln3_g, ln3_b,
                 ffn2_w_in, ffn2_w_out, ln4_g, ln4_b):
    x = layer_norm(ALPHA * x + 0.5 * swiglu_ffn(x, ffn1_w_in, ffn1_w_out), ln1_g, ln1_b)
    h = x @ w_in
    g_ssd, g_att, z, xbc, dt_raw, q, c_kv, q_idx, k_idx, w_idx = split_cols(h, IN_SPLITS)
    y_ssd = ssd_branch(z, xbc, dt_raw, conv_w, conv_b, dt_bias, a_log, d_skip, ssd_norm_w)
    y_att = dsa_branch(q, c_kv, q_idx, k_idx, w_idx, kv_norm_w, w_uk, w_uv)
    merged = jax.nn.sigmoid(g_ssd) * (y_ssd @ w_proj_ssd) + jax.nn.sigmoid(g_att) * (y_att @ w_proj_att)
    x = layer_norm(ALPHA * x + merged @ w_out, ln2_g, ln2_b)
    x = layer_norm(ALPHA * x + memory_xattn(x, mem, w_mq, w_mkv, w_mo), ln3_g, ln3_b)
    x = layer_norm(ALPHA * x + 0.5 * swiglu_ffn(x, ffn2_w_in, ffn2_w_out), ln4_g, ln4_b)
    return x


def setup_inputs(seed: int = 0) -> dict:
    key = jax.random.key(seed)
    keys = iter(jax.random.split(key, 48))
    f32 = jnp.float32
    L = DEPTH

    def nrm(shape, scale):
        return jax.random.normal(next(keys), shape, f32) * scale

    def gain(shape):
        return 1.0 + nrm(shape, 0.02)

    x = nrm((BATCH, SEQ, D_MODEL), 1.0)
    mem = nrm((BATCH, MEM_LEN, D_MODEL), 1.0)
    ffn1_w_in = nrm((L, D_MODEL, 2 * D_FF), D_MODEL ** -0.5)
    ffn1_w_out = nrm((L, D_FF, D_MODEL), BETA * D_FF ** -0.5)
    ln1_g = gain((L, D_MODEL))
    ln1_b = nrm((L, D_MODEL), 0.02)
    w_in = nrm((L, D_MODEL, D_IN), D_MODEL ** -0.5)
    conv_w = nrm((L, SSD_CONV, SSD_XBC), SSD_CONV ** -0.5)
    conv_b = nrm((L, SSD_XBC), 0.02)
    dt0 = jnp.exp(jax.random.uniform(next(keys), (L, SSD_HEADS), f32,
                                     minval=math.log(1e-3), maxval=math.log(1e-1)))
    dt_bias = dt0 + jnp.log(-jnp.expm1(-dt0))
    a_log = jnp.log(jax.random.uniform(next(keys), (L, SSD_HEADS), f32, minval=1.0, maxval=16.0))
    d_skip = gain((L, SSD_HEADS))
    ssd_norm_w = gain((L, SSD_INNER))
    kv_norm_w = gain((L, KV_RANK))
    w_uk = nrm((L, ATT_HEADS, ATT_HEAD_DIM, KV_RANK), KV_RANK ** -0.5)
    w_uv = nrm((L, ATT_HEADS, KV_RANK, ATT_HEAD_DIM), KV_RANK ** -0.5)
    w_proj_ssd = nrm((L, SSD_INNER, D_MODEL), SSD_INNER ** -0.5)
    w_proj_att = nrm((L, ATT_WIDTH, D_MODEL), ATT_WIDTH ** -0.5)
    w_out = nrm((L, D_MODEL, D_MODEL), BETA * D_MODEL ** -0.5)
    ln2_g = gain((L, D_MODEL))
    ln2_b = nrm((L, D_MODEL), 0.02)
    w_mq = nrm((L, D_MODEL, D_MODEL), D_MODEL ** -0.5)
    w_mkv = nrm((L, D_MODEL, 2 * D_MODEL), D_MODEL ** -0.5)
    w_mo = nrm((L, D_MODEL, D_MODEL), BETA * D_MODEL ** -0.5)
    ln3_g = gain((L, D_MODEL))
    ln3_b = nrm((L, D_MODEL), 0.02)
    ffn2_w_in = nrm((L, D_MODEL, 2 * D_FF), D_MODEL ** -0.5)
    ffn2_w_out = nrm((L, D_FF, D_MODEL), BETA * D_FF ** -0.5)
    ln4_g = gain((L, D_MODEL))
    ln4_b = nrm((L, D_MODEL), 0.02)
    return {"x": x, "mem": mem,
            "ffn1_w_in": ffn1_w_in, "ffn1_w_out": ffn1_w_out, "ln1_g": ln1_g, "ln1_b": ln1_b,
            "w_in": w_in, "conv_w": conv_w, "conv_b": conv_b, "dt_bias": dt_bias,
            "a_log": a_log, "d_skip": d_skip, "ssd_norm_w": ssd_norm_w,
            "kv_norm_w": kv_norm_w, "w_uk": w_uk, "w_uv": w_uv,
            "w_proj_ssd": w_proj_ssd, "w_proj_att": w_proj_att, "w_out": w_out,
            "ln2_g": ln2_g, "ln2_b": ln2_b,
            "w_mq": w_mq, "w_mkv": w_mkv, "w_mo": w_mo, "ln3_g": ln3_g, "ln3_b": ln3_b,
            "ffn2_w_in": ffn2_w_in, "ffn2_w_out": ffn2_w_out, "ln4_g": ln4_g, "ln4_b": ln4_b}


def reference(x, mem, ffn1_w_in, ffn1_w_out, ln1_g, ln1_b,
              w_in, conv_w, conv_b, dt_bias, a_log, d_skip, ssd_norm_w,
              kv_norm_w, w_uk, w_uv, w_proj_ssd, w_proj_att, w_out, ln2_g, ln2_b,
              w_mq, w_mkv, w_mo, ln3_g, ln3_b,
              ffn2_w_in, ffn2_w_out, ln4_g, ln4_b):
    for layer in range(DEPTH):
        x = hybrid_layer(x, mem,
                         ffn1_w_in[layer], ffn1_w_out[layer], ln1_g[layer], ln1_b[layer],
                         w_in[layer], conv_w[layer], conv_b[layer], dt_bias[layer],
                         a_log[layer], d_skip[layer], ssd_norm_w[layer],
                         kv_norm_w[layer], w_uk[layer], w_uv[layer],
                         w_proj_ssd[layer], w_proj_att[layer], w_out[layer],
                         ln2_g[layer], ln2_b[layer],
                         w_mq[layer], w_mkv[layer], w_mo[layer], ln3_g[layer], ln3_b[layer],
                         ffn2_w_in[layer], ffn2_w_out[layer], ln4_g[layer], ln4_b[layer])
    return x
```

```python
import contextlib
import numpy as np
import ml_dtypes
import concourse.bass as bass
import concourse.mybir as mybir
from concourse.bass_utils import run_bass_kernel_spmd

F32 = mybir.dt.float32
BF16 = mybir.dt.bfloat16
AF = mybir.ActivationFunctionType
ALU = mybir.AluOpType
AX = mybir.AxisListType
DSZ = {F32: 4, BF16: 2}
WHOLE = (0, 1 << 30, 0, 1 << 40)

D = 1024
SEQ = 4096
NBLK = 32
DFF = 2816
NF = DFF // 128
ALPHA = 2.0 ** 0.25
LN_EPS = 1e-5


class Ctx:
    def __init__(self, nc):
        self.nc = nc
        self.es = contextlib.ExitStack()
        self.eng = {'pe': nc.tensor, 'act': nc.scalar, 'dve': nc.vector,
                    'pool': nc.gpsimd, 'sp': nc.sync}
        self.esem = {k: self.es.enter_context(nc.semaphore('es_' + k))
                     for k in ('pe', 'act', 'dve', 'pool')}
        self.ecnt = {k: 0 for k in self.esem}
        self.seen = {k: {} for k in self.eng}
        self.trk = {}
        self.free_sems = []
        self.scnt = {}
        self.nsem = 0
        self.nwait = 0
        self.ninst = 0
        self.pstack = None
        self.pnames = None
        self.psi = 0

    def _reg(self, name, rowb, dram=False):
        self.trk[name] = dict(rowb=rowb, w=[], r=[], dsem=None, dram=dram)
        if self.pnames is not None and not dram:
            self.pnames.append(name)

    def sb(self, name, shape, dtype):
        self.uid = getattr(self, 'uid', 0) + 1
        name = '%s_u%d' % (name, self.uid)
        st = self.pstack if self.pstack is not None else self.es
        t = st.enter_context(self.nc.sbuf_tensor(name, list(shape), dtype))
        self._reg(name, int(np.prod(shape[1:])) * DSZ[dtype])
        return t

    def psum(self, name, shape, dtype=F32):
        st = self.pstack if self.pstack is not None else self.es
        t = st.enter_context(self.nc.psum_tensor(name, list(shape), dtype))
        self._reg(name, int(np.prod(shape[1:])) * DSZ[dtype])
        self.trk[name]['psum'] = True
        return t

    def dram(self, name, shape, dtype, kind="Internal"):
        t = self.nc.dram_tensor(name, list(shape), dtype, kind=kind)
        self._reg(name, 0, dram=True)
        return t

    def sem_for(self, name):
        t = self.trk[name]
        if t['dsem'] is None:
            if self.free_sems:
                t['dsem'] = self.free_sems.pop()
            else:
                self.nsem += 1
                s = self.es.enter_context(self.nc.semaphore('ds%d' % self.nsem))
                self.scnt[s.name] = 0
                t['dsem'] = s
        return t['dsem']

    @contextlib.contextmanager
    def phase(self):
        assert self.pstack is None
        self.pstack = contextlib.ExitStack()
        self.pnames = []
        try:
            yield
        finally:
            self.barrier()
            for n in self.pnames:
                t = self.trk.pop(n)
                if t['dsem'] is not None:
                    self.free_sems.append(t['dsem'])
            self.pstack.close()
            self.pstack = None
            self.pnames = None

    def barrier(self):
        for e, eng in self.eng.items():
            for k, s in self.esem.items():
                if k != e and self.seen[e].get(s.name, 0) < self.ecnt[k]:
                    eng.wait_ge(s, self.ecnt[k])
                    self.seen[e][s.name] = self.ecnt[k]
            for name, t in self.trk.items():
                s = t['dsem']
                if s is not None and self.seen[e].get(s.name, 0) < self.scnt[s.name]:
                    eng.wait_ge(s, self.scnt[s.name])
                    self.seen[e][s.name] = self.scnt[s.name]
        for t in self.trk.values():
            t['w'] = []
            t['r'] = []

    def region(self, ap):
        t = self.trk[ap.name]
        sz = DSZ[ap.dtype]
        pat = ap.ap
        off = ap.offset
        if t['dram']:
            ext = sum((c - 1) * abs(s) for s, c in pat) + 1
            return (0, 1, off * sz, (off + ext) * sz)
        if t.get('psum'):
            return (0, 128, 0, t['rowb'])
        rowe = t['rowb'] // sz
        p0 = off // rowe
        f0 = off % rowe
        ps, pc = pat[0]
        p1 = p0 + (pc - 1) * (ps // rowe) + 1
        ext = sum((c - 1) * abs(s) for s, c in pat[1:]) + 1
        return (p0, p1, f0 * sz, (f0 + ext) * sz)

    @staticmethod
    def _ov(a, b):
        return a[0] < b[1] and b[0] < a[1] and a[2] < b[3] and b[2] < a[3]

    @staticmethod
    def _cov(a, b):
        return a[0] <= b[0] and a[1] >= b[1] and a[2] <= b[2] and a[3] >= b[3]

    def _cur(self, t, rec):
        if rec[2] == 'dma' and rec[0] is t['dsem']:
            return (rec[0], self.scnt[rec[0].name], 'dma')
        return rec

    def _deps(self, outs, ins, whole_out=False, e=None):
        deps = []
        for ap in ins:
            t = self.trk[ap.name]
            R = self.region(ap)
            for (Q, rec) in t['w']:
                if self._ov(R, Q):
                    deps.append(self._cur(t, rec))
            if t.get('psum'):
                for (Q, rec) in t['r']:
                    if rec[2] != e:
                        deps.append(rec)
        for ap in outs:
            t = self.trk[ap.name]
            R = self.region(ap) if not whole_out else WHOLE
            for (Q, rec) in t['w']:
                if self._ov(R, Q):
                    deps.append(self._cur(t, rec))
            for (Q, rec) in t['r']:
                if self._ov(R, Q):
                    deps.append(self._cur(t, rec))
        return deps

    def _record(self, outs, ins, rec):
        for ap in ins:
            t = self.trk[ap.name]
            R = self.region(ap)
            t['r'] = [(Q, r) for (Q, r) in t['r']
                      if not (r[0] is rec[0] and self._cov(R, Q))]
            t['r'].append((R, rec))
            if len(t['r']) > 40:
                self._collapse(t, 'r')
        for ap in outs:
            t = self.trk[ap.name]
            R = self.region(ap)
            t['w'] = [(Q, r) for (Q, r) in t['w'] if not self._cov(R, Q)]
            t['r'] = [(Q, r) for (Q, r) in t['r'] if not self._cov(R, Q)]
            t['w'].append((R, rec))
            if len(t['w']) > 40:
                self._collapse(t, 'w')

    def _collapse(self, t, k):
        best = {}
        box = None
        for (Q, r) in t[k]:
            key = r[0].name
            if key not in best or best[key][1] < r[1]:
                best[key] = r
            box = Q if box is None else (min(box[0], Q[0]), max(box[1], Q[1]),
                                         min(box[2], Q[2]), max(box[3], Q[3]))
        t[k] = [(box, r) for r in best.values()]

    def _waits(self, e, deps):
        need = {}
        for (sem, val, src) in deps:
            if src == e and e == 'pe':
                continue
            if self.seen[e].get(sem.name, 0) >= val:
                continue
            if sem.name not in need or need[sem.name][1] < val:
                need[sem.name] = (sem, val)
        for (sem, val) in need.values():
            self.seen[e][sem.name] = val
        return list(need.values())

    def _autobb(self):
        tot = self.ninst + self.nwait
        if tot - getattr(self, 'lastbb', 0) > 500:
            self.lastbb = tot
            self.newbb()

    def emit(self, e, fn, outs, ins, attach=True):
        if self.ninst >= LIMIT:
            return None
        self._autobb()
        waits = self._waits(e, self._deps(outs, ins, e=e))
        eng = self.eng[e]
        self.nwait += len(waits)
        last = None
        if attach and waits:
            last = waits.pop()
        for (sem, val) in waits:
            eng.wait_ge(sem, val)
        ins_ = fn(eng)
        if last is not None:
            ins_._wait_ge(last[0], last[1])
        ins_.then_inc(self.esem[e], 1)
        self.ecnt[e] += 1
        self.ninst += 1
        rec = (self.esem[e], self.ecnt[e], e)
        self._record(outs, ins, rec)
        return ins_

    def dma(self, out, in_, q='sp', **kw):
        if self.ninst >= LIMIT:
            return None
        self._autobb()
        deps = self._deps([out], [in_], whole_out=True)
        sbside = in_ if self.trk[out.name]['dram'] else out
        sem = self.sem_for(sbside.name)
        deps = [d for d in deps if not (d[0] is sem and d[2] == 'dma')]
        waits = self._waits(q, deps)
        eng = self.eng[q]
        self.nwait += len(waits)
        for (s, val) in waits:
            eng.wait_ge(s, val)
        self.scnt[sem.name] += 16
        eng.dma_start(out=out, in_=in_, **kw).then_inc(sem, 16)
        self.ninst += 1
        rec = (sem, self.scnt[sem.name], 'dma')
        self._record([out], [in_], rec)

    def newbb(self):
        self.nbb = getattr(self, 'nbb', 0) + 1
        self.nc.switch_bb('kbb%d' % self.nbb)

    def ps(self):
        b = self.banks[self.psi % len(self.banks)]
        self.psi += 1
        return b

    def mm(self, out, lhsT, rhs, start=True, stop=True):
        return self.emit('pe', lambda e: e.matmul(out, lhsT=lhsT, rhs=rhs, start=start, stop=stop),
                         [out], [lhsT, rhs])

    def tr(self, out, in_, ident):
        return self.emit('pe', lambda e: e.transpose(out=out, in_=in_, identity=ident),
                         [out], [in_, ident])

    def act(self, out, in_, func, bias=None, scale=None, accum=None, e='act'):
        kw = {}
        ins = [in_]
        outs = [out]
        if bias is not None:
            kw['bias'] = bias
            if not isinstance(bias, (int, float)):
                ins.append(bias)
        if scale is not None:
            kw['scale'] = scale
            if not isinstance(scale, (int, float)):
                ins.append(scale)
        if accum is not None:
            kw['accum_out'] = accum
            outs.append(accum)
        return self.emit('act', lambda e_: e_.activation(out=out, in_=in_, func=func, **kw),
                         outs, ins, attach=accum is None)

    def ts(self, e, out, in0, s1, s2, op0, op1=None, accum=None):
        ins = [in0] + [s for s in (s1, s2) if s is not None and not isinstance(s, (int, float))]
        outs = [out] + ([accum] if accum is not None else [])
        kw = {}
        if op1 is not None:
            kw['op1'] = op1
        if accum is not None:
            kw['accum_out'] = accum
        return self.emit(e, lambda e_: e_.tensor_scalar(out=out, in0=in0, scalar1=s1, scalar2=s2,
                                                        op0=op0, **kw),
                         outs, ins, attach=accum is None)

    def tt(self, e, out, in0, in1, op):
        return self.emit(e, lambda e_: e_.tensor_tensor(out=out, in0=in0, in1=in1, op=op),
                         [out], [in0, in1])

    def stt(self, out, in0, scalar, in1, op0, op1):
        ins = [in0, in1] + ([scalar] if not isinstance(scalar, (int, float)) else [])
        return self.emit('dve', lambda e_: e_.scalar_tensor_tensor(out=out, in0=in0, scalar=scalar,
                                                                   in1=in1, op0=op0, op1=op1),
                         [out], ins)

    def copy(self, e, out, in_):
        if e == 'act':
            return self.emit('act', lambda e_: e_.copy(out=out, in_=in_), [out], [in_])
        return self.emit(e, lambda e_: e_.tensor_copy(out=out, in_=in_), [out], [in_])


def load_cast(c, dst, src, st, eng='pool'):
    shp = list(src.shape)
    n = int(np.prod(shp[1:]))
    sv = st[:, 0:n]
    if len(shp) == 3:
        sv = sv.rearrange("p (a b) -> p a b", a=shp[1])
    c.dma(sv, src)
    c.copy(eng, dst, sv)


def layer_norm_stats(c, S, y, eps):
    st6 = S['st6']
    mv = S['mv']
    for h in range(2):
        c.emit('dve', lambda e, h=h: e.bn_stats(out=st6[:, h, :], in_=y[:, h * 512:(h + 1) * 512]),
               [st6[:, h, :]], [y[:, h * 512:(h + 1) * 512]])
    c.emit('dve', lambda e: e.bn_aggr(out=mv[:, 0:2], in_=st6[:]), [mv[:, 0:2]], [st6[:]])
    c.act(mv[:, 2:3], mv[:, 1:2], AF.Sqrt, bias=S['epsc'][:, 0:1])
    c.emit('dve', lambda e: e.reciprocal(out=mv[:, 3:4], in_=mv[:, 2:3]), [mv[:, 3:4]], [mv[:, 2:3]])
    return mv[:, 0:1], mv[:, 3:4]


def ffn_phase(c, K, xsrc, nblk, w_in, w_out, g_d, b_d, post, name):
    TT = 1024
    NSUB = TT // 128
    assert (nblk * 128) % TT == 0
    with c.phase():
        W2 = c.sb('W2', [128, NF, D], BF16)
        xT = c.sb('xT', [128, 8, TT], BF16)
        hT = c.sb('hT', [128, NF, TT], BF16)
        W1 = [c.sb('W1_%d' % i, [128, 8, 512], BF16) for i in range(2)]
        wst = [c.sb('wst%d' % i, [128, 2048], F32) for i in range(3)]
        xin = [c.sb('xin%d' % i, [128, D], F32) for i in range(2)]
        xbf = [c.sb('xbf%d' % i, [128, D], BF16) for i in range(2)]
        sg = [c.sb('sg%d' % i, [128, 512], BF16) for i in range(2)]
        yb = [c.sb('yb%d' % i, [128, D], F32) for i in range(2)]
        S = dict(K)
        S['st6'] = c.sb('st6', [128, 2, 6], F32)
        S['mv'] = c.sb('mv', [128, 4], F32)
        S['epsc'] = c.sb('epsc', [128, 1], F32)
        S['gB'] = c.sb('gB', [128, D], F32)
        S['bB'] = c.sb('bB', [128, D], F32)
        S['gT'] = c.sb('gT', [128, 8], F32)
        S['bT'] = c.sb('bT', [128, 8], F32)
        c.emit('pool', lambda e: e.memset(S['epsc'][:], LN_EPS / (ALPHA * ALPHA)), [S['epsc'][:]], [])
        c.dma(S['gB'][:], g_d[0:1, :].partition_broadcast(128))
        c.dma(S['bB'][:], b_d[0:1, :].partition_broadcast(128))
        post('setup', S, None, None, None)
        wi = 0
        w2v = w_out.rearrange("(f p) n -> p f n", p=128)
        for f in range(0, NF, 2):
            load_cast(c, W2[:, f:f + 2, :], w2v[:, f:f + 2, :], wst[wi % 3])
            wi += 1
        w1v = w_in.rearrange("(k p) n -> p k n", p=128)
        xi = 0
        for t in range(nblk * 128 // TT):
            for s in range(NSUB):
                blk = t * NSUB + s
                xs_ = xin[xi % 2]
                xb_ = xbf[xi % 2]
                xi += 1
                c.dma(xs_[:], xsrc[blk * 128:(blk + 1) * 128, :])
                c.copy('act', xb_[:], xs_[:])
                pb = c.ps()
                pT = pb.bitcast(BF16)
                for k in range(8):
                    c.tr(pT[:, k * 128:(k + 1) * 128], xb_[:, k * 128:(k + 1) * 128], K['identb'][:])
                c.copy('dve', xT[:, :, s * 128:(s + 1) * 128],
                       pT[:, 0:1024].rearrange("p (k t) -> p k t", k=8))
            for fp in range(NF // 2):
                w1 = W1[fp % 2]
                for gu in range(2):
                    col = gu * DFF + fp * 256
                    load_cast(c, w1[:, :, gu * 256:(gu + 1) * 256], w1v[:, :, col:col + 256], wst[wi % 3],
                              eng='pool' if gu == 0 else 'dve')
                    wi += 1
                for fi in range(2):
                    f = fp * 2 + fi
                    for h in range(TT // 512):
                        pg = c.ps()
                        pu = c.ps()
                        for k in range(8):
                            c.mm(pg[:], w1[:, k, fi * 128:(fi + 1) * 128], xT[:, k, h * 512:(h + 1) * 512],
                                 start=(k == 0), stop=(k == 7))
                        for k in range(8):
                            c.mm(pu[:], w1[:, k, 256 + fi * 128:256 + (fi + 1) * 128],
                                 xT[:, k, h * 512:(h + 1) * 512], start=(k == 0), stop=(k == 7))
                        s_ = sg[(f * 2 + h) % 2]
                        c.act(s_[:], pg[:], AF.Silu)
                        c.tt('dve', hT[:, f, h * 512:(h + 1) * 512], s_[:], pu[:], ALU.mult)
            for s in range(NSUB):
                blk = t * NSUB + s
                xs_ = xin[xi % 2]
                y = yb[xi % 2]
                xi += 1
                c.dma(xs_[:], xsrc[blk * 128:(blk + 1) * 128, :])
                for h in range(2):
                    po = c.ps()
                    for f in range(NF):
                        c.mm(po[:], hT[:, f, s * 128:(s + 1) * 128], W2[:, f, h * 512:(h + 1) * 512],
                             start=(f == 0), stop=(f == NF - 1))
                    c.stt(y[:, h * 512:(h + 1) * 512], po[:], 0.5 / ALPHA, xs_[:, h * 512:(h + 1) * 512],
                          ALU.mult, ALU.add)
                mean, rstd = layer_norm_stats(c, S, y, None)
                post(blk, S, y, mean, rstd)


O_GS, O_GA, O_Z, O_XBC, O_DT, O_Q, O_CKV, O_QI, O_KI, O_WI = 0, 1024, 2048, 3072, 5120, 5136, 6160, 6416, 7440, 7504
D_IN = 7520
RMS_EPS = 1e-6


def proj_phase_all(c, K, G, nblk):
    w_in = G['w_in']
    wv = w_in.rearrange("(k p) n -> p k n", p=128)
    with c.phase():
        Wx = c.sb('Wx', [128, 8, 2048], BF16)
        Wk = c.sb('Wk', [128, 8, 128], BF16)
        Wd = c.sb('Wd', [128, 8, 16], BF16)
        Wc = c.sb('Wc', [128, 8, 256], BF16)
        wst = [c.sb('wst%d' % i, [128, 2048], F32) for i in range(3)]
        xg = [c.sb('xg%d' % i, [128, 8, 512], BF16) for i in range(2)]
        xo = [c.sb('xo%d' % i, [128, 16, 512], BF16) for i in range(2)]
        ko = [c.sb('ko%d' % i, [128, 512], BF16) for i in range(2)]
        dto = [c.sb('dto%d' % i, [128, 16], F32) for i in range(2)]
        dte = [c.sb('dte%d' % i, [128, 16], F32) for i in range(2)]
        cko = [c.sb('cko%d' % i, [128, 257], BF16) for i in range(2)]
        ckf = [c.sb('ckf%d' % i, [128, 256], F32) for i in range(2)]
        ckT = [c.sb('ckT%d' % i, [128, 2, 128], BF16) for i in range(2)]
        sq = c.sb('sq', [128, 256], F32)
        ss = c.sb('ss', [128, 4], F32)
        dtb = c.sb('dtb', [128, 16], F32)
        kvg = c.sb('kvg', [128, 256], F32)
        epsr = c.sb('epsr', [128, 1], F32)
        c.emit('pool', lambda e: e.memset(epsr[:], RMS_EPS), [epsr[:]], [])
        c.dma(dtb[:], G['dt_bias'][0:1, :].partition_broadcast(128))
        c.dma(kvg[:], G['kv_norm_w'][0:1, :].partition_broadcast(128))
        wi = 0
        for m in range(0, 16, 2):
            load_cast(c, Wx[:, :, m * 128:(m + 2) * 128], wv[:, :, O_XBC + m * 128:O_XBC + (m + 2) * 128], wst[wi % 3]); wi += 1
        load_cast(c, Wc[:], wv[:, :, O_CKV:O_CKV + 256], wst[wi % 3]); wi += 1
        for hf in range(2):
            load_cast(c, Wk[:, :, hf * 64:(hf + 1) * 64], wv[:, :, O_KI:O_KI + 64], wst[wi % 3]); wi += 1
        load_cast(c, Wd[:], wv[:, :, O_DT:O_DT + 16], wst[wi % 3]); wi += 1
        xbcv = G['xbcT_d'].rearrange("m p t -> p m t")
        for g in range(nblk // 4):
            x_ = xg[g % 2]
            for b in range(4):
                c.dma(x_[:, :, b * 128:(b + 1) * 128], G['x1T_d'][g * 4 + b])
            xo_ = xo[g % 2]
            for m in range(16):
                p = c.ps()
                for k in range(8):
                    c.mm(p[:], Wx[:, k, m * 128:(m + 1) * 128], x_[:, k, :], start=(k == 0), stop=(k == 7))
                c.copy('act' if m % 2 == 0 else 'dve', xo_[:, m, :], p[:])
            for mh in range(2):
                c.dma(xbcv[:, mh * 8:mh * 8 + 8, g * 512:(g + 1) * 512], xo_[:, mh * 8:mh * 8 + 8, :])
            p = c.ps()
            for k in range(8):
                c.mm(p[:], Wk[:, k, :], x_[:, k, :], start=(k == 0), stop=(k == 7))
            ko_ = ko[g % 2]
            c.copy('dve', ko_[:], p[:])
            c.dma(G['kiT_d'][:, g * 512:(g + 1) * 512], ko_[:])
            for b in range(4):
                blk = g * 4 + b
                p = c.ps()
                for k in range(8):
                    c.mm(p[:, 0:16], x_[:, k, b * 128:(b + 1) * 128], Wd[:, k, :], start=(k == 0), stop=(k == 7))
                e_ = dte[blk % 2]
                d_ = dto[blk % 2]
                c.tt('dve', e_[:], p[:, 0:16], dtb[:], ALU.add)
                c.act(e_[:], e_[:], AF.Exp)
                c.act(d_[:], e_[:], AF.Ln, bias=1.0)
                if blk == 0:
                    c.ts('dve', d_[:], d_[:], K['v0'][:, 0:1], None, ALU.mult)
                c.dma(G['dt_d'][blk * 128:(blk + 1) * 128, :], d_[:])
                p = c.ps()
                for k in range(8):
                    c.mm(p[:, 0:256], x_[:, k, b * 128:(b + 1) * 128], Wc[:, k, :], start=(k == 0), stop=(k == 7))
                f_ = ckf[blk % 2]
                c.copy('dve', f_[:], p[:, 0:256])
                c.act(sq[:], f_[:], AF.Square, accum=ss[:, 0:1])
                c.act(ss[:, 1:2], ss[:, 0:1], AF.Sqrt, bias=epsr[:, 0:1], scale=1.0 / 256)
                c.emit('dve', lambda e: e.reciprocal(out=ss[:, 2:3], in_=ss[:, 1:2]), [ss[:, 2:3]], [ss[:, 1:2]])
                o_ = cko[blk % 2]
                c.stt(o_[:, 0:256], f_[:], ss[:, 2:3], kvg[:], ALU.mult, ALU.mult)
                c.emit('pool', lambda e, o_=o_: e.memset(o_[:, 256:257], 1.0), [o_[:, 256:257]], [])
                c.dma(G['ckvtok_d'][blk], o_[:])
                pb = c.ps()
                pT = pb.bitcast(BF16)
                for r in range(2):
                    c.tr(pT[:, r * 128:(r + 1) * 128], o_[:, r * 128:(r + 1) * 128], K['identb'][:])
                t_ = ckT[blk % 2]
                c.copy('act', t_[:], pT[:, 0:256].rearrange("p (r t) -> p r t", r=2))
                c.dma(G['ckvT_d'].rearrange("r p t -> p r t")[:, :, blk * 128:(blk + 1) * 128], t_[:])


def proj_phase_own(c, K, G, nblk):
    wv = G['w_in'].rearrange("(k p) n -> p k n", p=128)
    nown = nblk // 2
    with c.phase():
        Wf = c.sb('Wf', [128, 8, 4096], BF16)
        Wz = c.sb('Wz', [128, 8, 1024], BF16)
        Ww = c.sb('Ww', [128, 8, 16], BF16)
        wst = [c.sb('wst%d' % i, [128, 2048], F32) for i in range(3)]
        xg = [c.sb('xg%d' % i, [128, 8, 512], BF16) for i in range(2)]
        fo = [c.sb('fo%d' % i, [128, 8, 512], BF16) for i in range(2)]
        zo = [c.sb('zo%d' % i, [128, 1024], BF16) for i in range(2)]
        wo = [c.sb('wo%d' % i, [128, 16], F32) for i in range(2)]
        wi = 0
        segs = [(O_GS, 'sgs_d', AF.Sigmoid), (O_GA, 'sga_d', AF.Sigmoid), (O_Q, 'qT_d', AF.Identity),
                (O_QI, 'qiT_d', AF.Identity)]
        for si, (off, _, _) in enumerate(segs):
            for m in range(0, 8, 2):
                load_cast(c, Wf[:, :, si * 1024 + m * 128:si * 1024 + (m + 2) * 128],
                          wv[:, :, off + m * 128:off + (m + 2) * 128], wst[wi % 3]); wi += 1
        for m in range(0, 8, 2):
            load_cast(c, Wz[:, :, m * 128:(m + 2) * 128], wv[:, :, O_Z + m * 128:O_Z + (m + 2) * 128], wst[wi % 3]); wi += 1
        load_cast(c, Ww[:], wv[:, :, O_WI:O_WI + 16], wst[wi % 3]); wi += 1
        fi = 0
        for g in range(nown // 4):
            x_ = xg[g % 2]
            for b in range(4):
                c.dma(x_[:, :, b * 128:(b + 1) * 128], G['x1T_d'][2 * (g * 4 + b) + 1])
            for si, (off, dst, fn) in enumerate(segs):
                f_ = fo[fi % 2]; fi += 1
                for m in range(8):
                    p = c.ps()
                    for k in range(8):
                        c.mm(p[:], Wf[:, k, si * 1024 + m * 128:si * 1024 + (m + 1) * 128], x_[:, k, :],
                             start=(k == 0), stop=(k == 7))
                    if fn == AF.Identity and m % 2 == 1:
                        c.copy('dve', f_[:, m, :], p[:])
                    else:
                        c.act(f_[:, m, :], p[:], fn)
                c.dma(G[dst].rearrange("m p t -> p m t")[:, :, g * 512:(g + 1) * 512], f_[:])
            for b in range(4):
                ob = g * 4 + b
                z_ = zo[ob % 2]
                for h in range(2):
                    p = c.ps()
                    for k in range(8):
                        c.mm(p[:], x_[:, k, b * 128:(b + 1) * 128], Wz[:, k, h * 512:(h + 1) * 512],
                             start=(k == 0), stop=(k == 7))
                    c.act(z_[:, h * 512:(h + 1) * 512], p[:], AF.Silu)
                c.dma(G['zs_d'][ob * 128:(ob + 1) * 128, :], z_[:])
                p = c.ps()
                for k in range(8):
                    c.mm(p[:, 0:16], x_[:, k, b * 128:(b + 1) * 128], Ww[:, k, :], start=(k == 0), stop=(k == 7))
                w_ = wo[ob % 2]
                c.act(w_[:], p[:, 0:16], AF.Copy, scale=0.25)
                c.dma(G['widx_d'][ob * 128:(ob + 1) * 128, :], w_[:])


def ssd_phase(c, K, G, nblk):
    with c.phase():
        cw = c.sb('cw', [128, 16, 4], F32)
        cb = c.sb('cb', [128, 16], F32)
        tri = c.sb('tri', [128, 128], BF16)
        ones = c.sb('ones', [128, 128], BF16)
        negm = c.sb('negm', [128, 512], BF16)
        sel = c.sb('sel', [128, 2048], BF16)
        apad = c.sb('apad', [128, 2, 128], BF16)
        ahl = c.sb('ahl', [128, 2, 16], BF16)
        ares = c.sb('ares', [128, 16], F32)
        Abc = c.sb('Abc', [128, 16], F32)
        Dbc = c.sb('Dbc', [128, 16], F32)
        nwB = c.sb('nwB', [128, D], F32)
        epsr = c.sb('epsr', [128, 1], F32)
        S32 = c.sb('S32', [128, 16, 64], F32)
        Sbf = c.sb('Sbf', [128, 16, 64], BF16)
        for t_, n_ in ((cw, 'cwT'), (cb, 'cbT'), (tri, 'trib'), (ones, 'ones128b'),
                       (negm, 'negmask4b'), (sel, 'sel16b')):
            c.dma(t_[:], G[n_])
        c.dma(Abc[:], G['a_log'][0:1, :].partition_broadcast(128))
        c.dma(Dbc[:], G['d_skip'][0:1, :].partition_broadcast(128))
        c.dma(nwB[:], G['ssd_norm_w'][0:1, :].partition_broadcast(128))
        c.act(Abc[:], Abc[:], AF.Exp)
        c.ts('dve', Abc[:], Abc[:], -1.0, None, ALU.mult)
        c.emit('dve', lambda e: e.memset(epsr[:], RMS_EPS), [epsr[:]], [])
        c.emit('dve', lambda e: e.memset(S32[:], 0.0), [S32[:]], [])
        c.emit('dve', lambda e: e.memset(Sbf[:], 0.0), [Sbf[:]], [])
        c.emit('dve', lambda e: e.memset(apad[:], 0.0), [apad[:]], [])
        xh = [c.sb('xh%d' % i, [128, 16, 132], BF16) for i in range(2)]
        t1 = [c.sb('t1_%d' % i, [128, 16, 128], F32) for i in range(3)]
        xc = [c.sb('xc%d' % i, [128, 16, 128], BF16) for i in range(2)]
        xtok = [c.sb('xtok%d' % i, [128, 16, 64], F32) for i in range(2)]
        Btok = [c.sb('Btok%d' % i, [128, 4, 128], BF16) for i in range(2)]
        xdt = [c.sb('xdt%d' % i, [128, 16, 64], BF16) for i in range(2)]
        xdd = [c.sb('xdd%d' % i, [128, 16, 64], BF16) for i in range(2)]
        dtt = [c.sb('dtt%d' % i, [128, 16], F32) for i in range(2)]
        sm = [c.sb('sm%d' % i, [128, 6, 16], F32) for i in range(2)]
        acsT = [c.sb('acsT%d' % i, [128, 2, 128], BF16) for i in range(2)]
        acsTf = c.sb('acsTf', [128, 128], F32)
        LT = c.sb('LT', [128, 16, 128], F32)
        MT = c.sb('MT', [128, 16, 128], BF16)
        yo = c.sb('yo', [128, 16, 64], F32)
        yy = c.sb('yy', [128, 16, 64], F32)
        zt = [c.sb('zt%d' % i, [128, D], BF16) for i in range(2)]
        sq = c.sb('sqs', [128, 256], F32)
        gs = c.sb('gs', [128, 12], F32)
        ynb = c.sb('ynb', [128, D], BF16)
        ynT = [c.sb('ynT%d' % i, [128, 8, 128], BF16) for i in range(2)]
        for i in range(2):
            c.emit('dve', lambda e, i=i: e.memset(xh[i][:], 0.0), [xh[i][:]], [])
        xbv = G['xbcT_d'].rearrange("m p t -> p m t")
        for ch in range(nblk):
            own = ch % 2 == 1
            x_ = xh[ch % 2]
            for mh in range(2):
                ms = slice(mh * 8, mh * 8 + 8)
                if ch == 0:
                    c.dma(x_[:, ms, 4:132], xbv[:, ms, 0:128])
                else:
                    c.dma(x_[:, ms, 0:132], xbv[:, ms, ch * 128 - 4:ch * 128 + 128])
            d_ = dtt[ch % 2]
            c.dma(d_[:], G['dt_d'][ch * 128:(ch + 1) * 128, :])
            a_ = t1[0]
            b_ = t1[1]
            c.tt('dve', a_[:], x_[:, :, 1:129], cw[:, :, 0:1].to_broadcast([128, 16, 128]), ALU.mult)
            for k in range(1, 4):
                b_ = t1[1 + (k % 2)]
                c.tt('dve', b_[:], x_[:, :, k + 1:k + 129], cw[:, :, k:k + 1].to_broadcast([128, 16, 128]), ALU.mult)
                c.tt('pool', a_[:], a_[:], b_[:], ALU.add)
            c.tt('dve', a_[:], a_[:], cb[:].unsqueeze(2).to_broadcast([128, 16, 128]), ALU.add)
            xc_ = xc[ch % 2]
            c.act(xc_[:], a_[:], AF.Silu)
            p0 = c.ps(); p0b = p0.bitcast(BF16)
            p1 = c.ps(); p1b = p1.bitcast(BF16)
            for m in range(8):
                c.tr(p0b[:, m * 128:(m + 1) * 128], xc_[:, m, :], K['identb'][:])
            for m in range(4):
                c.tr(p1b[:, m * 128:(m + 1) * 128], xc_[:, 8 + m, :], K['identb'][:])
            xt_ = xtok[ch % 2]
            Bt_ = Btok[ch % 2]
            c.copy('act', xt_[:], p0b[:, 0:1024].rearrange("p (h q) -> p h q", h=16))
            c.copy('act', Bt_[:], p1b[:, 0:512].rearrange("p (g n) -> p g n", g=4))
            xd_ = xdt[ch % 2]
            c.tt('dve', xd_[:], xt_[:], d_[:].unsqueeze(2).to_broadcast([128, 16, 64]), ALU.mult)
            s_ = sm[ch % 2]
            c.tt('dve', s_[:, 0, :], d_[:], Abc[:], ALU.mult)
            c.copy('dve', ahl[:, 0, :], s_[:, 0, :])
            c.tt('dve', ares[:], s_[:, 0, :], ahl[:, 0, :], ALU.subtract)
            c.copy('dve', ahl[:, 1, :], ares[:])
            c.copy('dve', apad[:, :, 0:16], ahl[:])
            pc = c.ps()
            for q_ in range(2):
                c.mm(pc[:, 0:16], tri[:], ahl[:, q_, :], start=(q_ == 0), stop=(q_ == 1))
            pc1 = c.ps()
            for q_ in range(2):
                c.mm(pc1[:, 0:16], ones[:], ahl[:, q_, :], start=(q_ == 0), stop=(q_ == 1))
            pc2 = c.ps()
            for q_ in range(2):
                c.mm(pc2[:, 0:128], apad[:, q_, :], tri[:], start=(q_ == 0), stop=(q_ == 1))
            c.copy('dve', s_[:, 1, :], pc[:, 0:16])
            c.ts('dve', s_[:, 2, :], pc[:, 0:16], -1.0, None, ALU.mult)
            c.act(s_[:, 3, :], pc[:, 0:16], AF.Exp)
            c.tt('dve', s_[:, 4, :], pc1[:, 0:16], s_[:, 1, :], ALU.subtract)
            c.act(s_[:, 4, :], s_[:, 4, :], AF.Exp)
            c.act(s_[:, 5, :], pc1[:, 0:16], AF.Exp)
            aT_ = acsT[ch % 2]
            c.copy('dve', acsTf[:], pc2[:, 0:128])
            c.copy('dve', aT_[:, 0, :], acsTf[:])
            c.tt('dve', acsTf[:], acsTf[:], aT_[:, 0, :], ALU.subtract)
            c.copy('dve', aT_[:, 1, :], acsTf[:])
            if own:
                ob = ch // 2
                for q in range(4):
                    pl = c.ps()
                    c.mm(pl[:], K['identb'][:], negm[:], start=True, stop=False)
                    for j in range(4):
                        h = q * 4 + j
                        for q_ in range(2):
                            c.mm(pl[:, j * 128:(j + 1) * 128], sel[:, h * 128:(h + 1) * 128], aT_[:, q_, :],
                                 start=False, stop=(j == 3 and q_ == 1))
                    for j in range(4):
                        h = q * 4 + j
                        c.act(LT[:, h, :], pl[:, j * 128:(j + 1) * 128], AF.Exp, bias=s_[:, 2, h:h + 1])
                pcb = c.ps()
                for g in range(4):
                    c.mm(pcb[:, g * 128:(g + 1) * 128], xc_[:, 8 + g, :], xc_[:, 12 + g, :])
                c.tt('dve', MT[:].rearrange("p (g j) l -> p g j l", g=4),
                     LT[:].rearrange("p (g j) l -> p g j l", g=4),
                     pcb[:].rearrange("p (g l) -> p g l", g=4).unsqueeze(2).to_broadcast([128, 4, 4, 128]),
                     ALU.mult)
                py = [c.ps(), c.ps()]
                for h in range(16):
                    c.mm(py[h // 8][:, (h % 8) * 64:(h % 8 + 1) * 64], MT[:, h, :], xd_[:, h, :])
                po = [c.ps(), c.ps()]
                for g in range(4):
                    c.mm(po[g // 2][:, (g % 2) * 256:(g % 2 + 1) * 256], xc_[:, 12 + g, :],
                         Sbf[:, 4 * g:4 * g + 4, :].rearrange("p h q -> p (h q)"))
                for hf in range(2):
                    hs = slice(hf * 8, hf * 8 + 8)
                    c.tt('dve', yo[:, hs, :], po[hf][:].rearrange("p (h q) -> p h q", h=8),
                         s_[:, 3, hs].unsqueeze(2).to_broadcast([128, 8, 64]), ALU.mult)
                    c.tt('dve', yy[:, hs, :], py[hf][:].rearrange("p (h q) -> p h q", h=8), yo[:, hs, :], ALU.add)
                c.tt('dve', yo[:], xt_[:], Dbc[:].unsqueeze(2).to_broadcast([128, 16, 64]), ALU.mult)
                c.tt('dve', yy[:], yy[:], yo[:], ALU.add)
                z_ = zt[ob % 2]
                c.dma(z_[:], G['zs_d'][ob * 128:(ob + 1) * 128, :])
                yf = yy[:].rearrange("p h q -> p (h q)")
                c.tt('dve', yf, yf, z_[:], ALU.mult)
                for g in range(4):
                    c.act(sq[:], yf[:, g * 256:(g + 1) * 256], AF.Square, accum=gs[:, g:g + 1])
                c.act(gs[:, 4:8], gs[:, 0:4], AF.Sqrt, bias=epsr[:, 0:1], scale=1.0 / 256)
                c.emit('dve', lambda e: e.reciprocal(out=gs[:, 8:12], in_=gs[:, 4:8]), [gs[:, 8:12]], [gs[:, 4:8]])
                c.tt('dve', yy[:].rearrange("p (g j) q -> p g (j q)", g=4),
                     yy[:].rearrange("p (g j) q -> p g (j q)", g=4),
                     gs[:, 8:12].unsqueeze(2).to_broadcast([128, 4, 256]), ALU.mult)
                c.tt('dve', ynb[:], yf, nwB[:], ALU.mult)
                pb = c.ps(); pT = pb.bitcast(BF16)
                for m in range(8):
                    c.tr(pT[:, m * 128:(m + 1) * 128], ynb[:, m * 128:(m + 1) * 128], K['identb'][:])
                yT_ = ynT[ob % 2]
                c.copy('act', yT_[:], pT[:, 0:1024].rearrange("p (m t) -> p m t", m=8))
                c.dma(G['yssdT_d'][ob], yT_[:])
            xe_ = xdd[ch % 2]
            c.tt('dve', xe_[:], xd_[:], s_[:, 4, :].unsqueeze(2).to_broadcast([128, 16, 64]), ALU.mult)
            pst = [c.ps(), c.ps()]
            for g in range(4):
                c.mm(pst[g // 2][:, (g % 2) * 256:(g % 2 + 1) * 256], Bt_[:, g, :],
                     xe_[:, 4 * g:4 * g + 4, :].rearrange("p h q -> p (h q)"))
            c.tt('dve', S32[:], S32[:], s_[:, 5, :].unsqueeze(2).to_broadcast([128, 16, 64]), ALU.mult)
            for hf in range(2):
                hs = slice(hf * 8, hf * 8 + 8)
                c.tt('dve', S32[:, hs, :], S32[:, hs, :], pst[hf][:].rearrange("p (h q) -> p h q", h=8), ALU.add)
            c.copy('act', Sbf[:], S32[:])


LAST_INPUTS = []
LIMIT = 10 ** 9
NIT_BISECT = 16
TOPK = 256


def dsa_phase(c, K, G, nblk):
    ntok = nblk * 128
    nown = nblk // 2
    with c.phase():
        kiT = c.sb('kiT', [128, ntok], BF16)
        ckvT = c.sb('ckvT', [128, 2, ntok], BF16)
        ckvtok = c.sb('ckvtok', [128, nblk, 257], BF16)
        Wuk = c.sb('Wuk', [128, 16, 256], BF16)
        qiz = c.sb('qiz', [128, 16, 128], BF16)
        Wuv = c.sb('Wuv', [128, 16, 2, 128], BF16)
        wst = [c.sb('wst%d' % i, [128, 2048], F32) for i in range(1)]
        LTt = [c.sb('LTt%d' % i, [128, ntok], BF16) for i in range(2)]
        Rt = [c.sb('Rt%d' % i, [128, 512], BF16) for i in range(4)]
        ohp = c.sb('ohp', [128, 16], F32)
        nbpad = c.sb('nbpad', [128, 128], BF16)
        prow1 = c.sb('prow1', [128, ntok], F32)
        negsl = c.sb('negsl', [128, 16], F32)
        negc = c.sb('negc', [128, 128], F32)
        identf = c.sb('identf2', [128, 128], F32)
        sc = c.sb('sc', [128, ntok], F32)
        tmpn = c.sb('tmpn', [128, ntok], F32)
        m01 = c.sb('m01', [128, ntok], BF16)
        mbT = c.sb('mbT', [128, nblk, 128], BF16)
        qT = [c.sb('qT%d' % i, [128, 8, 128], BF16) for i in range(2)]
        qiT = [c.sb('qiT%d' % i, [128, 8, 128], BF16) for i in range(2)]
        wid = [c.sb('wid%d' % i, [128, 16], F32) for i in range(2)]
        qlat = c.sb('qlat', [128, 2, 16, 128], BF16)
        rl = [c.sb('rl%d' % i, [128, 512], F32) for i in range(3)]
        PT = [c.sb('PT%d' % i, [128, 512], BF16) for i in range(3)]
        osb = c.sb('osb', [128, 4, 256], BF16)
        oT = c.sb('oT', [128, 16, 2, 128], BF16)
        yat = [c.sb('yat%d' % i, [128, 8, 128], BF16) for i in range(2)]
        bs = c.sb('bs', [128, 12], F32)
        nb = c.sb('nb', [128, 16], F32)
        c.dma(kiT[:], G['kiT_d'])
        c.dma(ckvT[:], G['ckvT_d'].rearrange("r p t -> p r t"))
        for b0 in range(0, nblk, 8):
            c.dma(ckvtok[:, b0:b0 + 8, :], G['ckvtok_d'].rearrange("b p f -> p b f")[:, b0:b0 + 8, :])
        for i in range(2):
            c.dma(LTt[i][:], G['alibiLT'][i][:, 0:ntok])
        for i in range(4):
            c.dma(Rt[i][:], G['alibiR'][i])
        c.emit('dve', lambda e: e.memset(qiz[:], 0.0), [qiz[:]], [])
        c.dma(ohp[:], G['ohp'])
        c.emit('dve', lambda e: e.memset(nbpad[:], 0.0), [nbpad[:]], [])
        c.dma(prow1[:], G['prow1'][0:1, 0:ntok].partition_broadcast(128))
        c.dma(negsl[:], G['negslope'][0:1, :].partition_broadcast(128))
        c.dma(negc[:], G['negcausal'])
        c.dma(identf[:], G['identf'])
        c.emit('dve', lambda e: e.memset(Wuk[:], 0.0), [Wuk[:]], [])
        svk = wst[0][:, 0:2048].rearrange("p (a b) -> p a b", a=8)
        c.dma(svk, G['w_uk'].rearrange("(c p) r -> p c r", p=128))
        Wukv = Wuk[:].rearrange("p (c e) r -> p c e r", e=2)
        c.copy('dve', Wukv[0:64, :, 0, :], svk[0:64, :, :])
        c.copy('dve', Wukv[64:128, :, 1, :], svk[64:128, :, :])
        c.emit('dve', lambda e: e.memset(Wuv[:], 0.0), [Wuv[:]], [])
        for h in range(16):
            st = wst[0]
            sv = st[:, 0:128].rearrange("p (a b) -> p a b", a=2)
            c.dma(sv, G['w_uv'][h].rearrange("(rc p) d -> p rc d", p=128))
            c.copy('dve', Wuv[:, h, :, (h % 2) * 64:(h % 2 + 1) * 64], sv)
        pool_banks = c.banks[0:4]
        acc_banks = c.banks[4:8]
        allbanks = c.banks
        for ob in range(nown):
            jb = 2 * ob + 1
            NKT = jb + 1
            NK = NKT * 128
            q_ = qT[ob % 2]; qi_ = qiT[ob % 2]; w_ = wid[ob % 2]
            c.dma(q_[:], G['qT_d'].rearrange("m p t -> p m t")[:, :, ob * 128:(ob + 1) * 128])
            c.dma(qi_[:], G['qiT_d'].rearrange("m p t -> p m t")[:, :, ob * 128:(ob + 1) * 128])
            c.dma(w_[:], G['widx_d'][ob * 128:(ob + 1) * 128, :])
            c.banks = allbanks
            for rc in range(2):
                for hg in range(4):
                    p = c.ps()
                    for j in range(4):
                        h = hg * 4 + j
                        c.mm(p[:, j * 128:(j + 1) * 128], Wuk[:, h, rc * 128:(rc + 1) * 128], q_[:, h // 2, :])
                    c.act(qlat[:, rc, hg * 4:hg * 4 + 4, :], p[:].rearrange("p (j t) -> p j t", j=4), AF.Copy, scale=0.125)
            qizv = qiz[:].rearrange("p (c e) t -> p c e t", e=2)
            c.copy('dve', qizv[0:64, :, 0, :], qi_[0:64, :, :])
            c.copy('dve', qizv[64:128, :, 1, :], qi_[64:128, :, :])
            ri = 0
            for s0 in range(0, NK, 512):
                w = min(512, NK - s0)
                for h in range(16):
                    p = c.ps()
                    c.mm(p[:, 0:w], qiz[:, h, :], kiT[:, s0:s0 + w])
                    r_ = rl[ri % 3]; ri += 1
                    c.act(r_[:, 0:w], p[:, 0:w], AF.Relu)
                    if h == 0:
                        c.ts('dve', sc[:, s0:s0 + w], r_[:, 0:w], w_[:, 0:1], None, ALU.mult)
                    else:
                        c.stt(sc[:, s0:s0 + w], r_[:, 0:w], w_[:, h:h + 1], sc[:, s0:s0 + w], ALU.mult, ALU.add)
            lo, hi, mid, cnt, ge, dd, n1 = [bs[:, i:i + 1] for i in range(7)]
            if ob > 0:
                c.emit('dve', lambda e: e.tensor_reduce(out=lo, in_=sc[:, 128:jb * 128], axis=AX.X, op=ALU.min),
                       [lo], [sc[:, 128:jb * 128]])
            c.tt('dve', sc[:, jb * 128:NK], sc[:, jb * 128:NK], negc[:], ALU.add)
            c.ts('dve', sc[:, 0:128], sc[:, 0:128], K['kb0'][:, 0:1], None, ALU.add)
            if ob > 0:
                c.emit('dve', lambda e: e.tensor_reduce(out=hi, in_=sc[:, 0:NK], axis=AX.X, op=ALU.max),
                       [hi], [sc[:, 0:NK]])
                c.ts('dve', hi, hi, 1.0, None, ALU.add)
                for it in range(NIT_BISECT):
                    c.ts('dve', mid, lo, hi, 0.5, ALU.add, ALU.mult)
                    c.ts('dve', tmpn[:, 0:NK], sc[:, 0:NK], mid, 0.0, ALU.is_ge, ALU.add, accum=cnt)
                    c.ts('dve', ge, cnt, TOPK - 0.5, None, ALU.is_ge)
                    c.tt('dve', dd, mid, lo, ALU.subtract)
                    c.stt(lo, dd, ge, lo, ALU.mult, ALU.add)
                    c.tt('dve', dd, hi, mid, ALU.subtract)
                    c.stt(hi, dd, ge, mid, ALU.mult, ALU.add)
                c.ts('dve', m01[:, 0:NK], sc[:, 0:NK], lo, None, ALU.is_ge)
            else:
                c.ts('dve', m01[:, 0:NK], sc[:, 0:NK], -1e29, None, ALU.is_ge)
            c.tt('dve', tmpn[:, 0:NK], m01[:, 0:NK], prow1[:, 0:NK], ALU.mult)
            c.emit('dve', lambda e: e.tensor_reduce(out=n1, in_=tmpn[:, 0:NK], axis=AX.X, op=ALU.max),
                   [n1], [tmpn[:, 0:NK]])
            c.ts('dve', n1, n1, -1.0, None, ALU.add)
            c.ts('dve', nbpad[:].rearrange("p (g q) -> p g q", g=2)[:, :, 0:16],
                 negsl[:].unsqueeze(1).to_broadcast([128, 2, 16]), n1, None, ALU.mult)
            pnb = c.ps()
            pn = pnb.bitcast(BF16)
            c.tr(pn[:, 0:128], nbpad[:], K['identb'][:])
            for h in range(16):
                hg = h // 4
                pr = slice(64 * (hg % 2), 64 * (hg % 2) + 16)
                c.ts('dve', Rt[hg][pr, (h % 4) * 128:(h % 4 + 1) * 128], pn[pr, 0:128],
                     ohp[pr, h:h + 1], None, ALU.mult)
            for k0 in range(0, NKT, 4):
                nk = min(4, NKT - k0)
                pb = c.ps(); pT = pb.bitcast(BF16)
                for i in range(nk):
                    c.tr(pT[:, i * 128:(i + 1) * 128], m01[:, (k0 + i) * 128:(k0 + i + 1) * 128], K['identb'][:])
                c.ts('dve', mbT[:, k0:k0 + nk, :], pT[:, 0:nk * 128].rearrange("p (k t) -> p k t", k=nk),
                     -1.0, 30000.0, ALU.add, ALU.mult)
            c.banks = pool_banks
            pi = 0
            for hg in range(4):
                for kt in range(NKT):
                    ks = slice(kt * 128, (kt + 1) * 128)
                    pS = c.ps()
                    c.mm(pS[:], ckvT[:, 0, ks], qlat[:, 0, hg * 4:hg * 4 + 4, :].rearrange("p j t -> p (j t)"),
                         start=True, stop=False)
                    c.mm(pS[:], ckvT[:, 1, ks], qlat[:, 1, hg * 4:hg * 4 + 4, :].rearrange("p j t -> p (j t)"),
                         start=False, stop=False)
                    c.mm(pS[:], LTt[hg // 2][:, ks], Rt[hg][:], start=False, stop=False)
                    c.mm(pS[:].rearrange("p (j t) -> p j t", j=4), K['identb'][:],
                         mbT[:, kt, :].unsqueeze(1).to_broadcast([128, 4, 128]), start=False, stop=True)
                    P_ = PT[pi % 3]; pi += 1
                    c.act(P_[:], pS[:], AF.Exp)
                    for j in range(4):
                        c.mm(acc_banks[j][:, 0:257], P_[:, j * 128:(j + 1) * 128], ckvtok[:, kt, :],
                             start=(kt == 0), stop=(kt == NKT - 1))
                for j in range(4):
                    rs = bs[:, 7 + j:8 + j]
                    c.emit('dve', lambda e, j=j, rs=rs: e.reciprocal(out=rs, in_=acc_banks[j][:, 256:257]),
                           [rs], [acc_banks[j][:, 256:257]])
                    c.act(osb[:, j, :], acc_banks[j][:, 0:256], AF.Copy, scale=rs)
                for rc in range(2):
                    pb = c.ps(); pT = pb.bitcast(BF16)
                    for j in range(4):
                        c.tr(pT[:, j * 128:(j + 1) * 128], osb[:, j, rc * 128:(rc + 1) * 128], K['identb'][:])
                    c.copy('dve', oT[:, hg * 4:hg * 4 + 4, rc, :], pT[:, 0:512].rearrange("p (j t) -> p j t", j=4))
            c.banks = allbanks
            y_ = yat[ob % 2]
            for cp in range(0, 8, 4):
                p = c.ps()
                for ci in range(4):
                    cidx = cp + ci
                    n_ = 0
                    for h in (2 * cidx, 2 * cidx + 1):
                        for rc in range(2):
                            c.mm(p[:, ci * 128:(ci + 1) * 128], Wuv[:, h, rc, :], oT[:, h, rc, :],
                                 start=(n_ == 0), stop=(n_ == 3))
                            n_ += 1
                c.copy('act', y_[:, cp:cp + 4, :], p[:].rearrange("p (c t) -> p c t", c=4))
            c.dma(G['yattT_d'][ob], y_[:])
        c.banks = allbanks


def load_w_resident(c, dst, src, wst, wi, ncols):
    sv = src.rearrange("(k p) n -> p k n", p=128)
    for m in range(0, ncols, 256):
        load_cast(c, dst[:, :, m:m + 256], sv[:, :, m:m + 256], wst[wi[0] % len(wst)])
        wi[0] += 1


def ln_block(c, S, y, eps_c, g_bc, b_bc, out32):
    st6, mv = S['st6'], S['mv']
    for h in range(2):
        c.emit('dve', lambda e, h=h: e.bn_stats(out=st6[:, h, :], in_=y[:, h * 512:(h + 1) * 512]),
               [st6[:, h, :]], [y[:, h * 512:(h + 1) * 512]])
    c.emit('dve', lambda e: e.bn_aggr(out=mv[:, 0:2], in_=st6[:]), [mv[:, 0:2]], [st6[:]])
    c.act(mv[:, 2:3], mv[:, 1:2], AF.Sqrt, bias=eps_c[:, 0:1])
    c.emit('dve', lambda e: e.reciprocal(out=mv[:, 3:4], in_=mv[:, 2:3]), [mv[:, 3:4]], [mv[:, 2:3]])
    c.ts('dve', out32, y, mv[:, 0:1], mv[:, 3:4], ALU.subtract, ALU.mult)
    c.tt('pool', out32, out32, g_bc, ALU.mult)
    c.tt('pool', out32, out32, b_bc, ALU.add)


def merge_phase(c, K, G, nblk):
    nown = nblk // 2
    with c.phase():
        Wps = c.sb('Wps', [128, 8, D], BF16)
        Wpa = c.sb('Wpa', [128, 8, D], BF16)
        Wo = c.sb('Wo', [128, 8, D], BF16)
        wst = [c.sb('wst%d' % i, [128, 2048], F32) for i in range(3)]
        wi = [0]
        load_w_resident(c, Wps, G['w_proj_ssd'], wst, wi, D)
        load_w_resident(c, Wpa, G['w_proj_att'], wst, wi, D)
        load_w_resident(c, Wo, G['w_out'], wst, wi, D)
        ys = [c.sb('ys%d' % i, [128, 8, 512], BF16) for i in range(2)]
        ya = [c.sb('ya%d' % i, [128, 8, 512], BF16) for i in range(2)]
        gs_ = [c.sb('gs%d' % i, [128, 8, 512], BF16) for i in range(2)]
        ga_ = [c.sb('ga%d' % i, [128, 8, 512], BF16) for i in range(2)]
        m1 = [c.sb('m1_%d' % i, [128, 512], F32) for i in range(2)]
        m2 = [c.sb('m2_%d' % i, [128, 512], F32) for i in range(2)]
        mT = c.sb('mT', [128, 8, 512], BF16)
        x1t = [c.sb('x1t%d' % i, [128, D], F32) for i in range(2)]
        yb = [c.sb('yb%d' % i, [128, D], F32) for i in range(2)]
        x2 = [c.sb('x2_%d' % i, [128, D], F32) for i in range(2)]
        x2b = [c.sb('x2b%d' % i, [128, D], BF16) for i in range(2)]
        x2T = [c.sb('x2T%d' % i, [128, 8, 128], BF16) for i in range(2)]
        S = dict(st6=c.sb('st6', [128, 2, 6], F32), mv=c.sb('mv', [128, 4], F32))
        epsc = c.sb('epsc', [128, 1], F32)
        gB = c.sb('gB', [128, D], F32); bB = c.sb('bB', [128, D], F32)
        c.emit('pool', lambda e: e.memset(epsc[:], LN_EPS / (ALPHA * ALPHA)), [epsc[:]], [])
        c.dma(gB[:], G['ln2_g'][0:1, :].partition_broadcast(128))
        c.dma(bB[:], G['ln2_b'][0:1, :].partition_broadcast(128))
        for g in range(nown // 4):
            i2 = g % 2
            for b in range(4):
                ob = g * 4 + b
                c.dma(ys[i2][:, :, b * 128:(b + 1) * 128], G['yssdT_d'][ob])
                c.dma(ya[i2][:, :, b * 128:(b + 1) * 128], G['yattT_d'][ob])
            c.dma(gs_[i2][:], G['sgs_d'].rearrange("m p t -> p m t")[:, :, g * 512:(g + 1) * 512])
            c.dma(ga_[i2][:], G['sga_d'].rearrange("m p t -> p m t")[:, :, g * 512:(g + 1) * 512])
            for m in range(8):
                p1 = c.ps(); p2 = c.ps()
                for k in range(8):
                    c.mm(p1[:], Wps[:, k, m * 128:(m + 1) * 128], ys[i2][:, k, :], start=(k == 0), stop=(k == 7))
                for k in range(8):
                    c.mm(p2[:], Wpa[:, k, m * 128:(m + 1) * 128], ya[i2][:, k, :], start=(k == 0), stop=(k == 7))
                c.tt('dve', m1[m % 2][:], p1[:], gs_[i2][:, m, :], ALU.mult)
                c.tt('dve', m2[m % 2][:], p2[:], ga_[i2][:, m, :], ALU.mult)
                c.tt('pool', mT[:, m, :], m1[m % 2][:], m2[m % 2][:], ALU.add)
            for b in range(4):
                ob = g * 4 + b
                i = ob % 2
                c.dma(x1t[i][:], G['x1_d'][ob * 128:(ob + 1) * 128, :])
                for h in range(2):
                    po = c.ps()
                    for k in range(8):
                        c.mm(po[:], mT[:, k, b * 128:(b + 1) * 128], Wo[:, k, h * 512:(h + 1) * 512],
                             start=(k == 0), stop=(k == 7))
                    c.stt(yb[i][:, h * 512:(h + 1) * 512], po[:], 1.0 / ALPHA, x1t[i][:, h * 512:(h + 1) * 512],
                          ALU.mult, ALU.add)
                ln_block(c, S, yb[i][:], epsc, gB[:], bB[:], x2[i][:])
                c.dma(G['x2_d'][ob * 128:(ob + 1) * 128, :], x2[i][:])
                c.copy('act', x2b[i][:], x2[i][:])
                pb = c.ps(); pT = pb.bitcast(BF16)
                for k in range(8):
                    c.tr(pT[:, k * 128:(k + 1) * 128], x2b[i][:, k * 128:(k + 1) * 128], K['identb'][:])
                c.copy('dve', x2T[i][:], pT[:, 0:1024].rearrange("p (k t) -> p k t", k=8))
                c.dma(G['x2T_d'][ob], x2T[i][:])


def memkv_phase(c, K, G):
    with c.phase():
        Wkv = c.sb('Wkv', [128, 8, 2048], BF16)
        wst = [c.sb('wst%d' % i, [128, 2048], F32) for i in range(3)]
        wi = [0]
        load_w_resident(c, Wkv, G['w_mkv'], wst, wi, 2048)
        mem32 = [c.sb('mem32_%d' % i, [128, D], F32) for i in range(2)]
        memb = [c.sb('memb%d' % i, [128, D], BF16) for i in range(2)]
        memT = c.sb('memT', [128, 8, 256], BF16)
        kmT = c.sb('kmT', [128, 8, 256], BF16)
        vm = c.sb('vm', [128, 2, D], BF16)
        for mt in range(2):
            c.dma(mem32[mt][:], G['mem'][mt * 128:(mt + 1) * 128, :])
            c.copy('act', memb[mt][:], mem32[mt][:])
            pb = c.ps(); pT = pb.bitcast(BF16)
            for k in range(8):
                c.tr(pT[:, k * 128:(k + 1) * 128], memb[mt][:, k * 128:(k + 1) * 128], K['identb'][:])
            c.copy('dve', memT[:, :, mt * 128:(mt + 1) * 128], pT[:, 0:1024].rearrange("p (k t) -> p k t", k=8))
        for m in range(8):
            p = c.ps()
            for k in range(8):
                c.mm(p[:, 0:256], Wkv[:, k, m * 128:(m + 1) * 128], memT[:, k, :], start=(k == 0), stop=(k == 7))
            c.copy('act', kmT[:, m, :], p[:, 0:256])
        for mt in range(2):
            for h in range(2):
                p = c.ps()
                for k in range(8):
                    c.mm(p[:], memT[:, k, mt * 128:(mt + 1) * 128], Wkv[:, k, 1024 + h * 512:1024 + (h + 1) * 512],
                         start=(k == 0), stop=(k == 7))
                c.copy('dve', vm[:, mt, h * 512:(h + 1) * 512], p[:])
        c.dma(G['kmT_d'], kmT[:])
        c.dma(G['vm_d'], vm[:])


def xattn_phase(c, K, G, nblk):
    nown = nblk // 2
    with c.phase():
        Wq = c.sb('Wq', [128, 8, D], BF16)
        Wmo = c.sb('Wmo', [128, 8, D], BF16)
        wst = [c.sb('wst%d' % i, [128, 2048], F32) for i in range(3)]
        wi = [0]
        load_w_resident(c, Wq, G['w_mq'], wst, wi, D)
        load_w_resident(c, Wmo, G['w_mo'], wst, wi, D)
        kmT = c.sb('kmT', [128, 8, 256], BF16)
        vm = c.sb('vm', [128, 2, D], BF16)
        c.dma(kmT[:], G['kmT_d'])
        c.dma(vm[:], G['vm_d'])
        xT = [c.sb('xT%d' % i, [128, 8, 512], BF16) for i in range(2)]
        qmT = c.sb('qmT', [128, 8, 512], BF16)
        x2t = [c.sb('x2t%d' % i, [128, D], F32) for i in range(2)]
        Pm = [c.sb('Pm%d' % i, [128, 4, 256], F32) for i in range(2)]
        Pn = [c.sb('Pn%d' % i, [128, 4, 256], BF16) for i in range(2)]
        PmT = [c.sb('PmT%d' % i, [128, 2, 4, 128], BF16) for i in range(2)]
        omT = [c.sb('omT%d' % i, [128, 8, 128], BF16) for i in range(2)]
        yb = [c.sb('yb%d' % i, [128, D], F32) for i in range(2)]
        x3 = [c.sb('x3_%d' % i, [128, D], F32) for i in range(2)]
        sm = c.sb('smx', [128, 16], F32)
        S = dict(st6=c.sb('st6', [128, 2, 6], F32), mv=c.sb('mv', [128, 4], F32))
        epsc = c.sb('epsc', [128, 1], F32)
        gB = c.sb('gB', [128, D], F32); bB = c.sb('bB', [128, D], F32)
        c.emit('pool', lambda e: e.memset(epsc[:], LN_EPS / (ALPHA * ALPHA)), [epsc[:]], [])
        c.dma(gB[:], G['ln3_g'][0:1, :].partition_broadcast(128))
        c.dma(bB[:], G['ln3_b'][0:1, :].partition_broadcast(128))
        for g in range(nown // 4):
            x_ = xT[g % 2]
            for b in range(4):
                c.dma(x_[:, :, b * 128:(b + 1) * 128], G['x2T_d'][g * 4 + b])
            for m in range(8):
                p = c.ps()
                for k in range(8):
                    c.mm(p[:], Wq[:, k, m * 128:(m + 1) * 128], x_[:, k, :], start=(k == 0), stop=(k == 7))
                c.act(qmT[:, m, :], p[:], AF.Copy, scale=1.0 / 16)
            for b in range(4):
                ob = g * 4 + b
                i = ob % 2
                c.dma(x2t[i][:], G['x2_d'][ob * 128:(ob + 1) * 128, :])
                pS = [c.ps(), c.ps()]
                for h in range(4):
                    for dc in range(2):
                        c.mm(pS[h // 2][:, (h % 2) * 256:(h % 2 + 1) * 256], qmT[:, 2 * h + dc, b * 128:(b + 1) * 128],
                             kmT[:, 2 * h + dc, :], start=(dc == 0), stop=(dc == 1))
                for hf in range(2):
                    c.emit('dve', lambda e, hf=hf: e.tensor_reduce(
                        out=sm[:, hf * 2:hf * 2 + 2], in_=pS[hf][:].rearrange("p (h m) -> p h m", h=2),
                        axis=AX.X, op=ALU.max), [sm[:, hf * 2:hf * 2 + 2]], [pS[hf][:]])
                c.ts('dve', sm[:, 4:8], sm[:, 0:4], -1.0, None, ALU.mult)
                for h in range(4):
                    c.act(Pm[i][:, h, :], pS[h // 2][:, (h % 2) * 256:(h % 2 + 1) * 256], AF.Exp,
                          bias=sm[:, 4 + h:5 + h], accum=sm[:, 8 + h:9 + h])
                c.emit('dve', lambda e: e.reciprocal(out=sm[:, 12:16], in_=sm[:, 8:12]), [sm[:, 12:16]], [sm[:, 8:12]])
                c.tt('dve', Pn[i][:], Pm[i][:], sm[:, 12:16].unsqueeze(2).to_broadcast([128, 4, 256]), ALU.mult)
                for mt in range(2):
                    pb = c.ps(); pT = pb.bitcast(BF16)
                    for h in range(4):
                        c.tr(pT[:, h * 128:(h + 1) * 128], Pn[i][:, h, mt * 128:(mt + 1) * 128], K['identb'][:])
                    c.copy('act', PmT[i][:, mt, :, :], pT[:, 0:512].rearrange("p (h t) -> p h t", h=4))
                for cp in range(0, 8, 4):
                    p = c.ps()
                    for ci in range(4):
                        cc = cp + ci
                        for mt in range(2):
                            c.mm(p[:, ci * 128:(ci + 1) * 128], vm[:, mt, cc * 128:(cc + 1) * 128],
                                 PmT[i][:, mt, cc // 2, :], start=(mt == 0), stop=(mt == 1))
                    c.copy('dve', omT[i][:, cp:cp + 4, :], p[:].rearrange("p (c t) -> p c t", c=4))
                for h in range(2):
                    po = c.ps()
                    for k in range(8):
                        c.mm(po[:], omT[i][:, k, :], Wmo[:, k, h * 512:(h + 1) * 512], start=(k == 0), stop=(k == 7))
                    c.stt(yb[i][:, h * 512:(h + 1) * 512], po[:], 1.0 / ALPHA, x2t[i][:, h * 512:(h + 1) * 512],
                          ALU.mult, ALU.add)
                ln_block(c, S, yb[i][:], epsc, gB[:], bB[:], x3[i][:])
                c.dma(G['x3_d'][ob * 128:(ob + 1) * 128, :], x3[i][:])


def build(stage=99, dbg=False, nblk=NBLK):
    nc = bass.Bass("TRN2", target_bir_lowering=False)
    c = Ctx(nc)
    kind_s = "ExternalOutput" if dbg else "Internal"
    G = {}

    def din(name, shape, dt=F32):
        G[name] = c.dram(name, shape, dt, kind="ExternalInput").ap()
        return G[name]

    def dsc(name, shape, dt):
        G[name] = c.dram(name, shape, dt, kind=kind_s).ap()
        return G[name]

    nown = nblk // 2 * 128
    ntok = nblk * 128
    xs = din("xs", [ntok, D])
    din("ffn1_w_in", [D, 2 * DFF]); din("ffn1_w_out", [DFF, D])
    din("ln1_g", [1, D]); din("ln1_b", [1, D]); din("ln1_gT", [128, 8]); din("ln1_bT", [128, 8])
    din("w_in", [D, D_IN]); din("dt_bias", [1, 16]); din("kv_norm_w", [1, 256])
    din("identb", [128, 128], BF16)
    din("v0", [128, 1])

    dsc("x1T_d", [nblk, 128, 8, 128], BF16)
    dsc("x1_d", [nown, D], F32)
    dsc("xbcT_d", [16, 128, ntok], BF16)
    dsc("kiT_d", [128, ntok], BF16)
    dsc("dt_d", [ntok, 16], F32)
    dsc("ckvtok_d", [nblk, 128, 257], BF16)
    dsc("ckvT_d", [2, 128, ntok], BF16)
    dsc("sgs_d", [8, 128, nown], BF16); dsc("sga_d", [8, 128, nown], BF16)
    dsc("qT_d", [8, 128, nown], BF16); dsc("qiT_d", [8, 128, nown], BF16)
    dsc("zs_d", [nown, D], BF16); dsc("widx_d", [nown, 16], F32)
    dsc("yssdT_d", [nblk // 2, 128, 8, 128], BF16)
    dsc("yattT_d", [nblk // 2, 128, 8, 128], BF16)
    dsc("x2_d", [nown, D], F32); dsc("x2T_d", [nblk // 2, 128, 8, 128], BF16)
    dsc("kmT_d", [128, 8, 256], BF16); dsc("vm_d", [128, 2, D], BF16); dsc("x3_d", [nown, D], F32)
    for wn in ("w_proj_ssd", "w_proj_att", "w_out", "w_mq", "w_mo"):
        din(wn, [D, D])
    din("w_mkv", [D, 2 * D]); din("mem", [256, D])
    for ln in ("ln2", "ln3", "ln4"):
        din(ln + "_g", [1, D]); din(ln + "_b", [1, D])
    din("ffn2_w_in", [D, 2 * DFF]); din("ffn2_w_out", [DFF, D])
    G['out'] = c.dram("out", [nown, D], F32, kind="ExternalOutput").ap()
    G['alibiLT'] = [din("alibiLT%d" % i, [128, SEQ], BF16) for i in range(2)]
    G['alibiR'] = [din("alibiR%d" % i, [128, 512], BF16) for i in range(4)]
    din("ohp", [128, 16])
    din("prow1", [1, SEQ]); din("negslope", [1, 16]); din("negcausal", [128, 128]); din("kb0", [128, 1])
    din("w_uk", [1024, 256]); G['w_uv'] = din("w_uv", [16, 256, 64])
    din("cwT", [128, 16, 4]); din("cbT", [128, 16]); din("trib", [128, 128], BF16); din("ones128b", [128, 128], BF16)
    din("identf", [128, 128]); din("negmask4b", [128, 512], BF16); din("sel16b", [128, 2048], BF16)
    din("a_log", [1, 16]); din("d_skip", [1, 16]); din("ssd_norm_w", [1, D])

    c.banks = [c.psum('bank%d' % i, [128, 512], F32) for i in range(8)]
    K = {}
    K['identb'] = c.sb('identb_s', [128, 128], BF16)
    c.dma(K['identb'][:], G['identb'])
    K['v0'] = c.sb('v0_s', [128, 1], F32)
    c.dma(K['v0'][:], G['v0'])
    K['kb0'] = c.sb('kb0_s', [128, 1], F32)
    c.dma(K['kb0'][:], G['kb0'])

    def post1(blk, S, y, mean, rstd):
        if blk == 'setup':
            S['zb'] = [c.sb('zb%d' % i, [128, D], BF16) for i in range(2)]
            S['x1s'] = [c.sb('x1s%d' % i, [128, 8, 128], BF16) for i in range(2)]
            S['z32'] = [c.sb('z32_%d' % i, [128, D], F32) for i in range(2)]
            c.dma(S['gT'][:], G['ln1_gT'])
            c.dma(S['bT'][:], G['ln1_bT'])
            return
        zb = S['zb'][blk % 2]
        x1s = S['x1s'][blk % 2]
        c.ts('dve', zb[:], y[:], mean, rstd, ALU.subtract, ALU.mult)
        pb = c.ps()
        pT = pb.bitcast(BF16)
        for k in range(8):
            c.tr(pT[:, k * 128:(k + 1) * 128], zb[:, k * 128:(k + 1) * 128], K['identb'][:])
        for k in range(8):
            c.act(x1s[:, k, :], pT[:, k * 128:(k + 1) * 128], AF.Identity,
                  bias=S['bT'][:, k:k + 1], scale=S['gT'][:, k:k + 1])
        if blk == 0:
            c.ts('dve', x1s[:], x1s[:], K['v0'][:, 0:1], None, ALU.mult)
        c.dma(G['x1T_d'][blk], x1s[:])
        if blk % 2 == 1:
            z = S['z32'][(blk // 2) % 2]
            c.ts('dve', z[:], y[:], mean, rstd, ALU.subtract, ALU.mult)
            c.tt('pool', z[:], z[:], S['gB'][:], ALU.mult)
            c.tt('pool', z[:], z[:], S['bB'][:], ALU.add)
            c.dma(G['x1_d'][(blk // 2) * 128:(blk // 2 + 1) * 128, :], z[:])

    if stage >= 1:
        ffn_phase(c, K, xs, nblk, G['ffn1_w_in'], G['ffn1_w_out'], G['ln1_g'], G['ln1_b'], post1, 'f1')
    if stage >= 2:
        proj_phase_all(c, K, G, nblk)
        proj_phase_own(c, K, G, nblk)
    if stage >= 3:
        ssd_phase(c, K, G, nblk)
    if stage >= 4:
        dsa_phase(c, K, G, nblk)
    if stage >= 5:
        merge_phase(c, K, G, nblk)
        memkv_phase(c, K, G)
        xattn_phase(c, K, G, nblk)
    if stage >= 6:
        def post4(blk, S, y, mean, rstd):
            if blk == 'setup':
                S['o32'] = [c.sb('o32_%d' % i, [128, D], F32) for i in range(2)]
                return
            o = S['o32'][blk % 2]
            c.ts('dve', o[:], y[:], mean, rstd, ALU.subtract, ALU.mult)
            c.tt('pool', o[:], o[:], S['gB'][:], ALU.mult)
            c.tt('pool', o[:], o[:], S['bB'][:], ALU.add)
            c.dma(G['out'][blk * 128:(blk + 1) * 128, :], o[:])
        ffn_phase(c, K, G['x3_d'], nblk // 2, G['ffn2_w_in'], G['ffn2_w_out'], G['ln4_g'], G['ln4_b'], post4, 'f2')

    c.barrier()
    c.es.close()
    global LAST_INPUTS
    LAST_INPUTS = [n for n in G if n not in ('out',) and not n.endswith('_d')]
    print("build: ninst=%d nwait=%d nsem=%d" % (c.ninst, c.nwait, c.nsem))
    return nc


def make_consts():
    i = np.arange(128)
    tri = (i[:, None] <= i[None, :]).astype(np.float32)
    negm = np.where(i[:, None] <= i[None, :], 0.0, -30000.0).astype(np.float32)
    sel = np.zeros((128, 16, 128), np.float32)
    for h in range(16):
        sel[h, h, :] = 1.0
    bf = ml_dtypes.bfloat16
    slopes = (2.0 ** (-8.0 * np.arange(1, 17, dtype=np.float64) / 16)).astype(np.float32)
    pos = np.arange(SEQ, dtype=np.float32)
    extra = {}
    ltt = [np.zeros((128, SEQ), np.float32) for _ in range(2)]
    rrt = [np.zeros((128, 512), np.float32) for _ in range(4)]
    ohp = np.zeros((128, 16), np.float32)
    for hg in range(4):
        r0 = 64 * (hg % 2)
        lt = ltt[hg // 2][r0:r0 + 28]
        lt[0:16] = 1.0
        rr = rrt[hg][r0:r0 + 28]
        for j in range(4):
            v = (slopes[hg * 4 + j] * pos).astype(np.float32)
            for k in range(3):
                part = v.astype(bf).astype(np.float32)
                lt[16 + 3 * j + k] = part
                v = v - part
                rr[16 + 3 * j + k, j * 128:(j + 1) * 128] = 1.0
            ohp[r0 + hg * 4 + j, hg * 4 + j] = 1.0
    for q_ in range(2):
        extra["alibiLT%d" % q_] = ltt[q_].astype(bf)
    for q_ in range(4):
        extra["alibiR%d" % q_] = rrt[q_].astype(bf)
    extra["ohp"] = ohp
    extra["prow1"] = (pos + 1.0)[None, :]
    extra["negslope"] = (-slopes)[None, :]
    extra["negcausal"] = np.where(i[None, :] <= i[:, None], 0.0, -1e30).astype(np.float32)
    return {**extra, "identb": np.eye(128, dtype=np.float32).astype(ml_dtypes.bfloat16),
            "identf": np.eye(128, dtype=np.float32), "trib": tri.astype(bf),
            "ones128b": np.ones((128, 128), np.float32).astype(bf),
            "negmask4b": np.ascontiguousarray(np.tile(negm, (1, 4))).astype(bf),
            "sel16b": sel.reshape(128, 2048).astype(bf)}


def make_in_maps(inputs):
    x = np.asarray(inputs["x"], dtype=np.float32)
    cs = make_consts()
    sq = lambda a: np.ascontiguousarray(np.asarray(a, dtype=np.float32)[0])
    shared = {
        "ffn1_w_in": sq(inputs["ffn1_w_in"]), "ffn1_w_out": sq(inputs["ffn1_w_out"]),
        "ln1_g": sq(inputs["ln1_g"])[None, :], "ln1_b": sq(inputs["ln1_b"])[None, :],
        "ln1_gT": np.ascontiguousarray(sq(inputs["ln1_g"]).reshape(8, 128).T),
        "ln1_bT": np.ascontiguousarray(sq(inputs["ln1_b"]).reshape(8, 128).T),
        "w_in": sq(inputs["w_in"]), "dt_bias": sq(inputs["dt_bias"])[None, :],
        "kv_norm_w": sq(inputs["kv_norm_w"])[None, :],
        "cwT": np.ascontiguousarray(sq(inputs["conv_w"]).reshape(4, 16, 128).transpose(2, 1, 0)),
        "cbT": np.ascontiguousarray(sq(inputs["conv_b"]).reshape(16, 128).T),
        "a_log": sq(inputs["a_log"])[None, :], "d_skip": sq(inputs["d_skip"])[None, :],
        "ssd_norm_w": sq(inputs["ssd_norm_w"])[None, :],
        "w_uk": sq(inputs["w_uk"]).reshape(1024, 256), "w_uv": sq(inputs["w_uv"]),
        "w_proj_ssd": sq(inputs["w_proj_ssd"]), "w_proj_att": sq(inputs["w_proj_att"]),
        "w_out": sq(inputs["w_out"]), "w_mq": sq(inputs["w_mq"]), "w_mo": sq(inputs["w_mo"]),
        "w_mkv": sq(inputs["w_mkv"]),
        "ln2_g": sq(inputs["ln2_g"])[None, :], "ln2_b": sq(inputs["ln2_b"])[None, :],
        "ln3_g": sq(inputs["ln3_g"])[None, :], "ln3_b": sq(inputs["ln3_b"])[None, :],
        "ln4_g": sq(inputs["ln4_g"])[None, :], "ln4_b": sq(inputs["ln4_b"])[None, :],
        "ffn2_w_in": sq(inputs["ffn2_w_in"]), "ffn2_w_out": sq(inputs["ffn2_w_out"]),
    }
    shared.update(cs)
    maps = []
    for core in range(8):
        b, par = core // 2, core % 2
        if par == 0:
            xs = np.concatenate([np.zeros((128, D), np.float32), x[b, :SEQ - 128]], axis=0)
        else:
            xs = x[b]
        m = dict(shared)
        m["xs"] = np.ascontiguousarray(xs)
        m["v0"] = np.full((128, 1), float(par), np.float32)
        m["kb0"] = np.full((128, 1), 0.0 if par else -1e30, np.float32)
        m["mem"] = np.ascontiguousarray(np.asarray(inputs["mem"], dtype=np.float32)[b])
        maps.append(m)
    return maps


def kernel(**inputs):
    nc = build()
    maps = make_in_maps(inputs)
    res = run_bass_kernel_spmd(nc, maps, core_ids=list(range(8)))
    out = np.zeros((4, SEQ, D), np.float32)
    for core in range(8):
        b, par = core // 2, core % 2
        o = np.asarray(res.results[core]["out"], dtype=np.float32).reshape(NBLK // 2, 128, D)
        out[b].reshape(NBLK, 128, D)[par::2] = o
    return out
```

```python
import contextlib
import numpy as np
import ml_dtypes
import concourse.bass as bass
import concourse.mybir as mybir
from concourse.bass_utils import run_bass_kernel_spmd

F32 = mybir.dt.float32
BF16 = mybir.dt.bfloat16
AF = mybir.ActivationFunctionType
ALU = mybir.AluOpType
AX = mybir.AxisListType
DSZ = {F32: 4, BF16: 2}
WHOLE = (0, 1 << 30, 0, 1 << 40)

D = 1024
SEQ = 4096
NBLK = 32
DFF = 2816
NF = DFF // 128
ALPHA = 2.0 ** 0.25
LN_EPS = 1e-5


class Ctx:
    def __init__(self, nc):
        self.nc = nc
        self.es = contextlib.ExitStack()
        self.eng = {'pe': nc.tensor, 'act': nc.scalar, 'dve': nc.vector,
                    'pool': nc.gpsimd, 'sp': nc.sync}
        self.esem = {k: self.es.enter_context(nc.semaphore('es_' + k))
                     for k in ('pe', 'act', 'dve', 'pool')}
        self.ecnt = {k: 0 for k in self.esem}
        self.seen = {k: {} for k in self.eng}
        self.trk = {}
        self.free_sems = []
        self.scnt = {}
        self.nsem = 0
        self.nwait = 0
        self.ninst = 0
        self.pstack = None
        self.pnames = None
        self.psi = 0

    def _reg(self, name, rowb, dram=False):
        self.trk[name] = dict(rowb=rowb, w=[], r=[], dsem=None, dram=dram)
        if self.pnames is not None and not dram:
            self.pnames.append(name)

    def sb(self, name, shape, dtype):
        self.uid = getattr(self, 'uid', 0) + 1
        name = '%s_u%d' % (name, self.uid)
        st = self.pstack if self.pstack is not None else self.es
        t = st.enter_context(self.nc.sbuf_tensor(name, list(shape), dtype))
        self._reg(name, int(np.prod(shape[1:])) * DSZ[dtype])
        return t

    def psum(self, name, shape, dtype=F32):
        st = self.pstack if self.pstack is not None else self.es
        t = st.enter_context(self.nc.psum_tensor(name, list(shape), dtype))
        self._reg(name, int(np.prod(shape[1:])) * DSZ[dtype])
        self.trk[name]['psum'] = True
        return t

    def dram(self, name, shape, dtype, kind="Internal"):
        t = self.nc.dram_tensor(name, list(shape), dtype, kind=kind)
        self._reg(name, 0, dram=True)
        return t

    def sem_for(self, name):
        t = self.trk[name]
        if t['dsem'] is None:
            if self.free_sems:
                t['dsem'] = self.free_sems.pop()
            else:
                self.nsem += 1
                s = self.es.enter_context(self.nc.semaphore('ds%d' % self.nsem))
                self.scnt[s.name] = 0
                t['dsem'] = s
        return t['dsem']

    @contextlib.contextmanager
    def phase(self):
        assert self.pstack is None
        self.pstack = contextlib.ExitStack()
        self.pnames = []
        try:
            yield
        finally:
            self.barrier()
            for n in self.pnames:
                t = self.trk.pop(n)
                if t['dsem'] is not None:
                    self.free_sems.append(t['dsem'])
            self.pstack.close()
            self.pstack = None
            self.pnames = None

    def barrier(self):
        for e, eng in self.eng.items():
            for k, s in self.esem.items():
                if k != e and self.seen[e].get(s.name, 0) < self.ecnt[k]:
                    eng.wait_ge(s, self.ecnt[k])
                    self.seen[e][s.name] = self.ecnt[k]
            for name, t in self.trk.items():
                s = t['dsem']
                if s is not None and self.seen[e].get(s.name, 0) < self.scnt[s.name]:
                    eng.wait_ge(s, self.scnt[s.name])
                    self.seen[e][s.name] = self.scnt[s.name]
        for t in self.trk.values():
            t['w'] = []
            t['r'] = []

    def region(self, ap):
        t = self.trk[ap.name]
        sz = DSZ[ap.dtype]
        pat = ap.ap
        off = ap.offset
        if t['dram']:
            ext = sum((c - 1) * abs(s) for s, c in pat) + 1
            return (0, 1, off * sz, (off + ext) * sz)
        if t.get('psum'):
            return (0, 128, 0, t['rowb'])
        rowe = t['rowb'] // sz
        p0 = off // rowe
        f0 = off % rowe
        ps, pc = pat[0]
        p1 = p0 + (pc - 1) * (ps // rowe) + 1
        ext = sum((c - 1) * abs(s) for s, c in pat[1:]) + 1
        return (p0, p1, f0 * sz, (f0 + ext) * sz)

    @staticmethod
    def _ov(a, b):
        return a[0] < b[1] and b[0] < a[1] and a[2] < b[3] and b[2] < a[3]

    @staticmethod
    def _cov(a, b):
        return a[0] <= b[0] and a[1] >= b[1] and a[2] <= b[2] and a[3] >= b[3]

    def _cur(self, t, rec):
        if rec[2] == 'dma' and rec[0] is t['dsem']:
            return (rec[0], self.scnt[rec[0].name], 'dma')
        return rec

    def _deps(self, outs, ins, whole_out=False, e=None):
        deps = []
        for ap in ins:
            t = self.trk[ap.name]
            R = self.region(ap)
            for (Q, rec) in t['w']:
                if self._ov(R, Q):
                    deps.append(self._cur(t, rec))
            if t.get('psum'):
                for (Q, rec) in t['r']:
                    if rec[2] != e:
                        deps.append(rec)
        for ap in outs:
            t = self.trk[ap.name]
            R = self.region(ap) if not whole_out else WHOLE
            for (Q, rec) in t['w']:
                if self._ov(R, Q):
                    deps.append(self._cur(t, rec))
            for (Q, rec) in t['r']:
                if self._ov(R, Q):
                    deps.append(self._cur(t, rec))
        return deps

    def _record(self, outs, ins, rec):
        for ap in ins:
            t = self.trk[ap.name]
            R = self.region(ap)
            t['r'] = [(Q, r) for (Q, r) in t['r']
                      if not (r[0] is rec[0] and self._cov(R, Q))]
            t['r'].append((R, rec))
            if len(t['r']) > 40:
                self._collapse(t, 'r')
        for ap in outs:
            t = self.trk[ap.name]
            R = self.region(ap)
            t['w'] = [(Q, r) for (Q, r) in t['w'] if not self._cov(R, Q)]
            t['r'] = [(Q, r) for (Q, r) in t['r'] if not self._cov(R, Q)]
            t['w'].append((R, rec))
            if len(t['w']) > 40:
                self._collapse(t, 'w')

    def _collapse(self, t, k):
        best = {}
        box = None
        for (Q, r) in t[k]:
            key = r[0].name
            if key not in best or best[key][1] < r[1]:
                best[key] = r
            box = Q if box is None else (min(box[0], Q[0]), max(box[1], Q[1]),
                                         min(box[2], Q[2]), max(box[3], Q[3]))
        t[k] = [(box, r) for r in best.values()]

    def _waits(self, e, deps):
        need = {}
        for (sem, val, src) in deps:
            if src == e and e == 'pe':
                continue
            if self.seen[e].get(sem.name, 0) >= val:
                continue
            if sem.name not in need or need[sem.name][1] < val:
                need[sem.name] = (sem, val)
        for (sem, val) in need.values():
            self.seen[e][sem.name] = val
        return list(need.values())

    def _autobb(self):
        tot = self.ninst + self.nwait
        if tot - getattr(self, 'lastbb', 0) > 500:
            self.lastbb = tot
            self.newbb()

    def emit(self, e, fn, outs, ins, attach=True):
        if self.ninst >= LIMIT:
            return None
        self._autobb()
        waits = self._waits(e, self._deps(outs, ins, e=e))
        eng = self.eng[e]
        self.nwait += len(waits)
        last = None
        if attach and waits:
            last = waits.pop()
        for (sem, val) in waits:
            eng.wait_ge(sem, val)
        ins_ = fn(eng)
        if last is not None:
            ins_._wait_ge(last[0], last[1])
        ins_.then_inc(self.esem[e], 1)
        self.ecnt[e] += 1
        self.ninst += 1
        rec = (self.esem[e], self.ecnt[e], e)
        self._record(outs, ins, rec)
        return ins_

    def dma(self, out, in_, q='sp', **kw):
        if self.ninst >= LIMIT:
            return None
        self._autobb()
        deps = self._deps([out], [in_], whole_out=True)
        sbside = in_ if self.trk[out.name]['dram'] else out
        sem = self.sem_for(sbside.name)
        deps = [d for d in deps if not (d[0] is sem and d[2] == 'dma')]
        waits = self._waits(q, deps)
        eng = self.eng[q]
        self.nwait += len(waits)
        for (s, val) in waits:
            eng.wait_ge(s, val)
        self.scnt[sem.name] += 16
        eng.dma_start(out=out, in_=in_, **kw).then_inc(sem, 16)
        self.ninst += 1
        rec = (sem, self.scnt[sem.name], 'dma')
        self._record([out], [in_], rec)

    def newbb(self):
        self.nbb = getattr(self, 'nbb', 0) + 1
        self.nc.switch_bb('kbb%d' % self.nbb)

    def ps(self):
        b = self.banks[self.psi % len(self.banks)]
        self.psi += 1
        return b

    def mm(self, out, lhsT, rhs, start=True, stop=True):
        return self.emit('pe', lambda e: e.matmul(out, lhsT=lhsT, rhs=rhs, start=start, stop=stop),
                         [out], [lhsT, rhs])

    def tr(self, out, in_, ident):
        return self.emit('pe', lambda e: e.transpose(out=out, in_=in_, identity=ident),
                         [out], [in_, ident])

    def act(self, out, in_, func, bias=None, scale=None, accum=None, e='act'):
        kw = {}
        ins = [in_]
        outs = [out]
        if bias is not None:
            kw['bias'] = bias
            if not isinstance(bias, (int, float)):
                ins.append(bias)
        if scale is not None:
            kw['scale'] = scale
            if not isinstance(scale, (int, float)):
                ins.append(scale)
        if accum is not None:
            kw['accum_out'] = accum
            outs.append(accum)
        return self.emit('act', lambda e_: e_.activation(out=out, in_=in_, func=func, **kw),
                         outs, ins, attach=accum is None)

    def ts(self, e, out, in0, s1, s2, op0, op1=None, accum=None):
        ins = [in0] + [s for s in (s1, s2) if s is not None and not isinstance(s, (int, float))]
        outs = [out] + ([accum] if accum is not None else [])
        kw = {}
        if op1 is not None:
            kw['op1'] = op1
        if accum is not None:
            kw['accum_out'] = accum
        return self.emit(e, lambda e_: e_.tensor_scalar(out=out, in0=in0, scalar1=s1, scalar2=s2,
                                                        op0=op0, **kw),
                         outs, ins, attach=accum is None)

    def tt(self, e, out, in0, in1, op):
        return self.emit(e, lambda e_: e_.tensor_tensor(out=out, in0=in0, in1=in1, op=op),
                         [out], [in0, in1])

    def stt(self, out, in0, scalar, in1, op0, op1):
        ins = [in0, in1] + ([scalar] if not isinstance(scalar, (int, float)) else [])
        return self.emit('dve', lambda e_: e_.scalar_tensor_tensor(out=out, in0=in0, scalar=scalar,
                                                                   in1=in1, op0=op0, op1=op1),
                         [out], ins)

    def copy(self, e, out, in_):
        if e == 'act':
            return self.emit('act', lambda e_: e_.copy(out=out, in_=in_), [out], [in_])
        return self.emit(e, lambda e_: e_.tensor_copy(out=out, in_=in_), [out], [in_])


def load_cast(c, dst, src, st, eng='pool'):
    shp = list(src.shape)
    n = int(np.prod(shp[1:]))
    sv = st[:, 0:n]
    if len(shp) == 3:
        sv = sv.rearrange("p (a b) -> p a b", a=shp[1])
    c.dma(sv, src)
    c.copy(eng, dst, sv)


def layer_norm_stats(c, S, y, eps):
    st6 = S['st6']
    mv = S['mv']
    for h in range(2):
        c.emit('dve', lambda e, h=h: e.bn_stats(out=st6[:, h, :], in_=y[:, h * 512:(h + 1) * 512]),
               [st6[:, h, :]], [y[:, h * 512:(h + 1) * 512]])
    c.emit('dve', lambda e: e.bn_aggr(out=mv[:, 0:2], in_=st6[:]), [mv[:, 0:2]], [st6[:]])
    c.act(mv[:, 2:3], mv[:, 1:2], AF.Sqrt, bias=S['epsc'][:, 0:1])
    c.emit('dve', lambda e: e.reciprocal(out=mv[:, 3:4], in_=mv[:, 2:3]), [mv[:, 3:4]], [mv[:, 2:3]])
    return mv[:, 0:1], mv[:, 3:4]


def ffn_phase(c, K, xsrc, nblk, w_in, w_out, g_d, b_d, post, name):
    TT = 1024
    NSUB = TT // 128
    assert (nblk * 128) % TT == 0
    with c.phase():
        W2 = c.sb('W2', [128, NF, D], BF16)
        xT = c.sb('xT', [128, 8, TT], BF16)
        hT = c.sb('hT', [128, NF, TT], BF16)
        W1 = [c.sb('W1_%d' % i, [128, 8, 512], BF16) for i in range(2)]
        wst = [c.sb('wst%d' % i, [128, 2048], F32) for i in range(3)]
        xin = [c.sb('xin%d' % i, [128, D], F32) for i in range(2)]
        xbf = [c.sb('xbf%d' % i, [128, D], BF16) for i in range(2)]
        sg = [c.sb('sg%d' % i, [128, 512], BF16) for i in range(2)]
        yb = [c.sb('yb%d' % i, [128, D], F32) for i in range(2)]
        S = dict(K)
        S['st6'] = c.sb('st6', [128, 2, 6], F32)
        S['mv'] = c.sb('mv', [128, 4], F32)
        S['epsc'] = c.sb('epsc', [128, 1], F32)
        S['gB'] = c.sb('gB', [128, D], F32)
        S['bB'] = c.sb('bB', [128, D], F32)
        S['gT'] = c.sb('gT', [128, 8], F32)
        S['bT'] = c.sb('bT', [128, 8], F32)
        c.emit('pool', lambda e: e.memset(S['epsc'][:], LN_EPS / (ALPHA * ALPHA)), [S['epsc'][:]], [])
        c.dma(S['gB'][:], g_d[0:1, :].partition_broadcast(128))
        c.dma(S['bB'][:], b_d[0:1, :].partition_broadcast(128))
        post('setup', S, None, None, None)
        wi = 0
        w2v = w_out.rearrange("(f p) n -> p f n", p=128)
        for f in range(0, NF, 2):
            load_cast(c, W2[:, f:f + 2, :], w2v[:, f:f + 2, :], wst[wi % 3])
            wi += 1
        w1v = w_in.rearrange("(k p) n -> p k n", p=128)
        xi = 0
        for t in range(nblk * 128 // TT):
            for s in range(NSUB):
                blk = t * NSUB + s
                xs_ = xin[xi % 2]
                xb_ = xbf[xi % 2]
                xi += 1
                c.dma(xs_[:], xsrc[blk * 128:(blk + 1) * 128, :])
                c.copy('act', xb_[:], xs_[:])
                pb = c.ps()
                pT = pb.bitcast(BF16)
                for k in range(8):
                    c.tr(pT[:, k * 128:(k + 1) * 128], xb_[:, k * 128:(k + 1) * 128], K['identb'][:])
                c.copy('dve', xT[:, :, s * 128:(s + 1) * 128],
                       pT[:, 0:1024].rearrange("p (k t) -> p k t", k=8))
            for fp in range(NF // 2):
                w1 = W1[fp % 2]
                for gu in range(2):
                    col = gu * DFF + fp * 256
                    load_cast(c, w1[:, :, gu * 256:(gu + 1) * 256], w1v[:, :, col:col + 256], wst[wi % 3],
                              eng='pool' if gu == 0 else 'dve')
                    wi += 1
                for fi in range(2):
                    f = fp * 2 + fi
                    for h in range(TT // 512):
                        pg = c.ps()
                        pu = c.ps()
                        for k in range(8):
                            c.mm(pg[:], w1[:, k, fi * 128:(fi + 1) * 128], xT[:, k, h * 512:(h + 1) * 512],
                                 start=(k == 0), stop=(k == 7))
                        for k in range(8):
                            c.mm(pu[:], w1[:, k, 256 + fi * 128:256 + (fi + 1) * 128],
                                 xT[:, k, h * 512:(h + 1) * 512], start=(k == 0), stop=(k == 7))
                        s_ = sg[(f * 2 + h) % 2]
                        c.act(s_[:], pg[:], AF.Silu)
                        c.tt('dve', hT[:, f, h * 512:(h + 1) * 512], s_[:], pu[:], ALU.mult)
            for s in range(NSUB):
                blk = t * NSUB + s
                xs_ = xin[xi % 2]
                y = yb[xi % 2]
                xi += 1
                c.dma(xs_[:], xsrc[blk * 128:(blk + 1) * 128, :])
                for h in range(2):
                    po = c.ps()
                    for f in range(NF):
                        c.mm(po[:], hT[:, f, s * 128:(s + 1) * 128], W2[:, f, h * 512:(h + 1) * 512],
                             start=(f == 0), stop=(f == NF - 1))
                    c.stt(y[:, h * 512:(h + 1) * 512], po[:], 0.5 / ALPHA, xs_[:, h * 512:(h + 1) * 512],
                          ALU.mult, ALU.add)
                mean, rstd = layer_norm_stats(c, S, y, None)
                post(blk, S, y, mean, rstd)


O_GS, O_GA, O_Z, O_XBC, O_DT, O_Q, O_CKV, O_QI, O_KI, O_WI = 0, 1024, 2048, 3072, 5120, 5136, 6160, 6416, 7440, 7504
D_IN = 7520
RMS_EPS = 1e-6


def proj_phase_all(c, K, G, nblk):
    w_in = G['w_in']
    wv = w_in.rearrange("(k p) n -> p k n", p=128)
    with c.phase():
        Wx = c.sb('Wx', [128, 8, 2048], BF16)
        Wk = c.sb('Wk', [128, 8, 128], BF16)
        Wd = c.sb('Wd', [128, 8, 16], BF16)
        Wc = c.sb('Wc', [128, 8, 256], BF16)
        wst = [c.sb('wst%d' % i, [128, 2048], F32) for i in range(3)]
        xg = [c.sb('xg%d' % i, [128, 8, 512], BF16) for i in range(2)]
        xo = [c.sb('xo%d' % i, [128, 16, 512], BF16) for i in range(2)]
        ko = [c.sb('ko%d' % i, [128, 512], BF16) for i in range(2)]
        dto = [c.sb('dto%d' % i, [128, 16], F32) for i in range(2)]
        dte = [c.sb('dte%d' % i, [128, 16], F32) for i in range(2)]
        cko = [c.sb('cko%d' % i, [128, 257], BF16) for i in range(2)]
        ckf = [c.sb('ckf%d' % i, [128, 256], F32) for i in range(2)]
        ckT = [c.sb('ckT%d' % i, [128, 2, 128], BF16) for i in range(2)]
        sq = c.sb('sq', [128, 256], F32)
        ss = c.sb('ss', [128, 4], F32)
        dtb = c.sb('dtb', [128, 16], F32)
        kvg = c.sb('kvg', [128, 256], F32)
        epsr = c.sb('epsr', [128, 1], F32)
        c.emit('pool', lambda e: e.memset(epsr[:], RMS_EPS), [epsr[:]], [])
        c.dma(dtb[:], G['dt_bias'][0:1, :].partition_broadcast(128))
        c.dma(kvg[:], G['kv_norm_w'][0:1, :].partition_broadcast(128))
        wi = 0
        for m in range(0, 16, 2):
            load_cast(c, Wx[:, :, m * 128:(m + 2) * 128], wv[:, :, O_XBC + m * 128:O_XBC + (m + 2) * 128], wst[wi % 3]); wi += 1
        load_cast(c, Wc[:], wv[:, :, O_CKV:O_CKV + 256], wst[wi % 3]); wi += 1
        for hf in range(2):
            load_cast(c, Wk[:, :, hf * 64:(hf + 1) * 64], wv[:, :, O_KI:O_KI + 64], wst[wi % 3]); wi += 1
        load_cast(c, Wd[:], wv[:, :, O_DT:O_DT + 16], wst[wi % 3]); wi += 1
        xbcv = G['xbcT_d'].rearrange("m p t -> p m t")
        for g in range(nblk // 4):
            x_ = xg[g % 2]
            for b in range(4):
                c.dma(x_[:, :, b * 128:(b + 1) * 128], G['x1T_d'][g * 4 + b])
            xo_ = xo[g % 2]
            for m in range(16):
                p = c.ps()
                for k in range(8):
                    c.mm(p[:], Wx[:, k, m * 128:(m + 1) * 128], x_[:, k, :], start=(k == 0), stop=(k == 7))
                c.copy('act' if m % 2 == 0 else 'dve', xo_[:, m, :], p[:])
            for mh in range(2):
                c.dma(xbcv[:, mh * 8:mh * 8 + 8, g * 512:(g + 1) * 512], xo_[:, mh * 8:mh * 8 + 8, :])
            p = c.ps()
            for k in range(8):
                c.mm(p[:], Wk[:, k, :], x_[:, k, :], start=(k == 0), stop=(k == 7))
            ko_ = ko[g % 2]
            c.copy('dve', ko_[:], p[:])
            c.dma(G['kiT_d'][:, g * 512:(g + 1) * 512], ko_[:])
            for b in range(4):
                blk = g * 4 + b
                p = c.ps()
                for k in range(8):
                    c.mm(p[:, 0:16], x_[:, k, b * 128:(b + 1) * 128], Wd[:, k, :], start=(k == 0), stop=(k == 7))
                e_ = dte[blk % 2]
                d_ = dto[blk % 2]
                c.tt('dve', e_[:], p[:, 0:16], dtb[:], ALU.add)
                c.act(e_[:], e_[:], AF.Exp)
                c.act(d_[:], e_[:], AF.Ln, bias=1.0)
                if blk == 0:
                    c.ts('dve', d_[:], d_[:], K['v0'][:, 0:1], None, ALU.mult)
                c.dma(G['dt_d'][blk * 128:(blk + 1) * 128, :], d_[:])
                p = c.ps()
                for k in range(8):
                    c.mm(p[:, 0:256], x_[:, k, b * 128:(b + 1) * 128], Wc[:, k, :], start=(k == 0), stop=(k == 7))
                f_ = ckf[blk % 2]
                c.copy('dve', f_[:], p[:, 0:256])
                c.act(sq[:], f_[:], AF.Square, accum=ss[:, 0:1])
                c.act(ss[:, 1:2], ss[:, 0:1], AF.Sqrt, bias=epsr[:, 0:1], scale=1.0 / 256)
                c.emit('dve', lambda e: e.reciprocal(out=ss[:, 2:3], in_=ss[:, 1:2]), [ss[:, 2:3]], [ss[:, 1:2]])
                o_ = cko[blk % 2]
                c.stt(o_[:, 0:256], f_[:], ss[:, 2:3], kvg[:], ALU.mult, ALU.mult)
                c.emit('pool', lambda e, o_=o_: e.memset(o_[:, 256:257], 1.0), [o_[:, 256:257]], [])
                c.dma(G['ckvtok_d'][blk], o_[:])
                pb = c.ps()
                pT = pb.bitcast(BF16)
                for r in range(2):
                    c.tr(pT[:, r * 128:(r + 1) * 128], o_[:, r * 128:(r + 1) * 128], K['identb'][:])
                t_ = ckT[blk % 2]
                c.copy('act', t_[:], pT[:, 0:256].rearrange("p (r t) -> p r t", r=2))
                c.dma(G['ckvT_d'].rearrange("r p t -> p r t")[:, :, blk * 128:(blk + 1) * 128], t_[:])


def proj_phase_own(c, K, G, nblk):
    wv = G['w_in'].rearrange("(k p) n -> p k n", p=128)
    nown = nblk // 2
    with c.phase():
        Wf = c.sb('Wf', [128, 8, 4096], BF16)
        Wz = c.sb('Wz', [128, 8, 1024], BF16)
        Ww = c.sb('Ww', [128, 8, 16], BF16)
        wst = [c.sb('wst%d' % i, [128, 2048], F32) for i in range(3)]
        xg = [c.sb('xg%d' % i, [128, 8, 512], BF16) for i in range(2)]
        fo = [c.sb('fo%d' % i, [128, 8, 512], BF16) for i in range(2)]
        zo = [c.sb('zo%d' % i, [128, 1024], BF16) for i in range(2)]
        wo = [c.sb('wo%d' % i, [128, 16], F32) for i in range(2)]
        wi = 0
        segs = [(O_GS, 'sgs_d', AF.Sigmoid), (O_GA, 'sga_d', AF.Sigmoid), (O_Q, 'qT_d', AF.Identity),
                (O_QI, 'qiT_d', AF.Identity)]
        for si, (off, _, _) in enumerate(segs):
            for m in range(0, 8, 2):
                load_cast(c, Wf[:, :, si * 1024 + m * 128:si * 1024 + (m + 2) * 128],
                          wv[:, :, off + m * 128:off + (m + 2) * 128], wst[wi % 3]); wi += 1
        for m in range(0, 8, 2):
            load_cast(c, Wz[:, :, m * 128:(m + 2) * 128], wv[:, :, O_Z + m * 128:O_Z + (m + 2) * 128], wst[wi % 3]); wi += 1
        load_cast(c, Ww[:], wv[:, :, O_WI:O_WI + 16], wst[wi % 3]); wi += 1
        fi = 0
        for g in range(nown // 4):
            x_ = xg[g % 2]
            for b in range(4):
                c.dma(x_[:, :, b * 128:(b + 1) * 128], G['x1T_d'][2 * (g * 4 + b) + 1])
            for si, (off, dst, fn) in enumerate(segs):
                f_ = fo[fi % 2]; fi += 1
                for m in range(8):
                    p = c.ps()
                    for k in range(8):
                        c.mm(p[:], Wf[:, k, si * 1024 + m * 128:si * 1024 + (m + 1) * 128], x_[:, k, :],
                             start=(k == 0), stop=(k == 7))
                    if fn == AF.Identity and m % 2 == 1:
                        c.copy('dve', f_[:, m, :], p[:])
                    else:
                        c.act(f_[:, m, :], p[:], fn)
                c.dma(G[dst].rearrange("m p t -> p m t")[:, :, g * 512:(g + 1) * 512], f_[:])
            for b in range(4):
                ob = g * 4 + b
                z_ = zo[ob % 2]
                for h in range(2):
                    p = c.ps()
                    for k in range(8):
                        c.mm(p[:], x_[:, k, b * 128:(b + 1) * 128], Wz[:, k, h * 512:(h + 1) * 512],
                             start=(k == 0), stop=(k == 7))
                    c.act(z_[:, h * 512:(h + 1) * 512], p[:], AF.Silu)
                c.dma(G['zs_d'][ob * 128:(ob + 1) * 128, :], z_[:])
                p = c.ps()
                for k in range(8):
                    c.mm(p[:, 0:16], x_[:, k, b * 128:(b + 1) * 128], Ww[:, k, :], start=(k == 0), stop=(k == 7))
                w_ = wo[ob % 2]
                c.act(w_[:], p[:, 0:16], AF.Copy, scale=0.25)
                c.dma(G['widx_d'][ob * 128:(ob + 1) * 128, :], w_[:])


def ssd_phase(c, K, G, nblk):
    with c.phase():
        cw = c.sb('cw', [128, 16, 4], F32)
        cb = c.sb('cb', [128, 16], F32)
        tri = c.sb('tri', [128, 128], BF16)
        ones = c.sb('ones', [128, 128], BF16)
        negm = c.sb('negm', [128, 512], BF16)
        sel = c.sb('sel', [128, 2048], BF16)
        apad = c.sb('apad', [128, 2, 128], BF16)
        ahl = c.sb('ahl', [128, 2, 16], BF16)
        ares = c.sb('ares', [128, 16], F32)
        Abc = c.sb('Abc', [128, 16], F32)
        Dbc = c.sb('Dbc', [128, 16], F32)
        nwB = c.sb('nwB', [128, D], F32)
        epsr = c.sb('epsr', [128, 1], F32)
        S32 = c.sb('S32', [128, 16, 64], F32)
        Sbf = c.sb('Sbf', [128, 16, 64], BF16)
        for t_, n_ in ((cw, 'cwT'), (cb, 'cbT'), (tri, 'trib'), (ones, 'ones128b'),
                       (negm, 'negmask4b'), (sel, 'sel16b')):
            c.dma(t_[:], G[n_])
        c.dma(Abc[:], G['a_log'][0:1, :].partition_broadcast(128))
        c.dma(Dbc[:], G['d_skip'][0:1, :].partition_broadcast(128))
        c.dma(nwB[:], G['ssd_norm_w'][0:1, :].partition_broadcast(128))
        c.act(Abc[:], Abc[:], AF.Exp)
        c.ts('dve', Abc[:], Abc[:], -1.0, None, ALU.mult)
        c.emit('dve', lambda e: e.memset(epsr[:], RMS_EPS), [epsr[:]], [])
        c.emit('dve', lambda e: e.memset(S32[:], 0.0), [S32[:]], [])
        c.emit('dve', lambda e: e.memset(Sbf[:], 0.0), [Sbf[:]], [])
        c.emit('dve', lambda e: e.memset(apad[:], 0.0), [apad[:]], [])
        xh = [c.sb('xh%d' % i, [128, 16, 132], BF16) for i in range(2)]
        t1 = [c.sb('t1_%d' % i, [128, 16, 128], F32) for i in range(3)]
        xc = [c.sb('xc%d' % i, [128, 16, 128], BF16) for i in range(2)]
        xtok = [c.sb('xtok%d' % i, [128, 16, 64], F32) for i in range(2)]
        Btok = [c.sb('Btok%d' % i, [128, 4, 128], BF16) for i in range(2)]
        xdt = [c.sb('xdt%d' % i, [128, 16, 64], BF16) for i in range(2)]
        xdd = [c.sb('xdd%d' % i, [128, 16, 64], BF16) for i in range(2)]
        dtt = [c.sb('dtt%d' % i, [128, 16], F32) for i in range(2)]
        sm = [c.sb('sm%d' % i, [128, 6, 16], F32) for i in range(2)]
        acsT = [c.sb('acsT%d' % i, [128, 2, 128], BF16) for i in range(2)]
        acsTf = c.sb('acsTf', [128, 128], F32)
        LT = c.sb('LT', [128, 16, 128], F32)
        MT = c.sb('MT', [128, 16, 128], BF16)
        yo = c.sb('yo', [128, 16, 64], F32)
        yy = c.sb('yy', [128, 16, 64], F32)
        zt = [c.sb('zt%d' % i, [128, D], BF16) for i in range(2)]
        sq = c.sb('sqs', [128, 256], F32)
        gs = c.sb('gs', [128, 12], F32)
        ynb = c.sb('ynb', [128, D], BF16)
        ynT = [c.sb('ynT%d' % i, [128, 8, 128], BF16) for i in range(2)]
        for i in range(2):
            c.emit('dve', lambda e, i=i: e.memset(xh[i][:], 0.0), [xh[i][:]], [])
        xbv = G['xbcT_d'].rearrange("m p t -> p m t")
        for ch in range(nblk):
            own = ch % 2 == 1
            x_ = xh[ch % 2]
            for mh in range(2):
                ms = slice(mh * 8, mh * 8 + 8)
                if ch == 0:
                    c.dma(x_[:, ms, 4:132], xbv[:, ms, 0:128])
                else:
                    c.dma(x_[:, ms, 0:132], xbv[:, ms, ch * 128 - 4:ch * 128 + 128])
            d_ = dtt[ch % 2]
            c.dma(d_[:], G['dt_d'][ch * 128:(ch + 1) * 128, :])
            a_ = t1[0]
            b_ = t1[1]
            c.tt('dve', a_[:], x_[:, :, 1:129], cw[:, :, 0:1].to_broadcast([128, 16, 128]), ALU.mult)
            for k in range(1, 4):
                b_ = t1[1 + (k % 2)]
                c.tt('dve', b_[:], x_[:, :, k + 1:k + 129], cw[:, :, k:k + 1].to_broadcast([128, 16, 128]), ALU.mult)
                c.tt('pool', a_[:], a_[:], b_[:], ALU.add)
            c.tt('dve', a_[:], a_[:], cb[:].unsqueeze(2).to_broadcast([128, 16, 128]), ALU.add)
            xc_ = xc[ch % 2]
            c.act(xc_[:], a_[:], AF.Silu)
            p0 = c.ps(); p0b = p0.bitcast(BF16)
            p1 = c.ps(); p1b = p1.bitcast(BF16)
            for m in range(8):
                c.tr(p0b[:, m * 128:(m + 1) * 128], xc_[:, m, :], K['identb'][:])
            for m in range(4):
                c.tr(p1b[:, m * 128:(m + 1) * 128], xc_[:, 8 + m, :], K['identb'][:])
            xt_ = xtok[ch % 2]
            Bt_ = Btok[ch % 2]
            c.copy('act', xt_[:], p0b[:, 0:1024].rearrange("p (h q) -> p h q", h=16))
            c.copy('act', Bt_[:], p1b[:, 0:512].rearrange("p (g n) -> p g n", g=4))
            xd_ = xdt[ch % 2]
            c.tt('dve', xd_[:], xt_[:], d_[:].unsqueeze(2).to_broadcast([128, 16, 64]), ALU.mult)
            s_ = sm[ch % 2]
            c.tt('dve', s_[:, 0, :], d_[:], Abc[:], ALU.mult)
            c.copy('dve', ahl[:, 0, :], s_[:, 0, :])
            c.tt('dve', ares[:], s_[:, 0, :], ahl[:, 0, :], ALU.subtract)
            c.copy('dve', ahl[:, 1, :], ares[:])
            c.copy('dve', apad[:, :, 0:16], ahl[:])
            pc = c.ps()
            for q_ in range(2):
                c.mm(pc[:, 0:16], tri[:], ahl[:, q_, :], start=(q_ == 0), stop=(q_ == 1))
            pc1 = c.ps()
            for q_ in range(2):
                c.mm(pc1[:, 0:16], ones[:], ahl[:, q_, :], start=(q_ == 0), stop=(q_ == 1))
            pc2 = c.ps()
            for q_ in range(2):
                c.mm(pc2[:, 0:128], apad[:, q_, :], tri[:], start=(q_ == 0), stop=(q_ == 1))
            c.copy('dve', s_[:, 1, :], pc[:, 0:16])
            c.ts('dve', s_[:, 2, :], pc[:, 0:16], -1.0, None, ALU.mult)
            c.act(s_[:, 3, :], pc[:, 0:16], AF.Exp)
            c.tt('dve', s_[:, 4, :], pc1[:, 0:16], s_[:, 1, :], ALU.subtract)
            c.act(s_[:, 4, :], s_[:, 4, :], AF.Exp)
            c.act(s_[:, 5, :], pc1[:, 0:16], AF.Exp)
            aT_ = acsT[ch % 2]
            c.copy('dve', acsTf[:], pc2[:, 0:128])
            c.copy('dve', aT_[:, 0, :], acsTf[:])
            c.tt('dve', acsTf[:], acsTf[:], aT_[:, 0, :], ALU.subtract)
            c.copy('dve', aT_[:, 1, :], acsTf[:])
            if own:
                ob = ch // 2
                for q in range(4):
                    pl = c.ps()
                    c.mm(pl[:], K['identb'][:], negm[:], start=True, stop=False)
                    for j in range(4):
                        h = q * 4 + j
                        for q_ in range(2):
                            c.mm(pl[:, j * 128:(j + 1) * 128], sel[:, h * 128:(h + 1) * 128], aT_[:, q_, :],
                                 start=False, stop=(j == 3 and q_ == 1))
                    for j in range(4):
                        h = q * 4 + j
                        c.act(LT[:, h, :], pl[:, j * 128:(j + 1) * 128], AF.Exp, bias=s_[:, 2, h:h + 1])
                pcb = c.ps()
                for g in range(4):
                    c.mm(pcb[:, g * 128:(g + 1) * 128], xc_[:, 8 + g, :], xc_[:, 12 + g, :])
                c.tt('dve', MT[:].rearrange("p (g j) l -> p g j l", g=4),
                     LT[:].rearrange("p (g j) l -> p g j l", g=4),
                     pcb[:].rearrange("p (g l) -> p g l", g=4).unsqueeze(2).to_broadcast([128, 4, 4, 128]),
                     ALU.mult)
                py = [c.ps(), c.ps()]
                for h in range(16):
                    c.mm(py[h // 8][:, (h % 8) * 64:(h % 8 + 1) * 64], MT[:, h, :], xd_[:, h, :])
                po = [c.ps(), c.ps()]
                for g in range(4):
                    c.mm(po[g // 2][:, (g % 2) * 256:(g % 2 + 1) * 256], xc_[:, 12 + g, :],
                         Sbf[:, 4 * g:4 * g + 4, :].rearrange("p h q -> p (h q)"))
                for hf in range(2):
                    hs = slice(hf * 8, hf * 8 + 8)
                    c.tt('dve', yo[:, hs, :], po[hf][:].rearrange("p (h q) -> p h q", h=8),
                         s_[:, 3, hs].unsqueeze(2).to_broadcast([128, 8, 64]), ALU.mult)
                    c.tt('dve', yy[:, hs, :], py[hf][:].rearrange("p (h q) -> p h q", h=8), yo[:, hs, :], ALU.add)
                c.tt('dve', yo[:], xt_[:], Dbc[:].unsqueeze(2).to_broadcast([128, 16, 64]), ALU.mult)
                c.tt('dve', yy[:], yy[:], yo[:], ALU.add)
                z_ = zt[ob % 2]
                c.dma(z_[:], G['zs_d'][ob * 128:(ob + 1) * 128, :])
                yf = yy[:].rearrange("p h q -> p (h q)")
                c.tt('dve', yf, yf, z_[:], ALU.mult)
                for g in range(4):
                    c.act(sq[:], yf[:, g * 256:(g + 1) * 256], AF.Square, accum=gs[:, g:g + 1])
                c.act(gs[:, 4:8], gs[:, 0:4], AF.Sqrt, bias=epsr[:, 0:1], scale=1.0 / 256)
                c.emit('dve', lambda e: e.reciprocal(out=gs[:, 8:12], in_=gs[:, 4:8]), [gs[:, 8:12]], [gs[:, 4:8]])
                c.tt('dve', yy[:].rearrange("p (g j) q -> p g (j q)", g=4),
                     yy[:].rearrange("p (g j) q -> p g (j q)", g=4),
                     gs[:, 8:12].unsqueeze(2).to_broadcast([128, 4, 256]), ALU.mult)
                c.tt('dve', ynb[:], yf, nwB[:], ALU.mult)
                pb = c.ps(); pT = pb.bitcast(BF16)
                for m in range(8):
                    c.tr(pT[:, m * 128:(m + 1) * 128], ynb[:, m * 128:(m + 1) * 128], K['identb'][:])
                yT_ = ynT[ob % 2]
                c.copy('act', yT_[:], pT[:, 0:1024].rearrange("p (m t) -> p m t", m=8))
                c.dma(G['yssdT_d'][ob], yT_[:])
            xe_ = xdd[ch % 2]
            c.tt('dve', xe_[:], xd_[:], s_[:, 4, :].unsqueeze(2).to_broadcast([128, 16, 64]), ALU.mult)
            pst = [c.ps(), c.ps()]
            for g in range(4):
                c.mm(pst[g // 2][:, (g % 2) * 256:(g % 2 + 1) * 256], Bt_[:, g, :],
                     xe_[:, 4 * g:4 * g + 4, :].rearrange("p h q -> p (h q)"))
            c.tt('dve', S32[:], S32[:], s_[:, 5, :].unsqueeze(2).to_broadcast([128, 16, 64]), ALU.mult)
            for hf in range(2):
                hs = slice(hf * 8, hf * 8 + 8)
                c.tt('dve', S32[:, hs, :], S32[:, hs, :], pst[hf][:].rearrange("p (h q) -> p h q", h=8), ALU.add)
            c.copy('act', Sbf[:], S32[:])


LAST_INPUTS = []
LIMIT = 10 ** 9
NIT_BISECT = 16
TOPK = 256


def dsa_phase(c, K, G, nblk):
    ntok = nblk * 128
    nown = nblk // 2
    with c.phase():
        kiT = c.sb('kiT', [128, ntok], BF16)
        ckvT = c.sb('ckvT', [128, 2, ntok], BF16)
        ckvtok = c.sb('ckvtok', [128, nblk, 257], BF16)
        Wuk = c.sb('Wuk', [128, 16, 256], BF16)
        qiz = c.sb('qiz', [128, 16, 128], BF16)
        Wuv = c.sb('Wuv', [128, 16, 2, 128], BF16)
        wst = [c.sb('wst%d' % i, [128, 2048], F32) for i in range(1)]
        LTt = [c.sb('LTt%d' % i, [128, ntok], BF16) for i in range(2)]
        Rt = [c.sb('Rt%d' % i, [128, 512], BF16) for i in range(4)]
        ohp = c.sb('ohp', [128, 16], F32)
        nbpad = c.sb('nbpad', [128, 128], BF16)
        prow1 = c.sb('prow1', [128, ntok], F32)
        negsl = c.sb('negsl', [128, 16], F32)
        negc = c.sb('negc', [128, 128], F32)
        identf = c.sb('identf2', [128, 128], F32)
        sc = c.sb('sc', [128, ntok], F32)
        tmpn = c.sb('tmpn', [128, ntok], F32)
        m01 = c.sb('m01', [128, ntok], BF16)
        mbT = c.sb('mbT', [128, nblk, 128], BF16)
        qT = [c.sb('qT%d' % i, [128, 8, 128], BF16) for i in range(2)]
        qiT = [c.sb('qiT%d' % i, [128, 8, 128], BF16) for i in range(2)]
        wid = [c.sb('wid%d' % i, [128, 16], F32) for i in range(2)]
        qlat = c.sb('qlat', [128, 2, 16, 128], BF16)
        rl = [c.sb('rl%d' % i, [128, 512], F32) for i in range(3)]
        PT = [c.sb('PT%d' % i, [128, 512], BF16) for i in range(3)]
        osb = c.sb('osb', [128, 4, 256], BF16)
        oT = c.sb('oT', [128, 16, 2, 128], BF16)
        yat = [c.sb('yat%d' % i, [128, 8, 128], BF16) for i in range(2)]
        bs = c.sb('bs', [128, 12], F32)
        nb = c.sb('nb', [128, 16], F32)
        c.dma(kiT[:], G['kiT_d'])
        c.dma(ckvT[:], G['ckvT_d'].rearrange("r p t -> p r t"))
        for b0 in range(0, nblk, 8):
            c.dma(ckvtok[:, b0:b0 + 8, :], G['ckvtok_d'].rearrange("b p f -> p b f")[:, b0:b0 + 8, :])
        for i in range(2):
            c.dma(LTt[i][:], G['alibiLT'][i][:, 0:ntok])
        for i in range(4):
            c.dma(Rt[i][:], G['alibiR'][i])
        c.emit('dve', lambda e: e.memset(qiz[:], 0.0), [qiz[:]], [])
        c.dma(ohp[:], G['ohp'])
        c.emit('dve', lambda e: e.memset(nbpad[:], 0.0), [nbpad[:]], [])
        c.dma(prow1[:], G['prow1'][0:1, 0:ntok].partition_broadcast(128))
        c.dma(negsl[:], G['negslope'][0:1, :].partition_broadcast(128))
        c.dma(negc[:], G['negcausal'])
        c.dma(identf[:], G['identf'])
        c.emit('dve', lambda e: e.memset(Wuk[:], 0.0), [Wuk[:]], [])
        svk = wst[0][:, 0:2048].rearrange("p (a b) -> p a b", a=8)
        c.dma(svk, G['w_uk'].rearrange("(c p) r -> p c r", p=128))
        Wukv = Wuk[:].rearrange("p (c e) r -> p c e r", e=2)
        c.copy('dve', Wukv[0:64, :, 0, :], svk[0:64, :, :])
        c.copy('dve', Wukv[64:128, :, 1, :], svk[64:128, :, :])
        c.emit('dve', lambda e: e.memset(Wuv[:], 0.0), [Wuv[:]], [])
        for h in range(16):
            st = wst[0]
            sv = st[:, 0:128].rearrange("p (a b) -> p a b", a=2)
            c.dma(sv, G['w_uv'][h].rearrange("(rc p) d -> p rc d", p=128))
            c.copy('dve', Wuv[:, h, :, (h % 2) * 64:(h % 2 + 1) * 64], sv)
        pool_banks = c.banks[0:4]
        acc_banks = c.banks[4:8]
        allbanks = c.banks
        lo, hi, mid, cnt, ge, dd, n1 = [bs[:, i:i + 1] for i in range(7)]

        def A1(ob):
            jb = 2 * ob + 1
            NK = (jb + 1) * 128
            q_ = qT[ob % 2]; qi_ = qiT[ob % 2]; w_ = wid[ob % 2]
            c.dma(q_[:], G['qT_d'].rearrange("m p t -> p m t")[:, :, ob * 128:(ob + 1) * 128])
            c.dma(qi_[:], G['qiT_d'].rearrange("m p t -> p m t")[:, :, ob * 128:(ob + 1) * 128])
            c.dma(w_[:], G['widx_d'][ob * 128:(ob + 1) * 128, :])
            qizv = qiz[:].rearrange("p (c e) t -> p c e t", e=2)
            c.copy('dve', qizv[0:64, :, 0, :], qi_[0:64, :, :])
            c.copy('dve', qizv[64:128, :, 1, :], qi_[64:128, :, :])
            ri = 0
            for s0 in range(0, NK, 512):
                w = min(512, NK - s0)
                for h in range(16):
                    p = c.ps()
                    c.mm(p[:, 0:w], qiz[:, h, :], kiT[:, s0:s0 + w])
                    r_ = rl[ri % 3]; ri += 1
                    c.act(r_[:, 0:w], p[:, 0:w], AF.Relu)
                    if h == 0:
                        c.ts('dve', sc[:, s0:s0 + w], r_[:, 0:w], w_[:, 0:1], None, ALU.mult)
                    else:
                        c.stt(sc[:, s0:s0 + w], r_[:, 0:w], w_[:, h:h + 1], sc[:, s0:s0 + w], ALU.mult, ALU.add)
                    if h % 4 == 3:
                        yield
            if ob > 0:
                c.emit('dve', lambda e: e.tensor_reduce(out=lo, in_=sc[:, 128:jb * 128], axis=AX.X, op=ALU.min),
                       [lo], [sc[:, 128:jb * 128]])
            c.tt('dve', sc[:, jb * 128:NK], sc[:, jb * 128:NK], negc[:], ALU.add)
            c.ts('dve', sc[:, 0:128], sc[:, 0:128], K['kb0'][:, 0:1], None, ALU.add)
            if ob > 0:
                c.emit('dve', lambda e: e.tensor_reduce(out=hi, in_=sc[:, 0:NK], axis=AX.X, op=ALU.max),
                       [hi], [sc[:, 0:NK]])
                c.ts('dve', hi, hi, 1.0, None, ALU.add)
                yield
                for it in range(NIT_BISECT):
                    c.ts('dve', mid, lo, hi, 0.5, ALU.add, ALU.mult)
                    c.ts('dve', tmpn[:, 0:NK], sc[:, 0:NK], mid, 0.0, ALU.is_ge, ALU.add, accum=cnt)
                    c.ts('dve', ge, cnt, TOPK - 0.5, None, ALU.is_ge)
                    c.tt('dve', dd, mid, lo, ALU.subtract)
                    c.stt(lo, dd, ge, lo, ALU.mult, ALU.add)
                    c.tt('dve', dd, hi, mid, ALU.subtract)
                    c.stt(hi, dd, ge, mid, ALU.mult, ALU.add)
                    if it % 2 == 1:
                        yield
                c.ts('dve', m01[:, 0:NK], sc[:, 0:NK], lo, None, ALU.is_ge)
            else:
                c.ts('dve', m01[:, 0:NK], sc[:, 0:NK], -1e29, None, ALU.is_ge)
            yield
            c.tt('dve', tmpn[:, 0:NK], m01[:, 0:NK], prow1[:, 0:NK], ALU.mult)
            c.emit('dve', lambda e: e.tensor_reduce(out=n1, in_=tmpn[:, 0:NK], axis=AX.X, op=ALU.max),
                   [n1], [tmpn[:, 0:NK]])
            c.ts('dve', n1, n1, -1.0, None, ALU.add)
            c.ts('dve', nbpad[:].rearrange("p (g q) -> p g q", g=2)[:, :, 0:16],
                 negsl[:].unsqueeze(1).to_broadcast([128, 2, 16]), n1, None, ALU.mult)

        def A2(ob):
            jb = 2 * ob + 1
            NKT = jb + 1
            q_ = qT[ob % 2]
            for rc in range(2):
                for hg in range(4):
                    p = c.ps()
                    for j in range(4):
                        h = hg * 4 + j
                        c.mm(p[:, j * 128:(j + 1) * 128], Wuk[:, h, rc * 128:(rc + 1) * 128], q_[:, h // 2, :])
                    c.act(qlat[:, rc, hg * 4:hg * 4 + 4, :], p[:].rearrange("p (j t) -> p j t", j=4), AF.Copy, scale=0.125)
            pnb = c.ps()
            pn = pnb.bitcast(BF16)
            c.tr(pn[:, 0:128], nbpad[:], K['identb'][:])
            for h in range(16):
                hg = h // 4
                pr = slice(64 * (hg % 2), 64 * (hg % 2) + 16)
                c.ts('dve', Rt[hg][pr, (h % 4) * 128:(h % 4 + 1) * 128], pn[pr, 0:128],
                     ohp[pr, h:h + 1], None, ALU.mult)
            for k0 in range(0, NKT, 4):
                nk = min(4, NKT - k0)
                pb = c.ps(); pT = pb.bitcast(BF16)
                for i in range(nk):
                    c.tr(pT[:, i * 128:(i + 1) * 128], m01[:, (k0 + i) * 128:(k0 + i + 1) * 128], K['identb'][:])
                c.ts('dve', mbT[:, k0:k0 + nk, :], pT[:, 0:nk * 128].rearrange("p (k t) -> p k t", k=nk),
                     -1.0, 30000.0, ALU.add, ALU.mult)

        def step(gen):
            if gen is not None:
                next(gen, None)

        def Att(ob, gen):
            jb = 2 * ob + 1
            NKT = jb + 1
            c.banks = pool_banks
            pi = 0
            for hg in range(4):
                for kt in range(NKT):
                    ks = slice(kt * 128, (kt + 1) * 128)
                    pS = c.ps()
                    c.mm(pS[:], ckvT[:, 0, ks], qlat[:, 0, hg * 4:hg * 4 + 4, :].rearrange("p j t -> p (j t)"),
                         start=True, stop=False)
                    c.mm(pS[:], ckvT[:, 1, ks], qlat[:, 1, hg * 4:hg * 4 + 4, :].rearrange("p j t -> p (j t)"),
                         start=False, stop=False)
                    c.mm(pS[:], LTt[hg // 2][:, ks], Rt[hg][:], start=False, stop=False)
                    c.mm(pS[:].rearrange("p (j t) -> p j t", j=4), K['identb'][:],
                         mbT[:, kt, :].unsqueeze(1).to_broadcast([128, 4, 128]), start=False, stop=True)
                    P_ = PT[pi % 3]; pi += 1
                    c.act(P_[:], pS[:], AF.Exp)
                    for j in range(4):
                        c.mm(acc_banks[j][:, 0:257], P_[:, j * 128:(j + 1) * 128], ckvtok[:, kt, :],
                             start=(kt == 0), stop=(kt == NKT - 1))
                    step(gen)
                for j in range(4):
                    rs = bs[:, 7 + j:8 + j]
                    c.emit('dve', lambda e, j=j, rs=rs: e.reciprocal(out=rs, in_=acc_banks[j][:, 256:257]),
                           [rs], [acc_banks[j][:, 256:257]])
                    c.act(osb[:, j, :], acc_banks[j][:, 0:256], AF.Copy, scale=rs)
                for rc in range(2):
                    pb = c.ps(); pT = pb.bitcast(BF16)
                    for j in range(4):
                        c.tr(pT[:, j * 128:(j + 1) * 128], osb[:, j, rc * 128:(rc + 1) * 128], K['identb'][:])
                    c.copy('dve', oT[:, hg * 4:hg * 4 + 4, rc, :], pT[:, 0:512].rearrange("p (j t) -> p j t", j=4))
            y_ = yat[ob % 2]
            for cp in range(0, 8, 4):
                p = c.ps()
                for ci in range(4):
                    cidx = cp + ci
                    n_ = 0
                    for h in (2 * cidx, 2 * cidx + 1):
                        for rc in range(2):
                            c.mm(p[:, ci * 128:(ci + 1) * 128], Wuv[:, h, rc, :], oT[:, h, rc, :],
                                 start=(n_ == 0), stop=(n_ == 3))
                            n_ += 1
                c.copy('act', y_[:, cp:cp + 4, :], p[:].rearrange("p (c t) -> p c t", c=4))
            c.dma(G['yattT_d'][ob], y_[:])
            if gen is not None:
                for _ in gen:
                    pass
            c.banks = allbanks

        c.banks = allbanks
        for _ in A1(0):
            pass
        A2(0)
        for ob in range(nown):
            gen = A1(ob + 1) if ob + 1 < nown else None
            Att(ob, gen)
            if ob + 1 < nown:
                A2(ob + 1)
        c.banks = allbanks


def load_w_resident(c, dst, src, wst, wi, ncols):
    sv = src.rearrange("(k p) n -> p k n", p=128)
    for m in range(0, ncols, 256):
        load_cast(c, dst[:, :, m:m + 256], sv[:, :, m:m + 256], wst[wi[0] % len(wst)])
        wi[0] += 1


def ln_block(c, S, y, eps_c, g_bc, b_bc, out32):
    st6, mv = S['st6'], S['mv']
    for h in range(2):
        c.emit('dve', lambda e, h=h: e.bn_stats(out=st6[:, h, :], in_=y[:, h * 512:(h + 1) * 512]),
               [st6[:, h, :]], [y[:, h * 512:(h + 1) * 512]])
    c.emit('dve', lambda e: e.bn_aggr(out=mv[:, 0:2], in_=st6[:]), [mv[:, 0:2]], [st6[:]])
    c.act(mv[:, 2:3], mv[:, 1:2], AF.Sqrt, bias=eps_c[:, 0:1])
    c.emit('dve', lambda e: e.reciprocal(out=mv[:, 3:4], in_=mv[:, 2:3]), [mv[:, 3:4]], [mv[:, 2:3]])
    c.ts('dve', out32, y, mv[:, 0:1], mv[:, 3:4], ALU.subtract, ALU.mult)
    c.tt('pool', out32, out32, g_bc, ALU.mult)
    c.tt('pool', out32, out32, b_bc, ALU.add)


def merge_phase(c, K, G, nblk):
    nown = nblk // 2
    with c.phase():
        Wps = c.sb('Wps', [128, 8, D], BF16)
        Wpa = c.sb('Wpa', [128, 8, D], BF16)
        Wo = c.sb('Wo', [128, 8, D], BF16)
        wst = [c.sb('wst%d' % i, [128, 2048], F32) for i in range(3)]
        wi = [0]
        load_w_resident(c, Wps, G['w_proj_ssd'], wst, wi, D)
        load_w_resident(c, Wpa, G['w_proj_att'], wst, wi, D)
        load_w_resident(c, Wo, G['w_out'], wst, wi, D)
        ys = [c.sb('ys%d' % i, [128, 8, 512], BF16) for i in range(2)]
        ya = [c.sb('ya%d' % i, [128, 8, 512], BF16) for i in range(2)]
        gs_ = [c.sb('gs%d' % i, [128, 8, 512], BF16) for i in range(2)]
        ga_ = [c.sb('ga%d' % i, [128, 8, 512], BF16) for i in range(2)]
        m1 = [c.sb('m1_%d' % i, [128, 512], F32) for i in range(2)]
        m2 = [c.sb('m2_%d' % i, [128, 512], F32) for i in range(2)]
        mT = c.sb('mT', [128, 8, 512], BF16)
        x1t = [c.sb('x1t%d' % i, [128, D], F32) for i in range(2)]
        yb = [c.sb('yb%d' % i, [128, D], F32) for i in range(2)]
        x2 = [c.sb('x2_%d' % i, [128, D], F32) for i in range(2)]
        x2b = [c.sb('x2b%d' % i, [128, D], BF16) for i in range(2)]
        x2T = [c.sb('x2T%d' % i, [128, 8, 128], BF16) for i in range(2)]
        S = dict(st6=c.sb('st6', [128, 2, 6], F32), mv=c.sb('mv', [128, 4], F32))
        epsc = c.sb('epsc', [128, 1], F32)
        gB = c.sb('gB', [128, D], F32); bB = c.sb('bB', [128, D], F32)
        c.emit('pool', lambda e: e.memset(epsc[:], LN_EPS / (ALPHA * ALPHA)), [epsc[:]], [])
        c.dma(gB[:], G['ln2_g'][0:1, :].partition_broadcast(128))
        c.dma(bB[:], G['ln2_b'][0:1, :].partition_broadcast(128))
        for g in range(nown // 4):
            i2 = g % 2
            for b in range(4):
                ob = g * 4 + b
                c.dma(ys[i2][:, :, b * 128:(b + 1) * 128], G['yssdT_d'][ob])
                c.dma(ya[i2][:, :, b * 128:(b + 1) * 128], G['yattT_d'][ob])
            c.dma(gs_[i2][:], G['sgs_d'].rearrange("m p t -> p m t")[:, :, g * 512:(g + 1) * 512])
            c.dma(ga_[i2][:], G['sga_d'].rearrange("m p t -> p m t")[:, :, g * 512:(g + 1) * 512])
            for m in range(8):
                p1 = c.ps(); p2 = c.ps()
                for k in range(8):
                    c.mm(p1[:], Wps[:, k, m * 128:(m + 1) * 128], ys[i2][:, k, :], start=(k == 0), stop=(k == 7))
                for k in range(8):
                    c.mm(p2[:], Wpa[:, k, m * 128:(m + 1) * 128], ya[i2][:, k, :], start=(k == 0), stop=(k == 7))
                c.tt('dve', m1[m % 2][:], p1[:], gs_[i2][:, m, :], ALU.mult)
                c.tt('dve', m2[m % 2][:], p2[:], ga_[i2][:, m, :], ALU.mult)
                c.tt('pool', mT[:, m, :], m1[m % 2][:], m2[m % 2][:], ALU.add)
            for b in range(4):
                ob = g * 4 + b
                i = ob % 2
                c.dma(x1t[i][:], G['x1_d'][ob * 128:(ob + 1) * 128, :])
                for h in range(2):
                    po = c.ps()
                    for k in range(8):
                        c.mm(po[:], mT[:, k, b * 128:(b + 1) * 128], Wo[:, k, h * 512:(h + 1) * 512],
                             start=(k == 0), stop=(k == 7))
                    c.stt(yb[i][:, h * 512:(h + 1) * 512], po[:], 1.0 / ALPHA, x1t[i][:, h * 512:(h + 1) * 512],
                          ALU.mult, ALU.add)
                ln_block(c, S, yb[i][:], epsc, gB[:], bB[:], x2[i][:])
                c.dma(G['x2_d'][ob * 128:(ob + 1) * 128, :], x2[i][:])
                c.copy('act', x2b[i][:], x2[i][:])
                pb = c.ps(); pT = pb.bitcast(BF16)
                for k in range(8):
                    c.tr(pT[:, k * 128:(k + 1) * 128], x2b[i][:, k * 128:(k + 1) * 128], K['identb'][:])
                c.copy('dve', x2T[i][:], pT[:, 0:1024].rearrange("p (k t) -> p k t", k=8))
                c.dma(G['x2T_d'][ob], x2T[i][:])


def memkv_phase(c, K, G):
    with c.phase():
        Wkv = c.sb('Wkv', [128, 8, 2048], BF16)
        wst = [c.sb('wst%d' % i, [128, 2048], F32) for i in range(3)]
        wi = [0]
        load_w_resident(c, Wkv, G['w_mkv'], wst, wi, 2048)
        mem32 = [c.sb('mem32_%d' % i, [128, D], F32) for i in range(2)]
        memb = [c.sb('memb%d' % i, [128, D], BF16) for i in range(2)]
        memT = c.sb('memT', [128, 8, 256], BF16)
        kmT = c.sb('kmT', [128, 8, 256], BF16)
        vm = c.sb('vm', [128, 2, D], BF16)
        for mt in range(2):
            c.dma(mem32[mt][:], G['mem'][mt * 128:(mt + 1) * 128, :])
            c.copy('act', memb[mt][:], mem32[mt][:])
            pb = c.ps(); pT = pb.bitcast(BF16)
            for k in range(8):
                c.tr(pT[:, k * 128:(k + 1) * 128], memb[mt][:, k * 128:(k + 1) * 128], K['identb'][:])
            c.copy('dve', memT[:, :, mt * 128:(mt + 1) * 128], pT[:, 0:1024].rearrange("p (k t) -> p k t", k=8))
        for m in range(8):
            p = c.ps()
            for k in range(8):
                c.mm(p[:, 0:256], Wkv[:, k, m * 128:(m + 1) * 128], memT[:, k, :], start=(k == 0), stop=(k == 7))
            c.copy('act', kmT[:, m, :], p[:, 0:256])
        for mt in range(2):
            for h in range(2):
                p = c.ps()
                for k in range(8):
                    c.mm(p[:], memT[:, k, mt * 128:(mt + 1) * 128], Wkv[:, k, 1024 + h * 512:1024 + (h + 1) * 512],
                         start=(k == 0), stop=(k == 7))
                c.copy('dve', vm[:, mt, h * 512:(h + 1) * 512], p[:])
        c.dma(G['kmT_d'], kmT[:])
        c.dma(G['vm_d'], vm[:])


def xattn_phase(c, K, G, nblk):
    nown = nblk // 2
    with c.phase():
        Wq = c.sb('Wq', [128, 8, D], BF16)
        Wmo = c.sb('Wmo', [128, 8, D], BF16)
        wst = [c.sb('wst%d' % i, [128, 2048], F32) for i in range(3)]
        wi = [0]
        load_w_resident(c, Wq, G['w_mq'], wst, wi, D)
        load_w_resident(c, Wmo, G['w_mo'], wst, wi, D)
        kmT = c.sb('kmT', [128, 8, 256], BF16)
        vm = c.sb('vm', [128, 2, D], BF16)
        c.dma(kmT[:], G['kmT_d'])
        c.dma(vm[:], G['vm_d'])
        xT = [c.sb('xT%d' % i, [128, 8, 512], BF16) for i in range(2)]
        qmT = c.sb('qmT', [128, 8, 512], BF16)
        x2t = [c.sb('x2t%d' % i, [128, D], F32) for i in range(2)]
        Pm = [c.sb('Pm%d' % i, [128, 4, 256], F32) for i in range(2)]
        Pn = [c.sb('Pn%d' % i, [128, 4, 256], BF16) for i in range(2)]
        PmT = [c.sb('PmT%d' % i, [128, 2, 4, 128], BF16) for i in range(2)]
        omT = [c.sb('omT%d' % i, [128, 8, 128], BF16) for i in range(2)]
        yb = [c.sb('yb%d' % i, [128, D], F32) for i in range(2)]
        x3 = [c.sb('x3_%d' % i, [128, D], F32) for i in range(2)]
        sm = c.sb('smx', [128, 16], F32)
        S = dict(st6=c.sb('st6', [128, 2, 6], F32), mv=c.sb('mv', [128, 4], F32))
        epsc = c.sb('epsc', [128, 1], F32)
        gB = c.sb('gB', [128, D], F32); bB = c.sb('bB', [128, D], F32)
        c.emit('pool', lambda e: e.memset(epsc[:], LN_EPS / (ALPHA * ALPHA)), [epsc[:]], [])
        c.dma(gB[:], G['ln3_g'][0:1, :].partition_broadcast(128))
        c.dma(bB[:], G['ln3_b'][0:1, :].partition_broadcast(128))
        for g in range(nown // 4):
            x_ = xT[g % 2]
            for b in range(4):
                c.dma(x_[:, :, b * 128:(b + 1) * 128], G['x2T_d'][g * 4 + b])
            for m in range(8):
                p = c.ps()
                for k in range(8):
                    c.mm(p[:], Wq[:, k, m * 128:(m + 1) * 128], x_[:, k, :], start=(k == 0), stop=(k == 7))
                c.act(qmT[:, m, :], p[:], AF.Copy, scale=1.0 / 16)
            for b in range(4):
                ob = g * 4 + b
                i = ob % 2
                c.dma(x2t[i][:], G['x2_d'][ob * 128:(ob + 1) * 128, :])
                pS = [c.ps(), c.ps()]
                for h in range(4):
                    for dc in range(2):
                        c.mm(pS[h // 2][:, (h % 2) * 256:(h % 2 + 1) * 256], qmT[:, 2 * h + dc, b * 128:(b + 1) * 128],
                             kmT[:, 2 * h + dc, :], start=(dc == 0), stop=(dc == 1))
                for hf in range(2):
                    c.emit('dve', lambda e, hf=hf: e.tensor_reduce(
                        out=sm[:, hf * 2:hf * 2 + 2], in_=pS[hf][:].rearrange("p (h m) -> p h m", h=2),
                        axis=AX.X, op=ALU.max), [sm[:, hf * 2:hf * 2 + 2]], [pS[hf][:]])
                c.ts('dve', sm[:, 4:8], sm[:, 0:4], -1.0, None, ALU.mult)
                for h in range(4):
                    c.act(Pm[i][:, h, :], pS[h // 2][:, (h % 2) * 256:(h % 2 + 1) * 256], AF.Exp,
                          bias=sm[:, 4 + h:5 + h], accum=sm[:, 8 + h:9 + h])
                c.emit('dve', lambda e: e.reciprocal(out=sm[:, 12:16], in_=sm[:, 8:12]), [sm[:, 12:16]], [sm[:, 8:12]])
                c.tt('dve', Pn[i][:], Pm[i][:], sm[:, 12:16].unsqueeze(2).to_broadcast([128, 4, 256]), ALU.mult)
                for mt in range(2):
                    pb = c.ps(); pT = pb.bitcast(BF16)
                    for h in range(4):
                        c.tr(pT[:, h * 128:(h + 1) * 128], Pn[i][:, h, mt * 128:(mt + 1) * 128], K['identb'][:])
                    c.copy('act', PmT[i][:, mt, :, :], pT[:, 0:512].rearrange("p (h t) -> p h t", h=4))
                for cp in range(0, 8, 4):
                    p = c.ps()
                    for ci in range(4):
                        cc = cp + ci
                        for mt in range(2):
                            c.mm(p[:, ci * 128:(ci + 1) * 128], vm[:, mt, cc * 128:(cc + 1) * 128],
                                 PmT[i][:, mt, cc // 2, :], start=(mt == 0), stop=(mt == 1))
                    c.copy('dve', omT[i][:, cp:cp + 4, :], p[:].rearrange("p (c t) -> p c t", c=4))
                for h in range(2):
                    po = c.ps()
                    for k in range(8):
                        c.mm(po[:], omT[i][:, k, :], Wmo[:, k, h * 512:(h + 1) * 512], start=(k == 0), stop=(k == 7))
                    c.stt(yb[i][:, h * 512:(h + 1) * 512], po[:], 1.0 / ALPHA, x2t[i][:, h * 512:(h + 1) * 512],
                          ALU.mult, ALU.add)
                ln_block(c, S, yb[i][:], epsc, gB[:], bB[:], x3[i][:])
                c.dma(G['x3_d'][ob * 128:(ob + 1) * 128, :], x3[i][:])


def build(stage=99, dbg=False, nblk=NBLK):
    nc = bass.Bass("TRN2", target_bir_lowering=False)
    c = Ctx(nc)
    kind_s = "ExternalOutput" if dbg else "Internal"
    G = {}

    def din(name, shape, dt=F32):
        G[name] = c.dram(name, shape, dt, kind="ExternalInput").ap()
        return G[name]

    def dsc(name, shape, dt):
        G[name] = c.dram(name, shape, dt, kind=kind_s).ap()
        return G[name]

    nown = nblk // 2 * 128
    ntok = nblk * 128
    xs = din("xs", [ntok, D])
    din("ffn1_w_in", [D, 2 * DFF]); din("ffn1_w_out", [DFF, D])
    din("ln1_g", [1, D]); din("ln1_b", [1, D]); din("ln1_gT", [128, 8]); din("ln1_bT", [128, 8])
    din("w_in", [D, D_IN]); din("dt_bias", [1, 16]); din("kv_norm_w", [1, 256])
    din("identb", [128, 128], BF16)
    din("v0", [128, 1])

    dsc("x1T_d", [nblk, 128, 8, 128], BF16)
    dsc("x1_d", [nown, D], F32)
    dsc("xbcT_d", [16, 128, ntok], BF16)
    dsc("kiT_d", [128, ntok], BF16)
    dsc("dt_d", [ntok, 16], F32)
    dsc("ckvtok_d", [nblk, 128, 257], BF16)
    dsc("ckvT_d", [2, 128, ntok], BF16)
    dsc("sgs_d", [8, 128, nown], BF16); dsc("sga_d", [8, 128, nown], BF16)
    dsc("qT_d", [8, 128, nown], BF16); dsc("qiT_d", [8, 128, nown], BF16)
    dsc("zs_d", [nown, D], BF16); dsc("widx_d", [nown, 16], F32)
    dsc("yssdT_d", [nblk // 2, 128, 8, 128], BF16)
    dsc("yattT_d", [nblk // 2, 128, 8, 128], BF16)
    dsc("x2_d", [nown, D], F32); dsc("x2T_d", [nblk // 2, 128, 8, 128], BF16)
    dsc("kmT_d", [128, 8, 256], BF16); dsc("vm_d", [128, 2, D], BF16); dsc("x3_d", [nown, D], F32)
    for wn in ("w_proj_ssd", "w_proj_att", "w_out", "w_mq", "w_mo"):
        din(wn, [D, D])
    din("w_mkv", [D, 2 * D]); din("mem", [256, D])
    for ln in ("ln2", "ln3", "ln4"):
        din(ln + "_g", [1, D]); din(ln + "_b", [1, D])
    din("ffn2_w_in", [D, 2 * DFF]); din("ffn2_w_out", [DFF, D])
    G['out'] = c.dram("out", [nown, D], F32, kind="ExternalOutput").ap()
    G['alibiLT'] = [din("alibiLT%d" % i, [128, SEQ], BF16) for i in range(2)]
    G['alibiR'] = [din("alibiR%d" % i, [128, 512], BF16) for i in range(4)]
    din("ohp", [128, 16])
    din("prow1", [1, SEQ]); din("negslope", [1, 16]); din("negcausal", [128, 128]); din("kb0", [128, 1])
    din("w_uk", [1024, 256]); G['w_uv'] = din("w_uv", [16, 256, 64])
    din("cwT", [128, 16, 4]); din("cbT", [128, 16]); din("trib", [128, 128], BF16); din("ones128b", [128, 128], BF16)
    din("identf", [128, 128]); din("negmask4b", [128, 512], BF16); din("sel16b", [128, 2048], BF16)
    din("a_log", [1, 16]); din("d_skip", [1, 16]); din("ssd_norm_w", [1, D])

    c.banks = [c.psum('bank%d' % i, [128, 512], F32) for i in range(8)]
    K = {}
    K['identb'] = c.sb('identb_s', [128, 128], BF16)
    c.dma(K['identb'][:], G['identb'])
    K['v0'] = c.sb('v0_s', [128, 1], F32)
    c.dma(K['v0'][:], G['v0'])
    K['kb0'] = c.sb('kb0_s', [128, 1], F32)
    c.dma(K['kb0'][:], G['kb0'])

    def post1(blk, S, y, mean, rstd):
        if blk == 'setup':
            S['zb'] = [c.sb('zb%d' % i, [128, D], BF16) for i in range(2)]
            S['x1s'] = [c.sb('x1s%d' % i, [128, 8, 128], BF16) for i in range(2)]
            S['z32'] = [c.sb('z32_%d' % i, [128, D], F32) for i in range(2)]
            c.dma(S['gT'][:], G['ln1_gT'])
            c.dma(S['bT'][:], G['ln1_bT'])
            return
        zb = S['zb'][blk % 2]
        x1s = S['x1s'][blk % 2]
        c.ts('dve', zb[:], y[:], mean, rstd, ALU.subtract, ALU.mult)
        pb = c.ps()
        pT = pb.bitcast(BF16)
        for k in range(8):
            c.tr(pT[:, k * 128:(k + 1) * 128], zb[:, k * 128:(k + 1) * 128], K['identb'][:])
        for k in range(8):
            c.act(x1s[:, k, :], pT[:, k * 128:(k + 1) * 128], AF.Identity,
                  bias=S['bT'][:, k:k + 1], scale=S['gT'][:, k:k + 1])
        if blk == 0:
            c.ts('dve', x1s[:], x1s[:], K['v0'][:, 0:1], None, ALU.mult)
        c.dma(G['x1T_d'][blk], x1s[:])
        if blk % 2 == 1:
            z = S['z32'][(blk // 2) % 2]
            c.ts('dve', z[:], y[:], mean, rstd, ALU.subtract, ALU.mult)
            c.tt('pool', z[:], z[:], S['gB'][:], ALU.mult)
            c.tt('pool', z[:], z[:], S['bB'][:], ALU.add)
            c.dma(G['x1_d'][(blk // 2) * 128:(blk // 2 + 1) * 128, :], z[:])

    if stage >= 1:
        ffn_phase(c, K, xs, nblk, G['ffn1_w_in'], G['ffn1_w_out'], G['ln1_g'], G['ln1_b'], post1, 'f1')
    if stage >= 2:
        proj_phase_all(c, K, G, nblk)
        proj_phase_own(c, K, G, nblk)
    if stage >= 3:
        ssd_phase(c, K, G, nblk)
    if stage >= 4:
        dsa_phase(c, K, G, nblk)
    if stage >= 5:
        merge_phase(c, K, G, nblk)
        memkv_phase(c, K, G)
        xattn_phase(c, K, G, nblk)
    if stage >= 6:
        def post4(blk, S, y, mean, rstd):
            if blk == 'setup':
                S['o32'] = [c.sb('o32_%d' % i, [128, D], F32) for i in range(2)]
                return
            o = S['o32'][blk % 2]
            c.ts('dve', o[:], y[:], mean, rstd, ALU.subtract, ALU.mult)
            c.tt('pool', o[:], o[:], S['gB'][:], ALU.mult)
            c.tt('pool', o[:], o[:], S['bB'][:], ALU.add)
            c.dma(G['out'][blk * 128:(blk + 1) * 128, :], o[:])
        ffn_phase(c, K, G['x3_d'], nblk // 2, G['ffn2_w_in'], G['ffn2_w_out'], G['ln4_g'], G['ln4_b'], post4, 'f2')

    c.barrier()
    c.es.close()
    global LAST_INPUTS
    LAST_INPUTS = [n for n in G if n not in ('out',) and not n.endswith('_d')]
    print("build: ninst=%d nwait=%d nsem=%d" % (c.ninst, c.nwait, c.nsem))
    return nc


def make_consts():
    i = np.arange(128)
    tri = (i[:, None] <= i[None, :]).astype(np.float32)
    negm = np.where(i[:, None] <= i[None, :], 0.0, -30000.0).astype(np.float32)
    sel = np.zeros((128, 16, 128), np.float32)
    for h in range(16):
        sel[h, h, :] = 1.0
    bf = ml_dtypes.bfloat16
    slopes = (2.0 ** (-8.0 * np.arange(1, 17, dtype=np.float64) / 16)).astype(np.float32)
    pos = np.arange(SEQ, dtype=np.float32)
    extra = {}
    ltt = [np.zeros((128, SEQ), np.float32) for _ in range(2)]
    rrt = [np.zeros((128, 512), np.float32) for _ in range(4)]
    ohp = np.zeros((128, 16), np.float32)
    for hg in range(4):
        r0 = 64 * (hg % 2)
        lt = ltt[hg // 2][r0:r0 + 28]
        lt[0:16] = 1.0
        rr = rrt[hg][r0:r0 + 28]
        for j in range(4):
            v = (slopes[hg * 4 + j] * pos).astype(np.float32)
            for k in range(3):
                part = v.astype(bf).astype(np.float32)
                lt[16 + 3 * j + k] = part
                v = v - part
                rr[16 + 3 * j + k, j * 128:(j + 1) * 128] = 1.0
            ohp[r0 + hg * 4 + j, hg * 4 + j] = 1.0
    for q_ in range(2):
        extra["alibiLT%d" % q_] = ltt[q_].astype(bf)
    for q_ in range(4):
        extra["alibiR%d" % q_] = rrt[q_].astype(bf)
    extra["ohp"] = ohp
    extra["prow1"] = (pos + 1.0)[None, :]
    extra["negslope"] = (-slopes)[None, :]
    extra["negcausal"] = np.where(i[None, :] <= i[:, None], 0.0, -1e30).astype(np.float32)
    return {**extra, "identb": np.eye(128, dtype=np.float32).astype(ml_dtypes.bfloat16),
            "identf": np.eye(128, dtype=np.float32), "trib": tri.astype(bf),
            "ones128b": np.ones((128, 128), np.float32).astype(bf),
            "negmask4b": np.ascontiguousarray(np.tile(negm, (1, 4))).astype(bf),
            "sel16b": sel.reshape(128, 2048).astype(bf)}


def make_in_maps(inputs):
    x = np.asarray(inputs["x"], dtype=np.float32)
    cs = make_consts()
    sq = lambda a: np.ascontiguousarray(np.asarray(a, dtype=np.float32)[0])
    shared = {
        "ffn1_w_in": sq(inputs["ffn1_w_in"]), "ffn1_w_out": sq(inputs["ffn1_w_out"]),
        "ln1_g": sq(inputs["ln1_g"])[None, :], "ln1_b": sq(inputs["ln1_b"])[None, :],
        "ln1_gT": np.ascontiguousarray(sq(inputs["ln1_g"]).reshape(8, 128).T),
        "ln1_bT": np.ascontiguousarray(sq(inputs["ln1_b"]).reshape(8, 128).T),
        "w_in": sq(inputs["w_in"]), "dt_bias": sq(inputs["dt_bias"])[None, :],
        "kv_norm_w": sq(inputs["kv_norm_w"])[None, :],
        "cwT": np.ascontiguousarray(sq(inputs["conv_w"]).reshape(4, 16, 128).transpose(2, 1, 0)),
        "cbT": np.ascontiguousarray(sq(inputs["conv_b"]).reshape(16, 128).T),
        "a_log": sq(inputs["a_log"])[None, :], "d_skip": sq(inputs["d_skip"])[None, :],
        "ssd_norm_w": sq(inputs["ssd_norm_w"])[None, :],
        "w_uk": sq(inputs["w_uk"]).reshape(1024, 256), "w_uv": sq(inputs["w_uv"]),
        "w_proj_ssd": sq(inputs["w_proj_ssd"]), "w_proj_att": sq(inputs["w_proj_att"]),
        "w_out": sq(inputs["w_out"]), "w_mq": sq(inputs["w_mq"]), "w_mo": sq(inputs["w_mo"]),
        "w_mkv": sq(inputs["w_mkv"]),
        "ln2_g": sq(inputs["ln2_g"])[None, :], "ln2_b": sq(inputs["ln2_b"])[None, :],
        "ln3_g": sq(inputs["ln3_g"])[None, :], "ln3_b": sq(inputs["ln3_b"])[None, :],
        "ln4_g": sq(inputs["ln4_g"])[None, :], "ln4_b": sq(inputs["ln4_b"])[None, :],
        "ffn2_w_in": sq(inputs["ffn2_w_in"]), "ffn2_w_out": sq(inputs["ffn2_w_out"]),
    }
    shared.update(cs)
    maps = []
    for core in range(8):
        b, par = core // 2, core % 2
        if par == 0:
            xs = np.concatenate([np.zeros((128, D), np.float32), x[b, :SEQ - 128]], axis=0)
        else:
            xs = x[b]
        m = dict(shared)
        m["xs"] = np.ascontiguousarray(xs)
        m["v0"] = np.full((128, 1), float(par), np.float32)
        m["kb0"] = np.full((128, 1), 0.0 if par else -1e30, np.float32)
        m["mem"] = np.ascontiguousarray(np.asarray(inputs["mem"], dtype=np.float32)[b])
        maps.append(m)
    return maps


def kernel(**inputs):
    nc = build()
    maps = make_in_maps(inputs)
    res = run_bass_kernel_spmd(nc, maps, core_ids=list(range(8)))
    out = np.zeros((4, SEQ, D), np.float32)
    for core in range(8):
        b, par = core // 2, core % 2
        o = np.asarray(res.results[core]["out"], dtype=np.float32).reshape(NBLK // 2, 128, D)
        out[b].reshape(NBLK, 128, D)[par::2] = o
    return out
```

```python
import contextlib
import numpy as np
import ml_dtypes
import concourse.bass as bass
import concourse.mybir as mybir
from concourse.bass_utils import run_bass_kernel_spmd

F32 = mybir.dt.float32
BF16 = mybir.dt.bfloat16
AF = mybir.ActivationFunctionType
ALU = mybir.AluOpType
AX = mybir.AxisListType
DSZ = {F32: 4, BF16: 2}
WHOLE = (0, 1 << 30, 0, 1 << 40)

D = 1024
SEQ = 4096
NBLK = 32
DFF = 2816
NF = DFF // 128
ALPHA = 2.0 ** 0.25
LN_EPS = 1e-5


class Ctx:
    def __init__(self, nc):
        self.nc = nc
        self.es = contextlib.ExitStack()
        self.eng = {'pe': nc.tensor, 'act': nc.scalar, 'dve': nc.vector,
                    'pool': nc.gpsimd, 'sp': nc.sync}
        self.esem = {k: self.es.enter_context(nc.semaphore('es_' + k))
                     for k in ('pe', 'act', 'dve', 'pool')}
        self.ecnt = {k: 0 for k in self.esem}
        self.seen = {k: {} for k in self.eng}
        self.trk = {}
        self.free_sems = []
        self.scnt = {}
        self.nsem = 0
        self.nwait = 0
        self.ninst = 0
        self.pstack = None
        self.pnames = None
        self.psi = 0

    def _reg(self, name, rowb, dram=False):
        self.trk[name] = dict(rowb=rowb, w=[], r=[], dsem=None, dram=dram)
        if self.pnames is not None and not dram:
            self.pnames.append(name)

    def sb(self, name, shape, dtype):
        self.uid = getattr(self, 'uid', 0) + 1
        name = '%s_u%d' % (name, self.uid)
        st = self.pstack if self.pstack is not None else self.es
        t = st.enter_context(self.nc.sbuf_tensor(name, list(shape), dtype))
        self._reg(name, int(np.prod(shape[1:])) * DSZ[dtype])
        return t

    def psum(self, name, shape, dtype=F32):
        st = self.pstack if self.pstack is not None else self.es
        t = st.enter_context(self.nc.psum_tensor(name, list(shape), dtype))
        self._reg(name, int(np.prod(shape[1:])) * DSZ[dtype])
        self.trk[name]['psum'] = True
        return t

    def dram(self, name, shape, dtype, kind="Internal"):
        t = self.nc.dram_tensor(name, list(shape), dtype, kind=kind)
        self._reg(name, 0, dram=True)
        return t

    def sem_for(self, name):
        t = self.trk[name]
        if t['dsem'] is None:
            if self.free_sems:
                t['dsem'] = self.free_sems.pop()
            else:
                self.nsem += 1
                s = self.es.enter_context(self.nc.semaphore('ds%d' % self.nsem))
                self.scnt[s.name] = 0
                t['dsem'] = s
        return t['dsem']

    @contextlib.contextmanager
    def phase(self):
        assert self.pstack is None
        self.pstack = contextlib.ExitStack()
        self.pnames = []
        try:
            yield
        finally:
            self.barrier()
            for n in self.pnames:
                t = self.trk.pop(n)
                if t['dsem'] is not None:
                    self.free_sems.append(t['dsem'])
            self.pstack.close()
            self.pstack = None
            self.pnames = None

    def barrier(self):
        for e, eng in self.eng.items():
            for k, s in self.esem.items():
                if k != e and self.seen[e].get(s.name, 0) < self.ecnt[k]:
                    eng.wait_ge(s, self.ecnt[k])
                    self.seen[e][s.name] = self.ecnt[k]
            for name, t in self.trk.items():
                s = t['dsem']
                if s is not None and self.seen[e].get(s.name, 0) < self.scnt[s.name]:
                    eng.wait_ge(s, self.scnt[s.name])
                    self.seen[e][s.name] = self.scnt[s.name]
        for t in self.trk.values():
            t['w'] = []
            t['r'] = []

    def region(self, ap):
        t = self.trk[ap.name]
        sz = DSZ[ap.dtype]
        pat = ap.ap
        off = ap.offset
        if t['dram']:
            ext = sum((c - 1) * abs(s) for s, c in pat) + 1
            return (0, 1, off * sz, (off + ext) * sz)
        if t.get('psum'):
            return (0, 128, 0, t['rowb'])
        rowe = t['rowb'] // sz
        p0 = off // rowe
        f0 = off % rowe
        ps, pc = pat[0]
        p1 = p0 + (pc - 1) * (ps // rowe) + 1
        ext = sum((c - 1) * abs(s) for s, c in pat[1:]) + 1
        return (p0, p1, f0 * sz, (f0 + ext) * sz)

    @staticmethod
    def _ov(a, b):
        return a[0] < b[1] and b[0] < a[1] and a[2] < b[3] and b[2] < a[3]

    @staticmethod
    def _cov(a, b):
        return a[0] <= b[0] and a[1] >= b[1] and a[2] <= b[2] and a[3] >= b[3]

    def _cur(self, t, rec):
        if rec[2] == 'dma' and rec[0] is t['dsem']:
            return (rec[0], self.scnt[rec[0].name], 'dma')
        return rec

    def _deps(self, outs, ins, whole_out=False, e=None):
        deps = []
        for ap in ins:
            t = self.trk[ap.name]
            R = self.region(ap)
            for (Q, rec) in t['w']:
                if self._ov(R, Q):
                    deps.append(self._cur(t, rec))
            if t.get('psum'):
                for (Q, rec) in t['r']:
                    if rec[2] != e:
                        deps.append(rec)
        for ap in outs:
            t = self.trk[ap.name]
            R = self.region(ap) if not whole_out else WHOLE
            for (Q, rec) in t['w']:
                if self._ov(R, Q):
                    deps.append(self._cur(t, rec))
            for (Q, rec) in t['r']:
                if self._ov(R, Q):
                    deps.append(self._cur(t, rec))
        return deps

    def _record(self, outs, ins, rec):
        for ap in ins:
            t = self.trk[ap.name]
            R = self.region(ap)
            t['r'] = [(Q, r) for (Q, r) in t['r']
                      if not (r[0] is rec[0] and self._cov(R, Q))]
            t['r'].append((R, rec))
            if len(t['r']) > 40:
                self._collapse(t, 'r')
        for ap in outs:
            t = self.trk[ap.name]
            R = self.region(ap)
            t['w'] = [(Q, r) for (Q, r) in t['w'] if not self._cov(R, Q)]
            t['r'] = [(Q, r) for (Q, r) in t['r'] if not self._cov(R, Q)]
            t['w'].append((R, rec))
            if len(t['w']) > 40:
                self._collapse(t, 'w')

    def _collapse(self, t, k):
        best = {}
        box = None
        for (Q, r) in t[k]:
            key = r[0].name
            if key not in best or best[key][1] < r[1]:
                best[key] = r
            box = Q if box is None else (min(box[0], Q[0]), max(box[1], Q[1]),
                                         min(box[2], Q[2]), max(box[3], Q[3]))
        t[k] = [(box, r) for r in best.values()]

    def _waits(self, e, deps):
        need = {}
        for (sem, val, src) in deps:
            if src == e and e == 'pe':
                continue
            if self.seen[e].get(sem.name, 0) >= val:
                continue
            if sem.name not in need or need[sem.name][1] < val:
                need[sem.name] = (sem, val)
        for (sem, val) in need.values():
            self.seen[e][sem.name] = val
        return list(need.values())

    def _autobb(self):
        tot = self.ninst + self.nwait
        if tot - getattr(self, 'lastbb', 0) > 500:
            self.lastbb = tot
            self.newbb()

    def emit(self, e, fn, outs, ins, attach=True):
        if self.ninst >= LIMIT:
            return None
        self._autobb()
        waits = self._waits(e, self._deps(outs, ins, e=e))
        eng = self.eng[e]
        self.nwait += len(waits)
        last = None
        if attach and waits:
            last = waits.pop()
        for (sem, val) in waits:
            eng.wait_ge(sem, val)
        ins_ = fn(eng)
        if last is not None:
            ins_._wait_ge(last[0], last[1])
        ins_.then_inc(self.esem[e], 1)
        self.ecnt[e] += 1
        self.ninst += 1
        rec = (self.esem[e], self.ecnt[e], e)
        self._record(outs, ins, rec)
        return ins_

    def dma(self, out, in_, q='sp', **kw):
        if self.ninst >= LIMIT:
            return None
        self._autobb()
        deps = self._deps([out], [in_], whole_out=True)
        sbside = in_ if self.trk[out.name]['dram'] else out
        sem = self.sem_for(sbside.name)
        deps = [d for d in deps if not (d[0] is sem and d[2] == 'dma')]
        waits = self._waits(q, deps)
        eng = self.eng[q]
        self.nwait += len(waits)
        for (s, val) in waits:
            eng.wait_ge(s, val)
        self.scnt[sem.name] += 16
        eng.dma_start(out=out, in_=in_, **kw).then_inc(sem, 16)
        self.ninst += 1
        rec = (sem, self.scnt[sem.name], 'dma')
        self._record([out], [in_], rec)

    def newbb(self):
        self.nbb = getattr(self, 'nbb', 0) + 1
        self.nc.switch_bb('kbb%d' % self.nbb)

    def ps(self):
        b = self.banks[self.psi % len(self.banks)]
        self.psi += 1
        return b

    def mm(self, out, lhsT, rhs, start=True, stop=True):
        return self.emit('pe', lambda e: e.matmul(out, lhsT=lhsT, rhs=rhs, start=start, stop=stop),
                         [out], [lhsT, rhs])

    def tr(self, out, in_, ident):
        return self.emit('pe', lambda e: e.transpose(out=out, in_=in_, identity=ident),
                         [out], [in_, ident])

    def act(self, out, in_, func, bias=None, scale=None, accum=None, e='act'):
        kw = {}
        ins = [in_]
        outs = [out]
        if bias is not None:
            kw['bias'] = bias
            if not isinstance(bias, (int, float)):
                ins.append(bias)
        if scale is not None:
            kw['scale'] = scale
            if not isinstance(scale, (int, float)):
                ins.append(scale)
        if accum is not None:
            kw['accum_out'] = accum
            outs.append(accum)
        return self.emit('act', lambda e_: e_.activation(out=out, in_=in_, func=func, **kw),
                         outs, ins, attach=accum is None)

    def ts(self, e, out, in0, s1, s2, op0, op1=None, accum=None):
        ins = [in0] + [s for s in (s1, s2) if s is not None and not isinstance(s, (int, float))]
        outs = [out] + ([accum] if accum is not None else [])
        kw = {}
        if op1 is not None:
            kw['op1'] = op1
        if accum is not None:
            kw['accum_out'] = accum
        return self.emit(e, lambda e_: e_.tensor_scalar(out=out, in0=in0, scalar1=s1, scalar2=s2,
                                                        op0=op0, **kw),
                         outs, ins, attach=accum is None)

    def tt(self, e, out, in0, in1, op):
        return self.emit(e, lambda e_: e_.tensor_tensor(out=out, in0=in0, in1=in1, op=op),
                         [out], [in0, in1])

    def stt(self, out, in0, scalar, in1, op0, op1):
        ins = [in0, in1] + ([scalar] if not isinstance(scalar, (int, float)) else [])
        return self.emit('dve', lambda e_: e_.scalar_tensor_tensor(out=out, in0=in0, scalar=scalar,
                                                                   in1=in1, op0=op0, op1=op1),
                         [out], ins)

    def copy(self, e, out, in_):
        if e == 'act':
            return self.emit('act', lambda e_: e_.copy(out=out, in_=in_), [out], [in_])
        return self.emit(e, lambda e_: e_.tensor_copy(out=out, in_=in_), [out], [in_])


def load_cast(c, dst, src, st, eng='pool'):
    shp = list(src.shape)
    n = int(np.prod(shp[1:]))
    sv = st[:, 0:n]
    if len(shp) == 3:
        sv = sv.rearrange("p (a b) -> p a b", a=shp[1])
    c.dma(sv, src)
    c.copy(eng, dst, sv)


def layer_norm_stats(c, S, y, eps):
    st6 = S['st6']
    mv = S['mv']
    for h in range(2):
        c.emit('dve', lambda e, h=h: e.bn_stats(out=st6[:, h, :], in_=y[:, h * 512:(h + 1) * 512]),
               [st6[:, h, :]], [y[:, h * 512:(h + 1) * 512]])
    c.emit('dve', lambda e: e.bn_aggr(out=mv[:, 0:2], in_=st6[:]), [mv[:, 0:2]], [st6[:]])
    c.act(mv[:, 2:3], mv[:, 1:2], AF.Sqrt, bias=S['epsc'][:, 0:1])
    c.emit('dve', lambda e: e.reciprocal(out=mv[:, 3:4], in_=mv[:, 2:3]), [mv[:, 3:4]], [mv[:, 2:3]])
    return mv[:, 0:1], mv[:, 3:4]


def ffn_phase(c, K, xsrc, nblk, w_in, w_out, g_d, b_d, post, name):
    TT = 1024
    NSUB = TT // 128
    assert (nblk * 128) % TT == 0
    with c.phase():
        W2 = c.sb('W2', [128, NF, D], BF16)
        xT = c.sb('xT', [128, 8, TT], BF16)
        hT = c.sb('hT', [128, NF, TT], BF16)
        W1 = [c.sb('W1_%d' % i, [128, 8, 512], BF16) for i in range(2)]
        wst = [c.sb('wst%d' % i, [128, 2048], F32) for i in range(3)]
        xin = [c.sb('xin%d' % i, [128, D], F32) for i in range(2)]
        xbf = [c.sb('xbf%d' % i, [128, D], BF16) for i in range(2)]
        sg = [c.sb('sg%d' % i, [128, 512], BF16) for i in range(2)]
        yb = [c.sb('yb%d' % i, [128, D], F32) for i in range(2)]
        S = dict(K)
        S['st6'] = c.sb('st6', [128, 2, 6], F32)
        S['mv'] = c.sb('mv', [128, 4], F32)
        S['epsc'] = c.sb('epsc', [128, 1], F32)
        S['gB'] = c.sb('gB', [128, D], F32)
        S['bB'] = c.sb('bB', [128, D], F32)
        S['gT'] = c.sb('gT', [128, 8], F32)
        S['bT'] = c.sb('bT', [128, 8], F32)
        c.emit('pool', lambda e: e.memset(S['epsc'][:], LN_EPS / (ALPHA * ALPHA)), [S['epsc'][:]], [])
        c.dma(S['gB'][:], g_d[0:1, :].partition_broadcast(128))
        c.dma(S['bB'][:], b_d[0:1, :].partition_broadcast(128))
        post('setup', S, None, None, None)
        wi = 0
        w2v = w_out.rearrange("(f p) n -> p f n", p=128)
        for f in range(0, NF, 2):
            load_cast(c, W2[:, f:f + 2, :], w2v[:, f:f + 2, :], wst[wi % 3])
            wi += 1
        w1v = w_in.rearrange("(k p) n -> p k n", p=128)
        xi = 0
        for t in range(nblk * 128 // TT):
            for s in range(NSUB):
                blk = t * NSUB + s
                xs_ = xin[xi % 2]
                xb_ = xbf[xi % 2]
                xi += 1
                c.dma(xs_[:], xsrc[blk * 128:(blk + 1) * 128, :])
                c.copy('act', xb_[:], xs_[:])
                pb = c.ps()
                pT = pb.bitcast(BF16)
                for k in range(8):
                    c.tr(pT[:, k * 128:(k + 1) * 128], xb_[:, k * 128:(k + 1) * 128], K['identb'][:])
                c.copy('dve', xT[:, :, s * 128:(s + 1) * 128],
                       pT[:, 0:1024].rearrange("p (k t) -> p k t", k=8))
            for fp in range(NF // 2):
                w1 = W1[fp % 2]
                for gu in range(2):
                    col = gu * DFF + fp * 256
                    load_cast(c, w1[:, :, gu * 256:(gu + 1) * 256], w1v[:, :, col:col + 256], wst[wi % 3],
                              eng='pool' if gu == 0 else 'dve')
                    wi += 1
                for fi in range(2):
                    f = fp * 2 + fi
                    for h in range(TT // 512):
                        pg = c.ps()
                        pu = c.ps()
                        for k in range(8):
                            c.mm(pg[:], w1[:, k, fi * 128:(fi + 1) * 128], xT[:, k, h * 512:(h + 1) * 512],
                                 start=(k == 0), stop=(k == 7))
                        for k in range(8):
                            c.mm(pu[:], w1[:, k, 256 + fi * 128:256 + (fi + 1) * 128],
                                 xT[:, k, h * 512:(h + 1) * 512], start=(k == 0), stop=(k == 7))
                        s_ = sg[(f * 2 + h) % 2]
                        c.act(s_[:], pg[:], AF.Silu)
                        c.tt('dve', hT[:, f, h * 512:(h + 1) * 512], s_[:], pu[:], ALU.mult)
            for s in range(NSUB):
                blk = t * NSUB + s
                xs_ = xin[xi % 2]
                y = yb[xi % 2]
                xi += 1
                c.dma(xs_[:], xsrc[blk * 128:(blk + 1) * 128, :])
                for h in range(2):
                    po = c.ps()
                    for f in range(NF):
                        c.mm(po[:], hT[:, f, s * 128:(s + 1) * 128], W2[:, f, h * 512:(h + 1) * 512],
                             start=(f == 0), stop=(f == NF - 1))
                    c.stt(y[:, h * 512:(h + 1) * 512], po[:], 0.5 / ALPHA, xs_[:, h * 512:(h + 1) * 512],
                          ALU.mult, ALU.add)
                mean, rstd = layer_norm_stats(c, S, y, None)
                post(blk, S, y, mean, rstd)


O_GS, O_GA, O_Z, O_XBC, O_DT, O_Q, O_CKV, O_QI, O_KI, O_WI = 0, 1024, 2048, 3072, 5120, 5136, 6160, 6416, 7440, 7504
D_IN = 7520
RMS_EPS = 1e-6


def proj_phase_all(c, K, G, nblk):
    w_in = G['w_in']
    wv = w_in.rearrange("(k p) n -> p k n", p=128)
    with c.phase():
        Wx = c.sb('Wx', [128, 8, 2048], BF16)
        Wk = c.sb('Wk', [128, 8, 128], BF16)
        Wd = c.sb('Wd', [128, 8, 16], BF16)
        Wc = c.sb('Wc', [128, 8, 256], BF16)
        wst = [c.sb('wst%d' % i, [128, 2048], F32) for i in range(3)]
        xg = [c.sb('xg%d' % i, [128, 8, 512], BF16) for i in range(2)]
        xo = [c.sb('xo%d' % i, [128, 16, 512], BF16) for i in range(2)]
        ko = [c.sb('ko%d' % i, [128, 512], BF16) for i in range(2)]
        dto = [c.sb('dto%d' % i, [128, 16], F32) for i in range(2)]
        dte = [c.sb('dte%d' % i, [128, 16], F32) for i in range(2)]
        cko = [c.sb('cko%d' % i, [128, 257], BF16) for i in range(2)]
        ckf = [c.sb('ckf%d' % i, [128, 256], F32) for i in range(2)]
        ckT = [c.sb('ckT%d' % i, [128, 2, 128], BF16) for i in range(2)]
        sq = c.sb('sq', [128, 256], F32)
        ss = c.sb('ss', [128, 4], F32)
        dtb = c.sb('dtb', [128, 16], F32)
        kvg = c.sb('kvg', [128, 256], F32)
        epsr = c.sb('epsr', [128, 1], F32)
        c.emit('pool', lambda e: e.memset(epsr[:], RMS_EPS), [epsr[:]], [])
        c.dma(dtb[:], G['dt_bias'][0:1, :].partition_broadcast(128))
        c.dma(kvg[:], G['kv_norm_w'][0:1, :].partition_broadcast(128))
        wi = 0
        for m in range(0, 16, 2):
            load_cast(c, Wx[:, :, m * 128:(m + 2) * 128], wv[:, :, O_XBC + m * 128:O_XBC + (m + 2) * 128], wst[wi % 3]); wi += 1
        load_cast(c, Wc[:], wv[:, :, O_CKV:O_CKV + 256], wst[wi % 3]); wi += 1
        for hf in range(2):
            load_cast(c, Wk[:, :, hf * 64:(hf + 1) * 64], wv[:, :, O_KI:O_KI + 64], wst[wi % 3]); wi += 1
        load_cast(c, Wd[:], wv[:, :, O_DT:O_DT + 16], wst[wi % 3]); wi += 1
        xbcv = G['xbcT_d'].rearrange("m p t -> p m t")
        for g in range(nblk // 4):
            x_ = xg[g % 2]
            for b in range(4):
                c.dma(x_[:, :, b * 128:(b + 1) * 128], G['x1T_d'][g * 4 + b])
            xo_ = xo[g % 2]
            for m in range(16):
                p = c.ps()
                for k in range(8):
                    c.mm(p[:], Wx[:, k, m * 128:(m + 1) * 128], x_[:, k, :], start=(k == 0), stop=(k == 7))
                c.copy('act' if m % 2 == 0 else 'dve', xo_[:, m, :], p[:])
            for mh in range(2):
                c.dma(xbcv[:, mh * 8:mh * 8 + 8, g * 512:(g + 1) * 512], xo_[:, mh * 8:mh * 8 + 8, :])
            p = c.ps()
            for k in range(8):
                c.mm(p[:], Wk[:, k, :], x_[:, k, :], start=(k == 0), stop=(k == 7))
            ko_ = ko[g % 2]
            c.copy('dve', ko_[:], p[:])
            c.dma(G['kiT_d'][:, g * 512:(g + 1) * 512], ko_[:])
            for b in range(4):
                blk = g * 4 + b
                p = c.ps()
                for k in range(8):
                    c.mm(p[:, 0:16], x_[:, k, b * 128:(b + 1) * 128], Wd[:, k, :], start=(k == 0), stop=(k == 7))
                e_ = dte[blk % 2]
                d_ = dto[blk % 2]
                c.tt('dve', e_[:], p[:, 0:16], dtb[:], ALU.add)
                c.act(e_[:], e_[:], AF.Exp)
                c.act(d_[:], e_[:], AF.Ln, bias=1.0)
                if blk == 0:
                    c.ts('dve', d_[:], d_[:], K['v0'][:, 0:1], None, ALU.mult)
                c.dma(G['dt_d'][blk * 128:(blk + 1) * 128, :], d_[:])
                p = c.ps()
                for k in range(8):
                    c.mm(p[:, 0:256], x_[:, k, b * 128:(b + 1) * 128], Wc[:, k, :], start=(k == 0), stop=(k == 7))
                f_ = ckf[blk % 2]
                c.copy('dve', f_[:], p[:, 0:256])
                c.act(sq[:], f_[:], AF.Square, accum=ss[:, 0:1])
                c.act(ss[:, 1:2], ss[:, 0:1], AF.Sqrt, bias=epsr[:, 0:1], scale=1.0 / 256)
                c.emit('dve', lambda e: e.reciprocal(out=ss[:, 2:3], in_=ss[:, 1:2]), [ss[:, 2:3]], [ss[:, 1:2]])
                o_ = cko[blk % 2]
                c.stt(o_[:, 0:256], f_[:], ss[:, 2:3], kvg[:], ALU.mult, ALU.mult)
                c.emit('pool', lambda e, o_=o_: e.memset(o_[:, 256:257], 1.0), [o_[:, 256:257]], [])
                c.dma(G['ckvtok_d'][blk], o_[:])
                pb = c.ps()
                pT = pb.bitcast(BF16)
                for r in range(2):
                    c.tr(pT[:, r * 128:(r + 1) * 128], o_[:, r * 128:(r + 1) * 128], K['identb'][:])
                t_ = ckT[blk % 2]
                c.copy('act', t_[:], pT[:, 0:256].rearrange("p (r t) -> p r t", r=2))
                c.dma(G['ckvT_d'].rearrange("r p t -> p r t")[:, :, blk * 128:(blk + 1) * 128], t_[:])


def proj_phase_own(c, K, G, nblk):
    wv = G['w_in'].rearrange("(k p) n -> p k n", p=128)
    nown = nblk // 2
    with c.phase():
        Wf = c.sb('Wf', [128, 8, 4096], BF16)
        Wz = c.sb('Wz', [128, 8, 1024], BF16)
        Ww = c.sb('Ww', [128, 8, 16], BF16)
        wst = [c.sb('wst%d' % i, [128, 2048], F32) for i in range(3)]
        xg = [c.sb('xg%d' % i, [128, 8, 512], BF16) for i in range(2)]
        fo = [c.sb('fo%d' % i, [128, 8, 512], BF16) for i in range(2)]
        zo = [c.sb('zo%d' % i, [128, 1024], BF16) for i in range(2)]
        wo = [c.sb('wo%d' % i, [128, 16], F32) for i in range(2)]
        wi = 0
        segs = [(O_GS, 'sgs_d', AF.Sigmoid), (O_GA, 'sga_d', AF.Sigmoid), (O_Q, 'qT_d', AF.Identity),
                (O_QI, 'qiT_d', AF.Identity)]
        for si, (off, _, _) in enumerate(segs):
            for m in range(0, 8, 2):
                load_cast(c, Wf[:, :, si * 1024 + m * 128:si * 1024 + (m + 2) * 128],
                          wv[:, :, off + m * 128:off + (m + 2) * 128], wst[wi % 3]); wi += 1
        for m in range(0, 8, 2):
            load_cast(c, Wz[:, :, m * 128:(m + 2) * 128], wv[:, :, O_Z + m * 128:O_Z + (m + 2) * 128], wst[wi % 3]); wi += 1
        load_cast(c, Ww[:], wv[:, :, O_WI:O_WI + 16], wst[wi % 3]); wi += 1
        fi = 0
        for g in range(nown // 4):
            x_ = xg[g % 2]
            for b in range(4):
                c.dma(x_[:, :, b * 128:(b + 1) * 128], G['x1T_d'][2 * (g * 4 + b) + 1])
            for si, (off, dst, fn) in enumerate(segs):
                f_ = fo[fi % 2]; fi += 1
                for m in range(8):
                    p = c.ps()
                    for k in range(8):
                        c.mm(p[:], Wf[:, k, si * 1024 + m * 128:si * 1024 + (m + 1) * 128], x_[:, k, :],
                             start=(k == 0), stop=(k == 7))
                    if fn == AF.Identity and m % 2 == 1:
                        c.copy('dve', f_[:, m, :], p[:])
                    else:
                        c.act(f_[:, m, :], p[:], fn)
                c.dma(G[dst].rearrange("m p t -> p m t")[:, :, g * 512:(g + 1) * 512], f_[:])
            for b in range(4):
                ob = g * 4 + b
                z_ = zo[ob % 2]
                for h in range(2):
                    p = c.ps()
                    for k in range(8):
                        c.mm(p[:], x_[:, k, b * 128:(b + 1) * 128], Wz[:, k, h * 512:(h + 1) * 512],
                             start=(k == 0), stop=(k == 7))
                    c.act(z_[:, h * 512:(h + 1) * 512], p[:], AF.Silu)
                c.dma(G['zs_d'][ob * 128:(ob + 1) * 128, :], z_[:])
                p = c.ps()
                for k in range(8):
                    c.mm(p[:, 0:16], x_[:, k, b * 128:(b + 1) * 128], Ww[:, k, :], start=(k == 0), stop=(k == 7))
                w_ = wo[ob % 2]
                c.act(w_[:], p[:, 0:16], AF.Copy, scale=0.25)
                c.dma(G['widx_d'][ob * 128:(ob + 1) * 128, :], w_[:])


def ssd_phase(c, K, G, nblk):
    with c.phase():
        cw = c.sb('cw', [128, 16, 4], F32)
        cb = c.sb('cb', [128, 16], F32)
        tri = c.sb('tri', [128, 128], BF16)
        ones = c.sb('ones', [128, 128], BF16)
        negm = c.sb('negm', [128, 512], BF16)
        sel = c.sb('sel', [128, 2048], BF16)
        apad = c.sb('apad', [128, 2, 128], BF16)
        ahl = c.sb('ahl', [128, 2, 16], BF16)
        ares = c.sb('ares', [128, 16], F32)
        Abc = c.sb('Abc', [128, 16], F32)
        Dbc = c.sb('Dbc', [128, 16], F32)
        nwB = c.sb('nwB', [128, D], F32)
        epsr = c.sb('epsr', [128, 1], F32)
        S32 = c.sb('S32', [128, 16, 64], F32)
        Sbf = c.sb('Sbf', [128, 16, 64], BF16)
        for t_, n_ in ((cw, 'cwT'), (cb, 'cbT'), (tri, 'trib'), (ones, 'ones128b'),
                       (negm, 'negmask4b'), (sel, 'sel16b')):
            c.dma(t_[:], G[n_])
        c.dma(Abc[:], G['a_log'][0:1, :].partition_broadcast(128))
        c.dma(Dbc[:], G['d_skip'][0:1, :].partition_broadcast(128))
        c.dma(nwB[:], G['ssd_norm_w'][0:1, :].partition_broadcast(128))
        c.act(Abc[:], Abc[:], AF.Exp)
        c.ts('dve', Abc[:], Abc[:], -1.0, None, ALU.mult)
        c.emit('dve', lambda e: e.memset(epsr[:], RMS_EPS), [epsr[:]], [])
        c.emit('dve', lambda e: e.memset(S32[:], 0.0), [S32[:]], [])
        c.emit('dve', lambda e: e.memset(Sbf[:], 0.0), [Sbf[:]], [])
        c.emit('dve', lambda e: e.memset(apad[:], 0.0), [apad[:]], [])
        xh = [c.sb('xh%d' % i, [128, 16, 132], BF16) for i in range(2)]
        t1 = [c.sb('t1_%d' % i, [128, 16, 128], F32) for i in range(3)]
        xc = [c.sb('xc%d' % i, [128, 16, 128], BF16) for i in range(2)]
        xtok = [c.sb('xtok%d' % i, [128, 16, 64], F32) for i in range(2)]
        Btok = [c.sb('Btok%d' % i, [128, 4, 128], BF16) for i in range(2)]
        xdt = [c.sb('xdt%d' % i, [128, 16, 64], BF16) for i in range(2)]
        xdd = [c.sb('xdd%d' % i, [128, 16, 64], BF16) for i in range(2)]
        dtt = [c.sb('dtt%d' % i, [128, 16], F32) for i in range(2)]
        sm = [c.sb('sm%d' % i, [128, 6, 16], F32) for i in range(2)]
        acsT = [c.sb('acsT%d' % i, [128, 2, 128], BF16) for i in range(2)]
        acsTf = c.sb('acsTf', [128, 128], F32)
        LT = c.sb('LT', [128, 16, 128], F32)
        MT = c.sb('MT', [128, 16, 128], BF16)
        yo = c.sb('yo', [128, 16, 64], F32)
        yy = c.sb('yy', [128, 16, 64], F32)
        zt = [c.sb('zt%d' % i, [128, D], BF16) for i in range(2)]
        sq = c.sb('sqs', [128, 256], F32)
        gs = c.sb('gs', [128, 12], F32)
        ynb = c.sb('ynb', [128, D], BF16)
        ynT = [c.sb('ynT%d' % i, [128, 8, 128], BF16) for i in range(2)]
        for i in range(2):
            c.emit('dve', lambda e, i=i: e.memset(xh[i][:], 0.0), [xh[i][:]], [])
        xbv = G['xbcT_d'].rearrange("m p t -> p m t")
        def front(ch):
            x_ = xh[ch % 2]
            for mh in range(2):
                ms = slice(mh * 8, mh * 8 + 8)
                if ch == 0:
                    c.dma(x_[:, ms, 4:132], xbv[:, ms, 0:128])
                else:
                    c.dma(x_[:, ms, 0:132], xbv[:, ms, ch * 128 - 4:ch * 128 + 128])
            d_ = dtt[ch % 2]
            c.dma(d_[:], G['dt_d'][ch * 128:(ch + 1) * 128, :])
            a_ = t1[0]
            b_ = t1[1]
            c.tt('dve', a_[:], x_[:, :, 1:129], cw[:, :, 0:1].to_broadcast([128, 16, 128]), ALU.mult)
            for k in range(1, 4):
                b_ = t1[1 + (k % 2)]
                c.tt('dve', b_[:], x_[:, :, k + 1:k + 129], cw[:, :, k:k + 1].to_broadcast([128, 16, 128]), ALU.mult)
                c.tt('pool', a_[:], a_[:], b_[:], ALU.add)
            c.tt('dve', a_[:], a_[:], cb[:].unsqueeze(2).to_broadcast([128, 16, 128]), ALU.add)
            xc_ = xc[ch % 2]
            c.act(xc_[:], a_[:], AF.Silu)
            p0 = c.ps(); p0b = p0.bitcast(BF16)
            p1 = c.ps(); p1b = p1.bitcast(BF16)
            for m in range(8):
                c.tr(p0b[:, m * 128:(m + 1) * 128], xc_[:, m, :], K['identb'][:])
            for m in range(4):
                c.tr(p1b[:, m * 128:(m + 1) * 128], xc_[:, 8 + m, :], K['identb'][:])
            xt_ = xtok[ch % 2]
            Bt_ = Btok[ch % 2]
            c.copy('act', xt_[:], p0b[:, 0:1024].rearrange("p (h q) -> p h q", h=16))
            c.copy('act', Bt_[:], p1b[:, 0:512].rearrange("p (g n) -> p g n", g=4))
            xd_ = xdt[ch % 2]
            c.tt('dve', xd_[:], xt_[:], d_[:].unsqueeze(2).to_broadcast([128, 16, 64]), ALU.mult)
            s_ = sm[ch % 2]
            c.tt('dve', s_[:, 0, :], d_[:], Abc[:], ALU.mult)
            c.copy('dve', ahl[:, 0, :], s_[:, 0, :])
            c.tt('dve', ares[:], s_[:, 0, :], ahl[:, 0, :], ALU.subtract)
            c.copy('dve', ahl[:, 1, :], ares[:])
            c.copy('dve', apad[:, :, 0:16], ahl[:])
            pc = c.ps()
            for q_ in range(2):
                c.mm(pc[:, 0:16], tri[:], ahl[:, q_, :], start=(q_ == 0), stop=(q_ == 1))
            pc1 = c.ps()
            for q_ in range(2):
                c.mm(pc1[:, 0:16], ones[:], ahl[:, q_, :], start=(q_ == 0), stop=(q_ == 1))
            pc2 = c.ps()
            for q_ in range(2):
                c.mm(pc2[:, 0:128], apad[:, q_, :], tri[:], start=(q_ == 0), stop=(q_ == 1))
            c.copy('dve', s_[:, 1, :], pc[:, 0:16])
            c.ts('dve', s_[:, 2, :], pc[:, 0:16], -1.0, None, ALU.mult)
            c.act(s_[:, 3, :], pc[:, 0:16], AF.Exp)
            c.tt('dve', s_[:, 4, :], pc1[:, 0:16], s_[:, 1, :], ALU.subtract)
            c.act(s_[:, 4, :], s_[:, 4, :], AF.Exp)
            c.act(s_[:, 5, :], pc1[:, 0:16], AF.Exp)
            aT_ = acsT[ch % 2]
            c.copy('dve', acsTf[:], pc2[:, 0:128])
            c.copy('dve', aT_[:, 0, :], acsTf[:])
            c.tt('dve', acsTf[:], acsTf[:], aT_[:, 0, :], ALU.subtract)
            c.copy('dve', aT_[:, 1, :], acsTf[:])
        def back(ch):
            own = ch % 2 == 1
            xc_ = xc[ch % 2]; xt_ = xtok[ch % 2]; Bt_ = Btok[ch % 2]; xd_ = xdt[ch % 2]
            s_ = sm[ch % 2]; aT_ = acsT[ch % 2]
            if own:
                ob = ch // 2
                for q in range(4):
                    pl = c.ps()
                    c.mm(pl[:], K['identb'][:], negm[:], start=True, stop=False)
                    for j in range(4):
                        h = q * 4 + j
                        for q_ in range(2):
                            c.mm(pl[:, j * 128:(j + 1) * 128], sel[:, h * 128:(h + 1) * 128], aT_[:, q_, :],
                                 start=False, stop=(j == 3 and q_ == 1))
                    for j in range(4):
                        h = q * 4 + j
                        c.act(LT[:, h, :], pl[:, j * 128:(j + 1) * 128], AF.Exp, bias=s_[:, 2, h:h + 1])
                pcb = c.ps()
                for g in range(4):
                    c.mm(pcb[:, g * 128:(g + 1) * 128], xc_[:, 8 + g, :], xc_[:, 12 + g, :])
                c.tt('dve', MT[:].rearrange("p (g j) l -> p g j l", g=4),
                     LT[:].rearrange("p (g j) l -> p g j l", g=4),
                     pcb[:].rearrange("p (g l) -> p g l", g=4).unsqueeze(2).to_broadcast([128, 4, 4, 128]),
                     ALU.mult)
                py = [c.ps(), c.ps()]
                for h in range(16):
                    c.mm(py[h // 8][:, (h % 8) * 64:(h % 8 + 1) * 64], MT[:, h, :], xd_[:, h, :])
                po = [c.ps(), c.ps()]
                for g in range(4):
                    c.mm(po[g // 2][:, (g % 2) * 256:(g % 2 + 1) * 256], xc_[:, 12 + g, :],
                         Sbf[:, 4 * g:4 * g + 4, :].rearrange("p h q -> p (h q)"))
                for hf in range(2):
                    hs = slice(hf * 8, hf * 8 + 8)
                    c.tt('dve', yo[:, hs, :], po[hf][:].rearrange("p (h q) -> p h q", h=8),
                         s_[:, 3, hs].unsqueeze(2).to_broadcast([128, 8, 64]), ALU.mult)
                    c.tt('dve', yy[:, hs, :], py[hf][:].rearrange("p (h q) -> p h q", h=8), yo[:, hs, :], ALU.add)
                c.tt('dve', yo[:], xt_[:], Dbc[:].unsqueeze(2).to_broadcast([128, 16, 64]), ALU.mult)
                c.tt('dve', yy[:], yy[:], yo[:], ALU.add)
                z_ = zt[ob % 2]
                c.dma(z_[:], G['zs_d'][ob * 128:(ob + 1) * 128, :])
                yf = yy[:].rearrange("p h q -> p (h q)")
                c.tt('dve', yf, yf, z_[:], ALU.mult)
                for g in range(4):
                    c.act(sq[:], yf[:, g * 256:(g + 1) * 256], AF.Square, accum=gs[:, g:g + 1])
                c.act(gs[:, 4:8], gs[:, 0:4], AF.Sqrt, bias=epsr[:, 0:1], scale=1.0 / 256)
                c.emit('dve', lambda e: e.reciprocal(out=gs[:, 8:12], in_=gs[:, 4:8]), [gs[:, 8:12]], [gs[:, 4:8]])
                c.tt('dve', yy[:].rearrange("p (g j) q -> p g (j q)", g=4),
                     yy[:].rearrange("p (g j) q -> p g (j q)", g=4),
                     gs[:, 8:12].unsqueeze(2).to_broadcast([128, 4, 256]), ALU.mult)
                c.tt('dve', ynb[:], yf, nwB[:], ALU.mult)
                pb = c.ps(); pT = pb.bitcast(BF16)
                for m in range(8):
                    c.tr(pT[:, m * 128:(m + 1) * 128], ynb[:, m * 128:(m + 1) * 128], K['identb'][:])
                yT_ = ynT[ob % 2]
                c.copy('act', yT_[:], pT[:, 0:1024].rearrange("p (m t) -> p m t", m=8))
                c.dma(G['yssdT_d'][ob], yT_[:])
            xe_ = xdd[ch % 2]
            c.tt('dve', xe_[:], xd_[:], s_[:, 4, :].unsqueeze(2).to_broadcast([128, 16, 64]), ALU.mult)
            pst = [c.ps(), c.ps()]
            for g in range(4):
                c.mm(pst[g // 2][:, (g % 2) * 256:(g % 2 + 1) * 256], Bt_[:, g, :],
                     xe_[:, 4 * g:4 * g + 4, :].rearrange("p h q -> p (h q)"))
            c.tt('dve', S32[:], S32[:], s_[:, 5, :].unsqueeze(2).to_broadcast([128, 16, 64]), ALU.mult)
            for hf in range(2):
                hs = slice(hf * 8, hf * 8 + 8)
                c.tt('dve', S32[:, hs, :], S32[:, hs, :], pst[hf][:].rearrange("p (h q) -> p h q", h=8), ALU.add)
            c.copy('act', Sbf[:], S32[:])

        front(0)
        for ch in range(nblk):
            if ch + 1 < nblk:
                front(ch + 1)
            back(ch)


LAST_INPUTS = []
LIMIT = 10 ** 9
NIT_BISECT = 16
TOPK = 256


def dsa_phase(c, K, G, nblk):
    ntok = nblk * 128
    nown = nblk // 2
    with c.phase():
        kiT = c.sb('kiT', [128, ntok], BF16)
        ckvT = c.sb('ckvT', [128, 2, ntok], BF16)
        ckvtok = c.sb('ckvtok', [128, nblk, 257], BF16)
        Wuk = c.sb('Wuk', [128, 16, 256], BF16)
        qiz = c.sb('qiz', [128, 16, 128], BF16)
        Wuv = c.sb('Wuv', [128, 16, 2, 128], BF16)
        wst = [c.sb('wst%d' % i, [128, 2048], F32) for i in range(1)]
        LTt = [c.sb('LTt%d' % i, [128, ntok], BF16) for i in range(2)]
        Rt = [c.sb('Rt%d' % i, [128, 512], BF16) for i in range(4)]
        ohp = c.sb('ohp', [128, 16], F32)
        nbpad = c.sb('nbpad', [128, 128], BF16)
        prow1 = c.sb('prow1', [128, ntok], F32)
        negsl = c.sb('negsl', [128, 16], F32)
        negc = c.sb('negc', [128, 128], F32)
        identf = c.sb('identf2', [128, 128], F32)
        sc = c.sb('sc', [128, ntok], F32)
        tmpn = c.sb('tmpn', [128, ntok], F32)
        m01 = c.sb('m01', [128, ntok], BF16)
        mbT = c.sb('mbT', [128, nblk, 128], BF16)
        qT = [c.sb('qT%d' % i, [128, 8, 128], BF16) for i in range(2)]
        qiT = [c.sb('qiT%d' % i, [128, 8, 128], BF16) for i in range(2)]
        wid = [c.sb('wid%d' % i, [128, 16], F32) for i in range(2)]
        qlat = c.sb('qlat', [128, 2, 16, 128], BF16)
        rl = [c.sb('rl%d' % i, [128, 512], F32) for i in range(3)]
        PT = [c.sb('PT%d' % i, [128, 512], BF16) for i in range(3)]
        osb = c.sb('osb', [128, 4, 256], BF16)
        oT = c.sb('oT', [128, 16, 2, 128], BF16)
        yat = [c.sb('yat%d' % i, [128, 8, 128], BF16) for i in range(2)]
        bs = c.sb('bs', [128, 12], F32)
        nb = c.sb('nb', [128, 16], F32)
        c.dma(kiT[:], G['kiT_d'])
        c.dma(ckvT[:], G['ckvT_d'].rearrange("r p t -> p r t"))
        for b0 in range(0, nblk, 8):
            c.dma(ckvtok[:, b0:b0 + 8, :], G['ckvtok_d'].rearrange("b p f -> p b f")[:, b0:b0 + 8, :])
        for i in range(2):
            c.dma(LTt[i][:], G['alibiLT'][i][:, 0:ntok])
        for i in range(4):
            c.dma(Rt[i][:], G['alibiR'][i])
        c.emit('dve', lambda e: e.memset(qiz[:], 0.0), [qiz[:]], [])
        c.dma(ohp[:], G['ohp'])
        c.emit('dve', lambda e: e.memset(nbpad[:], 0.0), [nbpad[:]], [])
        c.dma(prow1[:], G['prow1'][0:1, 0:ntok].partition_broadcast(128))
        c.dma(negsl[:], G['negslope'][0:1, :].partition_broadcast(128))
        c.dma(negc[:], G['negcausal'])
        c.dma(identf[:], G['identf'])
        c.emit('dve', lambda e: e.memset(Wuk[:], 0.0), [Wuk[:]], [])
        svk = wst[0][:, 0:2048].rearrange("p (a b) -> p a b", a=8)
        c.dma(svk, G['w_uk'].rearrange("(c p) r -> p c r", p=128))
        Wukv = Wuk[:].rearrange("p (c e) r -> p c e r", e=2)
        c.copy('dve', Wukv[0:64, :, 0, :], svk[0:64, :, :])
        c.copy('dve', Wukv[64:128, :, 1, :], svk[64:128, :, :])
        c.emit('dve', lambda e: e.memset(Wuv[:], 0.0), [Wuv[:]], [])
        for h in range(16):
            st = wst[0]
            sv = st[:, 0:128].rearrange("p (a b) -> p a b", a=2)
            c.dma(sv, G['w_uv'][h].rearrange("(rc p) d -> p rc d", p=128))
            c.copy('dve', Wuv[:, h, :, (h % 2) * 64:(h % 2 + 1) * 64], sv)
        pool_banks = c.banks[0:4]
        acc_banks = c.banks[4:8]
        allbanks = c.banks
        lo, hi, mid, cnt, ge, dd, n1 = [bs[:, i:i + 1] for i in range(7)]

        def A1(ob):
            jb = 2 * ob + 1
            NK = (jb + 1) * 128
            q_ = qT[ob % 2]; qi_ = qiT[ob % 2]; w_ = wid[ob % 2]
            c.dma(q_[:], G['qT_d'].rearrange("m p t -> p m t")[:, :, ob * 128:(ob + 1) * 128])
            c.dma(qi_[:], G['qiT_d'].rearrange("m p t -> p m t")[:, :, ob * 128:(ob + 1) * 128])
            c.dma(w_[:], G['widx_d'][ob * 128:(ob + 1) * 128, :])
            qizv = qiz[:].rearrange("p (c e) t -> p c e t", e=2)
            c.copy('dve', qizv[0:64, :, 0, :], qi_[0:64, :, :])
            c.copy('dve', qizv[64:128, :, 1, :], qi_[64:128, :, :])
            ri = 0
            for s0 in range(0, NK, 512):
                w = min(512, NK - s0)
                for h in range(16):
                    p = c.ps()
                    c.mm(p[:, 0:w], qiz[:, h, :], kiT[:, s0:s0 + w])
                    r_ = rl[ri % 3]; ri += 1
                    c.act(r_[:, 0:w], p[:, 0:w], AF.Relu)
                    if h == 0:
                        c.ts('dve', sc[:, s0:s0 + w], r_[:, 0:w], w_[:, 0:1], None, ALU.mult)
                    else:
                        c.stt(sc[:, s0:s0 + w], r_[:, 0:w], w_[:, h:h + 1], sc[:, s0:s0 + w], ALU.mult, ALU.add)
                    if h % 4 == 3:
                        yield
            if ob > 0:
                c.emit('dve', lambda e: e.tensor_reduce(out=lo, in_=sc[:, 128:jb * 128], axis=AX.X, op=ALU.min),
                       [lo], [sc[:, 128:jb * 128]])
            c.tt('dve', sc[:, jb * 128:NK], sc[:, jb * 128:NK], negc[:], ALU.add)
            c.ts('dve', sc[:, 0:128], sc[:, 0:128], K['kb0'][:, 0:1], None, ALU.add)
            if ob > 0:
                c.emit('dve', lambda e: e.tensor_reduce(out=hi, in_=sc[:, 0:NK], axis=AX.X, op=ALU.max),
                       [hi], [sc[:, 0:NK]])
                c.ts('dve', hi, hi, 1.0, None, ALU.add)
                yield
                for it in range(NIT_BISECT):
                    c.ts('dve', mid, lo, hi, 0.5, ALU.add, ALU.mult)
                    c.ts('dve', tmpn[:, 0:NK], sc[:, 0:NK], mid, 0.0, ALU.is_ge, ALU.add, accum=cnt)
                    c.ts('dve', ge, cnt, TOPK - 0.5, None, ALU.is_ge)
                    c.tt('dve', dd, mid, lo, ALU.subtract)
                    c.stt(lo, dd, ge, lo, ALU.mult, ALU.add)
                    c.tt('dve', dd, hi, mid, ALU.subtract)
                    c.stt(hi, dd, ge, mid, ALU.mult, ALU.add)
                    if it % 2 == 1:
                        yield
                c.ts('dve', m01[:, 0:NK], sc[:, 0:NK], lo, None, ALU.is_ge)
            else:
                c.ts('dve', m01[:, 0:NK], sc[:, 0:NK], -1e29, None, ALU.is_ge)
            yield
            c.tt('dve', tmpn[:, 0:NK], m01[:, 0:NK], prow1[:, 0:NK], ALU.mult)
            c.emit('dve', lambda e: e.tensor_reduce(out=n1, in_=tmpn[:, 0:NK], axis=AX.X, op=ALU.max),
                   [n1], [tmpn[:, 0:NK]])
            c.ts('dve', n1, n1, -1.0, None, ALU.add)
            c.ts('dve', nbpad[:].rearrange("p (g q) -> p g q", g=2)[:, :, 0:16],
                 negsl[:].unsqueeze(1).to_broadcast([128, 2, 16]), n1, None, ALU.mult)

        def A2(ob):
            jb = 2 * ob + 1
            NKT = jb + 1
            q_ = qT[ob % 2]
            for rc in range(2):
                for hg in range(4):
                    p = c.ps()
                    for j in range(4):
                        h = hg * 4 + j
                        c.mm(p[:, j * 128:(j + 1) * 128], Wuk[:, h, rc * 128:(rc + 1) * 128], q_[:, h // 2, :])
                    c.act(qlat[:, rc, hg * 4:hg * 4 + 4, :], p[:].rearrange("p (j t) -> p j t", j=4), AF.Copy, scale=0.125)
            pnb = c.ps()
            pn = pnb.bitcast(BF16)
            c.tr(pn[:, 0:128], nbpad[:], K['identb'][:])
            for h in range(16):
                hg = h // 4
                pr = slice(64 * (hg % 2), 64 * (hg % 2) + 16)
                c.ts('dve', Rt[hg][pr, (h % 4) * 128:(h % 4 + 1) * 128], pn[pr, 0:128],
                     ohp[pr, h:h + 1], None, ALU.mult)
            for k0 in range(0, NKT, 4):
                nk = min(4, NKT - k0)
                pb = c.ps(); pT = pb.bitcast(BF16)
                for i in range(nk):
                    c.tr(pT[:, i * 128:(i + 1) * 128], m01[:, (k0 + i) * 128:(k0 + i + 1) * 128], K['identb'][:])
                c.ts('dve', mbT[:, k0:k0 + nk, :], pT[:, 0:nk * 128].rearrange("p (k t) -> p k t", k=nk),
                     -1.0, 30000.0, ALU.add, ALU.mult)

        def step(gen):
            if gen is not None:
                next(gen, None)

        def Att(ob, gen):
            jb = 2 * ob + 1
            NKT = jb + 1
            c.banks = pool_banks
            pi = 0
            def qk(hg, kt):
                ks = slice(kt * 128, (kt + 1) * 128)
                pS = c.ps()
                c.mm(pS[:], ckvT[:, 0, ks], qlat[:, 0, hg * 4:hg * 4 + 4, :].rearrange("p j t -> p (j t)"),
                     start=True, stop=False)
                c.mm(pS[:], ckvT[:, 1, ks], qlat[:, 1, hg * 4:hg * 4 + 4, :].rearrange("p j t -> p (j t)"),
                     start=False, stop=False)
                c.mm(pS[:], LTt[hg // 2][:, ks], Rt[hg][:], start=False, stop=False)
                c.mm(pS[:].rearrange("p (j t) -> p j t", j=4), K['identb'][:],
                     mbT[:, kt, :].unsqueeze(1).to_broadcast([128, 4, 128]), start=False, stop=True)
                P_ = PT[pstate[0] % 3]; pstate[0] += 1
                c.act(P_[:], pS[:], AF.Exp)
                return P_

            pstate = [0]
            for hg in range(4):
                cur = qk(hg, 0)
                for kt in range(NKT):
                    nxt = qk(hg, kt + 1) if kt + 1 < NKT else None
                    for j in range(4):
                        c.mm(acc_banks[j][:, 0:257], cur[:, j * 128:(j + 1) * 128], ckvtok[:, kt, :],
                             start=(kt == 0), stop=(kt == NKT - 1))
                    step(gen)
                    cur = nxt
                for j in range(4):
                    rs = bs[:, 7 + j:8 + j]
                    c.emit('dve', lambda e, j=j, rs=rs: e.reciprocal(out=rs, in_=acc_banks[j][:, 256:257]),
                           [rs], [acc_banks[j][:, 256:257]])
                    c.act(osb[:, j, :], acc_banks[j][:, 0:256], AF.Copy, scale=rs)
                for rc in range(2):
                    pb = c.ps(); pT = pb.bitcast(BF16)
                    for j in range(4):
                        c.tr(pT[:, j * 128:(j + 1) * 128], osb[:, j, rc * 128:(rc + 1) * 128], K['identb'][:])
                    c.copy('dve', oT[:, hg * 4:hg * 4 + 4, rc, :], pT[:, 0:512].rearrange("p (j t) -> p j t", j=4))
            y_ = yat[ob % 2]
            for cp in range(0, 8, 4):
                p = c.ps()
                for ci in range(4):
                    cidx = cp + ci
                    n_ = 0
                    for h in (2 * cidx, 2 * cidx + 1):
                        for rc in range(2):
                            c.mm(p[:, ci * 128:(ci + 1) * 128], Wuv[:, h, rc, :], oT[:, h, rc, :],
                                 start=(n_ == 0), stop=(n_ == 3))
                            n_ += 1
                c.copy('act', y_[:, cp:cp + 4, :], p[:].rearrange("p (c t) -> p c t", c=4))
            c.dma(G['yattT_d'][ob], y_[:])
            if gen is not None:
                for _ in gen:
                    pass
            c.banks = allbanks

        c.banks = allbanks
        for _ in A1(0):
            pass
        A2(0)
        for ob in range(nown):
            gen = A1(ob + 1) if ob + 1 < nown else None
            Att(ob, gen)
            if ob + 1 < nown:
                A2(ob + 1)
        c.banks = allbanks


def load_w_resident(c, dst, src, wst, wi, ncols):
    sv = src.rearrange("(k p) n -> p k n", p=128)
    for m in range(0, ncols, 256):
        load_cast(c, dst[:, :, m:m + 256], sv[:, :, m:m + 256], wst[wi[0] % len(wst)])
        wi[0] += 1


def ln_block(c, S, y, eps_c, g_bc, b_bc, out32):
    st6, mv = S['st6'], S['mv']
    for h in range(2):
        c.emit('dve', lambda e, h=h: e.bn_stats(out=st6[:, h, :], in_=y[:, h * 512:(h + 1) * 512]),
               [st6[:, h, :]], [y[:, h * 512:(h + 1) * 512]])
    c.emit('dve', lambda e: e.bn_aggr(out=mv[:, 0:2], in_=st6[:]), [mv[:, 0:2]], [st6[:]])
    c.act(mv[:, 2:3], mv[:, 1:2], AF.Sqrt, bias=eps_c[:, 0:1])
    c.emit('dve', lambda e: e.reciprocal(out=mv[:, 3:4], in_=mv[:, 2:3]), [mv[:, 3:4]], [mv[:, 2:3]])
    c.ts('dve', out32, y, mv[:, 0:1], mv[:, 3:4], ALU.subtract, ALU.mult)
    c.tt('pool', out32, out32, g_bc, ALU.mult)
    c.tt('pool', out32, out32, b_bc, ALU.add)


def merge_phase(c, K, G, nblk):
    nown = nblk // 2
    with c.phase():
        Wps = c.sb('Wps', [128, 8, D], BF16)
        Wpa = c.sb('Wpa', [128, 8, D], BF16)
        Wo = c.sb('Wo', [128, 8, D], BF16)
        wst = [c.sb('wst%d' % i, [128, 2048], F32) for i in range(3)]
        wi = [0]
        load_w_resident(c, Wps, G['w_proj_ssd'], wst, wi, D)
        load_w_resident(c, Wpa, G['w_proj_att'], wst, wi, D)
        load_w_resident(c, Wo, G['w_out'], wst, wi, D)
        ys = [c.sb('ys%d' % i, [128, 8, 512], BF16) for i in range(2)]
        ya = [c.sb('ya%d' % i, [128, 8, 512], BF16) for i in range(2)]
        gs_ = [c.sb('gs%d' % i, [128, 8, 512], BF16) for i in range(2)]
        ga_ = [c.sb('ga%d' % i, [128, 8, 512], BF16) for i in range(2)]
        m1 = [c.sb('m1_%d' % i, [128, 512], F32) for i in range(2)]
        m2 = [c.sb('m2_%d' % i, [128, 512], F32) for i in range(2)]
        mT = c.sb('mT', [128, 8, 512], BF16)
        x1t = [c.sb('x1t%d' % i, [128, D], F32) for i in range(2)]
        yb = [c.sb('yb%d' % i, [128, D], F32) for i in range(2)]
        x2 = [c.sb('x2_%d' % i, [128, D], F32) for i in range(2)]
        x2b = [c.sb('x2b%d' % i, [128, D], BF16) for i in range(2)]
        x2T = [c.sb('x2T%d' % i, [128, 8, 128], BF16) for i in range(2)]
        S = dict(st6=c.sb('st6', [128, 2, 6], F32), mv=c.sb('mv', [128, 4], F32))
        epsc = c.sb('epsc', [128, 1], F32)
        gB = c.sb('gB', [128, D], F32); bB = c.sb('bB', [128, D], F32)
        c.emit('pool', lambda e: e.memset(epsc[:], LN_EPS / (ALPHA * ALPHA)), [epsc[:]], [])
        c.dma(gB[:], G['ln2_g'][0:1, :].partition_broadcast(128))
        c.dma(bB[:], G['ln2_b'][0:1, :].partition_broadcast(128))
        for g in range(nown // 4):
            i2 = g % 2
            for b in range(4):
                ob = g * 4 + b
                c.dma(ys[i2][:, :, b * 128:(b + 1) * 128], G['yssdT_d'][ob])
                c.dma(ya[i2][:, :, b * 128:(b + 1) * 128], G['yattT_d'][ob])
            c.dma(gs_[i2][:], G['sgs_d'].rearrange("m p t -> p m t")[:, :, g * 512:(g + 1) * 512])
            c.dma(ga_[i2][:], G['sga_d'].rearrange("m p t -> p m t")[:, :, g * 512:(g + 1) * 512])
            for m in range(8):
                p1 = c.ps(); p2 = c.ps()
                for k in range(8):
                    c.mm(p1[:], Wps[:, k, m * 128:(m + 1) * 128], ys[i2][:, k, :], start=(k == 0), stop=(k == 7))
                for k in range(8):
                    c.mm(p2[:], Wpa[:, k, m * 128:(m + 1) * 128], ya[i2][:, k, :], start=(k == 0), stop=(k == 7))
                c.tt('dve', m1[m % 2][:], p1[:], gs_[i2][:, m, :], ALU.mult)
                c.tt('dve', m2[m % 2][:], p2[:], ga_[i2][:, m, :], ALU.mult)
                c.tt('pool', mT[:, m, :], m1[m % 2][:], m2[m % 2][:], ALU.add)
            for b in range(4):
                ob = g * 4 + b
                i = ob % 2
                c.dma(x1t[i][:], G['x1_d'][ob * 128:(ob + 1) * 128, :])
                for h in range(2):
                    po = c.ps()
                    for k in range(8):
                        c.mm(po[:], mT[:, k, b * 128:(b + 1) * 128], Wo[:, k, h * 512:(h + 1) * 512],
                             start=(k == 0), stop=(k == 7))
                    c.stt(yb[i][:, h * 512:(h + 1) * 512], po[:], 1.0 / ALPHA, x1t[i][:, h * 512:(h + 1) * 512],
                          ALU.mult, ALU.add)
                ln_block(c, S, yb[i][:], epsc, gB[:], bB[:], x2[i][:])
                c.dma(G['x2_d'][ob * 128:(ob + 1) * 128, :], x2[i][:])
                c.copy('act', x2b[i][:], x2[i][:])
                pb = c.ps(); pT = pb.bitcast(BF16)
                for k in range(8):
                    c.tr(pT[:, k * 128:(k + 1) * 128], x2b[i][:, k * 128:(k + 1) * 128], K['identb'][:])
                c.copy('dve', x2T[i][:], pT[:, 0:1024].rearrange("p (k t) -> p k t", k=8))
                c.dma(G['x2T_d'][ob], x2T[i][:])


def memkv_phase(c, K, G):
    with c.phase():
        Wkv = c.sb('Wkv', [128, 8, 2048], BF16)
        wst = [c.sb('wst%d' % i, [128, 2048], F32) for i in range(3)]
        wi = [0]
        load_w_resident(c, Wkv, G['w_mkv'], wst, wi, 2048)
        mem32 = [c.sb('mem32_%d' % i, [128, D], F32) for i in range(2)]
        memb = [c.sb('memb%d' % i, [128, D], BF16) for i in range(2)]
        memT = c.sb('memT', [128, 8, 256], BF16)
        kmT = c.sb('kmT', [128, 8, 256], BF16)
        vm = c.sb('vm', [128, 2, D], BF16)
        for mt in range(2):
            c.dma(mem32[mt][:], G['mem'][mt * 128:(mt + 1) * 128, :])
            c.copy('act', memb[mt][:], mem32[mt][:])
            pb = c.ps(); pT = pb.bitcast(BF16)
            for k in range(8):
                c.tr(pT[:, k * 128:(k + 1) * 128], memb[mt][:, k * 128:(k + 1) * 128], K['identb'][:])
            c.copy('dve', memT[:, :, mt * 128:(mt + 1) * 128], pT[:, 0:1024].rearrange("p (k t) -> p k t", k=8))
        for m in range(8):
            p = c.ps()
            for k in range(8):
                c.mm(p[:, 0:256], Wkv[:, k, m * 128:(m + 1) * 128], memT[:, k, :], start=(k == 0), stop=(k == 7))
            c.copy('act', kmT[:, m, :], p[:, 0:256])
        for mt in range(2):
            for h in range(2):
                p = c.ps()
                for k in range(8):
                    c.mm(p[:], memT[:, k, mt * 128:(mt + 1) * 128], Wkv[:, k, 1024 + h * 512:1024 + (h + 1) * 512],
                         start=(k == 0), stop=(k == 7))
                c.copy('dve', vm[:, mt, h * 512:(h + 1) * 512], p[:])
        c.dma(G['kmT_d'], kmT[:])
        c.dma(G['vm_d'], vm[:])


def xattn_phase(c, K, G, nblk):
    nown = nblk // 2
    with c.phase():
        Wq = c.sb('Wq', [128, 8, D], BF16)
        Wmo = c.sb('Wmo', [128, 8, D], BF16)
        wst = [c.sb('wst%d' % i, [128, 2048], F32) for i in range(3)]
        wi = [0]
        load_w_resident(c, Wq, G['w_mq'], wst, wi, D)
        load_w_resident(c, Wmo, G['w_mo'], wst, wi, D)
        kmT = c.sb('kmT', [128, 8, 256], BF16)
        vm = c.sb('vm', [128, 2, D], BF16)
        c.dma(kmT[:], G['kmT_d'])
        c.dma(vm[:], G['vm_d'])
        xT = [c.sb('xT%d' % i, [128, 8, 512], BF16) for i in range(2)]
        qmT = c.sb('qmT', [128, 8, 512], BF16)
        x2t = [c.sb('x2t%d' % i, [128, D], F32) for i in range(2)]
        Pm = [c.sb('Pm%d' % i, [128, 4, 256], F32) for i in range(2)]
        Pn = [c.sb('Pn%d' % i, [128, 4, 256], BF16) for i in range(2)]
        PmT = [c.sb('PmT%d' % i, [128, 2, 4, 128], BF16) for i in range(2)]
        omT = [c.sb('omT%d' % i, [128, 8, 128], BF16) for i in range(2)]
        yb = [c.sb('yb%d' % i, [128, D], F32) for i in range(2)]
        x3 = [c.sb('x3_%d' % i, [128, D], F32) for i in range(2)]
        sm = c.sb('smx', [128, 16], F32)
        S = dict(st6=c.sb('st6', [128, 2, 6], F32), mv=c.sb('mv', [128, 4], F32))
        epsc = c.sb('epsc', [128, 1], F32)
        gB = c.sb('gB', [128, D], F32); bB = c.sb('bB', [128, D], F32)
        c.emit('pool', lambda e: e.memset(epsc[:], LN_EPS / (ALPHA * ALPHA)), [epsc[:]], [])
        c.dma(gB[:], G['ln3_g'][0:1, :].partition_broadcast(128))
        c.dma(bB[:], G['ln3_b'][0:1, :].partition_broadcast(128))
        for g in range(nown // 4):
            x_ = xT[g % 2]
            for b in range(4):
                c.dma(x_[:, :, b * 128:(b + 1) * 128], G['x2T_d'][g * 4 + b])
            for m in range(8):
                p = c.ps()
                for k in range(8):
                    c.mm(p[:], Wq[:, k, m * 128:(m + 1) * 128], x_[:, k, :], start=(k == 0), stop=(k == 7))
                c.act(qmT[:, m, :], p[:], AF.Copy, scale=1.0 / 16)
            for b in range(4):
                ob = g * 4 + b
                i = ob % 2
                c.dma(x2t[i][:], G['x2_d'][ob * 128:(ob + 1) * 128, :])
                pS = [c.ps(), c.ps()]
                for h in range(4):
                    for dc in range(2):
                        c.mm(pS[h // 2][:, (h % 2) * 256:(h % 2 + 1) * 256], qmT[:, 2 * h + dc, b * 128:(b + 1) * 128],
                             kmT[:, 2 * h + dc, :], start=(dc == 0), stop=(dc == 1))
                for hf in range(2):
                    c.emit('dve', lambda e, hf=hf: e.tensor_reduce(
                        out=sm[:, hf * 2:hf * 2 + 2], in_=pS[hf][:].rearrange("p (h m) -> p h m", h=2),
                        axis=AX.X, op=ALU.max), [sm[:, hf * 2:hf * 2 + 2]], [pS[hf][:]])
                c.ts('dve', sm[:, 4:8], sm[:, 0:4], -1.0, None, ALU.mult)
                for h in range(4):
                    c.act(Pm[i][:, h, :], pS[h // 2][:, (h % 2) * 256:(h % 2 + 1) * 256], AF.Exp,
                          bias=sm[:, 4 + h:5 + h], accum=sm[:, 8 + h:9 + h])
                c.emit('dve', lambda e: e.reciprocal(out=sm[:, 12:16], in_=sm[:, 8:12]), [sm[:, 12:16]], [sm[:, 8:12]])
                c.tt('dve', Pn[i][:], Pm[i][:], sm[:, 12:16].unsqueeze(2).to_broadcast([128, 4, 256]), ALU.mult)
                for mt in range(2):
                    pb = c.ps(); pT = pb.bitcast(BF16)
                    for h in range(4):
                        c.tr(pT[:, h * 128:(h + 1) * 128], Pn[i][:, h, mt * 128:(mt + 1) * 128], K['identb'][:])
                    c.copy('act', PmT[i][:, mt, :, :], pT[:, 0:512].rearrange("p (h t) -> p h t", h=4))
                for cp in range(0, 8, 4):
                    p = c.ps()
                    for ci in range(4):
                        cc = cp + ci
                        for mt in range(2):
                            c.mm(p[:, ci * 128:(ci + 1) * 128], vm[:, mt, cc * 128:(cc + 1) * 128],
                                 PmT[i][:, mt, cc // 2, :], start=(mt == 0), stop=(mt == 1))
                    c.copy('dve', omT[i][:, cp:cp + 4, :], p[:].rearrange("p (c t) -> p c t", c=4))
                for h in range(2):
                    po = c.ps()
                    for k in range(8):
                        c.mm(po[:], omT[i][:, k, :], Wmo[:, k, h * 512:(h + 1) * 512], start=(k == 0), stop=(k == 7))
                    c.stt(yb[i][:, h * 512:(h + 1) * 512], po[:], 1.0 / ALPHA, x2t[i][:, h * 512:(h + 1) * 512],
                          ALU.mult, ALU.add)
                ln_block(c, S, yb[i][:], epsc, gB[:], bB[:], x3[i][:])
                c.dma(G['x3_d'][ob * 128:(ob + 1) * 128, :], x3[i][:])


def build(stage=99, dbg=False, nblk=NBLK):
    nc = bass.Bass("TRN2", target_bir_lowering=False)
    c = Ctx(nc)
    kind_s = "ExternalOutput" if dbg else "Internal"
    G = {}

    def din(name, shape, dt=F32):
        G[name] = c.dram(name, shape, dt, kind="ExternalInput").ap()
        return G[name]

    def dsc(name, shape, dt):
        G[name] = c.dram(name, shape, dt, kind=kind_s).ap()
        return G[name]

    nown = nblk // 2 * 128
    ntok = nblk * 128
    xs = din("xs", [ntok, D])
    din("ffn1_w_in", [D, 2 * DFF]); din("ffn1_w_out", [DFF, D])
    din("ln1_g", [1, D]); din("ln1_b", [1, D]); din("ln1_gT", [128, 8]); din("ln1_bT", [128, 8])
    din("w_in", [D, D_IN]); din("dt_bias", [1, 16]); din("kv_norm_w", [1, 256])
    din("identb", [128, 128], BF16)
    din("v0", [128, 1])

    dsc("x1T_d", [nblk, 128, 8, 128], BF16)
    dsc("x1_d", [nown, D], F32)
    dsc("xbcT_d", [16, 128, ntok], BF16)
    dsc("kiT_d", [128, ntok], BF16)
    dsc("dt_d", [ntok, 16], F32)
    dsc("ckvtok_d", [nblk, 128, 257], BF16)
    dsc("ckvT_d", [2, 128, ntok], BF16)
    dsc("sgs_d", [8, 128, nown], BF16); dsc("sga_d", [8, 128, nown], BF16)
    dsc("qT_d", [8, 128, nown], BF16); dsc("qiT_d", [8, 128, nown], BF16)
    dsc("zs_d", [nown, D], BF16); dsc("widx_d", [nown, 16], F32)
    dsc("yssdT_d", [nblk // 2, 128, 8, 128], BF16)
    dsc("yattT_d", [nblk // 2, 128, 8, 128], BF16)
    dsc("x2_d", [nown, D], F32); dsc("x2T_d", [nblk // 2, 128, 8, 128], BF16)
    dsc("kmT_d", [128, 8, 256], BF16); dsc("vm_d", [128, 2, D], BF16); dsc("x3_d", [nown, D], F32)
    for wn in ("w_proj_ssd", "w_proj_att", "w_out", "w_mq", "w_mo"):
        din(wn, [D, D])
    din("w_mkv", [D, 2 * D]); din("mem", [256, D])
    for ln in ("ln2", "ln3", "ln4"):
        din(ln + "_g", [1, D]); din(ln + "_b", [1, D])
    din("ffn2_w_in", [D, 2 * DFF]); din("ffn2_w_out", [DFF, D])
    G['out'] = c.dram("out", [nown, D], F32, kind="ExternalOutput").ap()
    G['alibiLT'] = [din("alibiLT%d" % i, [128, SEQ], BF16) for i in range(2)]
    G['alibiR'] = [din("alibiR%d" % i, [128, 512], BF16) for i in range(4)]
    din("ohp", [128, 16])
    din("prow1", [1, SEQ]); din("negslope", [1, 16]); din("negcausal", [128, 128]); din("kb0", [128, 1])
    din("w_uk", [1024, 256]); G['w_uv'] = din("w_uv", [16, 256, 64])
    din("cwT", [128, 16, 4]); din("cbT", [128, 16]); din("trib", [128, 128], BF16); din("ones128b", [128, 128], BF16)
    din("identf", [128, 128]); din("negmask4b", [128, 512], BF16); din("sel16b", [128, 2048], BF16)
    din("a_log", [1, 16]); din("d_skip", [1, 16]); din("ssd_norm_w", [1, D])

    c.banks = [c.psum('bank%d' % i, [128, 512], F32) for i in range(8)]
    K = {}
    K['identb'] = c.sb('identb_s', [128, 128], BF16)
    c.dma(K['identb'][:], G['identb'])
    K['v0'] = c.sb('v0_s', [128, 1], F32)
    c.dma(K['v0'][:], G['v0'])
    K['kb0'] = c.sb('kb0_s', [128, 1], F32)
    c.dma(K['kb0'][:], G['kb0'])

    def post1(blk, S, y, mean, rstd):
        if blk == 'setup':
            S['zb'] = [c.sb('zb%d' % i, [128, D], BF16) for i in range(2)]
            S['x1s'] = [c.sb('x1s%d' % i, [128, 8, 128], BF16) for i in range(2)]
            S['z32'] = [c.sb('z32_%d' % i, [128, D], F32) for i in range(2)]
            c.dma(S['gT'][:], G['ln1_gT'])
            c.dma(S['bT'][:], G['ln1_bT'])
            return
        zb = S['zb'][blk % 2]
        x1s = S['x1s'][blk % 2]
        c.ts('dve', zb[:], y[:], mean, rstd, ALU.subtract, ALU.mult)
        pb = c.ps()
        pT = pb.bitcast(BF16)
        for k in range(8):
            c.tr(pT[:, k * 128:(k + 1) * 128], zb[:, k * 128:(k + 1) * 128], K['identb'][:])
        for k in range(8):
            c.act(x1s[:, k, :], pT[:, k * 128:(k + 1) * 128], AF.Identity,
                  bias=S['bT'][:, k:k + 1], scale=S['gT'][:, k:k + 1])
        if blk == 0:
            c.ts('dve', x1s[:], x1s[:], K['v0'][:, 0:1], None, ALU.mult)
        c.dma(G['x1T_d'][blk], x1s[:])
        if blk % 2 == 1:
            z = S['z32'][(blk // 2) % 2]
            c.ts('dve', z[:], y[:], mean, rstd, ALU.subtract, ALU.mult)
            c.tt('pool', z[:], z[:], S['gB'][:], ALU.mult)
            c.tt('pool', z[:], z[:], S['bB'][:], ALU.add)
            c.dma(G['x1_d'][(blk // 2) * 128:(blk // 2 + 1) * 128, :], z[:])

    if stage >= 1:
        ffn_phase(c, K, xs, nblk, G['ffn1_w_in'], G['ffn1_w_out'], G['ln1_g'], G['ln1_b'], post1, 'f1')
    if stage >= 2:
        proj_phase_all(c, K, G, nblk)
        proj_phase_own(c, K, G, nblk)
    if stage >= 3:
        ssd_phase(c, K, G, nblk)
    if stage >= 4:
        dsa_phase(c, K, G, nblk)
    if stage >= 5:
        merge_phase(c, K, G, nblk)
        memkv_phase(c, K, G)
        xattn_phase(c, K, G, nblk)
    if stage >= 6:
        def post4(blk, S, y, mean, rstd):
            if blk == 'setup':
                S['o32'] = [c.sb('o32_%d' % i, [128, D], F32) for i in range(2)]
                return
            o = S['o32'][blk % 2]
            c.ts('dve', o[:], y[:], mean, rstd, ALU.subtract, ALU.mult)
            c.tt('pool', o[:], o[:], S['gB'][:], ALU.mult)
            c.tt('pool', o[:], o[:], S['bB'][:], ALU.add)
            c.dma(G['out'][blk * 128:(blk + 1) * 128, :], o[:])
        ffn_phase(c, K, G['x3_d'], nblk // 2, G['ffn2_w_in'], G['ffn2_w_out'], G['ln4_g'], G['ln4_b'], post4, 'f2')

    c.barrier()
    c.es.close()
    global LAST_INPUTS
    LAST_INPUTS = [n for n in G if n not in ('out',) and not n.endswith('_d')]
    print("build: ninst=%d nwait=%d nsem=%d" % (c.ninst, c.nwait, c.nsem))
    return nc


def make_consts():
    i = np.arange(128)
    tri = (i[:, None] <= i[None, :]).astype(np.float32)
    negm = np.where(i[:, None] <= i[None, :], 0.0, -30000.0).astype(np.float32)
    sel = np.zeros((128, 16, 128), np.float32)
    for h in range(16):
        sel[h, h, :] = 1.0
    bf = ml_dtypes.bfloat16
    slopes = (2.0 ** (-8.0 * np.arange(1, 17, dtype=np.float64) / 16)).astype(np.float32)
    pos = np.arange(SEQ, dtype=np.float32)
    extra = {}
    ltt = [np.zeros((128, SEQ), np.float32) for _ in range(2)]
    rrt = [np.zeros((128, 512), np.float32) for _ in range(4)]
    ohp = np.zeros((128, 16), np.float32)
    for hg in range(4):
        r0 = 64 * (hg % 2)
        lt = ltt[hg // 2][r0:r0 + 28]
        lt[0:16] = 1.0
        rr = rrt[hg][r0:r0 + 28]
        for j in range(4):
            v = (slopes[hg * 4 + j] * pos).astype(np.float32)
            for k in range(3):
                part = v.astype(bf).astype(np.float32)
                lt[16 + 3 * j + k] = part
                v = v - part
                rr[16 + 3 * j + k, j * 128:(j + 1) * 128] = 1.0
            ohp[r0 + hg * 4 + j, hg * 4 + j] = 1.0
    for q_ in range(2):
        extra["alibiLT%d" % q_] = ltt[q_].astype(bf)
    for q_ in range(4):
        extra["alibiR%d" % q_] = rrt[q_].astype(bf)
    extra["ohp"] = ohp
    extra["prow1"] = (pos + 1.0)[None, :]
    extra["negslope"] = (-slopes)[None, :]
    extra["negcausal"] = np.where(i[None, :] <= i[:, None], 0.0, -1e30).astype(np.float32)
    return {**extra, "identb": np.eye(128, dtype=np.float32).astype(ml_dtypes.bfloat16),
            "identf": np.eye(128, dtype=np.float32), "trib": tri.astype(bf),
            "ones128b": np.ones((128, 128), np.float32).astype(bf),
            "negmask4b": np.ascontiguousarray(np.tile(negm, (1, 4))).astype(bf),
            "sel16b": sel.reshape(128, 2048).astype(bf)}


def make_in_maps(inputs):
    x = np.asarray(inputs["x"], dtype=np.float32)
    cs = make_consts()
    sq = lambda a: np.ascontiguousarray(np.asarray(a, dtype=np.float32)[0])
    shared = {
        "ffn1_w_in": sq(inputs["ffn1_w_in"]), "ffn1_w_out": sq(inputs["ffn1_w_out"]),
        "ln1_g": sq(inputs["ln1_g"])[None, :], "ln1_b": sq(inputs["ln1_b"])[None, :],
        "ln1_gT": np.ascontiguousarray(sq(inputs["ln1_g"]).reshape(8, 128).T),
        "ln1_bT": np.ascontiguousarray(sq(inputs["ln1_b"]).reshape(8, 128).T),
        "w_in": sq(inputs["w_in"]), "dt_bias": sq(inputs["dt_bias"])[None, :],
        "kv_norm_w": sq(inputs["kv_norm_w"])[None, :],
        "cwT": np.ascontiguousarray(sq(inputs["conv_w"]).reshape(4, 16, 128).transpose(2, 1, 0)),
        "cbT": np.ascontiguousarray(sq(inputs["conv_b"]).reshape(16, 128).T),
        "a_log": sq(inputs["a_log"])[None, :], "d_skip": sq(inputs["d_skip"])[None, :],
        "ssd_norm_w": sq(inputs["ssd_norm_w"])[None, :],
        "w_uk": sq(inputs["w_uk"]).reshape(1024, 256), "w_uv": sq(inputs["w_uv"]),
        "w_proj_ssd": sq(inputs["w_proj_ssd"]), "w_proj_att": sq(inputs["w_proj_att"]),
        "w_out": sq(inputs["w_out"]), "w_mq": sq(inputs["w_mq"]), "w_mo": sq(inputs["w_mo"]),
        "w_mkv": sq(inputs["w_mkv"]),
        "ln2_g": sq(inputs["ln2_g"])[None, :], "ln2_b": sq(inputs["ln2_b"])[None, :],
        "ln3_g": sq(inputs["ln3_g"])[None, :], "ln3_b": sq(inputs["ln3_b"])[None, :],
        "ln4_g": sq(inputs["ln4_g"])[None, :], "ln4_b": sq(inputs["ln4_b"])[None, :],
        "ffn2_w_in": sq(inputs["ffn2_w_in"]), "ffn2_w_out": sq(inputs["ffn2_w_out"]),
    }
    shared.update(cs)
    maps = []
    for core in range(8):
        b, par = core // 2, core % 2
        if par == 0:
            xs = np.concatenate([np.zeros((128, D), np.float32), x[b, :SEQ - 128]], axis=0)
        else:
            xs = x[b]
        m = dict(shared)
        m["xs"] = np.ascontiguousarray(xs)
        m["v0"] = np.full((128, 1), float(par), np.float32)
        m["kb0"] = np.full((128, 1), 0.0 if par else -1e30, np.float32)
        m["mem"] = np.ascontiguousarray(np.asarray(inputs["mem"], dtype=np.float32)[b])
        maps.append(m)
    return maps


def kernel(**inputs):
    nc = build()
    maps = make_in_maps(inputs)
    res = run_bass_kernel_spmd(nc, maps, core_ids=list(range(8)))
    out = np.zeros((4, SEQ, D), np.float32)
    for core in range(8):
        b, par = core // 2, core % 2
        o = np.asarray(res.results[core]["out"], dtype=np.float32).reshape(NBLK // 2, 128, D)
        out[b].reshape(NBLK, 128, D)[par::2] = o
    return out
```

```python
import contextlib
import numpy as np
import ml_dtypes
import concourse.bass as bass
import concourse.mybir as mybir
from concourse.bass_utils import run_bass_kernel_spmd

F32 = mybir.dt.float32
BF16 = mybir.dt.bfloat16
AF = mybir.ActivationFunctionType
ALU = mybir.AluOpType
AX = mybir.AxisListType
DSZ = {F32: 4, BF16: 2}
WHOLE = (0, 1 << 30, 0, 1 << 40)

D = 1024
SEQ = 4096
NBLK = 32
DFF = 2816
NF = DFF // 128
ALPHA = 2.0 ** 0.25
LN_EPS = 1e-5


class Ctx:
    def __init__(self, nc):
        self.nc = nc
        self.es = contextlib.ExitStack()
        self.eng = {'pe': nc.tensor, 'act': nc.scalar, 'dve': nc.vector,
                    'pool': nc.gpsimd, 'sp': nc.sync}
        self.esem = {k: self.es.enter_context(nc.semaphore('es_' + k))
                     for k in ('pe', 'act', 'dve', 'pool')}
        self.ecnt = {k: 0 for k in self.esem}
        self.seen = {k: {} for k in self.eng}
        self.trk = {}
        self.free_sems = []
        self.scnt = {}
        self.nsem = 0
        self.nwait = 0
        self.ninst = 0
        self.pstack = None
        self.pnames = None
        self.psi = 0

    def _reg(self, name, rowb, dram=False):
        self.trk[name] = dict(rowb=rowb, w=[], r=[], dsem=None, dram=dram)
        if self.pnames is not None and not dram:
            self.pnames.append(name)

    def sb(self, name, shape, dtype):
        self.uid = getattr(self, 'uid', 0) + 1
        name = '%s_u%d' % (name, self.uid)
        st = self.pstack if self.pstack is not None else self.es
        t = st.enter_context(self.nc.sbuf_tensor(name, list(shape), dtype))
        self._reg(name, int(np.prod(shape[1:])) * DSZ[dtype])
        return t

    def psum(self, name, shape, dtype=F32):
        st = self.pstack if self.pstack is not None else self.es
        t = st.enter_context(self.nc.psum_tensor(name, list(shape), dtype))
        self._reg(name, int(np.prod(shape[1:])) * DSZ[dtype])
        self.trk[name]['psum'] = True
        return t

    def dram(self, name, shape, dtype, kind="Internal"):
        t = self.nc.dram_tensor(name, list(shape), dtype, kind=kind)
        self._reg(name, 0, dram=True)
        return t

    def sem_for(self, name):
        t = self.trk[name]
        if t['dsem'] is None:
            if self.free_sems:
                t['dsem'] = self.free_sems.pop()
            else:
                self.nsem += 1
                s = self.es.enter_context(self.nc.semaphore('ds%d' % self.nsem))
                self.scnt[s.name] = 0
                t['dsem'] = s
        return t['dsem']

    @contextlib.contextmanager
    def phase(self):
        assert self.pstack is None
        self.pstack = contextlib.ExitStack()
        self.pnames = []
        try:
            yield
        finally:
            self.barrier()
            for n in self.pnames:
                t = self.trk.pop(n)
                if t['dsem'] is not None:
                    self.free_sems.append(t['dsem'])
            self.pstack.close()
            self.pstack = None
            self.pnames = None

    def barrier(self):
        for e, eng in self.eng.items():
            for k, s in self.esem.items():
                if k != e and self.seen[e].get(s.name, 0) < self.ecnt[k]:
                    eng.wait_ge(s, self.ecnt[k])
                    self.seen[e][s.name] = self.ecnt[k]
            for name, t in self.trk.items():
                s = t['dsem']
                if s is not None and self.seen[e].get(s.name, 0) < self.scnt[s.name]:
                    eng.wait_ge(s, self.scnt[s.name])
                    self.seen[e][s.name] = self.scnt[s.name]
        for t in self.trk.values():
            t['w'] = []
            t['r'] = []

    def region(self, ap):
        t = self.trk[ap.name]
        sz = DSZ[ap.dtype]
        pat = ap.ap
        off = ap.offset
        if t['dram']:
            ext = sum((c - 1) * abs(s) for s, c in pat) + 1
            return (0, 1, off * sz, (off + ext) * sz)
        if t.get('psum'):
            return (0, 128, 0, t['rowb'])
        rowe = t['rowb'] // sz
        p0 = off // rowe
        f0 = off % rowe
        ps, pc = pat[0]
        p1 = p0 + (pc - 1) * (ps // rowe) + 1
        ext = sum((c - 1) * abs(s) for s, c in pat[1:]) + 1
        return (p0, p1, f0 * sz, (f0 + ext) * sz)

    @staticmethod
    def _ov(a, b):
        return a[0] < b[1] and b[0] < a[1] and a[2] < b[3] and b[2] < a[3]

    @staticmethod
    def _cov(a, b):
        return a[0] <= b[0] and a[1] >= b[1] and a[2] <= b[2] and a[3] >= b[3]

    def _cur(self, t, rec):
        if rec[2] == 'dma' and rec[0] is t['dsem']:
            return (rec[0], self.scnt[rec[0].name], 'dma')
        return rec

    def _deps(self, outs, ins, whole_out=False, e=None):
        deps = []
        for ap in ins:
            t = self.trk[ap.name]
            R = self.region(ap)
            for (Q, rec) in t['w']:
                if self._ov(R, Q):
                    deps.append(self._cur(t, rec))
            if t.get('psum'):
                for (Q, rec) in t['r']:
                    if rec[2] != e:
                        deps.append(rec)
        for ap in outs:
            t = self.trk[ap.name]
            R = self.region(ap) if not whole_out else WHOLE
            for (Q, rec) in t['w']:
                if self._ov(R, Q):
                    deps.append(self._cur(t, rec))
            for (Q, rec) in t['r']:
                if self._ov(R, Q):
                    deps.append(self._cur(t, rec))
        return deps

    def _record(self, outs, ins, rec):
        for ap in ins:
            t = self.trk[ap.name]
            R = self.region(ap)
            t['r'] = [(Q, r) for (Q, r) in t['r']
                      if not (r[0] is rec[0] and self._cov(R, Q))]
            t['r'].append((R, rec))
            if len(t['r']) > 40:
                self._collapse(t, 'r')
        for ap in outs:
            t = self.trk[ap.name]
            R = self.region(ap)
            t['w'] = [(Q, r) for (Q, r) in t['w'] if not self._cov(R, Q)]
            t['r'] = [(Q, r) for (Q, r) in t['r'] if not self._cov(R, Q)]
            t['w'].append((R, rec))
            if len(t['w']) > 40:
                self._collapse(t, 'w')

    def _collapse(self, t, k):
        best = {}
        box = None
        for (Q, r) in t[k]:
            key = r[0].name
            if key not in best or best[key][1] < r[1]:
                best[key] = r
            box = Q if box is None else (min(box[0], Q[0]), max(box[1], Q[1]),
                                         min(box[2], Q[2]), max(box[3], Q[3]))
        t[k] = [(box, r) for r in best.values()]

    def _waits(self, e, deps):
        need = {}
        for (sem, val, src) in deps:
            if src == e and e == 'pe':
                continue
            if self.seen[e].get(sem.name, 0) >= val:
                continue
            if sem.name not in need or need[sem.name][1] < val:
                need[sem.name] = (sem, val)
        for (sem, val) in need.values():
            self.seen[e][sem.name] = val
        return list(need.values())

    def _autobb(self):
        tot = self.ninst + self.nwait
        if tot - getattr(self, 'lastbb', 0) > 500:
            self.lastbb = tot
            self.newbb()

    def emit(self, e, fn, outs, ins, attach=True):
        if self.ninst >= LIMIT:
            return None
        self._autobb()
        waits = self._waits(e, self._deps(outs, ins, e=e))
        eng = self.eng[e]
        self.nwait += len(waits)
        last = None
        if attach and waits:
            last = waits.pop()
        for (sem, val) in waits:
            eng.wait_ge(sem, val)
        ins_ = fn(eng)
        if last is not None:
            ins_._wait_ge(last[0], last[1])
        ins_.then_inc(self.esem[e], 1)
        self.ecnt[e] += 1
        self.ninst += 1
        rec = (self.esem[e], self.ecnt[e], e)
        self._record(outs, ins, rec)
        return ins_

    def dma(self, out, in_, q='sp', **kw):
        if self.ninst >= LIMIT:
            return None
        self._autobb()
        deps = self._deps([out], [in_], whole_out=True)
        sbside = in_ if self.trk[out.name]['dram'] else out
        sem = self.sem_for(sbside.name)
        deps = [d for d in deps if not (d[0] is sem and d[2] == 'dma')]
        waits = self._waits(q, deps)
        eng = self.eng[q]
        self.nwait += len(waits)
        for (s, val) in waits:
            eng.wait_ge(s, val)
        self.scnt[sem.name] += 16
        eng.dma_start(out=out, in_=in_, **kw).then_inc(sem, 16)
        self.ninst += 1
        rec = (sem, self.scnt[sem.name], 'dma')
        self._record([out], [in_], rec)

    def newbb(self):
        self.nbb = getattr(self, 'nbb', 0) + 1
        self.nc.switch_bb('kbb%d' % self.nbb)

    def ps(self):
        b = self.banks[self.psi % len(self.banks)]
        self.psi += 1
        return b

    def mm(self, out, lhsT, rhs, start=True, stop=True):
        return self.emit('pe', lambda e: e.matmul(out, lhsT=lhsT, rhs=rhs, start=start, stop=stop),
                         [out], [lhsT, rhs])

    def tr(self, out, in_, ident):
        return self.emit('pe', lambda e: e.transpose(out=out, in_=in_, identity=ident),
                         [out], [in_, ident])

    def act(self, out, in_, func, bias=None, scale=None, accum=None, e='act'):
        kw = {}
        ins = [in_]
        outs = [out]
        if bias is not None:
            kw['bias'] = bias
            if not isinstance(bias, (int, float)):
                ins.append(bias)
        if scale is not None:
            kw['scale'] = scale
            if not isinstance(scale, (int, float)):
                ins.append(scale)
        if accum is not None:
            kw['accum_out'] = accum
            outs.append(accum)
        return self.emit('act', lambda e_: e_.activation(out=out, in_=in_, func=func, **kw),
                         outs, ins, attach=accum is None)

    def ts(self, e, out, in0, s1, s2, op0, op1=None, accum=None):
        ins = [in0] + [s for s in (s1, s2) if s is not None and not isinstance(s, (int, float))]
        outs = [out] + ([accum] if accum is not None else [])
        kw = {}
        if op1 is not None:
            kw['op1'] = op1
        if accum is not None:
            kw['accum_out'] = accum
        return self.emit(e, lambda e_: e_.tensor_scalar(out=out, in0=in0, scalar1=s1, scalar2=s2,
                                                        op0=op0, **kw),
                         outs, ins, attach=accum is None)

    def tt(self, e, out, in0, in1, op):
        return self.emit(e, lambda e_: e_.tensor_tensor(out=out, in0=in0, in1=in1, op=op),
                         [out], [in0, in1])

    def stt(self, out, in0, scalar, in1, op0, op1):
        ins = [in0, in1] + ([scalar] if not isinstance(scalar, (int, float)) else [])
        return self.emit('dve', lambda e_: e_.scalar_tensor_tensor(out=out, in0=in0, scalar=scalar,
                                                                   in1=in1, op0=op0, op1=op1),
                         [out], ins)

    def copy(self, e, out, in_):
        if e == 'act':
            return self.emit('act', lambda e_: e_.copy(out=out, in_=in_), [out], [in_])
        return self.emit(e, lambda e_: e_.tensor_copy(out=out, in_=in_), [out], [in_])


def load_cast(c, dst, src, st, eng=None):
    if eng is None:
        c.rr = getattr(c, 'rr', 0) + 1
        eng = ('pool', 'dve', 'act')[c.rr % 3]
    shp = list(src.shape)
    n = int(np.prod(shp[1:]))
    sv = st[:, 0:n]
    if len(shp) == 3:
        sv = sv.rearrange("p (a b) -> p a b", a=shp[1])
    c.dma(sv, src)
    c.copy(eng, dst, sv)


def layer_norm_stats(c, S, y, eps):
    st6 = S['st6']
    mv = S['mv']
    for h in range(2):
        c.emit('dve', lambda e, h=h: e.bn_stats(out=st6[:, h, :], in_=y[:, h * 512:(h + 1) * 512]),
               [st6[:, h, :]], [y[:, h * 512:(h + 1) * 512]])
    c.emit('dve', lambda e: e.bn_aggr(out=mv[:, 0:2], in_=st6[:]), [mv[:, 0:2]], [st6[:]])
    c.act(mv[:, 2:3], mv[:, 1:2], AF.Sqrt, bias=S['epsc'][:, 0:1])
    c.emit('dve', lambda e: e.reciprocal(out=mv[:, 3:4], in_=mv[:, 2:3]), [mv[:, 3:4]], [mv[:, 2:3]])
    return mv[:, 0:1], mv[:, 3:4]


def ffn_phase(c, K, xsrc, nblk, w_in, w_out, g_d, b_d, post, name):
    TT = 1024
    NSUB = TT // 128
    assert (nblk * 128) % TT == 0
    with c.phase():
        W2 = c.sb('W2', [128, NF, D], BF16)
        xT = c.sb('xT', [128, 8, TT], BF16)
        hT = c.sb('hT', [128, NF, TT], BF16)
        W1 = [c.sb('W1_%d' % i, [128, 8, 512], BF16) for i in range(3)]
        wst = [c.sb('wst%d' % i, [128, 2048], F32) for i in range(3)]
        xin = [c.sb('xin%d' % i, [128, D], F32) for i in range(3)]
        xbf = [c.sb('xbf%d' % i, [128, D], BF16) for i in range(3)]
        sg = [c.sb('sg%d' % i, [128, 512], BF16) for i in range(2)]
        yb = [c.sb('yb%d' % i, [128, D], F32) for i in range(2)]
        S = dict(K)
        S['st6'] = c.sb('st6', [128, 2, 6], F32)
        S['mv'] = c.sb('mv', [128, 4], F32)
        S['epsc'] = c.sb('epsc', [128, 1], F32)
        S['gB'] = c.sb('gB', [128, D], F32)
        S['bB'] = c.sb('bB', [128, D], F32)
        S['gT'] = c.sb('gT', [128, 8], F32)
        S['bT'] = c.sb('bT', [128, 8], F32)
        c.emit('pool', lambda e: e.memset(S['epsc'][:], LN_EPS / (ALPHA * ALPHA)), [S['epsc'][:]], [])
        c.dma(S['gB'][:], g_d[0:1, :].partition_broadcast(128))
        c.dma(S['bB'][:], b_d[0:1, :].partition_broadcast(128))
        post('setup', S, None, None, None)
        wi = 0
        w2v = w_out.rearrange("(f p) n -> p f n", p=128)
        for f in range(0, NF, 2):
            load_cast(c, W2[:, f:f + 2, :], w2v[:, f:f + 2, :], wst[wi % 3])
            wi += 1
        w1v = w_in.rearrange("(k p) n -> p k n", p=128)
        xi = 0
        for t in range(nblk * 128 // TT):
            for s in range(NSUB):
                blk = t * NSUB + s
                xs_ = xin[xi % 3]
                xb_ = xbf[xi % 3]
                xi += 1
                c.dma(xs_[:], xsrc[blk * 128:(blk + 1) * 128, :])
                c.copy('act', xb_[:], xs_[:])
                pb = c.ps()
                pT = pb.bitcast(BF16)
                for k in range(8):
                    c.tr(pT[:, k * 128:(k + 1) * 128], xb_[:, k * 128:(k + 1) * 128], K['identb'][:])
                c.copy('dve', xT[:, :, s * 128:(s + 1) * 128],
                       pT[:, 0:1024].rearrange("p (k t) -> p k t", k=8))
            for fp in range(NF // 2):
                w1 = W1[fp % 3]
                for gu in range(2):
                    col = gu * DFF + fp * 256
                    load_cast(c, w1[:, :, gu * 256:(gu + 1) * 256], w1v[:, :, col:col + 256], wst[wi % 3],
                              eng='pool' if gu == 0 else 'dve')
                    wi += 1
                for fi in range(2):
                    f = fp * 2 + fi
                    for h in range(TT // 512):
                        pg = c.ps()
                        pu = c.ps()
                        for k in range(8):
                            c.mm(pg[:], w1[:, k, fi * 128:(fi + 1) * 128], xT[:, k, h * 512:(h + 1) * 512],
                                 start=(k == 0), stop=(k == 7))
                        for k in range(8):
                            c.mm(pu[:], w1[:, k, 256 + fi * 128:256 + (fi + 1) * 128],
                                 xT[:, k, h * 512:(h + 1) * 512], start=(k == 0), stop=(k == 7))
                        s_ = sg[(f * 2 + h) % 2]
                        c.act(s_[:], pg[:], AF.Silu)
                        c.tt('dve', hT[:, f, h * 512:(h + 1) * 512], s_[:], pu[:], ALU.mult)
            for s in range(NSUB):
                blk = t * NSUB + s
                xs_ = xin[xi % 3]
                y = yb[xi % 2]
                xi += 1
                c.dma(xs_[:], xsrc[blk * 128:(blk + 1) * 128, :])
                for h in range(2):
                    po = c.ps()
                    for f in range(NF):
                        c.mm(po[:], hT[:, f, s * 128:(s + 1) * 128], W2[:, f, h * 512:(h + 1) * 512],
                             start=(f == 0), stop=(f == NF - 1))
                    c.stt(y[:, h * 512:(h + 1) * 512], po[:], 0.5 / ALPHA, xs_[:, h * 512:(h + 1) * 512],
                          ALU.mult, ALU.add)
                mean, rstd = layer_norm_stats(c, S, y, None)
                post(blk, S, y, mean, rstd)


O_GS, O_GA, O_Z, O_XBC, O_DT, O_Q, O_CKV, O_QI, O_KI, O_WI = 0, 1024, 2048, 3072, 5120, 5136, 6160, 6416, 7440, 7504
D_IN = 7520
RMS_EPS = 1e-6


def proj_phase_all(c, K, G, nblk):
    w_in = G['w_in']
    wv = w_in.rearrange("(k p) n -> p k n", p=128)
    with c.phase():
        Wx = c.sb('Wx', [128, 8, 2048], BF16)
        Wk = c.sb('Wk', [128, 8, 128], BF16)
        Wd = c.sb('Wd', [128, 8, 16], BF16)
        Wc = c.sb('Wc', [128, 8, 256], BF16)
        wst = [c.sb('wst%d' % i, [128, 2048], F32) for i in range(3)]
        xg = [c.sb('xg%d' % i, [128, 8, 512], BF16) for i in range(2)]
        xo = [c.sb('xo%d' % i, [128, 16, 512], BF16) for i in range(2)]
        ko = [c.sb('ko%d' % i, [128, 512], BF16) for i in range(2)]
        dto = [c.sb('dto%d' % i, [128, 16], F32) for i in range(2)]
        dte = [c.sb('dte%d' % i, [128, 16], F32) for i in range(2)]
        cko = [c.sb('cko%d' % i, [128, 257], BF16) for i in range(2)]
        ckf = [c.sb('ckf%d' % i, [128, 256], F32) for i in range(2)]
        ckT = [c.sb('ckT%d' % i, [128, 2, 128], BF16) for i in range(2)]
        sq = c.sb('sq', [128, 256], F32)
        ss = c.sb('ss', [128, 4], F32)
        dtb = c.sb('dtb', [128, 16], F32)
        kvg = c.sb('kvg', [128, 256], F32)
        epsr = c.sb('epsr', [128, 1], F32)
        c.emit('pool', lambda e: e.memset(epsr[:], RMS_EPS), [epsr[:]], [])
        c.dma(dtb[:], G['dt_bias'][0:1, :].partition_broadcast(128))
        c.dma(kvg[:], G['kv_norm_w'][0:1, :].partition_broadcast(128))
        wi = 0
        for m in range(0, 16, 2):
            load_cast(c, Wx[:, :, m * 128:(m + 2) * 128], wv[:, :, O_XBC + m * 128:O_XBC + (m + 2) * 128], wst[wi % 3]); wi += 1
        load_cast(c, Wc[:], wv[:, :, O_CKV:O_CKV + 256], wst[wi % 3]); wi += 1
        for hf in range(2):
            load_cast(c, Wk[:, :, hf * 64:(hf + 1) * 64], wv[:, :, O_KI:O_KI + 64], wst[wi % 3]); wi += 1
        load_cast(c, Wd[:], wv[:, :, O_DT:O_DT + 16], wst[wi % 3]); wi += 1
        xbcv = G['xbcT_d'].rearrange("m p t -> p m t")
        for g in range(nblk // 4):
            x_ = xg[g % 2]
            for b in range(4):
                c.dma(x_[:, :, b * 128:(b + 1) * 128], G['x1T_d'][g * 4 + b])
            xo_ = xo[g % 2]
            for m in range(16):
                p = c.ps()
                for k in range(8):
                    c.mm(p[:], Wx[:, k, m * 128:(m + 1) * 128], x_[:, k, :], start=(k == 0), stop=(k == 7))
                c.copy('act' if m % 2 == 0 else 'dve', xo_[:, m, :], p[:])
            for mh in range(2):
                c.dma(xbcv[:, mh * 8:mh * 8 + 8, g * 512:(g + 1) * 512], xo_[:, mh * 8:mh * 8 + 8, :])
            p = c.ps()
            for k in range(8):
                c.mm(p[:], Wk[:, k, :], x_[:, k, :], start=(k == 0), stop=(k == 7))
            ko_ = ko[g % 2]
            c.copy('dve', ko_[:], p[:])
            c.dma(G['kiT_d'][:, g * 512:(g + 1) * 512], ko_[:])
            for b in range(4):
                blk = g * 4 + b
                p = c.ps()
                for k in range(8):
                    c.mm(p[:, 0:16], x_[:, k, b * 128:(b + 1) * 128], Wd[:, k, :], start=(k == 0), stop=(k == 7))
                e_ = dte[blk % 2]
                d_ = dto[blk % 2]
                c.tt('dve', e_[:], p[:, 0:16], dtb[:], ALU.add)
                c.act(e_[:], e_[:], AF.Exp)
                c.act(d_[:], e_[:], AF.Ln, bias=1.0)
                if blk == 0:
                    c.ts('dve', d_[:], d_[:], K['v0'][:, 0:1], None, ALU.mult)
                c.dma(G['dt_d'][blk * 128:(blk + 1) * 128, :], d_[:])
                p = c.ps()
                for k in range(8):
                    c.mm(p[:, 0:256], x_[:, k, b * 128:(b + 1) * 128], Wc[:, k, :], start=(k == 0), stop=(k == 7))
                f_ = ckf[blk % 2]
                c.copy('dve', f_[:], p[:, 0:256])
                c.act(sq[:], f_[:], AF.Square, accum=ss[:, 0:1])
                c.act(ss[:, 1:2], ss[:, 0:1], AF.Sqrt, bias=epsr[:, 0:1], scale=1.0 / 256)
                c.emit('dve', lambda e: e.reciprocal(out=ss[:, 2:3], in_=ss[:, 1:2]), [ss[:, 2:3]], [ss[:, 1:2]])
                o_ = cko[blk % 2]
                c.stt(o_[:, 0:256], f_[:], ss[:, 2:3], kvg[:], ALU.mult, ALU.mult)
                c.emit('pool', lambda e, o_=o_: e.memset(o_[:, 256:257], 1.0), [o_[:, 256:257]], [])
                c.dma(G['ckvtok_d'][blk], o_[:])
                pb = c.ps()
                pT = pb.bitcast(BF16)
                for r in range(2):
                    c.tr(pT[:, r * 128:(r + 1) * 128], o_[:, r * 128:(r + 1) * 128], K['identb'][:])
                t_ = ckT[blk % 2]
                c.copy('act', t_[:], pT[:, 0:256].rearrange("p (r t) -> p r t", r=2))
                c.dma(G['ckvT_d'].rearrange("r p t -> p r t")[:, :, blk * 128:(blk + 1) * 128], t_[:])


def proj_phase_own(c, K, G, nblk):
    wv = G['w_in'].rearrange("(k p) n -> p k n", p=128)
    nown = nblk // 2
    with c.phase():
        Wf = c.sb('Wf', [128, 8, 4096], BF16)
        Wz = c.sb('Wz', [128, 8, 1024], BF16)
        Ww = c.sb('Ww', [128, 8, 16], BF16)
        wst = [c.sb('wst%d' % i, [128, 2048], F32) for i in range(3)]
        xg = [c.sb('xg%d' % i, [128, 8, 512], BF16) for i in range(2)]
        fo = [c.sb('fo%d' % i, [128, 8, 512], BF16) for i in range(2)]
        zo = [c.sb('zo%d' % i, [128, 1024], BF16) for i in range(2)]
        wo = [c.sb('wo%d' % i, [128, 16], F32) for i in range(2)]
        wi = 0
        segs = [(O_GS, 'sgs_d', AF.Sigmoid), (O_GA, 'sga_d', AF.Sigmoid), (O_Q, 'qT_d', AF.Identity),
                (O_QI, 'qiT_d', AF.Identity)]
        for si, (off, _, _) in enumerate(segs):
            for m in range(0, 8, 2):
                load_cast(c, Wf[:, :, si * 1024 + m * 128:si * 1024 + (m + 2) * 128],
                          wv[:, :, off + m * 128:off + (m + 2) * 128], wst[wi % 3]); wi += 1
        for m in range(0, 8, 2):
            load_cast(c, Wz[:, :, m * 128:(m + 2) * 128], wv[:, :, O_Z + m * 128:O_Z + (m + 2) * 128], wst[wi % 3]); wi += 1
        load_cast(c, Ww[:], wv[:, :, O_WI:O_WI + 16], wst[wi % 3]); wi += 1
        fi = 0
        for g in range(nown // 4):
            x_ = xg[g % 2]
            for b in range(4):
                c.dma(x_[:, :, b * 128:(b + 1) * 128], G['x1T_d'][2 * (g * 4 + b) + 1])
            for si, (off, dst, fn) in enumerate(segs):
                f_ = fo[fi % 2]; fi += 1
                for m in range(8):
                    p = c.ps()
                    for k in range(8):
                        c.mm(p[:], Wf[:, k, si * 1024 + m * 128:si * 1024 + (m + 1) * 128], x_[:, k, :],
                             start=(k == 0), stop=(k == 7))
                    if fn == AF.Identity and m % 2 == 1:
                        c.copy('dve', f_[:, m, :], p[:])
                    else:
                        c.act(f_[:, m, :], p[:], fn)
                c.dma(G[dst].rearrange("m p t -> p m t")[:, :, g * 512:(g + 1) * 512], f_[:])
            for b in range(4):
                ob = g * 4 + b
                z_ = zo[ob % 2]
                for h in range(2):
                    p = c.ps()
                    for k in range(8):
                        c.mm(p[:], x_[:, k, b * 128:(b + 1) * 128], Wz[:, k, h * 512:(h + 1) * 512],
                             start=(k == 0), stop=(k == 7))
                    c.act(z_[:, h * 512:(h + 1) * 512], p[:], AF.Silu)
                c.dma(G['zs_d'][ob * 128:(ob + 1) * 128, :], z_[:])
                p = c.ps()
                for k in range(8):
                    c.mm(p[:, 0:16], x_[:, k, b * 128:(b + 1) * 128], Ww[:, k, :], start=(k == 0), stop=(k == 7))
                w_ = wo[ob % 2]
                c.act(w_[:], p[:, 0:16], AF.Copy, scale=0.25)
                c.dma(G['widx_d'][ob * 128:(ob + 1) * 128, :], w_[:])


def ssd_phase(c, K, G, nblk):
    with c.phase():
        cw = c.sb('cw', [128, 16, 4], F32)
        cb = c.sb('cb', [128, 16], F32)
        tri = c.sb('tri', [128, 128], BF16)
        ones = c.sb('ones', [128, 128], BF16)
        negm = c.sb('negm', [128, 512], BF16)
        sel = c.sb('sel', [128, 2048], BF16)
        apad = c.sb('apad', [128, 2, 128], BF16)
        ahl = c.sb('ahl', [128, 2, 16], BF16)
        ares = c.sb('ares', [128, 16], F32)
        Abc = c.sb('Abc', [128, 16], F32)
        Dbc = c.sb('Dbc', [128, 16], F32)
        nwB = c.sb('nwB', [128, D], F32)
        epsr = c.sb('epsr', [128, 1], F32)
        S32 = c.sb('S32', [128, 16, 64], F32)
        Sbf = c.sb('Sbf', [128, 16, 64], BF16)
        for t_, n_ in ((cw, 'cwT'), (cb, 'cbT'), (tri, 'trib'), (ones, 'ones128b'),
                       (negm, 'negmask4b'), (sel, 'sel16b')):
            c.dma(t_[:], G[n_])
        c.dma(Abc[:], G['a_log'][0:1, :].partition_broadcast(128))
        c.dma(Dbc[:], G['d_skip'][0:1, :].partition_broadcast(128))
        c.dma(nwB[:], G['ssd_norm_w'][0:1, :].partition_broadcast(128))
        c.act(Abc[:], Abc[:], AF.Exp)
        c.ts('dve', Abc[:], Abc[:], -1.0, None, ALU.mult)
        c.emit('dve', lambda e: e.memset(epsr[:], RMS_EPS), [epsr[:]], [])
        c.emit('dve', lambda e: e.memset(S32[:], 0.0), [S32[:]], [])
        c.emit('dve', lambda e: e.memset(Sbf[:], 0.0), [Sbf[:]], [])
        c.emit('dve', lambda e: e.memset(apad[:], 0.0), [apad[:]], [])
        xh = [c.sb('xh%d' % i, [128, 16, 132], BF16) for i in range(2)]
        t1 = [c.sb('t1_%d' % i, [128, 16, 128], F32) for i in range(3)]
        xc = [c.sb('xc%d' % i, [128, 16, 128], BF16) for i in range(2)]
        xtok = [c.sb('xtok%d' % i, [128, 16, 64], F32) for i in range(2)]
        Btok = [c.sb('Btok%d' % i, [128, 4, 128], BF16) for i in range(2)]
        xdt = [c.sb('xdt%d' % i, [128, 16, 64], BF16) for i in range(2)]
        xdd = [c.sb('xdd%d' % i, [128, 16, 64], BF16) for i in range(2)]
        dtt = [c.sb('dtt%d' % i, [128, 16], F32) for i in range(2)]
        sm = [c.sb('sm%d' % i, [128, 6, 16], F32) for i in range(2)]
        acsT = [c.sb('acsT%d' % i, [128, 2, 128], BF16) for i in range(2)]
        acsTf = c.sb('acsTf', [128, 128], F32)
        LT = c.sb('LT', [128, 16, 128], F32)
        MT = c.sb('MT', [128, 16, 128], BF16)
        yo = c.sb('yo', [128, 16, 64], F32)
        yy = c.sb('yy', [128, 16, 64], F32)
        zt = [c.sb('zt%d' % i, [128, D], BF16) for i in range(2)]
        sq = c.sb('sqs', [128, 256], F32)
        gs = c.sb('gs', [128, 12], F32)
        ynb = c.sb('ynb', [128, D], BF16)
        ynT = [c.sb('ynT%d' % i, [128, 8, 128], BF16) for i in range(2)]
        for i in range(2):
            c.emit('dve', lambda e, i=i: e.memset(xh[i][:], 0.0), [xh[i][:]], [])
        xbv = G['xbcT_d'].rearrange("m p t -> p m t")
        def front(ch):
            x_ = xh[ch % 2]
            for mh in range(2):
                ms = slice(mh * 8, mh * 8 + 8)
                if ch == 0:
                    c.dma(x_[:, ms, 4:132], xbv[:, ms, 0:128])
                else:
                    c.dma(x_[:, ms, 0:132], xbv[:, ms, ch * 128 - 4:ch * 128 + 128])
            d_ = dtt[ch % 2]
            c.dma(d_[:], G['dt_d'][ch * 128:(ch + 1) * 128, :])
            a_ = t1[0]
            b_ = t1[1]
            c.tt('dve', a_[:], x_[:, :, 1:129], cw[:, :, 0:1].to_broadcast([128, 16, 128]), ALU.mult)
            for k in range(1, 4):
                b_ = t1[1 + (k % 2)]
                c.tt('dve', b_[:], x_[:, :, k + 1:k + 129], cw[:, :, k:k + 1].to_broadcast([128, 16, 128]), ALU.mult)
                c.tt('pool', a_[:], a_[:], b_[:], ALU.add)
            c.tt('dve', a_[:], a_[:], cb[:].unsqueeze(2).to_broadcast([128, 16, 128]), ALU.add)
            xc_ = xc[ch % 2]
            c.act(xc_[:], a_[:], AF.Silu)
            p0 = c.ps(); p0b = p0.bitcast(BF16)
            p1 = c.ps(); p1b = p1.bitcast(BF16)
            for m in range(8):
                c.tr(p0b[:, m * 128:(m + 1) * 128], xc_[:, m, :], K['identb'][:])
            for m in range(4):
                c.tr(p1b[:, m * 128:(m + 1) * 128], xc_[:, 8 + m, :], K['identb'][:])
            xt_ = xtok[ch % 2]
            Bt_ = Btok[ch % 2]
            c.copy('act', xt_[:], p0b[:, 0:1024].rearrange("p (h q) -> p h q", h=16))
            c.copy('act', Bt_[:], p1b[:, 0:512].rearrange("p (g n) -> p g n", g=4))
            xd_ = xdt[ch % 2]
            c.tt('dve', xd_[:], xt_[:], d_[:].unsqueeze(2).to_broadcast([128, 16, 64]), ALU.mult)
            s_ = sm[ch % 2]
            c.tt('dve', s_[:, 0, :], d_[:], Abc[:], ALU.mult)
            c.copy('dve', ahl[:, 0, :], s_[:, 0, :])
            c.tt('dve', ares[:], s_[:, 0, :], ahl[:, 0, :], ALU.subtract)
            c.copy('dve', ahl[:, 1, :], ares[:])
            c.copy('dve', apad[:, :, 0:16], ahl[:])
            pc = c.ps()
            for q_ in range(2):
                c.mm(pc[:, 0:16], tri[:], ahl[:, q_, :], start=(q_ == 0), stop=(q_ == 1))
            pc1 = c.ps()
            for q_ in range(2):
                c.mm(pc1[:, 0:16], ones[:], ahl[:, q_, :], start=(q_ == 0), stop=(q_ == 1))
            pc2 = c.ps()
            for q_ in range(2):
                c.mm(pc2[:, 0:128], apad[:, q_, :], tri[:], start=(q_ == 0), stop=(q_ == 1))
            c.copy('dve', s_[:, 1, :], pc[:, 0:16])
            c.ts('dve', s_[:, 2, :], pc[:, 0:16], -1.0, None, ALU.mult)
            c.act(s_[:, 3, :], pc[:, 0:16], AF.Exp)
            c.tt('dve', s_[:, 4, :], pc1[:, 0:16], s_[:, 1, :], ALU.subtract)
            c.act(s_[:, 4, :], s_[:, 4, :], AF.Exp)
            c.act(s_[:, 5, :], pc1[:, 0:16], AF.Exp)
            aT_ = acsT[ch % 2]
            c.copy('dve', acsTf[:], pc2[:, 0:128])
            c.copy('dve', aT_[:, 0, :], acsTf[:])
            c.tt('dve', acsTf[:], acsTf[:], aT_[:, 0, :], ALU.subtract)
            c.copy('dve', aT_[:, 1, :], acsTf[:])
        def back(ch):
            own = ch % 2 == 1
            xc_ = xc[ch % 2]; xt_ = xtok[ch % 2]; Bt_ = Btok[ch % 2]; xd_ = xdt[ch % 2]
            s_ = sm[ch % 2]; aT_ = acsT[ch % 2]
            if own:
                ob = ch // 2
                for q in range(4):
                    pl = c.ps()
                    c.mm(pl[:], K['identb'][:], negm[:], start=True, stop=False)
                    for j in range(4):
                        h = q * 4 + j
                        for q_ in range(2):
                            c.mm(pl[:, j * 128:(j + 1) * 128], sel[:, h * 128:(h + 1) * 128], aT_[:, q_, :],
                                 start=False, stop=(j == 3 and q_ == 1))
                    for j in range(4):
                        h = q * 4 + j
                        c.act(LT[:, h, :], pl[:, j * 128:(j + 1) * 128], AF.Exp, bias=s_[:, 2, h:h + 1])
                pcb = c.ps()
                for g in range(4):
                    c.mm(pcb[:, g * 128:(g + 1) * 128], xc_[:, 8 + g, :], xc_[:, 12 + g, :])
                c.tt('dve', MT[:].rearrange("p (g j) l -> p g j l", g=4),
                     LT[:].rearrange("p (g j) l -> p g j l", g=4),
                     pcb[:].rearrange("p (g l) -> p g l", g=4).unsqueeze(2).to_broadcast([128, 4, 4, 128]),
                     ALU.mult)
                py = [c.ps(), c.ps()]
                for h in range(16):
                    c.mm(py[h // 8][:, (h % 8) * 64:(h % 8 + 1) * 64], MT[:, h, :], xd_[:, h, :])
                po = [c.ps(), c.ps()]
                for g in range(4):
                    c.mm(po[g // 2][:, (g % 2) * 256:(g % 2 + 1) * 256], xc_[:, 12 + g, :],
                         Sbf[:, 4 * g:4 * g + 4, :].rearrange("p h q -> p (h q)"))
                for hf in range(2):
                    hs = slice(hf * 8, hf * 8 + 8)
                    c.tt('dve', yo[:, hs, :], po[hf][:].rearrange("p (h q) -> p h q", h=8),
                         s_[:, 3, hs].unsqueeze(2).to_broadcast([128, 8, 64]), ALU.mult)
                    c.tt('dve', yy[:, hs, :], py[hf][:].rearrange("p (h q) -> p h q", h=8), yo[:, hs, :], ALU.add)
                c.tt('dve', yo[:], xt_[:], Dbc[:].unsqueeze(2).to_broadcast([128, 16, 64]), ALU.mult)
                c.tt('dve', yy[:], yy[:], yo[:], ALU.add)
                z_ = zt[ob % 2]
                c.dma(z_[:], G['zs_d'][ob * 128:(ob + 1) * 128, :])
                yf = yy[:].rearrange("p h q -> p (h q)")
                c.tt('dve', yf, yf, z_[:], ALU.mult)
                for g in range(4):
                    c.act(sq[:], yf[:, g * 256:(g + 1) * 256], AF.Square, accum=gs[:, g:g + 1])
                c.act(gs[:, 4:8], gs[:, 0:4], AF.Sqrt, bias=epsr[:, 0:1], scale=1.0 / 256)
                c.emit('dve', lambda e: e.reciprocal(out=gs[:, 8:12], in_=gs[:, 4:8]), [gs[:, 8:12]], [gs[:, 4:8]])
                c.tt('dve', yy[:].rearrange("p (g j) q -> p g (j q)", g=4),
                     yy[:].rearrange("p (g j) q -> p g (j q)", g=4),
                     gs[:, 8:12].unsqueeze(2).to_broadcast([128, 4, 256]), ALU.mult)
                c.tt('dve', ynb[:], yf, nwB[:], ALU.mult)
                pb = c.ps(); pT = pb.bitcast(BF16)
                for m in range(8):
                    c.tr(pT[:, m * 128:(m + 1) * 128], ynb[:, m * 128:(m + 1) * 128], K['identb'][:])
                yT_ = ynT[ob % 2]
                c.copy('act', yT_[:], pT[:, 0:1024].rearrange("p (m t) -> p m t", m=8))
                c.dma(G['yssdT_d'][ob], yT_[:])
            xe_ = xdd[ch % 2]
            c.tt('dve', xe_[:], xd_[:], s_[:, 4, :].unsqueeze(2).to_broadcast([128, 16, 64]), ALU.mult)
            pst = [c.ps(), c.ps()]
            for g in range(4):
                c.mm(pst[g // 2][:, (g % 2) * 256:(g % 2 + 1) * 256], Bt_[:, g, :],
                     xe_[:, 4 * g:4 * g + 4, :].rearrange("p h q -> p (h q)"))
            c.tt('dve', S32[:], S32[:], s_[:, 5, :].unsqueeze(2).to_broadcast([128, 16, 64]), ALU.mult)
            for hf in range(2):
                hs = slice(hf * 8, hf * 8 + 8)
                c.tt('dve', S32[:, hs, :], S32[:, hs, :], pst[hf][:].rearrange("p (h q) -> p h q", h=8), ALU.add)
            c.copy('act', Sbf[:], S32[:])

        front(0)
        for ch in range(nblk):
            if ch + 1 < nblk:
                front(ch + 1)
            back(ch)


LAST_INPUTS = []
LIMIT = 10 ** 9
NIT_BISECT = 16
TOPK = 256


def dsa_phase(c, K, G, nblk):
    ntok = nblk * 128
    nown = nblk // 2
    with c.phase():
        kiT = c.sb('kiT', [128, ntok], BF16)
        ckvT = c.sb('ckvT', [128, 2, ntok], BF16)
        ckvtok = c.sb('ckvtok', [128, nblk, 257], BF16)
        Wuk = c.sb('Wuk', [128, 16, 256], BF16)
        qiz = c.sb('qiz', [128, 16, 128], BF16)
        Wuv = c.sb('Wuv', [128, 16, 2, 128], BF16)
        wst = [c.sb('wst%d' % i, [128, 2048], F32) for i in range(1)]
        LTt = [c.sb('LTt%d' % i, [128, ntok], BF16) for i in range(2)]
        Rt = [c.sb('Rt%d' % i, [128, 512], BF16) for i in range(4)]
        ohp = c.sb('ohp', [128, 16], F32)
        nbpad = c.sb('nbpad', [128, 128], BF16)
        prow1 = c.sb('prow1', [128, ntok], F32)
        negsl = c.sb('negsl', [128, 16], F32)
        negc = c.sb('negc', [128, 128], F32)
        identf = c.sb('identf2', [128, 128], F32)
        sc = c.sb('sc', [128, ntok], F32)
        tmpn = c.sb('tmpn', [128, ntok], F32)
        m01 = c.sb('m01', [128, ntok], BF16)
        mbT = c.sb('mbT', [128, nblk, 128], BF16)
        qT = [c.sb('qT%d' % i, [128, 8, 128], BF16) for i in range(2)]
        qiT = [c.sb('qiT%d' % i, [128, 8, 128], BF16) for i in range(2)]
        wid = [c.sb('wid%d' % i, [128, 16], F32) for i in range(2)]
        qlat = c.sb('qlat', [128, 2, 16, 128], BF16)
        rl = [c.sb('rl%d' % i, [128, 512], F32) for i in range(3)]
        PT = [c.sb('PT%d' % i, [128, 512], BF16) for i in range(3)]
        osb = c.sb('osb', [128, 4, 256], BF16)
        oT = c.sb('oT', [128, 16, 2, 128], BF16)
        yat = [c.sb('yat%d' % i, [128, 8, 128], BF16) for i in range(2)]
        bs = c.sb('bs', [128, 12], F32)
        nb = c.sb('nb', [128, 16], F32)
        c.dma(kiT[:], G['kiT_d'])
        c.dma(ckvT[:], G['ckvT_d'].rearrange("r p t -> p r t"))
        for b0 in range(0, nblk, 8):
            c.dma(ckvtok[:, b0:b0 + 8, :], G['ckvtok_d'].rearrange("b p f -> p b f")[:, b0:b0 + 8, :])
        for i in range(2):
            c.dma(LTt[i][:], G['alibiLT'][i][:, 0:ntok])
        for i in range(4):
            c.dma(Rt[i][:], G['alibiR'][i])
        c.emit('dve', lambda e: e.memset(qiz[:], 0.0), [qiz[:]], [])
        c.dma(ohp[:], G['ohp'])
        c.emit('dve', lambda e: e.memset(nbpad[:], 0.0), [nbpad[:]], [])
        c.dma(prow1[:], G['prow1'][0:1, 0:ntok].partition_broadcast(128))
        c.dma(negsl[:], G['negslope'][0:1, :].partition_broadcast(128))
        c.dma(negc[:], G['negcausal'])
        c.dma(identf[:], G['identf'])
        c.emit('dve', lambda e: e.memset(Wuk[:], 0.0), [Wuk[:]], [])
        svk = wst[0][:, 0:2048].rearrange("p (a b) -> p a b", a=8)
        c.dma(svk, G['w_uk'].rearrange("(c p) r -> p c r", p=128))
        Wukv = Wuk[:].rearrange("p (c e) r -> p c e r", e=2)
        c.copy('dve', Wukv[0:64, :, 0, :], svk[0:64, :, :])
        c.copy('dve', Wukv[64:128, :, 1, :], svk[64:128, :, :])
        c.emit('dve', lambda e: e.memset(Wuv[:], 0.0), [Wuv[:]], [])
        for h in range(16):
            st = wst[0]
            sv = st[:, 0:128].rearrange("p (a b) -> p a b", a=2)
            c.dma(sv, G['w_uv'][h].rearrange("(rc p) d -> p rc d", p=128))
            c.copy('dve', Wuv[:, h, :, (h % 2) * 64:(h % 2 + 1) * 64], sv)
        pool_banks = c.banks[0:4]
        acc_banks = c.banks[4:8]
        allbanks = c.banks
        lo, hi, mid, cnt, ge, dd, n1 = [bs[:, i:i + 1] for i in range(7)]

        def A1(ob):
            jb = 2 * ob + 1
            NK = (jb + 1) * 128
            q_ = qT[ob % 2]; qi_ = qiT[ob % 2]; w_ = wid[ob % 2]
            c.dma(q_[:], G['qT_d'].rearrange("m p t -> p m t")[:, :, ob * 128:(ob + 1) * 128])
            c.dma(qi_[:], G['qiT_d'].rearrange("m p t -> p m t")[:, :, ob * 128:(ob + 1) * 128])
            c.dma(w_[:], G['widx_d'][ob * 128:(ob + 1) * 128, :])
            qizv = qiz[:].rearrange("p (c e) t -> p c e t", e=2)
            c.copy('dve', qizv[0:64, :, 0, :], qi_[0:64, :, :])
            c.copy('dve', qizv[64:128, :, 1, :], qi_[64:128, :, :])
            ri = 0
            for s0 in range(0, NK, 512):
                w = min(512, NK - s0)
                for h in range(16):
                    p = c.ps()
                    c.mm(p[:, 0:w], qiz[:, h, :], kiT[:, s0:s0 + w])
                    r_ = rl[ri % 3]; ri += 1
                    c.act(r_[:, 0:w], p[:, 0:w], AF.Relu)
                    if h == 0:
                        c.ts('dve', sc[:, s0:s0 + w], r_[:, 0:w], w_[:, 0:1], None, ALU.mult)
                    else:
                        c.stt(sc[:, s0:s0 + w], r_[:, 0:w], w_[:, h:h + 1], sc[:, s0:s0 + w], ALU.mult, ALU.add)
                    if h % 4 == 3:
                        yield
            if ob > 0:
                c.emit('dve', lambda e: e.tensor_reduce(out=lo, in_=sc[:, 128:jb * 128], axis=AX.X, op=ALU.min),
                       [lo], [sc[:, 128:jb * 128]])
            c.tt('dve', sc[:, jb * 128:NK], sc[:, jb * 128:NK], negc[:], ALU.add)
            c.ts('dve', sc[:, 0:128], sc[:, 0:128], K['kb0'][:, 0:1], None, ALU.add)
            if ob > 0:
                c.emit('dve', lambda e: e.tensor_reduce(out=hi, in_=sc[:, 0:NK], axis=AX.X, op=ALU.max),
                       [hi], [sc[:, 0:NK]])
                c.ts('dve', hi, hi, 1.0, None, ALU.add)
                yield
                for it in range(NIT_BISECT):
                    c.ts('dve', mid, lo, hi, 0.5, ALU.add, ALU.mult)
                    c.ts('dve', tmpn[:, 0:NK], sc[:, 0:NK], mid, 0.0, ALU.is_ge, ALU.add, accum=cnt)
                    c.ts('dve', ge, cnt, TOPK - 0.5, None, ALU.is_ge)
                    c.tt('dve', dd, mid, lo, ALU.subtract)
                    c.stt(lo, dd, ge, lo, ALU.mult, ALU.add)
                    c.tt('dve', dd, hi, mid, ALU.subtract)
                    c.stt(hi, dd, ge, mid, ALU.mult, ALU.add)
                    if it % 2 == 1:
                        yield
                c.ts('dve', m01[:, 0:NK], sc[:, 0:NK], lo, None, ALU.is_ge)
            else:
                c.ts('dve', m01[:, 0:NK], sc[:, 0:NK], -1e29, None, ALU.is_ge)
            yield
            c.tt('dve', tmpn[:, 0:NK], m01[:, 0:NK], prow1[:, 0:NK], ALU.mult)
            c.emit('dve', lambda e: e.tensor_reduce(out=n1, in_=tmpn[:, 0:NK], axis=AX.X, op=ALU.max),
                   [n1], [tmpn[:, 0:NK]])
            c.ts('dve', n1, n1, -1.0, None, ALU.add)
            c.ts('dve', nbpad[:].rearrange("p (g q) -> p g q", g=2)[:, :, 0:16],
                 negsl[:].unsqueeze(1).to_broadcast([128, 2, 16]), n1, None, ALU.mult)

        def A2(ob):
            jb = 2 * ob + 1
            NKT = jb + 1
            q_ = qT[ob % 2]
            for rc in range(2):
                for hg in range(4):
                    p = c.ps()
                    for j in range(4):
                        h = hg * 4 + j
                        c.mm(p[:, j * 128:(j + 1) * 128], Wuk[:, h, rc * 128:(rc + 1) * 128], q_[:, h // 2, :])
                    c.act(qlat[:, rc, hg * 4:hg * 4 + 4, :], p[:].rearrange("p (j t) -> p j t", j=4), AF.Copy, scale=0.125)
            pnb = c.ps()
            pn = pnb.bitcast(BF16)
            c.tr(pn[:, 0:128], nbpad[:], K['identb'][:])
            for h in range(16):
                hg = h // 4
                pr = slice(64 * (hg % 2), 64 * (hg % 2) + 16)
                c.ts('dve', Rt[hg][pr, (h % 4) * 128:(h % 4 + 1) * 128], pn[pr, 0:128],
                     ohp[pr, h:h + 1], None, ALU.mult)
            for k0 in range(0, NKT, 4):
                nk = min(4, NKT - k0)
                pb = c.ps(); pT = pb.bitcast(BF16)
                for i in range(nk):
                    c.tr(pT[:, i * 128:(i + 1) * 128], m01[:, (k0 + i) * 128:(k0 + i + 1) * 128], K['identb'][:])
                c.ts('dve', mbT[:, k0:k0 + nk, :], pT[:, 0:nk * 128].rearrange("p (k t) -> p k t", k=nk),
                     -1.0, 30000.0, ALU.add, ALU.mult)

        def step(gen):
            if gen is not None:
                next(gen, None)

        def Att(ob, gen):
            jb = 2 * ob + 1
            NKT = jb + 1
            c.banks = pool_banks
            pi = 0
            def qk(hg, kt):
                ks = slice(kt * 128, (kt + 1) * 128)
                pS = c.ps()
                c.mm(pS[:], ckvT[:, 0, ks], qlat[:, 0, hg * 4:hg * 4 + 4, :].rearrange("p j t -> p (j t)"),
                     start=True, stop=False)
                c.mm(pS[:], ckvT[:, 1, ks], qlat[:, 1, hg * 4:hg * 4 + 4, :].rearrange("p j t -> p (j t)"),
                     start=False, stop=False)
                c.mm(pS[:], LTt[hg // 2][:, ks], Rt[hg][:], start=False, stop=False)
                c.mm(pS[:].rearrange("p (j t) -> p j t", j=4), K['identb'][:],
                     mbT[:, kt, :].unsqueeze(1).to_broadcast([128, 4, 128]), start=False, stop=True)
                P_ = PT[pstate[0] % 3]; pstate[0] += 1
                c.act(P_[:], pS[:], AF.Exp)
                return P_

            pstate = [0]
            for hg in range(4):
                cur = qk(hg, 0)
                for kt in range(NKT):
                    nxt = qk(hg, kt + 1) if kt + 1 < NKT else None
                    for j in range(4):
                        c.mm(acc_banks[j][:, 0:257], cur[:, j * 128:(j + 1) * 128], ckvtok[:, kt, :],
                             start=(kt == 0), stop=(kt == NKT - 1))
                    step(gen)
                    cur = nxt
                for j in range(4):
                    rs = bs[:, 7 + j:8 + j]
                    c.emit('dve', lambda e, j=j, rs=rs: e.reciprocal(out=rs, in_=acc_banks[j][:, 256:257]),
                           [rs], [acc_banks[j][:, 256:257]])
                    c.act(osb[:, j, :], acc_banks[j][:, 0:256], AF.Copy, scale=rs)
                for rc in range(2):
                    pb = c.ps(); pT = pb.bitcast(BF16)
                    for j in range(4):
                        c.tr(pT[:, j * 128:(j + 1) * 128], osb[:, j, rc * 128:(rc + 1) * 128], K['identb'][:])
                    c.copy('dve', oT[:, hg * 4:hg * 4 + 4, rc, :], pT[:, 0:512].rearrange("p (j t) -> p j t", j=4))
            y_ = yat[ob % 2]
            for cp in range(0, 8, 4):
                p = c.ps()
                for ci in range(4):
                    cidx = cp + ci
                    n_ = 0
                    for h in (2 * cidx, 2 * cidx + 1):
                        for rc in range(2):
                            c.mm(p[:, ci * 128:(ci + 1) * 128], Wuv[:, h, rc, :], oT[:, h, rc, :],
                                 start=(n_ == 0), stop=(n_ == 3))
                            n_ += 1
                c.copy('act', y_[:, cp:cp + 4, :], p[:].rearrange("p (c t) -> p c t", c=4))
            c.dma(G['yattT_d'][ob], y_[:])
            if gen is not None:
                for _ in gen:
                    pass
            c.banks = allbanks

        c.banks = allbanks
        for _ in A1(0):
            pass
        A2(0)
        for ob in range(nown):
            gen = A1(ob + 1) if ob + 1 < nown else None
            Att(ob, gen)
            if ob + 1 < nown:
                A2(ob + 1)
        c.banks = allbanks


def load_w_resident(c, dst, src, wst, wi, ncols):
    sv = src.rearrange("(k p) n -> p k n", p=128)
    for m in range(0, ncols, 256):
        load_cast(c, dst[:, :, m:m + 256], sv[:, :, m:m + 256], wst[wi[0] % len(wst)])
        wi[0] += 1


def ln_block(c, S, y, eps_c, g_bc, b_bc, out32):
    st6, mv = S['st6'], S['mv']
    for h in range(2):
        c.emit('dve', lambda e, h=h: e.bn_stats(out=st6[:, h, :], in_=y[:, h * 512:(h + 1) * 512]),
               [st6[:, h, :]], [y[:, h * 512:(h + 1) * 512]])
    c.emit('dve', lambda e: e.bn_aggr(out=mv[:, 0:2], in_=st6[:]), [mv[:, 0:2]], [st6[:]])
    c.act(mv[:, 2:3], mv[:, 1:2], AF.Sqrt, bias=eps_c[:, 0:1])
    c.emit('dve', lambda e: e.reciprocal(out=mv[:, 3:4], in_=mv[:, 2:3]), [mv[:, 3:4]], [mv[:, 2:3]])
    c.ts('dve', out32, y, mv[:, 0:1], mv[:, 3:4], ALU.subtract, ALU.mult)
    c.tt('pool', out32, out32, g_bc, ALU.mult)
    c.tt('pool', out32, out32, b_bc, ALU.add)


def merge_phase(c, K, G, nblk):
    nown = nblk // 2
    with c.phase():
        Wps = c.sb('Wps', [128, 8, D], BF16)
        Wpa = c.sb('Wpa', [128, 8, D], BF16)
        Wo = c.sb('Wo', [128, 8, D], BF16)
        wst = [c.sb('wst%d' % i, [128, 2048], F32) for i in range(3)]
        wi = [0]
        load_w_resident(c, Wps, G['w_proj_ssd'], wst, wi, D)
        load_w_resident(c, Wpa, G['w_proj_att'], wst, wi, D)
        load_w_resident(c, Wo, G['w_out'], wst, wi, D)
        ys = [c.sb('ys%d' % i, [128, 8, 512], BF16) for i in range(2)]
        ya = [c.sb('ya%d' % i, [128, 8, 512], BF16) for i in range(2)]
        gs_ = [c.sb('gs%d' % i, [128, 8, 512], BF16) for i in range(2)]
        ga_ = [c.sb('ga%d' % i, [128, 8, 512], BF16) for i in range(2)]
        m1 = [c.sb('m1_%d' % i, [128, 512], F32) for i in range(2)]
        m2 = [c.sb('m2_%d' % i, [128, 512], F32) for i in range(2)]
        mT = c.sb('mT', [128, 8, 512], BF16)
        x1t = [c.sb('x1t%d' % i, [128, D], F32) for i in range(2)]
        yb = [c.sb('yb%d' % i, [128, D], F32) for i in range(2)]
        x2 = [c.sb('x2_%d' % i, [128, D], F32) for i in range(2)]
        x2b = [c.sb('x2b%d' % i, [128, D], BF16) for i in range(2)]
        x2T = [c.sb('x2T%d' % i, [128, 8, 128], BF16) for i in range(2)]
        S = dict(st6=c.sb('st6', [128, 2, 6], F32), mv=c.sb('mv', [128, 4], F32))
        epsc = c.sb('epsc', [128, 1], F32)
        gB = c.sb('gB', [128, D], F32); bB = c.sb('bB', [128, D], F32)
        c.emit('pool', lambda e: e.memset(epsc[:], LN_EPS / (ALPHA * ALPHA)), [epsc[:]], [])
        c.dma(gB[:], G['ln2_g'][0:1, :].partition_broadcast(128))
        c.dma(bB[:], G['ln2_b'][0:1, :].partition_broadcast(128))
        for g in range(nown // 4):
            i2 = g % 2
            for b in range(4):
                ob = g * 4 + b
                c.dma(ys[i2][:, :, b * 128:(b + 1) * 128], G['yssdT_d'][ob])
                c.dma(ya[i2][:, :, b * 128:(b + 1) * 128], G['yattT_d'][ob])
            c.dma(gs_[i2][:], G['sgs_d'].rearrange("m p t -> p m t")[:, :, g * 512:(g + 1) * 512])
            c.dma(ga_[i2][:], G['sga_d'].rearrange("m p t -> p m t")[:, :, g * 512:(g + 1) * 512])
            for m in range(8):
                p1 = c.ps(); p2 = c.ps()
                for k in range(8):
                    c.mm(p1[:], Wps[:, k, m * 128:(m + 1) * 128], ys[i2][:, k, :], start=(k == 0), stop=(k == 7))
                for k in range(8):
                    c.mm(p2[:], Wpa[:, k, m * 128:(m + 1) * 128], ya[i2][:, k, :], start=(k == 0), stop=(k == 7))
                c.tt('dve', m1[m % 2][:], p1[:], gs_[i2][:, m, :], ALU.mult)
                c.tt('dve', m2[m % 2][:], p2[:], ga_[i2][:, m, :], ALU.mult)
                c.tt('pool', mT[:, m, :], m1[m % 2][:], m2[m % 2][:], ALU.add)
            for b in range(4):
                ob = g * 4 + b
                i = ob % 2
                c.dma(x1t[i][:], G['x1_d'][ob * 128:(ob + 1) * 128, :])
                for h in range(2):
                    po = c.ps()
                    for k in range(8):
                        c.mm(po[:], mT[:, k, b * 128:(b + 1) * 128], Wo[:, k, h * 512:(h + 1) * 512],
                             start=(k == 0), stop=(k == 7))
                    c.stt(yb[i][:, h * 512:(h + 1) * 512], po[:], 1.0 / ALPHA, x1t[i][:, h * 512:(h + 1) * 512],
                          ALU.mult, ALU.add)
                ln_block(c, S, yb[i][:], epsc, gB[:], bB[:], x2[i][:])
                c.dma(G['x2_d'][ob * 128:(ob + 1) * 128, :], x2[i][:])
                c.copy('act', x2b[i][:], x2[i][:])
                pb = c.ps(); pT = pb.bitcast(BF16)
                for k in range(8):
                    c.tr(pT[:, k * 128:(k + 1) * 128], x2b[i][:, k * 128:(k + 1) * 128], K['identb'][:])
                c.copy('dve', x2T[i][:], pT[:, 0:1024].rearrange("p (k t) -> p k t", k=8))
                c.dma(G['x2T_d'][ob], x2T[i][:])


def memkv_phase(c, K, G):
    with c.phase():
        Wkv = c.sb('Wkv', [128, 8, 2048], BF16)
        wst = [c.sb('wst%d' % i, [128, 2048], F32) for i in range(3)]
        wi = [0]
        load_w_resident(c, Wkv, G['w_mkv'], wst, wi, 2048)
        mem32 = [c.sb('mem32_%d' % i, [128, D], F32) for i in range(2)]
        memb = [c.sb('memb%d' % i, [128, D], BF16) for i in range(2)]
        memT = c.sb('memT', [128, 8, 256], BF16)
        kmT = c.sb('kmT', [128, 8, 256], BF16)
        vm = c.sb('vm', [128, 2, D], BF16)
        for mt in range(2):
            c.dma(mem32[mt][:], G['mem'][mt * 128:(mt + 1) * 128, :])
            c.copy('act', memb[mt][:], mem32[mt][:])
            pb = c.ps(); pT = pb.bitcast(BF16)
            for k in range(8):
                c.tr(pT[:, k * 128:(k + 1) * 128], memb[mt][:, k * 128:(k + 1) * 128], K['identb'][:])
            c.copy('dve', memT[:, :, mt * 128:(mt + 1) * 128], pT[:, 0:1024].rearrange("p (k t) -> p k t", k=8))
        for m in range(8):
            p = c.ps()
            for k in range(8):
                c.mm(p[:, 0:256], Wkv[:, k, m * 128:(m + 1) * 128], memT[:, k, :], start=(k == 0), stop=(k == 7))
            c.copy('act', kmT[:, m, :], p[:, 0:256])
        for mt in range(2):
            for h in range(2):
                p = c.ps()
                for k in range(8):
                    c.mm(p[:], memT[:, k, mt * 128:(mt + 1) * 128], Wkv[:, k, 1024 + h * 512:1024 + (h + 1) * 512],
                         start=(k == 0), stop=(k == 7))
                c.copy('dve', vm[:, mt, h * 512:(h + 1) * 512], p[:])
        c.dma(G['kmT_d'], kmT[:])
        c.dma(G['vm_d'], vm[:])


def xattn_phase(c, K, G, nblk):
    nown = nblk // 2
    with c.phase():
        Wq = c.sb('Wq', [128, 8, D], BF16)
        Wmo = c.sb('Wmo', [128, 8, D], BF16)
        wst = [c.sb('wst%d' % i, [128, 2048], F32) for i in range(3)]
        wi = [0]
        load_w_resident(c, Wq, G['w_mq'], wst, wi, D)
        load_w_resident(c, Wmo, G['w_mo'], wst, wi, D)
        kmT = c.sb('kmT', [128, 8, 256], BF16)
        vm = c.sb('vm', [128, 2, D], BF16)
        c.dma(kmT[:], G['kmT_d'])
        c.dma(vm[:], G['vm_d'])
        xT = [c.sb('xT%d' % i, [128, 8, 512], BF16) for i in range(2)]
        qmT = c.sb('qmT', [128, 8, 512], BF16)
        x2t = [c.sb('x2t%d' % i, [128, D], F32) for i in range(2)]
        Pm = [c.sb('Pm%d' % i, [128, 4, 256], F32) for i in range(2)]
        Pn = [c.sb('Pn%d' % i, [128, 4, 256], BF16) for i in range(2)]
        PmT = [c.sb('PmT%d' % i, [128, 2, 4, 128], BF16) for i in range(2)]
        omT = [c.sb('omT%d' % i, [128, 8, 128], BF16) for i in range(2)]
        yb = [c.sb('yb%d' % i, [128, D], F32) for i in range(2)]
        x3 = [c.sb('x3_%d' % i, [128, D], F32) for i in range(2)]
        sm = c.sb('smx', [128, 16], F32)
        S = dict(st6=c.sb('st6', [128, 2, 6], F32), mv=c.sb('mv', [128, 4], F32))
        epsc = c.sb('epsc', [128, 1], F32)
        gB = c.sb('gB', [128, D], F32); bB = c.sb('bB', [128, D], F32)
        c.emit('pool', lambda e: e.memset(epsc[:], LN_EPS / (ALPHA * ALPHA)), [epsc[:]], [])
        c.dma(gB[:], G['ln3_g'][0:1, :].partition_broadcast(128))
        c.dma(bB[:], G['ln3_b'][0:1, :].partition_broadcast(128))
        for g in range(nown // 4):
            x_ = xT[g % 2]
            for b in range(4):
                c.dma(x_[:, :, b * 128:(b + 1) * 128], G['x2T_d'][g * 4 + b])
            for m in range(8):
                p = c.ps()
                for k in range(8):
                    c.mm(p[:], Wq[:, k, m * 128:(m + 1) * 128], x_[:, k, :], start=(k == 0), stop=(k == 7))
                c.act(qmT[:, m, :], p[:], AF.Copy, scale=1.0 / 16)
            for b in range(4):
                ob = g * 4 + b
                i = ob % 2
                c.dma(x2t[i][:], G['x2_d'][ob * 128:(ob + 1) * 128, :])
                pS = [c.ps(), c.ps()]
                for h in range(4):
                    for dc in range(2):
                        c.mm(pS[h // 2][:, (h % 2) * 256:(h % 2 + 1) * 256], qmT[:, 2 * h + dc, b * 128:(b + 1) * 128],
                             kmT[:, 2 * h + dc, :], start=(dc == 0), stop=(dc == 1))
                for hf in range(2):
                    c.emit('dve', lambda e, hf=hf: e.tensor_reduce(
                        out=sm[:, hf * 2:hf * 2 + 2], in_=pS[hf][:].rearrange("p (h m) -> p h m", h=2),
                        axis=AX.X, op=ALU.max), [sm[:, hf * 2:hf * 2 + 2]], [pS[hf][:]])
                c.ts('dve', sm[:, 4:8], sm[:, 0:4], -1.0, None, ALU.mult)
                for h in range(4):
                    c.act(Pm[i][:, h, :], pS[h // 2][:, (h % 2) * 256:(h % 2 + 1) * 256], AF.Exp,
                          bias=sm[:, 4 + h:5 + h], accum=sm[:, 8 + h:9 + h])
                c.emit('dve', lambda e: e.reciprocal(out=sm[:, 12:16], in_=sm[:, 8:12]), [sm[:, 12:16]], [sm[:, 8:12]])
                c.tt('dve', Pn[i][:], Pm[i][:], sm[:, 12:16].unsqueeze(2).to_broadcast([128, 4, 256]), ALU.mult)
                for mt in range(2):
                    pb = c.ps(); pT = pb.bitcast(BF16)
                    for h in range(4):
                        c.tr(pT[:, h * 128:(h + 1) * 128], Pn[i][:, h, mt * 128:(mt + 1) * 128], K['identb'][:])
                    c.copy('act', PmT[i][:, mt, :, :], pT[:, 0:512].rearrange("p (h t) -> p h t", h=4))
                for cp in range(0, 8, 4):
                    p = c.ps()
                    for ci in range(4):
                        cc = cp + ci
                        for mt in range(2):
                            c.mm(p[:, ci * 128:(ci + 1) * 128], vm[:, mt, cc * 128:(cc + 1) * 128],
                                 PmT[i][:, mt, cc // 2, :], start=(mt == 0), stop=(mt == 1))
                    c.copy('dve', omT[i][:, cp:cp + 4, :], p[:].rearrange("p (c t) -> p c t", c=4))
                for h in range(2):
                    po = c.ps()
                    for k in range(8):
                        c.mm(po[:], omT[i][:, k, :], Wmo[:, k, h * 512:(h + 1) * 512], start=(k == 0), stop=(k == 7))
                    c.stt(yb[i][:, h * 512:(h + 1) * 512], po[:], 1.0 / ALPHA, x2t[i][:, h * 512:(h + 1) * 512],
                          ALU.mult, ALU.add)
                ln_block(c, S, yb[i][:], epsc, gB[:], bB[:], x3[i][:])
                c.dma(G['x3_d'][ob * 128:(ob + 1) * 128, :], x3[i][:])


def build(stage=99, dbg=False, nblk=NBLK):
    nc = bass.Bass("TRN2", target_bir_lowering=False)
    c = Ctx(nc)
    kind_s = "ExternalOutput" if dbg else "Internal"
    G = {}

    def din(name, shape, dt=F32):
        G[name] = c.dram(name, shape, dt, kind="ExternalInput").ap()
        return G[name]

    def dsc(name, shape, dt):
        G[name] = c.dram(name, shape, dt, kind=kind_s).ap()
        return G[name]

    nown = nblk // 2 * 128
    ntok = nblk * 128
    xs = din("xs", [ntok, D])
    din("ffn1_w_in", [D, 2 * DFF]); din("ffn1_w_out", [DFF, D])
    din("ln1_g", [1, D]); din("ln1_b", [1, D]); din("ln1_gT", [128, 8]); din("ln1_bT", [128, 8])
    din("w_in", [D, D_IN]); din("dt_bias", [1, 16]); din("kv_norm_w", [1, 256])
    din("identb", [128, 128], BF16)
    din("v0", [128, 1])

    dsc("x1T_d", [nblk, 128, 8, 128], BF16)
    dsc("x1_d", [nown, D], F32)
    dsc("xbcT_d", [16, 128, ntok], BF16)
    dsc("kiT_d", [128, ntok], BF16)
    dsc("dt_d", [ntok, 16], F32)
    dsc("ckvtok_d", [nblk, 128, 257], BF16)
    dsc("ckvT_d", [2, 128, ntok], BF16)
    dsc("sgs_d", [8, 128, nown], BF16); dsc("sga_d", [8, 128, nown], BF16)
    dsc("qT_d", [8, 128, nown], BF16); dsc("qiT_d", [8, 128, nown], BF16)
    dsc("zs_d", [nown, D], BF16); dsc("widx_d", [nown, 16], F32)
    dsc("yssdT_d", [nblk // 2, 128, 8, 128], BF16)
    dsc("yattT_d", [nblk // 2, 128, 8, 128], BF16)
    dsc("x2_d", [nown, D], F32); dsc("x2T_d", [nblk // 2, 128, 8, 128], BF16)
    dsc("kmT_d", [128, 8, 256], BF16); dsc("vm_d", [128, 2, D], BF16); dsc("x3_d", [nown, D], F32)
    for wn in ("w_proj_ssd", "w_proj_att", "w_out", "w_mq", "w_mo"):
        din(wn, [D, D])
    din("w_mkv", [D, 2 * D]); din("mem", [256, D])
    for ln in ("ln2", "ln3", "ln4"):
        din(ln + "_g", [1, D]); din(ln + "_b", [1, D])
    din("ffn2_w_in", [D, 2 * DFF]); din("ffn2_w_out", [DFF, D])
    G['out'] = c.dram("out", [nown, D], F32, kind="ExternalOutput").ap()
    G['alibiLT'] = [din("alibiLT%d" % i, [128, SEQ], BF16) for i in range(2)]
    G['alibiR'] = [din("alibiR%d" % i, [128, 512], BF16) for i in range(4)]
    din("ohp", [128, 16])
    din("prow1", [1, SEQ]); din("negslope", [1, 16]); din("negcausal", [128, 128]); din("kb0", [128, 1])
    din("w_uk", [1024, 256]); G['w_uv'] = din("w_uv", [16, 256, 64])
    din("cwT", [128, 16, 4]); din("cbT", [128, 16]); din("trib", [128, 128], BF16); din("ones128b", [128, 128], BF16)
    din("identf", [128, 128]); din("negmask4b", [128, 512], BF16); din("sel16b", [128, 2048], BF16)
    din("a_log", [1, 16]); din("d_skip", [1, 16]); din("ssd_norm_w", [1, D])

    c.banks = [c.psum('bank%d' % i, [128, 512], F32) for i in range(8)]
    K = {}
    K['identb'] = c.sb('identb_s', [128, 128], BF16)
    c.dma(K['identb'][:], G['identb'])
    K['v0'] = c.sb('v0_s', [128, 1], F32)
    c.dma(K['v0'][:], G['v0'])
    K['kb0'] = c.sb('kb0_s', [128, 1], F32)
    c.dma(K['kb0'][:], G['kb0'])

    def post1(blk, S, y, mean, rstd):
        if blk == 'setup':
            S['zb'] = [c.sb('zb%d' % i, [128, D], BF16) for i in range(2)]
            S['x1s'] = [c.sb('x1s%d' % i, [128, 8, 128], BF16) for i in range(2)]
            S['z32'] = [c.sb('z32_%d' % i, [128, D], F32) for i in range(2)]
            c.dma(S['gT'][:], G['ln1_gT'])
            c.dma(S['bT'][:], G['ln1_bT'])
            return
        zb = S['zb'][blk % 2]
        x1s = S['x1s'][blk % 2]
        c.ts('dve', zb[:], y[:], mean, rstd, ALU.subtract, ALU.mult)
        pb = c.ps()
        pT = pb.bitcast(BF16)
        for k in range(8):
            c.tr(pT[:, k * 128:(k + 1) * 128], zb[:, k * 128:(k + 1) * 128], K['identb'][:])
        for k in range(8):
            c.act(x1s[:, k, :], pT[:, k * 128:(k + 1) * 128], AF.Identity,
                  bias=S['bT'][:, k:k + 1], scale=S['gT'][:, k:k + 1])
        if blk == 0:
            c.ts('dve', x1s[:], x1s[:], K['v0'][:, 0:1], None, ALU.mult)
        c.dma(G['x1T_d'][blk], x1s[:])
        if blk % 2 == 1:
            z = S['z32'][(blk // 2) % 2]
            c.ts('dve', z[:], y[:], mean, rstd, ALU.subtract, ALU.mult)
            c.tt('pool', z[:], z[:], S['gB'][:], ALU.mult)
            c.tt('pool', z[:], z[:], S['bB'][:], ALU.add)
            c.dma(G['x1_d'][(blk // 2) * 128:(blk // 2 + 1) * 128, :], z[:])

    if stage >= 1:
        ffn_phase(c, K, xs, nblk, G['ffn1_w_in'], G['ffn1_w_out'], G['ln1_g'], G['ln1_b'], post1, 'f1')
    if stage >= 2:
        proj_phase_all(c, K, G, nblk)
        proj_phase_own(c, K, G, nblk)
    if stage >= 3:
        ssd_phase(c, K, G, nblk)
    if stage >= 4:
        dsa_phase(c, K, G, nblk)
    if stage >= 5:
        merge_phase(c, K, G, nblk)
        memkv_phase(c, K, G)
        xattn_phase(c, K, G, nblk)
    if stage >= 6:
        def post4(blk, S, y, mean, rstd):
            if blk == 'setup':
                S['o32'] = [c.sb('o32_%d' % i, [128, D], F32) for i in range(2)]
                return
            o = S['o32'][blk % 2]
            c.ts('dve', o[:], y[:], mean, rstd, ALU.subtract, ALU.mult)
            c.tt('pool', o[:], o[:], S['gB'][:], ALU.mult)
            c.tt('pool', o[:], o[:], S['bB'][:], ALU.add)
            c.dma(G['out'][blk * 128:(blk + 1) * 128, :], o[:])
        ffn_phase(c, K, G['x3_d'], nblk // 2, G['ffn2_w_in'], G['ffn2_w_out'], G['ln4_g'], G['ln4_b'], post4, 'f2')

    c.barrier()
    c.es.close()
    global LAST_INPUTS
    LAST_INPUTS = [n for n in G if n not in ('out',) and not n.endswith('_d')]
    print("build: ninst=%d nwait=%d nsem=%d" % (c.ninst, c.nwait, c.nsem))
    return nc


def make_consts():
    i = np.arange(128)
    tri = (i[:, None] <= i[None, :]).astype(np.float32)
    negm = np.where(i[:, None] <= i[None, :], 0.0, -30000.0).astype(np.float32)
    sel = np.zeros((128, 16, 128), np.float32)
    for h in range(16):
        sel[h, h, :] = 1.0
    bf = ml_dtypes.bfloat16
    slopes = (2.0 ** (-8.0 * np.arange(1, 17, dtype=np.float64) / 16)).astype(np.float32)
    pos = np.arange(SEQ, dtype=np.float32)
    extra = {}
    ltt = [np.zeros((128, SEQ), np.float32) for _ in range(2)]
    rrt = [np.zeros((128, 512), np.float32) for _ in range(4)]
    ohp = np.zeros((128, 16), np.float32)
    for hg in range(4):
        r0 = 64 * (hg % 2)
        lt = ltt[hg // 2][r0:r0 + 28]
        lt[0:16] = 1.0
        rr = rrt[hg][r0:r0 + 28]
        for j in range(4):
            v = (slopes[hg * 4 + j] * pos).astype(np.float32)
            for k in range(3):
                part = v.astype(bf).astype(np.float32)
                lt[16 + 3 * j + k] = part
                v = v - part
                rr[16 + 3 * j + k, j * 128:(j + 1) * 128] = 1.0
            ohp[r0 + hg * 4 + j, hg * 4 + j] = 1.0
    for q_ in range(2):
        extra["alibiLT%d" % q_] = ltt[q_].astype(bf)
    for q_ in range(4):
        extra["alibiR%d" % q_] = rrt[q_].astype(bf)
    extra["ohp"] = ohp
    extra["prow1"] = (pos + 1.0)[None, :]
    extra["negslope"] = (-slopes)[None, :]
    extra["negcausal"] = np.where(i[None, :] <= i[:, None], 0.0, -1e30).astype(np.float32)
    return {**extra, "identb": np.eye(128, dtype=np.float32).astype(ml_dtypes.bfloat16),
            "identf": np.eye(128, dtype=np.float32), "trib": tri.astype(bf),
            "ones128b": np.ones((128, 128), np.float32).astype(bf),
            "negmask4b": np.ascontiguousarray(np.tile(negm, (1, 4))).astype(bf),
            "sel16b": sel.reshape(128, 2048).astype(bf)}


def make_in_maps(inputs):
    x = np.asarray(inputs["x"], dtype=np.float32)
    cs = make_consts()
    sq = lambda a: np.ascontiguousarray(np.asarray(a, dtype=np.float32)[0])
    shared = {
        "ffn1_w_in": sq(inputs["ffn1_w_in"]), "ffn1_w_out": sq(inputs["ffn1_w_out"]),
        "ln1_g": sq(inputs["ln1_g"])[None, :], "ln1_b": sq(inputs["ln1_b"])[None, :],
        "ln1_gT": np.ascontiguousarray(sq(inputs["ln1_g"]).reshape(8, 128).T),
        "ln1_bT": np.ascontiguousarray(sq(inputs["ln1_b"]).reshape(8, 128).T),
        "w_in": sq(inputs["w_in"]), "dt_bias": sq(inputs["dt_bias"])[None, :],
        "kv_norm_w": sq(inputs["kv_norm_w"])[None, :],
        "cwT": np.ascontiguousarray(sq(inputs["conv_w"]).reshape(4, 16, 128).transpose(2, 1, 0)),
        "cbT": np.ascontiguousarray(sq(inputs["conv_b"]).reshape(16, 128).T),
        "a_log": sq(inputs["a_log"])[None, :], "d_skip": sq(inputs["d_skip"])[None, :],
        "ssd_norm_w": sq(inputs["ssd_norm_w"])[None, :],
        "w_uk": sq(inputs["w_uk"]).reshape(1024, 256), "w_uv": sq(inputs["w_uv"]),
        "w_proj_ssd": sq(inputs["w_proj_ssd"]), "w_proj_att": sq(inputs["w_proj_att"]),
        "w_out": sq(inputs["w_out"]), "w_mq": sq(inputs["w_mq"]), "w_mo": sq(inputs["w_mo"]),
        "w_mkv": sq(inputs["w_mkv"]),
        "ln2_g": sq(inputs["ln2_g"])[None, :], "ln2_b": sq(inputs["ln2_b"])[None, :],
        "ln3_g": sq(inputs["ln3_g"])[None, :], "ln3_b": sq(inputs["ln3_b"])[None, :],
        "ln4_g": sq(inputs["ln4_g"])[None, :], "ln4_b": sq(inputs["ln4_b"])[None, :],
        "ffn2_w_in": sq(inputs["ffn2_w_in"]), "ffn2_w_out": sq(inputs["ffn2_w_out"]),
    }
    shared.update(cs)
    maps = []
    for core in range(8):
        b, par = core // 2, core % 2
        if par == 0:
            xs = np.concatenate([np.zeros((128, D), np.float32), x[b, :SEQ - 128]], axis=0)
        else:
            xs = x[b]
        m = dict(shared)
        m["xs"] = np.ascontiguousarray(xs)
        m["v0"] = np.full((128, 1), float(par), np.float32)
        m["kb0"] = np.full((128, 1), 0.0 if par else -1e30, np.float32)
        m["mem"] = np.ascontiguousarray(np.asarray(inputs["mem"], dtype=np.float32)[b])
        maps.append(m)
    return maps


def kernel(**inputs):
    nc = build()
    maps = make_in_maps(inputs)
    res = run_bass_kernel_spmd(nc, maps, core_ids=list(range(8)))
    out = np.zeros((4, SEQ, D), np.float32)
    for core in range(8):
        b, par = core // 2, core % 2
        o = np.asarray(res.results[core]["out"], dtype=np.float32).reshape(NBLK // 2, 128, D)
        out[b].reshape(NBLK, 128, D)[par::2] = o
    return out
```

```python
import contextlib
import numpy as np
import ml_dtypes
import concourse.bass as bass
import concourse.mybir as mybir
from concourse.bass_utils import run_bass_kernel_spmd

F32 = mybir.dt.float32
BF16 = mybir.dt.bfloat16
AF = mybir.ActivationFunctionType
ALU = mybir.AluOpType
AX = mybir.AxisListType
DSZ = {F32: 4, BF16: 2}
WHOLE = (0, 1 << 30, 0, 1 << 40)

D = 1024
SEQ = 4096
NBLK = 32
DFF = 2816
NF = DFF // 128
ALPHA = 2.0 ** 0.25
LN_EPS = 1e-5


class Ctx:
    def __init__(self, nc):
        self.nc = nc
        self.es = contextlib.ExitStack()
        self.eng = {'pe': nc.tensor, 'act': nc.scalar, 'dve': nc.vector,
                    'pool': nc.gpsimd, 'sp': nc.sync}
        self.esem = {k: self.es.enter_context(nc.semaphore('es_' + k))
                     for k in ('pe', 'act', 'dve', 'pool')}
        self.ecnt = {k: 0 for k in self.esem}
        self.seen = {k: {} for k in self.eng}
        self.trk = {}
        self.free_sems = []
        self.scnt = {}
        self.nsem = 0
        self.nwait = 0
        self.ninst = 0
        self.pstack = None
        self.pnames = None
        self.psi = 0

    def _reg(self, name, rowb, dram=False):
        self.trk[name] = dict(rowb=rowb, w=[], r=[], dsem=None, dram=dram)
        if self.pnames is not None and not dram:
            self.pnames.append(name)

    def sb(self, name, shape, dtype):
        self.uid = getattr(self, 'uid', 0) + 1
        name = '%s_u%d' % (name, self.uid)
        st = self.pstack if self.pstack is not None else self.es
        t = st.enter_context(self.nc.sbuf_tensor(name, list(shape), dtype))
        self._reg(name, int(np.prod(shape[1:])) * DSZ[dtype])
        return t

    def psum(self, name, shape, dtype=F32):
        st = self.pstack if self.pstack is not None else self.es
        t = st.enter_context(self.nc.psum_tensor(name, list(shape), dtype))
        self._reg(name, int(np.prod(shape[1:])) * DSZ[dtype])
        self.trk[name]['psum'] = True
        return t

    def dram(self, name, shape, dtype, kind="Internal"):
        t = self.nc.dram_tensor(name, list(shape), dtype, kind=kind)
        self._reg(name, 0, dram=True)
        return t

    def sem_for(self, name):
        t = self.trk[name]
        if t['dsem'] is None:
            if self.free_sems:
                t['dsem'] = self.free_sems.pop()
            else:
                self.nsem += 1
                s = self.es.enter_context(self.nc.semaphore('ds%d' % self.nsem))
                self.scnt[s.name] = 0
                t['dsem'] = s
        return t['dsem']

    @contextlib.contextmanager
    def phase(self):
        assert self.pstack is None
        self.pstack = contextlib.ExitStack()
        self.pnames = []
        try:
            yield
        finally:
            self.barrier()
            for n in self.pnames:
                t = self.trk.pop(n)
                if t['dsem'] is not None:
                    self.free_sems.append(t['dsem'])
            self.pstack.close()
            self.pstack = None
            self.pnames = None

    def barrier(self):
        for e, eng in self.eng.items():
            for k, s in self.esem.items():
                if k != e and self.seen[e].get(s.name, 0) < self.ecnt[k]:
                    eng.wait_ge(s, self.ecnt[k])
                    self.seen[e][s.name] = self.ecnt[k]
            for name, t in self.trk.items():
                s = t['dsem']
                if s is not None and self.seen[e].get(s.name, 0) < self.scnt[s.name]:
                    eng.wait_ge(s, self.scnt[s.name])
                    self.seen[e][s.name] = self.scnt[s.name]
        for t in self.trk.values():
            t['w'] = []
            t['r'] = []

    def region(self, ap):
        t = self.trk[ap.name]
        sz = DSZ[ap.dtype]
        pat = ap.ap
        off = ap.offset
        if t['dram']:
            ext = sum((c - 1) * abs(s) for s, c in pat) + 1
            return (0, 1, off * sz, (off + ext) * sz)
        if t.get('psum'):
            return (0, 128, 0, t['rowb'])
        rowe = t['rowb'] // sz
        p0 = off // rowe
        f0 = off % rowe
        ps, pc = pat[0]
        p1 = p0 + (pc - 1) * (ps // rowe) + 1
        ext = sum((c - 1) * abs(s) for s, c in pat[1:]) + 1
        return (p0, p1, f0 * sz, (f0 + ext) * sz)

    @staticmethod
    def _ov(a, b):
        return a[0] < b[1] and b[0] < a[1] and a[2] < b[3] and b[2] < a[3]

    @staticmethod
    def _cov(a, b):
        return a[0] <= b[0] and a[1] >= b[1] and a[2] <= b[2] and a[3] >= b[3]

    def _cur(self, t, rec):
        if rec[2] == 'dma' and rec[0] is t['dsem']:
            return (rec[0], self.scnt[rec[0].name], 'dma')
        return rec

    def _deps(self, outs, ins, whole_out=False, e=None):
        deps = []
        for ap in ins:
            t = self.trk[ap.name]
            R = self.region(ap)
            for (Q, rec) in t['w']:
                if self._ov(R, Q):
                    deps.append(self._cur(t, rec))
            if t.get('psum'):
                for (Q, rec) in t['r']:
                    if rec[2] != e:
                        deps.append(rec)
        for ap in outs:
            t = self.trk[ap.name]
            R = self.region(ap) if not whole_out else WHOLE
            for (Q, rec) in t['w']:
                if self._ov(R, Q):
                    deps.append(self._cur(t, rec))
            for (Q, rec) in t['r']:
                if self._ov(R, Q):
                    deps.append(self._cur(t, rec))
        return deps

    def _record(self, outs, ins, rec):
        for ap in ins:
            t = self.trk[ap.name]
            R = self.region(ap)
            t['r'] = [(Q, r) for (Q, r) in t['r']
                      if not (r[0] is rec[0] and self._cov(R, Q))]
            t['r'].append((R, rec))
            if len(t['r']) > 40:
                self._collapse(t, 'r')
        for ap in outs:
            t = self.trk[ap.name]
            R = self.region(ap)
            t['w'] = [(Q, r) for (Q, r) in t['w'] if not self._cov(R, Q)]
            t['r'] = [(Q, r) for (Q, r) in t['r'] if not self._cov(R, Q)]
            t['w'].append((R, rec))
            if len(t['w']) > 40:
                self._collapse(t, 'w')

    def _collapse(self, t, k):
        best = {}
        box = None
        for (Q, r) in t[k]:
            key = r[0].name
            if key not in best or best[key][1] < r[1]:
                best[key] = r
            box = Q if box is None else (min(box[0], Q[0]), max(box[1], Q[1]),
                                         min(box[2], Q[2]), max(box[3], Q[3]))
        t[k] = [(box, r) for r in best.values()]

    def _waits(self, e, deps):
        need = {}
        for (sem, val, src) in deps:
            if src == e and e == 'pe':
                continue
            if self.seen[e].get(sem.name, 0) >= val:
                continue
            if sem.name not in need or need[sem.name][1] < val:
                need[sem.name] = (sem, val)
        for (sem, val) in need.values():
            self.seen[e][sem.name] = val
        return list(need.values())

    def _autobb(self):
        tot = self.ninst + self.nwait
        if tot - getattr(self, 'lastbb', 0) > 500:
            self.lastbb = tot
            self.newbb()

    def emit(self, e, fn, outs, ins, attach=True):
        if self.ninst >= LIMIT:
            return None
        self._autobb()
        waits = self._waits(e, self._deps(outs, ins, e=e))
        eng = self.eng[e]
        self.nwait += len(waits)
        last = None
        if attach and waits:
            last = waits.pop()
        for (sem, val) in waits:
            eng.wait_ge(sem, val)
        ins_ = fn(eng)
        if last is not None:
            ins_._wait_ge(last[0], last[1])
        ins_.then_inc(self.esem[e], 1)
        self.ecnt[e] += 1
        self.ninst += 1
        rec = (self.esem[e], self.ecnt[e], e)
        self._record(outs, ins, rec)
        return ins_

    def dma(self, out, in_, q='sp', **kw):
        if self.ninst >= LIMIT:
            return None
        self._autobb()
        deps = self._deps([out], [in_], whole_out=True)
        sbside = in_ if self.trk[out.name]['dram'] else out
        sem = self.sem_for(sbside.name)
        deps = [d for d in deps if not (d[0] is sem and d[2] == 'dma')]
        waits = self._waits(q, deps)
        eng = self.eng[q]
        self.nwait += len(waits)
        for (s, val) in waits:
            eng.wait_ge(s, val)
        self.scnt[sem.name] += 16
        eng.dma_start(out=out, in_=in_, **kw).then_inc(sem, 16)
        self.ninst += 1
        rec = (sem, self.scnt[sem.name], 'dma')
        self._record([out], [in_], rec)

    def newbb(self):
        self.nbb = getattr(self, 'nbb', 0) + 1
        self.nc.switch_bb('kbb%d' % self.nbb)

    def ps(self):
        b = self.banks[self.psi % len(self.banks)]
        self.psi += 1
        return b

    def mm(self, out, lhsT, rhs, start=True, stop=True):
        return self.emit('pe', lambda e: e.matmul(out, lhsT=lhsT, rhs=rhs, start=start, stop=stop),
                         [out], [lhsT, rhs])

    def tr(self, out, in_, ident):
        return self.emit('pe', lambda e: e.transpose(out=out, in_=in_, identity=ident),
                         [out], [in_, ident])

    def act(self, out, in_, func, bias=None, scale=None, accum=None, e='act'):
        kw = {}
        ins = [in_]
        outs = [out]
        if bias is not None:
            kw['bias'] = bias
            if not isinstance(bias, (int, float)):
                ins.append(bias)
        if scale is not None:
            kw['scale'] = scale
            if not isinstance(scale, (int, float)):
                ins.append(scale)
        if accum is not None:
            kw['accum_out'] = accum
            outs.append(accum)
        return self.emit('act', lambda e_: e_.activation(out=out, in_=in_, func=func, **kw),
                         outs, ins, attach=accum is None)

    def ts(self, e, out, in0, s1, s2, op0, op1=None, accum=None):
        ins = [in0] + [s for s in (s1, s2) if s is not None and not isinstance(s, (int, float))]
        outs = [out] + ([accum] if accum is not None else [])
        kw = {}
        if op1 is not None:
            kw['op1'] = op1
        if accum is not None:
            kw['accum_out'] = accum
        return self.emit(e, lambda e_: e_.tensor_scalar(out=out, in0=in0, scalar1=s1, scalar2=s2,
                                                        op0=op0, **kw),
                         outs, ins, attach=accum is None)

    def tt(self, e, out, in0, in1, op):
        return self.emit(e, lambda e_: e_.tensor_tensor(out=out, in0=in0, in1=in1, op=op),
                         [out], [in0, in1])

    def stt(self, out, in0, scalar, in1, op0, op1):
        ins = [in0, in1] + ([scalar] if not isinstance(scalar, (int, float)) else [])
        return self.emit('dve', lambda e_: e_.scalar_tensor_tensor(out=out, in0=in0, scalar=scalar,
                                                                   in1=in1, op0=op0, op1=op1),
                         [out], ins)

    def copy(self, e, out, in_):
        if e == 'act':
            return self.emit('act', lambda e_: e_.copy(out=out, in_=in_), [out], [in_])
        return self.emit(e, lambda e_: e_.tensor_copy(out=out, in_=in_), [out], [in_])


def load_cast(c, dst, src, st, eng=None):
    if eng is None:
        c.rr = getattr(c, 'rr', 0) + 1
        eng = ('pool', 'dve', 'act')[c.rr % 3]
    shp = list(src.shape)
    n = int(np.prod(shp[1:]))
    sv = st[:, 0:n]
    if len(shp) == 3:
        sv = sv.rearrange("p (a b) -> p a b", a=shp[1])
    c.dma(sv, src)
    c.copy(eng, dst, sv)


def layer_norm_stats(c, S, y, eps):
    st6 = S['st6']
    mv = S['mv']
    for h in range(2):
        c.emit('dve', lambda e, h=h: e.bn_stats(out=st6[:, h, :], in_=y[:, h * 512:(h + 1) * 512]),
               [st6[:, h, :]], [y[:, h * 512:(h + 1) * 512]])
    c.emit('dve', lambda e: e.bn_aggr(out=mv[:, 0:2], in_=st6[:]), [mv[:, 0:2]], [st6[:]])
    c.act(mv[:, 2:3], mv[:, 1:2], AF.Sqrt, bias=S['epsc'][:, 0:1])
    c.emit('dve', lambda e: e.reciprocal(out=mv[:, 3:4], in_=mv[:, 2:3]), [mv[:, 3:4]], [mv[:, 2:3]])
    return mv[:, 0:1], mv[:, 3:4]


def ffn_phase(c, K, xsrc, nblk, w_in, w_out, g_d, b_d, post, name):
    TT = 1024
    NSUB = TT // 128
    assert (nblk * 128) % TT == 0
    with c.phase():
        W2 = c.sb('W2', [128, NF, D], BF16)
        xT = c.sb('xT', [128, 8, TT], BF16)
        hT = c.sb('hT', [128, NF, TT], BF16)
        W1 = [c.sb('W1_%d' % i, [128, 8, 512], BF16) for i in range(3)]
        wst = [c.sb('wst%d' % i, [128, 2048], F32) for i in range(3)]
        xin = [c.sb('xin%d' % i, [128, D], F32) for i in range(3)]
        xbf = [c.sb('xbf%d' % i, [128, D], BF16) for i in range(3)]
        sg = [c.sb('sg%d' % i, [128, 512], BF16) for i in range(3)]
        yb = [c.sb('yb%d' % i, [128, D], F32) for i in range(2)]
        S = dict(K)
        S['st6'] = c.sb('st6', [128, 2, 6], F32)
        S['mv'] = c.sb('mv', [128, 4], F32)
        S['epsc'] = c.sb('epsc', [128, 1], F32)
        S['gB'] = c.sb('gB', [128, D], F32)
        S['bB'] = c.sb('bB', [128, D], F32)
        S['gT'] = c.sb('gT', [128, 8], F32)
        S['bT'] = c.sb('bT', [128, 8], F32)
        c.emit('pool', lambda e: e.memset(S['epsc'][:], LN_EPS / (ALPHA * ALPHA)), [S['epsc'][:]], [])
        c.dma(S['gB'][:], g_d[0:1, :].partition_broadcast(128))
        c.dma(S['bB'][:], b_d[0:1, :].partition_broadcast(128))
        post('setup', S, None, None, None)
        wi = 0
        w2v = w_out.rearrange("(f p) n -> p f n", p=128)
        for f in range(0, NF, 2):
            load_cast(c, W2[:, f:f + 2, :], w2v[:, f:f + 2, :], wst[wi % 3])
            wi += 1
        w1v = w_in.rearrange("(k p) n -> p k n", p=128)
        xi = 0
        for t in range(nblk * 128 // TT):
            for s in range(NSUB):
                blk = t * NSUB + s
                xs_ = xin[xi % 3]
                xb_ = xbf[xi % 3]
                xi += 1
                c.dma(xs_[:], xsrc[blk * 128:(blk + 1) * 128, :])
                c.copy('act', xb_[:], xs_[:])
                pb = c.ps()
                pT = pb.bitcast(BF16)
                for k in range(8):
                    c.tr(pT[:, k * 128:(k + 1) * 128], xb_[:, k * 128:(k + 1) * 128], K['identb'][:])
                c.copy('dve', xT[:, :, s * 128:(s + 1) * 128],
                       pT[:, 0:1024].rearrange("p (k t) -> p k t", k=8))
            for fp in range(NF // 2):
                w1 = W1[fp % 3]
                for gu in range(2):
                    col = gu * DFF + fp * 256
                    load_cast(c, w1[:, :, gu * 256:(gu + 1) * 256], w1v[:, :, col:col + 256], wst[wi % 3],
                              eng='pool' if gu == 0 else 'dve')
                    wi += 1
                for fi in range(2):
                    f = fp * 2 + fi
                    for h in range(TT // 512):
                        pg = c.ps()
                        pu = c.ps()
                        for k in range(8):
                            c.mm(pg[:], w1[:, k, fi * 128:(fi + 1) * 128], xT[:, k, h * 512:(h + 1) * 512],
                                 start=(k == 0), stop=(k == 7))
                        for k in range(8):
                            c.mm(pu[:], w1[:, k, 256 + fi * 128:256 + (fi + 1) * 128],
                                 xT[:, k, h * 512:(h + 1) * 512], start=(k == 0), stop=(k == 7))
                        s_ = sg[(f * 2 + h) % 3]
                        c.act(s_[:], pg[:], AF.Silu)
                        c.tt('dve', hT[:, f, h * 512:(h + 1) * 512], s_[:], pu[:], ALU.mult)
            for s in range(NSUB):
                blk = t * NSUB + s
                xs_ = xin[xi % 3]
                y = yb[xi % 2]
                xi += 1
                c.dma(xs_[:], xsrc[blk * 128:(blk + 1) * 128, :])
                for h in range(2):
                    po = c.ps()
                    for f in range(NF):
                        c.mm(po[:], hT[:, f, s * 128:(s + 1) * 128], W2[:, f, h * 512:(h + 1) * 512],
                             start=(f == 0), stop=(f == NF - 1))
                    c.stt(y[:, h * 512:(h + 1) * 512], po[:], 0.5 / ALPHA, xs_[:, h * 512:(h + 1) * 512],
                          ALU.mult, ALU.add)
                mean, rstd = layer_norm_stats(c, S, y, None)
                post(blk, S, y, mean, rstd)


O_GS, O_GA, O_Z, O_XBC, O_DT, O_Q, O_CKV, O_QI, O_KI, O_WI = 0, 1024, 2048, 3072, 5120, 5136, 6160, 6416, 7440, 7504
D_IN = 7520
RMS_EPS = 1e-6


def proj_phase_all(c, K, G, nblk):
    w_in = G['w_in']
    wv = w_in.rearrange("(k p) n -> p k n", p=128)
    with c.phase():
        Wx = c.sb('Wx', [128, 8, 2048], BF16)
        Wk = c.sb('Wk', [128, 8, 128], BF16)
        Wd = c.sb('Wd', [128, 8, 16], BF16)
        Wc = c.sb('Wc', [128, 8, 256], BF16)
        wst = [c.sb('wst%d' % i, [128, 2048], F32) for i in range(3)]
        xg = [c.sb('xg%d' % i, [128, 8, 512], BF16) for i in range(2)]
        xo = [c.sb('xo%d' % i, [128, 16, 512], BF16) for i in range(2)]
        ko = [c.sb('ko%d' % i, [128, 512], BF16) for i in range(2)]
        dto = [c.sb('dto%d' % i, [128, 16], F32) for i in range(2)]
        dte = [c.sb('dte%d' % i, [128, 16], F32) for i in range(2)]
        cko = [c.sb('cko%d' % i, [128, 257], BF16) for i in range(2)]
        ckf = [c.sb('ckf%d' % i, [128, 256], F32) for i in range(2)]
        ckT = [c.sb('ckT%d' % i, [128, 2, 128], BF16) for i in range(2)]
        sq = c.sb('sq', [128, 256], F32)
        ss = c.sb('ss', [128, 4], F32)
        dtb = c.sb('dtb', [128, 16], F32)
        kvg = c.sb('kvg', [128, 256], F32)
        epsr = c.sb('epsr', [128, 1], F32)
        c.emit('pool', lambda e: e.memset(epsr[:], RMS_EPS), [epsr[:]], [])
        c.dma(dtb[:], G['dt_bias'][0:1, :].partition_broadcast(128))
        c.dma(kvg[:], G['kv_norm_w'][0:1, :].partition_broadcast(128))
        wi = 0
        for m in range(0, 16, 2):
            load_cast(c, Wx[:, :, m * 128:(m + 2) * 128], wv[:, :, O_XBC + m * 128:O_XBC + (m + 2) * 128], wst[wi % 3]); wi += 1
        load_cast(c, Wc[:], wv[:, :, O_CKV:O_CKV + 256], wst[wi % 3]); wi += 1
        for hf in range(2):
            load_cast(c, Wk[:, :, hf * 64:(hf + 1) * 64], wv[:, :, O_KI:O_KI + 64], wst[wi % 3]); wi += 1
        load_cast(c, Wd[:], wv[:, :, O_DT:O_DT + 16], wst[wi % 3]); wi += 1
        xbcv = G['xbcT_d'].rearrange("m p t -> p m t")
        for g in range(nblk // 4):
            x_ = xg[g % 2]
            for b in range(4):
                c.dma(x_[:, :, b * 128:(b + 1) * 128], G['x1T_d'][g * 4 + b])
            xo_ = xo[g % 2]
            for m in range(16):
                p = c.ps()
                for k in range(8):
                    c.mm(p[:], Wx[:, k, m * 128:(m + 1) * 128], x_[:, k, :], start=(k == 0), stop=(k == 7))
                c.copy('act' if m % 2 == 0 else 'dve', xo_[:, m, :], p[:])
            for mh in range(2):
                c.dma(xbcv[:, mh * 8:mh * 8 + 8, g * 512:(g + 1) * 512], xo_[:, mh * 8:mh * 8 + 8, :])
            p = c.ps()
            for k in range(8):
                c.mm(p[:], Wk[:, k, :], x_[:, k, :], start=(k == 0), stop=(k == 7))
            ko_ = ko[g % 2]
            c.copy('dve', ko_[:], p[:])
            c.dma(G['kiT_d'][:, g * 512:(g + 1) * 512], ko_[:])
            for b in range(4):
                blk = g * 4 + b
                p = c.ps()
                for k in range(8):
                    c.mm(p[:, 0:16], x_[:, k, b * 128:(b + 1) * 128], Wd[:, k, :], start=(k == 0), stop=(k == 7))
                e_ = dte[blk % 2]
                d_ = dto[blk % 2]
                c.tt('dve', e_[:], p[:, 0:16], dtb[:], ALU.add)
                c.act(e_[:], e_[:], AF.Exp)
                c.act(d_[:], e_[:], AF.Ln, bias=1.0)
                if blk == 0:
                    c.ts('dve', d_[:], d_[:], K['v0'][:, 0:1], None, ALU.mult)
                c.dma(G['dt_d'][blk * 128:(blk + 1) * 128, :], d_[:])
                p = c.ps()
                for k in range(8):
                    c.mm(p[:, 0:256], x_[:, k, b * 128:(b + 1) * 128], Wc[:, k, :], start=(k == 0), stop=(k == 7))
                f_ = ckf[blk % 2]
                c.copy('dve', f_[:], p[:, 0:256])
                c.act(sq[:], f_[:], AF.Square, accum=ss[:, 0:1])
                c.act(ss[:, 1:2], ss[:, 0:1], AF.Sqrt, bias=epsr[:, 0:1], scale=1.0 / 256)
                c.emit('dve', lambda e: e.reciprocal(out=ss[:, 2:3], in_=ss[:, 1:2]), [ss[:, 2:3]], [ss[:, 1:2]])
                o_ = cko[blk % 2]
                c.stt(o_[:, 0:256], f_[:], ss[:, 2:3], kvg[:], ALU.mult, ALU.mult)
                c.emit('pool', lambda e, o_=o_: e.memset(o_[:, 256:257], 1.0), [o_[:, 256:257]], [])
                c.dma(G['ckvtok_d'][blk], o_[:])
                pb = c.ps()
                pT = pb.bitcast(BF16)
                for r in range(2):
                    c.tr(pT[:, r * 128:(r + 1) * 128], o_[:, r * 128:(r + 1) * 128], K['identb'][:])
                t_ = ckT[blk % 2]
                c.copy('act', t_[:], pT[:, 0:256].rearrange("p (r t) -> p r t", r=2))
                c.dma(G['ckvT_d'].rearrange("r p t -> p r t")[:, :, blk * 128:(blk + 1) * 128], t_[:])


def proj_phase_own(c, K, G, nblk):
    wv = G['w_in'].rearrange("(k p) n -> p k n", p=128)
    nown = nblk // 2
    with c.phase():
        Wf = c.sb('Wf', [128, 8, 4096], BF16)
        Wz = c.sb('Wz', [128, 8, 1024], BF16)
        Ww = c.sb('Ww', [128, 8, 16], BF16)
        wst = [c.sb('wst%d' % i, [128, 2048], F32) for i in range(3)]
        xg = [c.sb('xg%d' % i, [128, 8, 512], BF16) for i in range(2)]
        fo = [c.sb('fo%d' % i, [128, 8, 512], BF16) for i in range(2)]
        zo = [c.sb('zo%d' % i, [128, 1024], BF16) for i in range(2)]
        wo = [c.sb('wo%d' % i, [128, 16], F32) for i in range(2)]
        wi = 0
        segs = [(O_GS, 'sgs_d', AF.Sigmoid), (O_GA, 'sga_d', AF.Sigmoid), (O_Q, 'qT_d', AF.Identity),
                (O_QI, 'qiT_d', AF.Identity)]
        for si, (off, _, _) in enumerate(segs):
            for m in range(0, 8, 2):
                load_cast(c, Wf[:, :, si * 1024 + m * 128:si * 1024 + (m + 2) * 128],
                          wv[:, :, off + m * 128:off + (m + 2) * 128], wst[wi % 3]); wi += 1
        for m in range(0, 8, 2):
            load_cast(c, Wz[:, :, m * 128:(m + 2) * 128], wv[:, :, O_Z + m * 128:O_Z + (m + 2) * 128], wst[wi % 3]); wi += 1
        load_cast(c, Ww[:], wv[:, :, O_WI:O_WI + 16], wst[wi % 3]); wi += 1
        fi = 0
        for g in range(nown // 4):
            x_ = xg[g % 2]
            for b in range(4):
                c.dma(x_[:, :, b * 128:(b + 1) * 128], G['x1T_d'][2 * (g * 4 + b) + 1])
            for si, (off, dst, fn) in enumerate(segs):
                f_ = fo[fi % 2]; fi += 1
                for m in range(8):
                    p = c.ps()
                    for k in range(8):
                        c.mm(p[:], Wf[:, k, si * 1024 + m * 128:si * 1024 + (m + 1) * 128], x_[:, k, :],
                             start=(k == 0), stop=(k == 7))
                    if fn == AF.Identity and m % 2 == 1:
                        c.copy('dve', f_[:, m, :], p[:])
                    else:
                        c.act(f_[:, m, :], p[:], fn)
                c.dma(G[dst].rearrange("m p t -> p m t")[:, :, g * 512:(g + 1) * 512], f_[:])
            for b in range(4):
                ob = g * 4 + b
                z_ = zo[ob % 2]
                for h in range(2):
                    p = c.ps()
                    for k in range(8):
                        c.mm(p[:], x_[:, k, b * 128:(b + 1) * 128], Wz[:, k, h * 512:(h + 1) * 512],
                             start=(k == 0), stop=(k == 7))
                    c.act(z_[:, h * 512:(h + 1) * 512], p[:], AF.Silu)
                c.dma(G['zs_d'][ob * 128:(ob + 1) * 128, :], z_[:])
                p = c.ps()
                for k in range(8):
                    c.mm(p[:, 0:16], x_[:, k, b * 128:(b + 1) * 128], Ww[:, k, :], start=(k == 0), stop=(k == 7))
                w_ = wo[ob % 2]
                c.act(w_[:], p[:, 0:16], AF.Copy, scale=0.25)
                c.dma(G['widx_d'][ob * 128:(ob + 1) * 128, :], w_[:])


def ssd_phase(c, K, G, nblk):
    with c.phase():
        cw = c.sb('cw', [128, 16, 4], F32)
        cb = c.sb('cb', [128, 16], F32)
        tri = c.sb('tri', [128, 128], BF16)
        ones = c.sb('ones', [128, 128], BF16)
        negm = c.sb('negm', [128, 512], BF16)
        sel = c.sb('sel', [128, 2048], BF16)
        apad = c.sb('apad', [128, 2, 128], BF16)
        ahl = c.sb('ahl', [128, 2, 16], BF16)
        ares = c.sb('ares', [128, 16], F32)
        Abc = c.sb('Abc', [128, 16], F32)
        Dbc = c.sb('Dbc', [128, 16], F32)
        nwB = c.sb('nwB', [128, D], F32)
        epsr = c.sb('epsr', [128, 1], F32)
        S32 = c.sb('S32', [128, 16, 64], F32)
        Sbf = c.sb('Sbf', [128, 16, 64], BF16)
        for t_, n_ in ((cw, 'cwT'), (cb, 'cbT'), (tri, 'trib'), (ones, 'ones128b'),
                       (negm, 'negmask4b'), (sel, 'sel16b')):
            c.dma(t_[:], G[n_])
        c.dma(Abc[:], G['a_log'][0:1, :].partition_broadcast(128))
        c.dma(Dbc[:], G['d_skip'][0:1, :].partition_broadcast(128))
        c.dma(nwB[:], G['ssd_norm_w'][0:1, :].partition_broadcast(128))
        c.act(Abc[:], Abc[:], AF.Exp)
        c.ts('dve', Abc[:], Abc[:], -1.0, None, ALU.mult)
        c.emit('dve', lambda e: e.memset(epsr[:], RMS_EPS), [epsr[:]], [])
        c.emit('dve', lambda e: e.memset(S32[:], 0.0), [S32[:]], [])
        c.emit('dve', lambda e: e.memset(Sbf[:], 0.0), [Sbf[:]], [])
        c.emit('dve', lambda e: e.memset(apad[:], 0.0), [apad[:]], [])
        xh = [c.sb('xh%d' % i, [128, 16, 132], BF16) for i in range(2)]
        t1 = [c.sb('t1_%d' % i, [128, 16, 128], F32) for i in range(3)]
        xc = [c.sb('xc%d' % i, [128, 16, 128], BF16) for i in range(2)]
        xtok = [c.sb('xtok%d' % i, [128, 16, 64], F32) for i in range(2)]
        Btok = [c.sb('Btok%d' % i, [128, 4, 128], BF16) for i in range(2)]
        xdt = [c.sb('xdt%d' % i, [128, 16, 64], BF16) for i in range(2)]
        xdd = [c.sb('xdd%d' % i, [128, 16, 64], BF16) for i in range(2)]
        dtt = [c.sb('dtt%d' % i, [128, 16], F32) for i in range(2)]
        sm = [c.sb('sm%d' % i, [128, 6, 16], F32) for i in range(2)]
        acsT = [c.sb('acsT%d' % i, [128, 2, 128], BF16) for i in range(2)]
        acsTf = c.sb('acsTf', [128, 128], F32)
        LT = c.sb('LT', [128, 16, 128], F32)
        MT = c.sb('MT', [128, 16, 128], BF16)
        yo = c.sb('yo', [128, 16, 64], F32)
        yy = c.sb('yy', [128, 16, 64], F32)
        zt = [c.sb('zt%d' % i, [128, D], BF16) for i in range(2)]
        sq = c.sb('sqs', [128, 256], F32)
        gs = c.sb('gs', [128, 12], F32)
        ynb = c.sb('ynb', [128, D], BF16)
        ynT = [c.sb('ynT%d' % i, [128, 8, 128], BF16) for i in range(2)]
        for i in range(2):
            c.emit('dve', lambda e, i=i: e.memset(xh[i][:], 0.0), [xh[i][:]], [])
        xbv = G['xbcT_d'].rearrange("m p t -> p m t")
        def front(ch):
            x_ = xh[ch % 2]
            for mh in range(2):
                ms = slice(mh * 8, mh * 8 + 8)
                if ch == 0:
                    c.dma(x_[:, ms, 4:132], xbv[:, ms, 0:128])
                else:
                    c.dma(x_[:, ms, 0:132], xbv[:, ms, ch * 128 - 4:ch * 128 + 128])
            d_ = dtt[ch % 2]
            c.dma(d_[:], G['dt_d'][ch * 128:(ch + 1) * 128, :])
            a_ = t1[0]
            b_ = t1[1]
            c.tt('dve', a_[:], x_[:, :, 1:129], cw[:, :, 0:1].to_broadcast([128, 16, 128]), ALU.mult)
            for k in range(1, 4):
                b_ = t1[1 + (k % 2)]
                c.tt('dve', b_[:], x_[:, :, k + 1:k + 129], cw[:, :, k:k + 1].to_broadcast([128, 16, 128]), ALU.mult)
                c.tt('pool', a_[:], a_[:], b_[:], ALU.add)
            c.tt('dve', a_[:], a_[:], cb[:].unsqueeze(2).to_broadcast([128, 16, 128]), ALU.add)
            xc_ = xc[ch % 2]
            c.act(xc_[:], a_[:], AF.Silu)
            p0 = c.ps(); p0b = p0.bitcast(BF16)
            p1 = c.ps(); p1b = p1.bitcast(BF16)
            for m in range(8):
                c.tr(p0b[:, m * 128:(m + 1) * 128], xc_[:, m, :], K['identb'][:])
            for m in range(4):
                c.tr(p1b[:, m * 128:(m + 1) * 128], xc_[:, 8 + m, :], K['identb'][:])
            xt_ = xtok[ch % 2]
            Bt_ = Btok[ch % 2]
            c.copy('act', xt_[:], p0b[:, 0:1024].rearrange("p (h q) -> p h q", h=16))
            c.copy('act', Bt_[:], p1b[:, 0:512].rearrange("p (g n) -> p g n", g=4))
            xd_ = xdt[ch % 2]
            c.tt('dve', xd_[:], xt_[:], d_[:].unsqueeze(2).to_broadcast([128, 16, 64]), ALU.mult)
            s_ = sm[ch % 2]
            c.tt('dve', s_[:, 0, :], d_[:], Abc[:], ALU.mult)
            c.copy('dve', ahl[:, 0, :], s_[:, 0, :])
            c.tt('dve', ares[:], s_[:, 0, :], ahl[:, 0, :], ALU.subtract)
            c.copy('dve', ahl[:, 1, :], ares[:])
            c.copy('dve', apad[:, :, 0:16], ahl[:])
            pc = c.ps()
            for q_ in range(2):
                c.mm(pc[:, 0:16], tri[:], ahl[:, q_, :], start=(q_ == 0), stop=(q_ == 1))
            pc1 = c.ps()
            for q_ in range(2):
                c.mm(pc1[:, 0:16], ones[:], ahl[:, q_, :], start=(q_ == 0), stop=(q_ == 1))
            pc2 = c.ps()
            for q_ in range(2):
                c.mm(pc2[:, 0:128], apad[:, q_, :], tri[:], start=(q_ == 0), stop=(q_ == 1))
            c.copy('dve', s_[:, 1, :], pc[:, 0:16])
            c.ts('dve', s_[:, 2, :], pc[:, 0:16], -1.0, None, ALU.mult)
            c.act(s_[:, 3, :], pc[:, 0:16], AF.Exp)
            c.tt('dve', s_[:, 4, :], pc1[:, 0:16], s_[:, 1, :], ALU.subtract)
            c.act(s_[:, 4, :], s_[:, 4, :], AF.Exp)
            c.act(s_[:, 5, :], pc1[:, 0:16], AF.Exp)
            aT_ = acsT[ch % 2]
            c.copy('dve', acsTf[:], pc2[:, 0:128])
            c.copy('dve', aT_[:, 0, :], acsTf[:])
            c.tt('dve', acsTf[:], acsTf[:], aT_[:, 0, :], ALU.subtract)
            c.copy('dve', aT_[:, 1, :], acsTf[:])
        def back(ch):
            own = ch % 2 == 1
            xc_ = xc[ch % 2]; xt_ = xtok[ch % 2]; Bt_ = Btok[ch % 2]; xd_ = xdt[ch % 2]
            s_ = sm[ch % 2]; aT_ = acsT[ch % 2]
            if own:
                ob = ch // 2
                for q in range(4):
                    pl = c.ps()
                    c.mm(pl[:], K['identb'][:], negm[:], start=True, stop=False)
                    for j in range(4):
                        h = q * 4 + j
                        for q_ in range(2):
                            c.mm(pl[:, j * 128:(j + 1) * 128], sel[:, h * 128:(h + 1) * 128], aT_[:, q_, :],
                                 start=False, stop=(j == 3 and q_ == 1))
                    for j in range(4):
                        h = q * 4 + j
                        c.act(LT[:, h, :], pl[:, j * 128:(j + 1) * 128], AF.Exp, bias=s_[:, 2, h:h + 1])
                pcb = c.ps()
                for g in range(4):
                    c.mm(pcb[:, g * 128:(g + 1) * 128], xc_[:, 8 + g, :], xc_[:, 12 + g, :])
                c.tt('dve', MT[:].rearrange("p (g j) l -> p g j l", g=4),
                     LT[:].rearrange("p (g j) l -> p g j l", g=4),
                     pcb[:].rearrange("p (g l) -> p g l", g=4).unsqueeze(2).to_broadcast([128, 4, 4, 128]),
                     ALU.mult)
                py = [c.ps(), c.ps()]
                for h in range(16):
                    c.mm(py[h // 8][:, (h % 8) * 64:(h % 8 + 1) * 64], MT[:, h, :], xd_[:, h, :])
                po = [c.ps(), c.ps()]
                for g in range(4):
                    c.mm(po[g // 2][:, (g % 2) * 256:(g % 2 + 1) * 256], xc_[:, 12 + g, :],
                         Sbf[:, 4 * g:4 * g + 4, :].rearrange("p h q -> p (h q)"))
                for hf in range(2):
                    hs = slice(hf * 8, hf * 8 + 8)
                    c.tt('dve', yo[:, hs, :], po[hf][:].rearrange("p (h q) -> p h q", h=8),
                         s_[:, 3, hs].unsqueeze(2).to_broadcast([128, 8, 64]), ALU.mult)
                    c.tt('dve', yy[:, hs, :], py[hf][:].rearrange("p (h q) -> p h q", h=8), yo[:, hs, :], ALU.add)
                c.tt('dve', yo[:], xt_[:], Dbc[:].unsqueeze(2).to_broadcast([128, 16, 64]), ALU.mult)
                c.tt('dve', yy[:], yy[:], yo[:], ALU.add)
                z_ = zt[ob % 2]
                c.dma(z_[:], G['zs_d'][ob * 128:(ob + 1) * 128, :])
                yf = yy[:].rearrange("p h q -> p (h q)")
                c.tt('dve', yf, yf, z_[:], ALU.mult)
                for g in range(4):
                    c.act(sq[:], yf[:, g * 256:(g + 1) * 256], AF.Square, accum=gs[:, g:g + 1])
                c.act(gs[:, 4:8], gs[:, 0:4], AF.Sqrt, bias=epsr[:, 0:1], scale=1.0 / 256)
                c.emit('dve', lambda e: e.reciprocal(out=gs[:, 8:12], in_=gs[:, 4:8]), [gs[:, 8:12]], [gs[:, 4:8]])
                c.tt('dve', yy[:].rearrange("p (g j) q -> p g (j q)", g=4),
                     yy[:].rearrange("p (g j) q -> p g (j q)", g=4),
                     gs[:, 8:12].unsqueeze(2).to_broadcast([128, 4, 256]), ALU.mult)
                c.tt('dve', ynb[:], yf, nwB[:], ALU.mult)
                pb = c.ps(); pT = pb.bitcast(BF16)
                for m in range(8):
                    c.tr(pT[:, m * 128:(m + 1) * 128], ynb[:, m * 128:(m + 1) * 128], K['identb'][:])
                yT_ = ynT[ob % 2]
                c.copy('act', yT_[:], pT[:, 0:1024].rearrange("p (m t) -> p m t", m=8))
                c.dma(G['yssdT_d'][ob], yT_[:])
            xe_ = xdd[ch % 2]
            c.tt('dve', xe_[:], xd_[:], s_[:, 4, :].unsqueeze(2).to_broadcast([128, 16, 64]), ALU.mult)
            pst = [c.ps(), c.ps()]
            for g in range(4):
                c.mm(pst[g // 2][:, (g % 2) * 256:(g % 2 + 1) * 256], Bt_[:, g, :],
                     xe_[:, 4 * g:4 * g + 4, :].rearrange("p h q -> p (h q)"))
            c.tt('dve', S32[:], S32[:], s_[:, 5, :].unsqueeze(2).to_broadcast([128, 16, 64]), ALU.mult)
            for hf in range(2):
                hs = slice(hf * 8, hf * 8 + 8)
                c.tt('dve', S32[:, hs, :], S32[:, hs, :], pst[hf][:].rearrange("p (h q) -> p h q", h=8), ALU.add)
            c.copy('act', Sbf[:], S32[:])

        front(0)
        for ch in range(nblk):
            if ch + 1 < nblk:
                front(ch + 1)
            back(ch)


LAST_INPUTS = []
LIMIT = 10 ** 9
NIT_BISECT = 16
TOPK = 256


def dsa_phase(c, K, G, nblk):
    ntok = nblk * 128
    nown = nblk // 2
    with c.phase():
        kiT = c.sb('kiT', [128, ntok], BF16)
        ckvT = c.sb('ckvT', [128, 2, ntok], BF16)
        ckvtok = c.sb('ckvtok', [128, nblk, 257], BF16)
        Wuk = c.sb('Wuk', [128, 16, 256], BF16)
        qiz = c.sb('qiz', [128, 16, 128], BF16)
        Wuv = c.sb('Wuv', [128, 16, 2, 128], BF16)
        wst = [c.sb('wst%d' % i, [128, 2048], F32) for i in range(1)]
        LTt = [c.sb('LTt%d' % i, [128, ntok], BF16) for i in range(2)]
        Rt = [c.sb('Rt%d' % i, [128, 512], BF16) for i in range(4)]
        ohp = c.sb('ohp', [128, 16], F32)
        nbpad = c.sb('nbpad', [128, 128], BF16)
        prow1 = c.sb('prow1', [128, ntok], F32)
        negsl = c.sb('negsl', [128, 16], F32)
        negc = c.sb('negc', [128, 128], F32)
        identf = c.sb('identf2', [128, 128], F32)
        sc = c.sb('sc', [128, ntok], F32)
        tmpn = c.sb('tmpn', [128, ntok], F32)
        m01 = c.sb('m01', [128, ntok], BF16)
        mbT = c.sb('mbT', [128, nblk, 128], BF16)
        qT = [c.sb('qT%d' % i, [128, 8, 128], BF16) for i in range(2)]
        qiT = [c.sb('qiT%d' % i, [128, 8, 128], BF16) for i in range(2)]
        wid = [c.sb('wid%d' % i, [128, 16], F32) for i in range(2)]
        qlat = c.sb('qlat', [128, 2, 16, 128], BF16)
        rl = [c.sb('rl%d' % i, [128, 512], F32) for i in range(4)]
        PT = [c.sb('PT%d' % i, [128, 512], BF16) for i in range(4)]
        osb = c.sb('osb', [128, 4, 256], BF16)
        oT = c.sb('oT', [128, 16, 2, 128], BF16)
        yat = [c.sb('yat%d' % i, [128, 8, 128], BF16) for i in range(2)]
        bs = c.sb('bs', [128, 12], F32)
        nb = c.sb('nb', [128, 16], F32)
        c.dma(kiT[:], G['kiT_d'])
        c.dma(ckvT[:], G['ckvT_d'].rearrange("r p t -> p r t"))
        for b0 in range(0, nblk, 8):
            c.dma(ckvtok[:, b0:b0 + 8, :], G['ckvtok_d'].rearrange("b p f -> p b f")[:, b0:b0 + 8, :])
        for i in range(2):
            c.dma(LTt[i][:], G['alibiLT'][i][:, 0:ntok])
        for i in range(4):
            c.dma(Rt[i][:], G['alibiR'][i])
        c.emit('dve', lambda e: e.memset(qiz[:], 0.0), [qiz[:]], [])
        c.dma(ohp[:], G['ohp'])
        c.emit('dve', lambda e: e.memset(nbpad[:], 0.0), [nbpad[:]], [])
        c.dma(prow1[:], G['prow1'][0:1, 0:ntok].partition_broadcast(128))
        c.dma(negsl[:], G['negslope'][0:1, :].partition_broadcast(128))
        c.dma(negc[:], G['negcausal'])
        c.dma(identf[:], G['identf'])
        c.emit('dve', lambda e: e.memset(Wuk[:], 0.0), [Wuk[:]], [])
        svk = wst[0][:, 0:2048].rearrange("p (a b) -> p a b", a=8)
        c.dma(svk, G['w_uk'].rearrange("(c p) r -> p c r", p=128))
        Wukv = Wuk[:].rearrange("p (c e) r -> p c e r", e=2)
        c.copy('dve', Wukv[0:64, :, 0, :], svk[0:64, :, :])
        c.copy('dve', Wukv[64:128, :, 1, :], svk[64:128, :, :])
        c.emit('dve', lambda e: e.memset(Wuv[:], 0.0), [Wuv[:]], [])
        for h in range(16):
            st = wst[0]
            sv = st[:, 0:128].rearrange("p (a b) -> p a b", a=2)
            c.dma(sv, G['w_uv'][h].rearrange("(rc p) d -> p rc d", p=128))
            c.copy('dve', Wuv[:, h, :, (h % 2) * 64:(h % 2 + 1) * 64], sv)
        pool_banks = c.banks[0:4]
        acc_banks = c.banks[4:8]
        allbanks = c.banks
        lo, hi, mid, cnt, ge, dd, n1 = [bs[:, i:i + 1] for i in range(7)]

        def A1(ob):
            jb = 2 * ob + 1
            NK = (jb + 1) * 128
            q_ = qT[ob % 2]; qi_ = qiT[ob % 2]; w_ = wid[ob % 2]
            c.dma(q_[:], G['qT_d'].rearrange("m p t -> p m t")[:, :, ob * 128:(ob + 1) * 128])
            c.dma(qi_[:], G['qiT_d'].rearrange("m p t -> p m t")[:, :, ob * 128:(ob + 1) * 128])
            c.dma(w_[:], G['widx_d'][ob * 128:(ob + 1) * 128, :])
            qizv = qiz[:].rearrange("p (c e) t -> p c e t", e=2)
            c.copy('dve', qizv[0:64, :, 0, :], qi_[0:64, :, :])
            c.copy('dve', qizv[64:128, :, 1, :], qi_[64:128, :, :])
            ri = 0
            for s0 in range(0, NK, 512):
                w = min(512, NK - s0)
                for h in range(16):
                    p = c.ps()
                    c.mm(p[:, 0:w], qiz[:, h, :], kiT[:, s0:s0 + w])
                    r_ = rl[ri % 4]; ri += 1
                    c.act(r_[:, 0:w], p[:, 0:w], AF.Relu)
                    if h == 0:
                        c.ts('dve', sc[:, s0:s0 + w], r_[:, 0:w], w_[:, 0:1], None, ALU.mult)
                    else:
                        c.stt(sc[:, s0:s0 + w], r_[:, 0:w], w_[:, h:h + 1], sc[:, s0:s0 + w], ALU.mult, ALU.add)
                    if h % 4 == 3:
                        yield
            if ob > 0:
                c.emit('dve', lambda e: e.tensor_reduce(out=lo, in_=sc[:, 128:jb * 128], axis=AX.X, op=ALU.min),
                       [lo], [sc[:, 128:jb * 128]])
            c.tt('dve', sc[:, jb * 128:NK], sc[:, jb * 128:NK], negc[:], ALU.add)
            c.ts('dve', sc[:, 0:128], sc[:, 0:128], K['kb0'][:, 0:1], None, ALU.add)
            if ob > 0:
                c.emit('dve', lambda e: e.tensor_reduce(out=hi, in_=sc[:, 0:NK], axis=AX.X, op=ALU.max),
                       [hi], [sc[:, 0:NK]])
                c.ts('dve', hi, hi, 1.0, None, ALU.add)
                yield
                for it in range(NIT_BISECT):
                    c.ts('dve', mid, lo, hi, 0.5, ALU.add, ALU.mult)
                    c.ts('dve', tmpn[:, 0:NK], sc[:, 0:NK], mid, 0.0, ALU.is_ge, ALU.add, accum=cnt)
                    c.ts('dve', ge, cnt, TOPK - 0.5, None, ALU.is_ge)
                    c.tt('dve', dd, mid, lo, ALU.subtract)
                    c.stt(lo, dd, ge, lo, ALU.mult, ALU.add)
                    c.tt('dve', dd, hi, mid, ALU.subtract)
                    c.stt(hi, dd, ge, mid, ALU.mult, ALU.add)
                    if it % 2 == 1:
                        yield
                c.ts('dve', m01[:, 0:NK], sc[:, 0:NK], lo, None, ALU.is_ge)
            else:
                c.ts('dve', m01[:, 0:NK], sc[:, 0:NK], -1e29, None, ALU.is_ge)
            yield
            c.tt('dve', tmpn[:, 0:NK], m01[:, 0:NK], prow1[:, 0:NK], ALU.mult)
            c.emit('dve', lambda e: e.tensor_reduce(out=n1, in_=tmpn[:, 0:NK], axis=AX.X, op=ALU.max),
                   [n1], [tmpn[:, 0:NK]])
            c.ts('dve', n1, n1, -1.0, None, ALU.add)
            c.ts('dve', nbpad[:].rearrange("p (g q) -> p g q", g=2)[:, :, 0:16],
                 negsl[:].unsqueeze(1).to_broadcast([128, 2, 16]), n1, None, ALU.mult)

        def A2(ob):
            jb = 2 * ob + 1
            NKT = jb + 1
            q_ = qT[ob % 2]
            for rc in range(2):
                for hg in range(4):
                    p = c.ps()
                    for j in range(4):
                        h = hg * 4 + j
                        c.mm(p[:, j * 128:(j + 1) * 128], Wuk[:, h, rc * 128:(rc + 1) * 128], q_[:, h // 2, :])
                    c.act(qlat[:, rc, hg * 4:hg * 4 + 4, :], p[:].rearrange("p (j t) -> p j t", j=4), AF.Copy, scale=0.125)
            pnb = c.ps()
            pn = pnb.bitcast(BF16)
            c.tr(pn[:, 0:128], nbpad[:], K['identb'][:])
            for h in range(16):
                hg = h // 4
                pr = slice(64 * (hg % 2), 64 * (hg % 2) + 16)
                c.ts('dve', Rt[hg][pr, (h % 4) * 128:(h % 4 + 1) * 128], pn[pr, 0:128],
                     ohp[pr, h:h + 1], None, ALU.mult)
            for k0 in range(0, NKT, 4):
                nk = min(4, NKT - k0)
                pb = c.ps(); pT = pb.bitcast(BF16)
                for i in range(nk):
                    c.tr(pT[:, i * 128:(i + 1) * 128], m01[:, (k0 + i) * 128:(k0 + i + 1) * 128], K['identb'][:])
                c.ts('dve', mbT[:, k0:k0 + nk, :], pT[:, 0:nk * 128].rearrange("p (k t) -> p k t", k=nk),
                     -1.0, 30000.0, ALU.add, ALU.mult)

        def step(gen):
            if gen is not None:
                next(gen, None)

        def Att(ob, gen):
            jb = 2 * ob + 1
            NKT = jb + 1
            c.banks = pool_banks
            pi = 0
            def qk(hg, kt):
                ks = slice(kt * 128, (kt + 1) * 128)
                pS = c.ps()
                c.mm(pS[:], ckvT[:, 0, ks], qlat[:, 0, hg * 4:hg * 4 + 4, :].rearrange("p j t -> p (j t)"),
                     start=True, stop=False)
                c.mm(pS[:], ckvT[:, 1, ks], qlat[:, 1, hg * 4:hg * 4 + 4, :].rearrange("p j t -> p (j t)"),
                     start=False, stop=False)
                c.mm(pS[:], LTt[hg // 2][:, ks], Rt[hg][:], start=False, stop=False)
                c.mm(pS[:].rearrange("p (j t) -> p j t", j=4), K['identb'][:],
                     mbT[:, kt, :].unsqueeze(1).to_broadcast([128, 4, 128]), start=False, stop=True)
                P_ = PT[pstate[0] % 4]; pstate[0] += 1
                c.act(P_[:], pS[:], AF.Exp)
                return P_

            pstate = [0]
            for hg in range(4):
                cur = qk(hg, 0)
                for kt in range(NKT):
                    nxt = qk(hg, kt + 1) if kt + 1 < NKT else None
                    for j in range(4):
                        c.mm(acc_banks[j][:, 0:257], cur[:, j * 128:(j + 1) * 128], ckvtok[:, kt, :],
                             start=(kt == 0), stop=(kt == NKT - 1))
                    step(gen)
                    cur = nxt
                for j in range(4):
                    rs = bs[:, 7 + j:8 + j]
                    c.emit('dve', lambda e, j=j, rs=rs: e.reciprocal(out=rs, in_=acc_banks[j][:, 256:257]),
                           [rs], [acc_banks[j][:, 256:257]])
                    c.act(osb[:, j, :], acc_banks[j][:, 0:256], AF.Copy, scale=rs)
                for rc in range(2):
                    pb = c.ps(); pT = pb.bitcast(BF16)
                    for j in range(4):
                        c.tr(pT[:, j * 128:(j + 1) * 128], osb[:, j, rc * 128:(rc + 1) * 128], K['identb'][:])
                    c.copy('dve', oT[:, hg * 4:hg * 4 + 4, rc, :], pT[:, 0:512].rearrange("p (j t) -> p j t", j=4))
            y_ = yat[ob % 2]
            for cp in range(0, 8, 4):
                p = c.ps()
                for ci in range(4):
                    cidx = cp + ci
                    n_ = 0
                    for h in (2 * cidx, 2 * cidx + 1):
                        for rc in range(2):
                            c.mm(p[:, ci * 128:(ci + 1) * 128], Wuv[:, h, rc, :], oT[:, h, rc, :],
                                 start=(n_ == 0), stop=(n_ == 3))
                            n_ += 1
                c.copy('act', y_[:, cp:cp + 4, :], p[:].rearrange("p (c t) -> p c t", c=4))
            c.dma(G['yattT_d'][ob], y_[:])
            if gen is not None:
                for _ in gen:
                    pass
            c.banks = allbanks

        c.banks = allbanks
        for _ in A1(0):
            pass
        A2(0)
        for ob in range(nown):
            gen = A1(ob + 1) if ob + 1 < nown else None
            Att(ob, gen)
            if ob + 1 < nown:
                A2(ob + 1)
        c.banks = allbanks


def load_w_resident(c, dst, src, wst, wi, ncols):
    sv = src.rearrange("(k p) n -> p k n", p=128)
    for m in range(0, ncols, 256):
        load_cast(c, dst[:, :, m:m + 256], sv[:, :, m:m + 256], wst[wi[0] % len(wst)])
        wi[0] += 1


def ln_block(c, S, y, eps_c, g_bc, b_bc, out32):
    st6, mv = S['st6'], S['mv']
    for h in range(2):
        c.emit('dve', lambda e, h=h: e.bn_stats(out=st6[:, h, :], in_=y[:, h * 512:(h + 1) * 512]),
               [st6[:, h, :]], [y[:, h * 512:(h + 1) * 512]])
    c.emit('dve', lambda e: e.bn_aggr(out=mv[:, 0:2], in_=st6[:]), [mv[:, 0:2]], [st6[:]])
    c.act(mv[:, 2:3], mv[:, 1:2], AF.Sqrt, bias=eps_c[:, 0:1])
    c.emit('dve', lambda e: e.reciprocal(out=mv[:, 3:4], in_=mv[:, 2:3]), [mv[:, 3:4]], [mv[:, 2:3]])
    c.ts('dve', out32, y, mv[:, 0:1], mv[:, 3:4], ALU.subtract, ALU.mult)
    c.tt('pool', out32, out32, g_bc, ALU.mult)
    c.tt('pool', out32, out32, b_bc, ALU.add)


def merge_phase(c, K, G, nblk):
    nown = nblk // 2
    with c.phase():
        Wps = c.sb('Wps', [128, 8, D], BF16)
        Wpa = c.sb('Wpa', [128, 8, D], BF16)
        Wo = c.sb('Wo', [128, 8, D], BF16)
        wst = [c.sb('wst%d' % i, [128, 2048], F32) for i in range(3)]
        wi = [0]
        load_w_resident(c, Wps, G['w_proj_ssd'], wst, wi, D)
        load_w_resident(c, Wpa, G['w_proj_att'], wst, wi, D)
        load_w_resident(c, Wo, G['w_out'], wst, wi, D)
        ys = [c.sb('ys%d' % i, [128, 8, 512], BF16) for i in range(2)]
        ya = [c.sb('ya%d' % i, [128, 8, 512], BF16) for i in range(2)]
        gs_ = [c.sb('gs%d' % i, [128, 8, 512], BF16) for i in range(2)]
        ga_ = [c.sb('ga%d' % i, [128, 8, 512], BF16) for i in range(2)]
        m1 = [c.sb('m1_%d' % i, [128, 512], F32) for i in range(2)]
        m2 = [c.sb('m2_%d' % i, [128, 512], F32) for i in range(2)]
        mT = c.sb('mT', [128, 8, 512], BF16)
        x1t = [c.sb('x1t%d' % i, [128, D], F32) for i in range(2)]
        yb = [c.sb('yb%d' % i, [128, D], F32) for i in range(2)]
        x2 = [c.sb('x2_%d' % i, [128, D], F32) for i in range(2)]
        x2b = [c.sb('x2b%d' % i, [128, D], BF16) for i in range(2)]
        x2T = [c.sb('x2T%d' % i, [128, 8, 128], BF16) for i in range(2)]
        S = dict(st6=c.sb('st6', [128, 2, 6], F32), mv=c.sb('mv', [128, 4], F32))
        epsc = c.sb('epsc', [128, 1], F32)
        gB = c.sb('gB', [128, D], F32); bB = c.sb('bB', [128, D], F32)
        c.emit('pool', lambda e: e.memset(epsc[:], LN_EPS / (ALPHA * ALPHA)), [epsc[:]], [])
        c.dma(gB[:], G['ln2_g'][0:1, :].partition_broadcast(128))
        c.dma(bB[:], G['ln2_b'][0:1, :].partition_broadcast(128))
        for g in range(nown // 4):
            i2 = g % 2
            for b in range(4):
                ob = g * 4 + b
                c.dma(ys[i2][:, :, b * 128:(b + 1) * 128], G['yssdT_d'][ob])
                c.dma(ya[i2][:, :, b * 128:(b + 1) * 128], G['yattT_d'][ob])
            c.dma(gs_[i2][:], G['sgs_d'].rearrange("m p t -> p m t")[:, :, g * 512:(g + 1) * 512])
            c.dma(ga_[i2][:], G['sga_d'].rearrange("m p t -> p m t")[:, :, g * 512:(g + 1) * 512])
            for m in range(8):
                p1 = c.ps(); p2 = c.ps()
                for k in range(8):
                    c.mm(p1[:], Wps[:, k, m * 128:(m + 1) * 128], ys[i2][:, k, :], start=(k == 0), stop=(k == 7))
                for k in range(8):
                    c.mm(p2[:], Wpa[:, k, m * 128:(m + 1) * 128], ya[i2][:, k, :], start=(k == 0), stop=(k == 7))
                c.tt('dve', m1[m % 2][:], p1[:], gs_[i2][:, m, :], ALU.mult)
                c.tt('dve', m2[m % 2][:], p2[:], ga_[i2][:, m, :], ALU.mult)
                c.tt('pool', mT[:, m, :], m1[m % 2][:], m2[m % 2][:], ALU.add)
            for b in range(4):
                ob = g * 4 + b
                i = ob % 2
                c.dma(x1t[i][:], G['x1_d'][ob * 128:(ob + 1) * 128, :])
                for h in range(2):
                    po = c.ps()
                    for k in range(8):
                        c.mm(po[:], mT[:, k, b * 128:(b + 1) * 128], Wo[:, k, h * 512:(h + 1) * 512],
                             start=(k == 0), stop=(k == 7))
                    c.stt(yb[i][:, h * 512:(h + 1) * 512], po[:], 1.0 / ALPHA, x1t[i][:, h * 512:(h + 1) * 512],
                          ALU.mult, ALU.add)
                ln_block(c, S, yb[i][:], epsc, gB[:], bB[:], x2[i][:])
                c.dma(G['x2_d'][ob * 128:(ob + 1) * 128, :], x2[i][:])
                c.copy('act', x2b[i][:], x2[i][:])
                pb = c.ps(); pT = pb.bitcast(BF16)
                for k in range(8):
                    c.tr(pT[:, k * 128:(k + 1) * 128], x2b[i][:, k * 128:(k + 1) * 128], K['identb'][:])
                c.copy('dve', x2T[i][:], pT[:, 0:1024].rearrange("p (k t) -> p k t", k=8))
                c.dma(G['x2T_d'][ob], x2T[i][:])


def memkv_phase(c, K, G):
    with c.phase():
        Wkv = c.sb('Wkv', [128, 8, 2048], BF16)
        wst = [c.sb('wst%d' % i, [128, 2048], F32) for i in range(3)]
        wi = [0]
        load_w_resident(c, Wkv, G['w_mkv'], wst, wi, 2048)
        mem32 = [c.sb('mem32_%d' % i, [128, D], F32) for i in range(2)]
        memb = [c.sb('memb%d' % i, [128, D], BF16) for i in range(2)]
        memT = c.sb('memT', [128, 8, 256], BF16)
        kmT = c.sb('kmT', [128, 8, 256], BF16)
        vm = c.sb('vm', [128, 2, D], BF16)
        for mt in range(2):
            c.dma(mem32[mt][:], G['mem'][mt * 128:(mt + 1) * 128, :])
            c.copy('act', memb[mt][:], mem32[mt][:])
            pb = c.ps(); pT = pb.bitcast(BF16)
            for k in range(8):
                c.tr(pT[:, k * 128:(k + 1) * 128], memb[mt][:, k * 128:(k + 1) * 128], K['identb'][:])
            c.copy('dve', memT[:, :, mt * 128:(mt + 1) * 128], pT[:, 0:1024].rearrange("p (k t) -> p k t", k=8))
        for m in range(8):
            p = c.ps()
            for k in range(8):
                c.mm(p[:, 0:256], Wkv[:, k, m * 128:(m + 1) * 128], memT[:, k, :], start=(k == 0), stop=(k == 7))
            c.copy('act', kmT[:, m, :], p[:, 0:256])
        for mt in range(2):
            for h in range(2):
                p = c.ps()
                for k in range(8):
                    c.mm(p[:], memT[:, k, mt * 128:(mt + 1) * 128], Wkv[:, k, 1024 + h * 512:1024 + (h + 1) * 512],
                         start=(k == 0), stop=(k == 7))
                c.copy('dve', vm[:, mt, h * 512:(h + 1) * 512], p[:])
        c.dma(G['kmT_d'], kmT[:])
        c.dma(G['vm_d'], vm[:])


def xattn_phase(c, K, G, nblk):
    nown = nblk // 2
    with c.phase():
        Wq = c.sb('Wq', [128, 8, D], BF16)
        Wmo = c.sb('Wmo', [128, 8, D], BF16)
        wst = [c.sb('wst%d' % i, [128, 2048], F32) for i in range(3)]
        wi = [0]
        load_w_resident(c, Wq, G['w_mq'], wst, wi, D)
        load_w_resident(c, Wmo, G['w_mo'], wst, wi, D)
        kmT = c.sb('kmT', [128, 8, 256], BF16)
        vm = c.sb('vm', [128, 2, D], BF16)
        c.dma(kmT[:], G['kmT_d'])
        c.dma(vm[:], G['vm_d'])
        xT = [c.sb('xT%d' % i, [128, 8, 512], BF16) for i in range(2)]
        qmT = c.sb('qmT', [128, 8, 512], BF16)
        x2t = [c.sb('x2t%d' % i, [128, D], F32) for i in range(2)]
        Pm = [c.sb('Pm%d' % i, [128, 4, 256], F32) for i in range(2)]
        Pn = [c.sb('Pn%d' % i, [128, 4, 256], BF16) for i in range(2)]
        PmT = [c.sb('PmT%d' % i, [128, 2, 4, 128], BF16) for i in range(2)]
        omT = [c.sb('omT%d' % i, [128, 8, 128], BF16) for i in range(2)]
        yb = [c.sb('yb%d' % i, [128, D], F32) for i in range(2)]
        x3 = [c.sb('x3_%d' % i, [128, D], F32) for i in range(2)]
        sm = c.sb('smx', [128, 16], F32)
        S = dict(st6=c.sb('st6', [128, 2, 6], F32), mv=c.sb('mv', [128, 4], F32))
        epsc = c.sb('epsc', [128, 1], F32)
        gB = c.sb('gB', [128, D], F32); bB = c.sb('bB', [128, D], F32)
        c.emit('pool', lambda e: e.memset(epsc[:], LN_EPS / (ALPHA * ALPHA)), [epsc[:]], [])
        c.dma(gB[:], G['ln3_g'][0:1, :].partition_broadcast(128))
        c.dma(bB[:], G['ln3_b'][0:1, :].partition_broadcast(128))
        for g in range(nown // 4):
            x_ = xT[g % 2]
            for b in range(4):
                c.dma(x_[:, :, b * 128:(b + 1) * 128], G['x2T_d'][g * 4 + b])
            for m in range(8):
                p = c.ps()
                for k in range(8):
                    c.mm(p[:], Wq[:, k, m * 128:(m + 1) * 128], x_[:, k, :], start=(k == 0), stop=(k == 7))
                c.act(qmT[:, m, :], p[:], AF.Copy, scale=1.0 / 16)
            for b in range(4):
                ob = g * 4 + b
                i = ob % 2
                c.dma(x2t[i][:], G['x2_d'][ob * 128:(ob + 1) * 128, :])
                pS = [c.ps(), c.ps()]
                for h in range(4):
                    for dc in range(2):
                        c.mm(pS[h // 2][:, (h % 2) * 256:(h % 2 + 1) * 256], qmT[:, 2 * h + dc, b * 128:(b + 1) * 128],
                             kmT[:, 2 * h + dc, :], start=(dc == 0), stop=(dc == 1))
                for hf in range(2):
                    c.emit('dve', lambda e, hf=hf: e.tensor_reduce(
                        out=sm[:, hf * 2:hf * 2 + 2], in_=pS[hf][:].rearrange("p (h m) -> p h m", h=2),
                        axis=AX.X, op=ALU.max), [sm[:, hf * 2:hf * 2 + 2]], [pS[hf][:]])
                c.ts('dve', sm[:, 4:8], sm[:, 0:4], -1.0, None, ALU.mult)
                for h in range(4):
                    c.act(Pm[i][:, h, :], pS[h // 2][:, (h % 2) * 256:(h % 2 + 1) * 256], AF.Exp,
                          bias=sm[:, 4 + h:5 + h], accum=sm[:, 8 + h:9 + h])
                c.emit('dve', lambda e: e.reciprocal(out=sm[:, 12:16], in_=sm[:, 8:12]), [sm[:, 12:16]], [sm[:, 8:12]])
                c.tt('dve', Pn[i][:], Pm[i][:], sm[:, 12:16].unsqueeze(2).to_broadcast([128, 4, 256]), ALU.mult)
                for mt in range(2):
                    pb = c.ps(); pT = pb.bitcast(BF16)
                    for h in range(4):
                        c.tr(pT[:, h * 128:(h + 1) * 128], Pn[i][:, h, mt * 128:(mt + 1) * 128], K['identb'][:])
                    c.copy('act', PmT[i][:, mt, :, :], pT[:, 0:512].rearrange("p (h t) -> p h t", h=4))
                for cp in range(0, 8, 4):
                    p = c.ps()
                    for ci in range(4):
                        cc = cp + ci
                        for mt in range(2):
                            c.mm(p[:, ci * 128:(ci + 1) * 128], vm[:, mt, cc * 128:(cc + 1) * 128],
                                 PmT[i][:, mt, cc // 2, :], start=(mt == 0), stop=(mt == 1))
                    c.copy('dve', omT[i][:, cp:cp + 4, :], p[:].rearrange("p (c t) -> p c t", c=4))
                for h in range(2):
                    po = c.ps()
                    for k in range(8):
                        c.mm(po[:], omT[i][:, k, :], Wmo[:, k, h * 512:(h + 1) * 512], start=(k == 0), stop=(k == 7))
                    c.stt(yb[i][:, h * 512:(h + 1) * 512], po[:], 1.0 / ALPHA, x2t[i][:, h * 512:(h + 1) * 512],
                          ALU.mult, ALU.add)
                ln_block(c, S, yb[i][:], epsc, gB[:], bB[:], x3[i][:])
                c.dma(G['x3_d'][ob * 128:(ob + 1) * 128, :], x3[i][:])


def build(stage=99, dbg=False, nblk=NBLK):
    nc = bass.Bass("TRN2", target_bir_lowering=False)
    c = Ctx(nc)
    kind_s = "ExternalOutput" if dbg else "Internal"
    G = {}

    def din(name, shape, dt=F32):
        G[name] = c.dram(name, shape, dt, kind="ExternalInput").ap()
        return G[name]

    def dsc(name, shape, dt):
        G[name] = c.dram(name, shape, dt, kind=kind_s).ap()
        return G[name]

    nown = nblk // 2 * 128
    ntok = nblk * 128
    xs = din("xs", [ntok, D])
    din("ffn1_w_in", [D, 2 * DFF]); din("ffn1_w_out", [DFF, D])
    din("ln1_g", [1, D]); din("ln1_b", [1, D]); din("ln1_gT", [128, 8]); din("ln1_bT", [128, 8])
    din("w_in", [D, D_IN]); din("dt_bias", [1, 16]); din("kv_norm_w", [1, 256])
    din("identb", [128, 128], BF16)
    din("v0", [128, 1])

    dsc("x1T_d", [nblk, 128, 8, 128], BF16)
    dsc("x1_d", [nown, D], F32)
    dsc("xbcT_d", [16, 128, ntok], BF16)
    dsc("kiT_d", [128, ntok], BF16)
    dsc("dt_d", [ntok, 16], F32)
    dsc("ckvtok_d", [nblk, 128, 257], BF16)
    dsc("ckvT_d", [2, 128, ntok], BF16)
    dsc("sgs_d", [8, 128, nown], BF16); dsc("sga_d", [8, 128, nown], BF16)
    dsc("qT_d", [8, 128, nown], BF16); dsc("qiT_d", [8, 128, nown], BF16)
    dsc("zs_d", [nown, D], BF16); dsc("widx_d", [nown, 16], F32)
    dsc("yssdT_d", [nblk // 2, 128, 8, 128], BF16)
    dsc("yattT_d", [nblk // 2, 128, 8, 128], BF16)
    dsc("x2_d", [nown, D], F32); dsc("x2T_d", [nblk // 2, 128, 8, 128], BF16)
    dsc("kmT_d", [128, 8, 256], BF16); dsc("vm_d", [128, 2, D], BF16); dsc("x3_d", [nown, D], F32)
    for wn in ("w_proj_ssd", "w_proj_att", "w_out", "w_mq", "w_mo"):
        din(wn, [D, D])
    din("w_mkv", [D, 2 * D]); din("mem", [256, D])
    for ln in ("ln2", "ln3", "ln4"):
        din(ln + "_g", [1, D]); din(ln + "_b", [1, D])
    din("ffn2_w_in", [D, 2 * DFF]); din("ffn2_w_out", [DFF, D])
    G['out'] = c.dram("out", [nown, D], F32, kind="ExternalOutput").ap()
    G['alibiLT'] = [din("alibiLT%d" % i, [128, SEQ], BF16) for i in range(2)]
    G['alibiR'] = [din("alibiR%d" % i, [128, 512], BF16) for i in range(4)]
    din("ohp", [128, 16])
    din("prow1", [1, SEQ]); din("negslope", [1, 16]); din("negcausal", [128, 128]); din("kb0", [128, 1])
    din("w_uk", [1024, 256]); G['w_uv'] = din("w_uv", [16, 256, 64])
    din("cwT", [128, 16, 4]); din("cbT", [128, 16]); din("trib", [128, 128], BF16); din("ones128b", [128, 128], BF16)
    din("identf", [128, 128]); din("negmask4b", [128, 512], BF16); din("sel16b", [128, 2048], BF16)
    din("a_log", [1, 16]); din("d_skip", [1, 16]); din("ssd_norm_w", [1, D])

    c.banks = [c.psum('bank%d' % i, [128, 512], F32) for i in range(8)]
    K = {}
    K['identb'] = c.sb('identb_s', [128, 128], BF16)
    c.dma(K['identb'][:], G['identb'])
    K['v0'] = c.sb('v0_s', [128, 1], F32)
    c.dma(K['v0'][:], G['v0'])
    K['kb0'] = c.sb('kb0_s', [128, 1], F32)
    c.dma(K['kb0'][:], G['kb0'])

    def post1(blk, S, y, mean, rstd):
        if blk == 'setup':
            S['zb'] = [c.sb('zb%d' % i, [128, D], BF16) for i in range(2)]
            S['x1s'] = [c.sb('x1s%d' % i, [128, 8, 128], BF16) for i in range(2)]
            S['z32'] = [c.sb('z32_%d' % i, [128, D], F32) for i in range(2)]
            c.dma(S['gT'][:], G['ln1_gT'])
            c.dma(S['bT'][:], G['ln1_bT'])
            return
        zb = S['zb'][blk % 2]
        x1s = S['x1s'][blk % 2]
        c.ts('dve', zb[:], y[:], mean, rstd, ALU.subtract, ALU.mult)
        pb = c.ps()
        pT = pb.bitcast(BF16)
        for k in range(8):
            c.tr(pT[:, k * 128:(k + 1) * 128], zb[:, k * 128:(k + 1) * 128], K['identb'][:])
        for k in range(8):
            c.act(x1s[:, k, :], pT[:, k * 128:(k + 1) * 128], AF.Identity,
                  bias=S['bT'][:, k:k + 1], scale=S['gT'][:, k:k + 1])
        if blk == 0:
            c.ts('dve', x1s[:], x1s[:], K['v0'][:, 0:1], None, ALU.mult)
        c.dma(G['x1T_d'][blk], x1s[:])
        if blk % 2 == 1:
            z = S['z32'][(blk // 2) % 2]
            c.ts('dve', z[:], y[:], mean, rstd, ALU.subtract, ALU.mult)
            c.tt('pool', z[:], z[:], S['gB'][:], ALU.mult)
            c.tt('pool', z[:], z[:], S['bB'][:], ALU.add)
            c.dma(G['x1_d'][(blk // 2) * 128:(blk // 2 + 1) * 128, :], z[:])

    if stage >= 1:
        ffn_phase(c, K, xs, nblk, G['ffn1_w_in'], G['ffn1_w_out'], G['ln1_g'], G['ln1_b'], post1, 'f1')
    if stage >= 2:
        proj_phase_all(c, K, G, nblk)
        proj_phase_own(c, K, G, nblk)
    if stage >= 3:
        ssd_phase(c, K, G, nblk)
    if stage >= 4:
        dsa_phase(c, K, G, nblk)
    if stage >= 5:
        merge_phase(c, K, G, nblk)
        memkv_phase(c, K, G)
        xattn_phase(c, K, G, nblk)
    if stage >= 6:
        def post4(blk, S, y, mean, rstd):
            if blk == 'setup':
                S['o32'] = [c.sb('o32_%d' % i, [128, D], F32) for i in range(2)]
                return
            o = S['o32'][blk % 2]
            c.ts('dve', o[:], y[:], mean, rstd, ALU.subtract, ALU.mult)
            c.tt('pool', o[:], o[:], S['gB'][:], ALU.mult)
            c.tt('pool', o[:], o[:], S['bB'][:], ALU.add)
            c.dma(G['out'][blk * 128:(blk + 1) * 128, :], o[:])
        ffn_phase(c, K, G['x3_d'], nblk // 2, G['ffn2_w_in'], G['ffn2_w_out'], G['ln4_g'], G['ln4_b'], post4, 'f2')

    c.barrier()
    c.es.close()
    global LAST_INPUTS
    LAST_INPUTS = [n for n in G if n not in ('out',) and not n.endswith('_d')]
    print("build: ninst=%d nwait=%d nsem=%d" % (c.ninst, c.nwait, c.nsem))
    return nc


def make_consts():
    i = np.arange(128)
    tri = (i[:, None] <= i[None, :]).astype(np.float32)
    negm = np.where(i[:, None] <= i[None, :], 0.0, -30000.0).astype(np.float32)
    sel = np.zeros((128, 16, 128), np.float32)
    for h in range(16):
        sel[h, h, :] = 1.0
    bf = ml_dtypes.bfloat16
    slopes = (2.0 ** (-8.0 * np.arange(1, 17, dtype=np.float64) / 16)).astype(np.float32)
    pos = np.arange(SEQ, dtype=np.float32)
    extra = {}
    ltt = [np.zeros((128, SEQ), np.float32) for _ in range(2)]
    rrt = [np.zeros((128, 512), np.float32) for _ in range(4)]
    ohp = np.zeros((128, 16), np.float32)
    for hg in range(4):
        r0 = 64 * (hg % 2)
        lt = ltt[hg // 2][r0:r0 + 28]
        lt[0:16] = 1.0
        rr = rrt[hg][r0:r0 + 28]
        for j in range(4):
            v = (slopes[hg * 4 + j] * pos).astype(np.float32)
            for k in range(3):
                part = v.astype(bf).astype(np.float32)
                lt[16 + 3 * j + k] = part
                v = v - part
                rr[16 + 3 * j + k, j * 128:(j + 1) * 128] = 1.0
            ohp[r0 + hg * 4 + j, hg * 4 + j] = 1.0
    for q_ in range(2):
        extra["alibiLT%d" % q_] = ltt[q_].astype(bf)
    for q_ in range(4):
        extra["alibiR%d" % q_] = rrt[q_].astype(bf)
    extra["ohp"] = ohp
    extra["prow1"] = (pos + 1.0)[None, :]
    extra["negslope"] = (-slopes)[None, :]
    extra["negcausal"] = np.where(i[None, :] <= i[:, None], 0.0, -1e30).astype(np.float32)
    return {**extra, "identb": np.eye(128, dtype=np.float32).astype(ml_dtypes.bfloat16),
            "identf": np.eye(128, dtype=np.float32), "trib": tri.astype(bf),
            "ones128b": np.ones((128, 128), np.float32).astype(bf),
            "negmask4b": np.ascontiguousarray(np.tile(negm, (1, 4))).astype(bf),
            "sel16b": sel.reshape(128, 2048).astype(bf)}


def make_in_maps(inputs):
    x = np.asarray(inputs["x"], dtype=np.float32)
    cs = make_consts()
    sq = lambda a: np.ascontiguousarray(np.asarray(a, dtype=np.float32)[0])
    shared = {
        "ffn1_w_in": sq(inputs["ffn1_w_in"]), "ffn1_w_out": sq(inputs["ffn1_w_out"]),
        "ln1_g": sq(inputs["ln1_g"])[None, :], "ln1_b": sq(inputs["ln1_b"])[None, :],
        "ln1_gT": np.ascontiguousarray(sq(inputs["ln1_g"]).reshape(8, 128).T),
        "ln1_bT": np.ascontiguousarray(sq(inputs["ln1_b"]).reshape(8, 128).T),
        "w_in": sq(inputs["w_in"]), "dt_bias": sq(inputs["dt_bias"])[None, :],
        "kv_norm_w": sq(inputs["kv_norm_w"])[None, :],
        "cwT": np.ascontiguousarray(sq(inputs["conv_w"]).reshape(4, 16, 128).transpose(2, 1, 0)),
        "cbT": np.ascontiguousarray(sq(inputs["conv_b"]).reshape(16, 128).T),
        "a_log": sq(inputs["a_log"])[None, :], "d_skip": sq(inputs["d_skip"])[None, :],
        "ssd_norm_w": sq(inputs["ssd_norm_w"])[None, :],
        "w_uk": sq(inputs["w_uk"]).reshape(1024, 256), "w_uv": sq(inputs["w_uv"]),
        "w_proj_ssd": sq(inputs["w_proj_ssd"]), "w_proj_att": sq(inputs["w_proj_att"]),
        "w_out": sq(inputs["w_out"]), "w_mq": sq(inputs["w_mq"]), "w_mo": sq(inputs["w_mo"]),
        "w_mkv": sq(inputs["w_mkv"]),
        "ln2_g": sq(inputs["ln2_g"])[None, :], "ln2_b": sq(inputs["ln2_b"])[None, :],
        "ln3_g": sq(inputs["ln3_g"])[None, :], "ln3_b": sq(inputs["ln3_b"])[None, :],
        "ln4_g": sq(inputs["ln4_g"])[None, :], "ln4_b": sq(inputs["ln4_b"])[None, :],
        "ffn2_w_in": sq(inputs["ffn2_w_in"]), "ffn2_w_out": sq(inputs["ffn2_w_out"]),
    }
    shared.update(cs)
    maps = []
    for core in range(8):
        b, par = core // 2, core % 2
        if par == 0:
            xs = np.concatenate([np.zeros((128, D), np.float32), x[b, :SEQ - 128]], axis=0)
        else:
            xs = x[b]
        m = dict(shared)
        m["xs"] = np.ascontiguousarray(xs)
        m["v0"] = np.full((128, 1), float(par), np.float32)
        m["kb0"] = np.full((128, 1), 0.0 if par else -1e30, np.float32)
        m["mem"] = np.ascontiguousarray(np.asarray(inputs["mem"], dtype=np.float32)[b])
        maps.append(m)
    return maps


def kernel(**inputs):
    nc = build()
    maps = make_in_maps(inputs)
    res = run_bass_kernel_spmd(nc, maps, core_ids=list(range(8)))
    out = np.zeros((4, SEQ, D), np.float32)
    for core in range(8):
        b, par = core // 2, core % 2
        o = np.asarray(res.results[core]["out"], dtype=np.float32).reshape(NBLK // 2, 128, D)
        out[b].reshape(NBLK, 128, D)[par::2] = o
    return out
```
